# Optimizing a Trainium2 kernel written in Bass

```python
import math
import jax, jax.numpy as jnp
from jax import lax
import numpy as np

D_MODEL = 1024
BATCH = 4
SEQ = 4096
DEPTH = 2

HEAD_DIM = D_MODEL // 16
A_HEADS = 4
B_HEADS = 4
C_HEADS = 8
C_KV_HEADS = 2
DIFF_DK = HEAD_DIM // 2
DIFF_DV = HEAD_DIM
CMP_LEN = 32
CMP_STRIDE = 16
CMP_HIDDEN = 2 * HEAD_DIM
SLC_BLOCK = 64
SLC_TOPN = 16
NSA_WINDOW = 512
SWA_WINDOW = 128
QBLK = 128
N_EXPERTS = 8
TOP_K = 2
D_FF_EXPERT = 7 * D_MODEL // 2
D_FF_DENSE = 2816
MOE_BLK = 256
PLE_DIM = 256
LN_EPS = 1e-5
NEG_INF = -1e30
FORCE_SCORE = 1e9
DN_ALPHA = (2 * DEPTH) ** 0.25
DN_BETA = (8 * DEPTH) ** -0.25
N_DENSE = (DEPTH + 1) // 2
N_MOE = DEPTH // 2
PROJ_SIZES = (A_HEADS * HEAD_DIM, 6 * HEAD_DIM, 3 * A_HEADS,
              B_HEADS * 2 * DIFF_DK, B_HEADS * 2 * DIFF_DK, B_HEADS * DIFF_DV,
              C_HEADS * HEAD_DIM, C_KV_HEADS * HEAD_DIM, C_KV_HEADS * HEAD_DIM)
N_PROJ = sum(PROJ_SIZES)

kernel_name = 'hybrid_nsa_diff_swa_moe_deepnorm'


def layer_norm(x, g, b):
    xf = x.astype(jnp.float32)
    mu = jnp.mean(xf, axis=-1, keepdims=True)
    var = jnp.mean(jnp.square(xf - mu), axis=-1, keepdims=True)
    return ((xf - mu) * lax.rsqrt(var + LN_EPS) * g + b).astype(x.dtype)


def rms_norm(x, g):
    xf = x.astype(jnp.float32)
    y = xf * lax.rsqrt(jnp.mean(jnp.square(xf), axis=-1, keepdims=True) + LN_EPS) * g
    return y.astype(x.dtype)


def masked_softmax(s, mask):
    s = jnp.where(mask, s.astype(jnp.float32), NEG_INF)
    m = jnp.max(s, axis=-1, keepdims=True)
    e = jnp.where(mask, jnp.exp(s - m), 0.0)
    return e / jnp.maximum(jnp.sum(e, axis=-1, keepdims=True), 1e-30)


def sink_softmax(s, mask, sink):
    s = jnp.where(mask, s.astype(jnp.float32), NEG_INF)
    sink = sink.astype(jnp.float32)
    m = jnp.maximum(jnp.max(s, axis=-1, keepdims=True), sink)
    e = jnp.where(mask, jnp.exp(s - m), 0.0)
    return e / (jnp.sum(e, axis=-1, keepdims=True) + jnp.exp(sink - m))


def alibi_slopes():
    n = A_HEADS + B_HEADS + C_HEADS
    s = 2.0 ** (-8.0 * jnp.arange(1, n + 1, dtype=jnp.float32) / n)
    rest = s[C_HEADS:]
    return rest[0::2], rest[1::2], s[:C_HEADS]


def band_blocks(a, n_prev):
    B, T = a.shape[:2]
    nb = T // QBLK
    pad = [(0, 0), (n_prev * QBLK, 0)] + [(0, 0)] * (a.ndim - 2)
    c = jnp.pad(a, pad).reshape(B, nb + n_prev, QBLK, *a.shape[2:])
    return jnp.concatenate([c[:, i:i + nb] for i in range(n_prev + 1)], axis=2)


def banded_attention(q, k, v, window, slopes, sinks):
    B, T, G, R, dh = q.shape
    nb = T // QBLK
    n_prev = window // QBLK
    kw = (n_prev + 1) * QBLK
    kb = band_blocks(k, n_prev)
    vb = band_blocks(v, n_prev)
    qb = q.reshape(B, nb, QBLK, G, R, dh)
    qq = jnp.arange(QBLK)[:, None]
    kk = jnp.arange(kw)[None, :]
    dist = n_prev * QBLK + qq - kk
    s_pos = (jnp.arange(nb)[:, None, None] - n_prev) * QBLK + kk[None]
    mask = (((dist >= 0) & (dist < window))[None] & (s_pos >= 0))[None, :, None, None]
    s = jnp.einsum('bnqgrd,bnkgd->bngrqk', qb, kb).astype(jnp.float32) * (dh ** -0.5)
    s = s - slopes[:, :, None, None] * dist.astype(jnp.float32)
    if sinks is None:
        p = masked_softmax(s, mask)
    else:
        p = sink_softmax(s, mask, sinks[:, :, None, None])
    o = jnp.einsum('bngrqk,bnkgd->bnqgrd', p.astype(v.dtype), vb)
    return o.reshape(B, T, G, R, dh)


def compress_blocks(kv, pos, w1, w2):
    B, T, dh = kv.shape
    n_chunk = T // CMP_STRIDE
    r = CMP_LEN // CMP_STRIDE
    n_cmp = n_chunk - r + 1
    c = kv.reshape(B, n_chunk, CMP_STRIDE, dh)
    blocks = jnp.concatenate([c[:, i:i + n_cmp] for i in range(r)], axis=2) + pos
    h = jax.nn.gelu(blocks.reshape(B, n_cmp, CMP_LEN * dh) @ w1)
    return h @ w2


def selected_attention(q, k, v, idx, slopes):
    B, T, H, dh = q.shape
    n = idx.shape[-1]
    nsl = T // SLC_BLOCK
    nq = T // QBLK
    kb = k.reshape(B, nsl, SLC_BLOCK, dh)
    vb = v.reshape(B, nsl, SLC_BLOCK, dh)
    qc = q.reshape(B, nq, QBLK, H, dh).transpose(1, 0, 2, 3, 4)
    ic = idx.reshape(B, nq, QBLK, n).transpose(1, 0, 2, 3)
    bidx = jnp.arange(B)[:, None, None]
    off = jnp.arange(SLC_BLOCK)

    def one(args):
        qj, ij, j = args
        kg = kb[bidx, ij].reshape(B, QBLK, n * SLC_BLOCK, dh)
        vg = vb[bidx, ij].reshape(B, QBLK, n * SLC_BLOCK, dh)
        s_pos = (ij[..., None] * SLC_BLOCK + off).reshape(B, QBLK, n * SLC_BLOCK)
        dist = (j * QBLK + jnp.arange(QBLK))[None, :, None] - s_pos
        s = jnp.einsum('bqhd,bqkd->bqhk', qj, kg).astype(jnp.float32) * (dh ** -0.5)
        s = s - slopes[None, None, :, None] * dist.astype(jnp.float32)[:, :, None, :]
        p = masked_softmax(s, (dist >= 0)[:, :, None, :])
        return jnp.einsum('bqhk,bqkd->bqhd', p.astype(vg.dtype), vg)

    o = lax.map(one, (qc, ic, jnp.arange(nq)))
    return o.transpose(1, 0, 2, 3, 4).reshape(B, T, H, dh)


def nsa_mixer(q, k_cmp, v_cmp, k_slc, v_slc, k_win, v_win, gates, cmp_pos, cmp_w1, cmp_w2, slopes):
    B, T, H, dh = q.shape
    kc = compress_blocks(k_cmp, cmp_pos[0], cmp_w1[0], cmp_w2[0])
    vc = compress_blocks(v_cmp, cmp_pos[1], cmp_w1[1], cmp_w2[1])
    n_cmp = kc.shape[1]
    t = jnp.arange(T)
    c_start = jnp.arange(n_cmp) * CMP_STRIDE
    dist_c = (t[:, None] - (c_start + CMP_LEN - 1)[None, :]).astype(jnp.float32)
    s_c = jnp.einsum('bthd,bnd->bthn', q, kc).astype(jnp.float32) * (dh ** -0.5)
    s_c = s_c - slopes[None, :, None] * dist_c[:, None, :]
    p_c = masked_softmax(s_c, (dist_c >= 0)[:, None, :])
    o_c = jnp.einsum('bthn,bnd->bthd', p_c.astype(vc.dtype), vc)
    nsl = T // SLC_BLOCK
    s_start = jnp.arange(nsl) * SLC_BLOCK
    overlap = jnp.clip(jnp.minimum(c_start[:, None] + CMP_LEN, s_start[None, :] + SLC_BLOCK)
                       - jnp.maximum(c_start[:, None], s_start[None, :]), 0, None).astype(jnp.float32) / CMP_LEN
    imp = jnp.einsum('bthn,nj->btj', p_c, overlap)
    t_blk = t // SLC_BLOCK
    j = jnp.arange(nsl)
    forced = (j[None] == 0) | (j[None] == t_blk[:, None]) | (j[None] == t_blk[:, None] - 1)
    future = j[None] > t_blk[:, None]
    imp = jnp.where(forced[None], FORCE_SCORE, jnp.where(future[None], -1.0, imp))
    _, idx = lax.top_k(imp, min(SLC_TOPN, nsl))
    o_s = selected_attention(q, k_slc, v_slc, idx, slopes)
    o_w = banded_attention(q[:, :, None], k_win[:, :, None], v_win[:, :, None],
                           NSA_WINDOW, slopes[None], None)[:, :, 0]
    o = gates[..., 0:1] * o_c + gates[..., 1:2] * o_s + gates[..., 2:3] * o_w
    return o.reshape(B, T, H * dh)


def diff_attention(q, k, v, lam, slopes):
    B, T, H, _, dk = q.shape
    nq = T // QBLK
    qc = q.reshape(B, nq, QBLK, H, 2, dk).transpose(1, 0, 2, 3, 4, 5)
    s_pos = jnp.arange(T)

    def one(args):
        qj, j = args
        dist = (j * QBLK + jnp.arange(QBLK))[:, None] - s_pos[None, :]
        bias = -slopes[:, None, None] * dist.astype(jnp.float32)
        s = jnp.einsum('bqhcd,bkhcd->bchqk', qj, k).astype(jnp.float32) * (dk ** -0.5) + bias
        p = masked_softmax(s, dist >= 0)
        w = p[:, 0] - lam * p[:, 1]
        return jnp.einsum('bhqk,bkhd->bqhd', w.astype(v.dtype), v)

    o = lax.map(one, (qc, jnp.arange(nq)))
    return o.transpose(1, 0, 2, 3, 4).reshape(B, T, H, -1)


def swiglu(h, wg, wu, wd):
    return (jax.nn.silu(h @ wg) * (h @ wu)) @ wd


def moe_swiglu(x, w_router, w_gate, w_up, w_down):
    B, T, D = x.shape
    n_tok = B * T
    xf = x.reshape(n_tok, D)
    logits = (xf @ w_router).astype(jnp.float32)
    top_logit, top_e = lax.top_k(logits, TOP_K)
    gate = jax.nn.softmax(top_logit, axis=-1)
    n_asg = n_tok * TOP_K
    e_flat = top_e.reshape(n_asg)
    tok_flat = jnp.arange(n_asg) // TOP_K
    g_flat = gate.reshape(n_asg)
    order = jnp.argsort(e_flat)
    e_sorted = e_flat[order]
    counts = jnp.bincount(e_flat, length=N_EXPERTS)
    padded = (counts + MOE_BLK - 1) // MOE_BLK * MOE_BLK
    pad_end = jnp.cumsum(padded)
    start = jnp.cumsum(counts) - counts
    dest = pad_end[e_sorted] - padded[e_sorted] + jnp.arange(n_asg) - start[e_sorted]
    n_blk = -(-n_asg // MOE_BLK) + N_EXPERTS
    cap = n_blk * MOE_BLK
    tok_buf = jnp.zeros((cap,), jnp.int32).at[dest].set(tok_flat[order])
    g_buf = jnp.zeros((cap,), jnp.float32).at[dest].set(g_flat[order])
    blk_e = jnp.minimum(jnp.sum(pad_end[None, :] <= (jnp.arange(n_blk) * MOE_BLK)[:, None], axis=-1),
                        N_EXPERTS - 1)
    x_buf = xf[tok_buf].reshape(n_blk, MOE_BLK, D)

    def expert_block(args):
        xb, e = args
        return swiglu(xb, w_gate[e], w_up[e], w_down[e])

    y = lax.map(expert_block, (x_buf, blk_e)).reshape(cap, D)
    out = jnp.zeros((n_tok, D), jnp.float32).at[tok_buf].add(y.astype(jnp.float32) * g_buf[:, None])
    return out.astype(x.dtype).reshape(B, T, D)


def setup_inputs(seed: int = 0) -> dict:
    key = jax.random.key(seed)
    keys = iter(jax.random.split(key, 32))

    def nrm(shape, scale):
        return jax.random.normal(next(keys), shape, jnp.float32) * scale

    D = D_MODEL
    return {
        'x': nrm((BATCH, SEQ, D), 1.0),
        'p': nrm((DEPTH, BATCH, SEQ, PLE_DIM), 1.0),
        'w_in': nrm((DEPTH, D, N_PROJ), D ** -0.5),
        'cmp_pos': nrm((DEPTH, 2, CMP_LEN, HEAD_DIM), 0.1),
        'cmp_w1': nrm((DEPTH, 2, CMP_LEN * HEAD_DIM, CMP_HIDDEN), (CMP_LEN * HEAD_DIM) ** -0.5),
        'cmp_w2': nrm((DEPTH, 2, CMP_HIDDEN, HEAD_DIM), CMP_HIDDEN ** -0.5),
        'diff_lambda': nrm((DEPTH, 4, DIFF_DK), 0.1),
        'diff_subln': 1.0 + nrm((DEPTH, DIFF_DV), 0.1),
        'sinks': nrm((DEPTH, C_HEADS), 1.0),
        'w_out': nrm((DEPTH, D, D), D ** -0.5 * DN_BETA),
        'ln1_g': 1.0 + nrm((DEPTH, D), 0.1),
        'ln1_b': nrm((DEPTH, D), 0.02),
        'ffn_w_gate': nrm((N_DENSE, D, D_FF_DENSE), D ** -0.5),
        'ffn_w_up': nrm((N_DENSE, D, D_FF_DENSE), D ** -0.5),
        'ffn_w_down': nrm((N_DENSE, D_FF_DENSE, D), D_FF_DENSE ** -0.5 * DN_BETA),
        'moe_router': nrm((N_MOE, D, N_EXPERTS), D ** -0.5),
        'moe_w_gate': nrm((N_MOE, N_EXPERTS, D, D_FF_EXPERT), D ** -0.5),
        'moe_w_up': nrm((N_MOE, N_EXPERTS, D, D_FF_EXPERT), D ** -0.5),
        'moe_w_down': nrm((N_MOE, N_EXPERTS, D_FF_EXPERT, D), D_FF_EXPERT ** -0.5 * DN_BETA),
        'ln2_g': 1.0 + nrm((DEPTH, D), 0.1),
        'ln2_b': nrm((DEPTH, D), 0.02),
        'ple_gate': nrm((DEPTH, D, D), D ** -0.5),
        'ple_proj': nrm((DEPTH, PLE_DIM, D), PLE_DIM ** -0.5 * DN_BETA),
        'ln3_g': 1.0 + nrm((DEPTH, D), 0.1),
        'ln3_b': nrm((DEPTH, D), 0.02),
    }


def reference(x, p, w_in, cmp_pos, cmp_w1, cmp_w2, diff_lambda, diff_subln, sinks, w_out,
              ln1_g, ln1_b, ffn_w_gate, ffn_w_up, ffn_w_down, moe_router, moe_w_gate, moe_w_up,
              moe_w_down, ln2_g, ln2_b, ple_gate, ple_proj, ln3_g, ln3_b):
    B, T, _ = x.shape
    slopes_a, slopes_b, slopes_c = alibi_slopes()
    offs = np.cumsum(PROJ_SIZES)[:-1].tolist()
    for i in range(DEPTH):
        proj = jnp.einsum('btd,dn->btn', x, w_in[i])
        q_a, kv_a, g_a, q_b, k_b, v_b, q_c, k_c, v_c = jnp.split(proj, offs, axis=-1)
        kv_a = kv_a.reshape(B, T, 6, HEAD_DIM)
        o_a = nsa_mixer(q_a.reshape(B, T, A_HEADS, HEAD_DIM),
                        kv_a[:, :, 0], kv_a[:, :, 1], kv_a[:, :, 2], kv_a[:, :, 3], kv_a[:, :, 4], kv_a[:, :, 5],
                        jax.nn.sigmoid(g_a).reshape(B, T, A_HEADS, 3),
                        cmp_pos[i], cmp_w1[i], cmp_w2[i], slopes_a)
        lam_init = 0.8 - 0.6 * math.exp(-0.3 * i)
        dl = diff_lambda[i].astype(jnp.float32)
        lam = jnp.exp(jnp.sum(dl[0] * dl[1])) - jnp.exp(jnp.sum(dl[2] * dl[3])) + lam_init
        o_b = diff_attention(q_b.reshape(B, T, B_HEADS, 2, DIFF_DK), k_b.reshape(B, T, B_HEADS, 2, DIFF_DK),
                             v_b.reshape(B, T, B_HEADS, DIFF_DV), lam, slopes_b)
        o_b = rms_norm(o_b, diff_subln[i]) * (1.0 - lam_init)
        o_c = banded_attention(q_c.reshape(B, T, C_KV_HEADS, C_HEADS // C_KV_HEADS, HEAD_DIM),
                               k_c.reshape(B, T, C_KV_HEADS, HEAD_DIM), v_c.reshape(B, T, C_KV_HEADS, HEAD_DIM),
                               SWA_WINDOW, slopes_c.reshape(C_KV_HEADS, -1), sinks[i].reshape(C_KV_HEADS, -1))
        mix = jnp.concatenate([o_a, o_b.reshape(B, T, -1), o_c.reshape(B, T, -1)], axis=-1) @ w_out[i]
        x = layer_norm(DN_ALPHA * x + mix, ln1_g[i], ln1_b[i])
        if i % 2 == 0:
            f = swiglu(x, ffn_w_gate[i // 2], ffn_w_up[i // 2], ffn_w_down[i // 2])
        else:
            f = moe_swiglu(x, moe_router[i // 2], moe_w_gate[i // 2], moe_w_up[i // 2], moe_w_down[i // 2])
        x = layer_norm(DN_ALPHA * x + f, ln2_g[i], ln2_b[i])
        e = jax.nn.sigmoid(x @ ple_gate[i]) * (p[i] @ ple_proj[i])
        x = layer_norm(DN_ALPHA * x + e, ln3_g[i], ln3_b[i])
    return x
```

```python
import contextlib, math
import numpy as np
import ml_dtypes
import concourse.bass as bass
import concourse.mybir as mybir

F32 = mybir.dt.float32
BF16 = mybir.dt.bfloat16
AF = mybir.ActivationFunctionType
ALU = mybir.AluOpType
AX = mybir.AxisListType

PHASE = [0]


def uname(name):
    return "%s_%d" % (name, PHASE[0])


ENGS = ("pe", "act", "dve", "pool", "sp")


class Tok:
    __slots__ = ("name", "w", "rs")

    def __init__(self, name=""):
        self.name = name
        self.w = None
        self.rs = []


class Op:
    __slots__ = ("eng", "fn", "deps", "dma", "sig", "needs", "gi", "inc")

    def __init__(self, eng, fn, dma):
        self.eng = eng
        self.fn = fn
        self.deps = set()
        self.dma = dma
        self.sig = None
        self.needs = False
        self.gi = 0
        self.inc = 16


class Sched:
    NDMA = 12
    NSCHED = 0

    def __init__(self, nc, same_engine_sync=True):
        self.nc = nc
        self.ops = []
        self.same = same_engine_sync

    def add(self, eng, fn, r=(), w=(), dma=False, inc=16):
        op = Op(eng, fn, dma)
        op.inc = inc
        op.gi = len(self.ops)
        for t in r:
            if t.w is not None:
                op.deps.add(t.w)
        for t in w:
            if t.w is not None:
                op.deps.add(t.w)
            for x in t.rs:
                op.deps.add(x)
        for t in r:
            t.rs.append(op)
        for t in w:
            t.w = op
            t.rs = []
        op.deps.discard(op)
        self.ops.append(op)
        return op

    def dma(self, q, out, in_, r=(), w=(), **kw):
        return self.add(q, lambda e: e.dma_start(out=out, in_=in_, **kw), r, w, dma=True)

    def emit(self, final_ops=()):
        nc = self.nc
        ops = self.ops
        for op in ops:
            for d in op.deps:
                if d.dma:
                    continue
                if d.eng != op.eng or (self.same and d.eng != "pe"):
                    d.needs = True
        for op in final_ops:
            op.needs = True
        cnt = {e: 0 for e in ENGS}
        dcnt = {e: [0] * self.NDMA for e in ENGS}
        drr = {e: 0 for e in ENGS}
        prev_dma_wait = {}
        ncoll = 0
        coll_keys = []
        for op in ops:
            if op.dma and op.inc != 16:
                ncoll += 1
                prev_dma_wait[op] = (op.eng, 0, 0)
                op.sig = (("k", op.eng, ncoll), op.inc)
                coll_keys.append(op.sig[0])
            elif op.dma:
                k = drr[op.eng]
                drr[op.eng] = (k + 1) % self.NDMA
                prev_dma_wait[op] = (op.eng, k, dcnt[op.eng][k])
                dcnt[op.eng][k] += op.inc
                op.sig = (("d", op.eng, k), dcnt[op.eng][k])
            elif op.needs:
                cnt[op.eng] += 1
                op.sig = (("c", op.eng), cnt[op.eng])
        per = {e: [o for o in ops if o.eng == e] for e in ENGS}
        used = [e for e in ENGS if per[e]]
        import contextlib
        with contextlib.ExitStack() as st:
            sems = {}
            for e in ENGS:
                if cnt[e] > 0:
                    sems[("c", e)] = nc.alloc_semaphore(name="c_%s_%d" % (e, Sched.NSCHED))
                for k in range(self.NDMA):
                    if dcnt[e][k] > 0:
                        sems[("d", e, k)] = nc.alloc_semaphore(name="d_%s_%d_%d" % (e, k, Sched.NSCHED))
            for key in coll_keys:
                sems[key] = nc.alloc_semaphore(name="k_%s_%d_%d" % (key[1], key[2], Sched.NSCHED))
            Sched.NSCHED += 1
            block = st.enter_context(nc.Block())

            def run(ename, eng):
                waited = {}
                for op in per[ename]:
                    need = {}
                    for d in op.deps:
                        if d.sig is None:
                            continue
                        if (not d.dma) and d.eng == ename and (ename == "pe" or not self.same):
                            continue
                        key, val = d.sig
                        if need.get(key, 0) < val:
                            need[key] = val
                    if op.dma:
                        _, k, v = prev_dma_wait[op]
                        if v > 0:
                            key = ("d", ename, k)
                            if need.get(key, 0) < v:
                                need[key] = v
                    for key, val in need.items():
                        if waited.get(key, 0) < val:
                            eng.wait_ge(sems[key], val)
                            waited[key] = val
                    ins = op.fn(eng)
                    if op.sig is not None:
                        key, val = op.sig
                        ins.then_inc(sems[key], op.inc if op.dma else 1)
                if ename == "sp":
                    for op in final_ops:
                        key, val = op.sig
                        if waited.get(key, 0) < val:
                            eng.wait_ge(sems[key], val)
                            waited[key] = val

            if per["sp"] or final_ops:
                block.sync(lambda e: run("sp", e))
            if per["pe"]:
                block.tensor(lambda e: run("pe", e))
            if per["act"]:
                block.scalar(lambda e: run("act", e))
            if per["dve"]:
                block.vector(lambda e: run("dve", e))
            if per["pool"]:
                block.gpsimd(lambda e: run("pool", e))
        return {e: len(per[e]) for e in ENGS}

NT = 2048
D = 1024
ALPHA = 4 ** 0.25
EPS = 1e-5


def dram(nc, name, shape, dt=F32, kind="ExternalInput"):
    return nc.dram_tensor(name, list(shape), dt, kind=kind).ap()


class Ctx:
    pass


def setup_psum(nc, st, c):
    c.PSB = [st.enter_context(nc.psum_tensor(uname("psb%d" % i), [128, 1024], F32)) for i in range(4)]
    c.TPS = [[Tok("ps%d_%d" % (i, h)) for h in range(2)] for i in range(4)]
    c.psrr = 0


def ps_full(c):
    i = c.psrr % 4
    c.psrr += 1
    return c.PSB[i], c.TPS[i]


def ps_half(c):
    if not hasattr(c, "hrr"):
        c.hrr = 0
    k = c.hrr % 8
    c.hrr += 1
    return c.PSB[k // 2][:, (k % 2) * 512:(k % 2 + 1) * 512], c.TPS[k // 2][k % 2]


RG_PAIRS = [[0, 1], [2, 3], [4, 5], [6, 7]]


def load_oT(nc, st, s, io, OT, tOT, H1, tH1):
    if "oTown" not in io:
        s.dma("sp", OT, io["oT"].rearrange("c p t -> p c t"), w=[tOT])
        return
    HS = st.enter_context(nc.sbuf_tensor(uname("HS"), [128, 2], F32)); tHS = Tok()
    s.dma("sp", HS[:], io["hsel"].partition_broadcast(128).rearrange("p a b -> p (a b)"), w=[tHS])
    for fc in range(8):
        s.dma("sp", OT[:, fc, :], io["oTg"][0][fc * 128:(fc + 1) * 128, :], w=[tOT])
        s.dma("sp", H1[:, fc, :], io["oTg"][1][fc * 128:(fc + 1) * 128, :], w=[tH1])
    for fc in range(8):
        s.add("dve", lambda e, fc=fc: e.tensor_scalar(H1[:, fc, :], H1[:, fc, :], HS[:, 1:2], None, ALU.mult), r=[tHS, tH1], w=[tH1])
        s.add("dve", lambda e, fc=fc: e.scalar_tensor_tensor(out=OT[:, fc, :], in0=OT[:, fc, :], scalar=HS[:, 0:1], in1=H1[:, fc, :], op0=ALU.mult, op1=ALU.add),
              r=[tHS, tH1, tOT], w=[tOT])


def layernorm_rows(s, c, src, tsrc, dst, tdst, G, Bt, tG):
    ST, tST = c.ST, c.tST
    s.add("dve", lambda e: e.bn_stats(out=ST[:, 0:6], in_=src[:, 0:512]), r=[tsrc], w=[tST])
    s.add("dve", lambda e: e.bn_stats(out=ST[:, 6:12], in_=src[:, 512:1024]), r=[tsrc], w=[tST])
    s.add("dve", lambda e: e.bn_aggr(out=ST[:, 12:14], in_=ST[:, 0:12]), r=[tST], w=[tST])
    s.add("act", lambda e: e.activation(out=ST[:, 15:16], in_=ST[:, 13:14], func=AF.Sqrt, bias=c.EPSC[:, 0:1]), r=[tST, c.tEPSC], w=[tST])
    s.add("dve", lambda e: e.reciprocal(out=ST[:, 14:15], in_=ST[:, 15:16]), r=[tST], w=[tST])
    s.add("dve", lambda e: e.tensor_scalar(dst, src, ST[:, 12:13], ST[:, 14:15], ALU.subtract, ALU.mult), r=[tsrc, tST], w=[tdst])
    s.add("dve", lambda e: e.tensor_tensor(out=dst, in0=dst, in1=G, op=ALU.mult), r=[tdst, tG], w=[tdst])
    s.add("dve", lambda e: e.tensor_tensor(out=dst, in0=dst, in1=Bt, op=ALU.add), r=[tdst, tG], w=[tdst])


def transpose_rows(s, c, src, tsrc, XT, tXT, col0, nchunk=8):
    for g0 in range(0, nchunk, 4):
        n = min(4, nchunk - g0)
        ps, tps = ps_half(c)
        for j in range(n):
            dc = g0 + j
            s.add("pe", lambda e, ps=ps, j=j, dc=dc: e.transpose(ps[:, j * 128:(j + 1) * 128], src[:, dc * 128:(dc + 1) * 128], c.IDN[:]),
                  r=[tsrc, c.tIDN], w=[tps])
        s.add("act", lambda e, ps=ps, g0=g0, n=n: e.activation(
            out=XT[:, g0:g0 + n, col0:col0 + 128], in_=ps[:, 0:n * 128].rearrange("p (a b) -> p a b", b=128), func=AF.Copy),
            r=[tps], w=[tXT])


def expert_ffn(s, c, XS, tXS, C, wg, wu, wd, dff, aT, taT, WG, tWG, WU, tWU, WD, tWD, out_cb, wd_preloaded=False):
    nfc = dff // 128
    ncb = dff // 256
    wg_v = wg.rearrange("(c p) f -> p c f", p=128)
    wu_v = wu.rearrange("(c p) f -> p c f", p=128)
    wd_v = wd.rearrange("(c p) n -> p c n", p=128)
    groups = [(g0, min(512, C - g0)) for g0 in range(0, C, 512)]
    for cb in range(ncb):
        b = cb % 2
        s.dma("pool", WG[b][:], wg_v[:, :, cb * 256:(cb + 1) * 256], w=[tWG[b]])
        s.dma("pool", WU[b][:], wu_v[:, :, cb * 256:(cb + 1) * 256], w=[tWU[b]])
        if not wd_preloaded:
            s.dma("pool", WD[:, 2 * cb:2 * cb + 2, :], wd_v[:, 2 * cb:2 * cb + 2, :], w=[tWD])
        for fl in range(2):
            fc = cb * 2 + fl
            for (g0, gn) in groups:
                pg, tpg = ps_half(c)
                pu, tpu = ps_half(c)
                for dc in range(8):
                    s.add("pe", lambda e, pg=pg, b=b, dc=dc, fl=fl, g0=g0, gn=gn: e.matmul(
                        pg[:, 0:gn], WG[b][:, dc, fl * 128:(fl + 1) * 128], XS[:, dc, g0:g0 + gn], start=(dc == 0), stop=(dc == 7)),
                        r=[tWG[b], tXS], w=[tpg])
                for dc in range(8):
                    s.add("pe", lambda e, pu=pu, b=b, dc=dc, fl=fl, g0=g0, gn=gn: e.matmul(
                        pu[:, 0:gn], WU[b][:, dc, fl * 128:(fl + 1) * 128], XS[:, dc, g0:g0 + gn], start=(dc == 0), stop=(dc == 7)),
                        r=[tWU[b], tXS], w=[tpu])
                k = c.sgrr % 2
                c.sgrr += 1
                SG, tSG = c.SG[k], c.tSG[k]
                s.add("act", lambda e, pg=pg, SG=SG, gn=gn: e.activation(out=SG[:, 0:gn], in_=pg[:, 0:gn], func=AF.Silu),
                      r=[tpg], w=[tSG])
                s.add("dve", lambda e, pu=pu, SG=SG, fc=fc, g0=g0, gn=gn: e.tensor_tensor(
                    out=aT[:, fc, g0:g0 + gn], in0=SG[:, 0:gn], in1=pu[:, 0:gn], op=ALU.mult),
                    r=[tSG, tpu], w=[taT])
    for sc in range(C // 128):
        py, tpy = ps_full(c)
        for nh in range(2):
            for fc in range(nfc):
                s.add("pe", lambda e, py=py, nh=nh, fc=fc, sc=sc: e.matmul(
                    py[:, nh * 512:(nh + 1) * 512], aT[:, fc, sc * 128:(sc + 1) * 128], WD[:, fc, nh * 512:(nh + 1) * 512],
                    start=(fc == 0), stop=(fc == nfc - 1)),
                    r=[taT, tWD], w=[tpy[nh]])
        out_cb(sc, py, tpy)


def build_B_dense(nc, st, s, c, io):
    DFF = 2816
    NFC = DFF // 128
    sb = lambda name, shape, dt=F32: st.enter_context(nc.sbuf_tensor(uname(name), list(shape), dt))
    c.IDN = sb("IDN", [128, 128]); c.tIDN = Tok("idn")
    c.ST = sb("ST", [128, 16]); c.tST = Tok("st")
    c.EPSC = sb("EPSC", [128, 1]); c.tEPSC = Tok("eps")
    s.add("dve", lambda e: e.memset(c.EPSC[:], EPS), w=[c.tEPSC])
    c.SG = [sb("SG%d" % i, [128, 512], BF16) for i in range(2)]; c.tSG = [Tok(), Tok()]; c.sgrr = 0
    BIG = sb("BIG", [128, NFC * 1024], BF16)
    OT = BIG[:, 0:8 * NT].rearrange("p (c t) -> p c t", t=NT); tBIG = Tok("big")
    aT = BIG[:, 0:NFC * 1024].rearrange("p (c t) -> p c t", t=1024)
    W16 = sb("W16", [128, 8, 1024], BF16); tW16 = Tok("w16")
    LNP = sb("LNP", [128, 2, 1024]); tLNP = Tok("lnp")
    XT = sb("XT", [128, 8, NT], BF16); tXT = Tok("xt")
    XR = [sb("XR%d" % i, [128, 1024]) for i in range(3)]; tXR = [Tok(), Tok(), Tok()]
    XR3, tXR3 = XR, tXR
    TT = [sb("TT%d" % i, [128, 1024]) for i in range(2)]; tTT = [Tok(), Tok()]
    WG = [sb("WG%d" % i, [128, 8, 256], BF16) for i in range(2)]; tWG = [Tok(), Tok()]
    WU = [sb("WU%d" % i, [128, 8, 256], BF16) for i in range(2)]; tWU = [Tok(), Tok()]
    WD = sb("WD", [128, NFC, 1024], BF16); tWD = Tok("wd")
    PP = sb("PP", [128, 2, 1024], BF16); tPP = Tok("pp")
    PTt = sb("PTt", [128, 2, NT], BF16); tPTt = Tok("ptt")
    PR = [sb("PR%d" % i, [128, 256]) for i in range(2)]; tPR = [Tok(), Tok()]
    xs1 = dram(nc, uname("xs1"), [NT, D], kind="Internal")
    xs2 = dram(nc, uname("xs2"), [NT, D], kind="Internal")
    txs1, txs2 = Tok("xs1"), Tok("xs2")

    def load_ln(k):
        s.dma("sp", LNP[:], io["lnp"][2 * k:2 * k + 2, :].partition_broadcast(128), w=[tLNP])

    s.dma("sp", c.IDN[:], io["idn"], w=[c.tIDN])
    load_oT(nc, st, s, io, OT, tBIG, XT, tXT)
    s.dma("pool", W16[:], io["w_out"].rearrange("(c p) n -> p c n", p=128), w=[tW16])
    load_ln(0)
    for i in range(NT // 128):
        k = i % 2
        k3 = i % 3
        s.dma("sp", XR3[k3][:], io["xres"][i * 128:(i + 1) * 128, :], w=[tXR3[k3]])
        py, tpy = ps_full(c)
        for nh in range(2):
            for fc in range(8):
                s.add("pe", lambda e, py=py, nh=nh, fc=fc, i=i: e.matmul(
                    py[:, nh * 512:(nh + 1) * 512], OT[:, fc, i * 128:(i + 1) * 128], W16[:, fc, nh * 512:(nh + 1) * 512],
                    start=(fc == 0), stop=(fc == 7)), r=[tBIG, tW16], w=[tpy[nh]])
        s.add("dve", lambda e, py=py, k=k, k3=k3: e.scalar_tensor_tensor(out=TT[k][:], in0=XR3[k3][:], scalar=ALPHA, in1=py[:], op0=ALU.mult, op1=ALU.add),
              r=[tXR3[k3], tpy[0], tpy[1]], w=[tTT[k]])
        layernorm_rows(s, c, TT[k][:], tTT[k], XR3[i % 3][:], tXR3[i % 3], LNP[:, 0, :], LNP[:, 1, :], tLNP)
        s.dma("sp", xs1[i * 128:(i + 1) * 128, :], XR3[i % 3][:], r=[tXR3[i % 3]], w=[txs1])
        if i >= 1:
            transpose_rows(s, c, XR3[(i - 1) % 3][:], tXR3[(i - 1) % 3], XT, tXT, (i - 1) * 128)
    transpose_rows(s, c, XR3[15 % 3][:], tXR3[15 % 3], XT, tXT, 15 * 128)
    load_ln(1)
    s.dma("pool", W16[:], io["ple_gate"].rearrange("(c p) n -> p c n", p=128), w=[tW16])
    s.dma("pool", PP[:], io["ple_proj"].rearrange("(c p) n -> p c n", p=128), w=[tPP])
    for grp in range(NT // 1024):
        def out_cb(sc, py, tpy, grp=grp):
            i = grp * 8 + sc
            k = i % 2
            s.dma("sp", XR[k][:], xs1[i * 128:(i + 1) * 128, :], r=[txs1], w=[tXR[k]])
            s.add("dve", lambda e: e.scalar_tensor_tensor(out=TT[k][:], in0=XR[k][:], scalar=ALPHA, in1=py[:], op0=ALU.mult, op1=ALU.add),
                  r=[tXR[k], tpy[0], tpy[1]], w=[tTT[k]])
            layernorm_rows(s, c, TT[k][:], tTT[k], XR[k][:], tXR[k], LNP[:, 0, :], LNP[:, 1, :], tLNP)
            s.dma("sp", xs2[i * 128:(i + 1) * 128, :], XR[k][:], r=[tXR[k]], w=[txs2])

        expert_ffn(s, c, XT[:, :, grp * 1024:(grp + 1) * 1024], tXT, 1024, io["wg"], io["wu"], io["wd"], DFF,
                   aT, tBIG, WG, tWG, WU, tWU, WD, tWD, out_cb, wd_preloaded=(grp > 0))
    for i in range(NT // 128):
        k = i % 2
        s.dma("sp", XR[k][:], xs2[i * 128:(i + 1) * 128, :], r=[txs2], w=[tXR[k]])
        transpose_rows(s, c, XR[k][:], tXR[k], XT, tXT, i * 128)
        s.dma("sp", PR[k][:], io["p"][i * 128:(i + 1) * 128, :], w=[tPR[k]])
        transpose_rows(s, c, PR[k][:], tPR[k], PTt, tPTt, i * 128, nchunk=2)
    load_ln(2)
    outs = []
    touts = []
    for i in range(NT // 128):
        k = i % 2
        s.dma("sp", XR[k][:], xs2[i * 128:(i + 1) * 128, :], r=[txs2], w=[tXR[k]])
        pg, tpg = ps_full(c)
        pe_, tpe = ps_full(c)
        for nh in range(2):
            for dc in range(8):
                s.add("pe", lambda e, pg=pg, nh=nh, dc=dc, i=i: e.matmul(
                    pg[:, nh * 512:(nh + 1) * 512], XT[:, dc, i * 128:(i + 1) * 128], W16[:, dc, nh * 512:(nh + 1) * 512],
                    start=(dc == 0), stop=(dc == 7)), r=[tXT, tW16], w=[tpg[nh]])
            for dc in range(2):
                s.add("pe", lambda e, pe_=pe_, nh=nh, dc=dc, i=i: e.matmul(
                    pe_[:, nh * 512:(nh + 1) * 512], PTt[:, dc, i * 128:(i + 1) * 128], PP[:, dc, nh * 512:(nh + 1) * 512],
                    start=(dc == 0), stop=(dc == 1)), r=[tPTt, tPP], w=[tpe[nh]])
        s.add("act", lambda e, pg=pg, k=k: e.activation(out=TT[k][:], in_=pg[:], func=AF.Sigmoid), r=[tpg[0], tpg[1]], w=[tTT[k]])
        s.add("dve", lambda e, pe_=pe_, k=k: e.tensor_tensor(out=TT[k][:], in0=TT[k][:], in1=pe_[:], op=ALU.mult),
              r=[tTT[k], tpe[0], tpe[1]], w=[tTT[k]])
        s.add("dve", lambda e, k=k: e.scalar_tensor_tensor(out=TT[k][:], in0=XR[k][:], scalar=ALPHA, in1=TT[k][:], op0=ALU.mult, op1=ALU.add),
              r=[tXR[k], tTT[k]], w=[tTT[k]])
        layernorm_rows(s, c, TT[k][:], tTT[k], XR[k][:], tXR[k], LNP[:, 0, :], LNP[:, 1, :], tLNP)
        touts.append(Tok())
        outs.append(s.dma("sp", io["out"][i * 128:(i + 1) * 128, :], XR[k][:], r=[tXR[k]], w=[touts[-1]]))
        if "xg" in io and i % 4 == 3:
            j = i // 4
            outs.append(s.add("pool", lambda e, j=j: e.collective_compute("AllGather", ALU.bypass, replica_groups=RG_PAIRS,
                                                                          ins=[io["out"][j * 512:(j + 1) * 512, :].opt()], outs=[io["xg"][j].opt()]),
                              r=touts[-4:], w=[Tok()], dma=True, inc=1))
    return outs


CAP = 640
NSC = CAP // 128


def build_B_moe(nc, st, s, c, io):
    DFF = 3584
    NFC = DFF // 128
    sb = lambda name, shape, dt=F32: st.enter_context(nc.sbuf_tensor(uname(name), list(shape), dt))
    c.IDN = sb("IDN", [128, 128]); c.tIDN = Tok("idn")
    IDNb = sb("IDNb", [128, 128], BF16); tIDNb = Tok()
    UT = sb("UT", [128, 128], BF16); ONESb = sb("ONESb", [128, 128], BF16); tUT = Tok()
    IOTA = sb("IOTA", [128, CAP]); tIOTA = Tok()
    c.ST = sb("ST", [128, 16]); c.tST = Tok("st")
    c.EPSC = sb("EPSC", [128, 1]); c.tEPSC = Tok("eps")
    s.add("dve", lambda e: e.memset(c.EPSC[:], EPS), w=[c.tEPSC])
    BIG = sb("BIG", [128, NFC * CAP], BF16); tBIG = Tok("big")
    OT = BIG[:, 0:8 * NT].rearrange("p (c t) -> p c t", t=NT)
    aT = BIG[:, 0:NFC * CAP].rearrange("p (c t) -> p c t", t=CAP)
    W16 = sb("W16", [128, 16 * CAP], BF16); tW16 = Tok("w16")
    WO = W16[:, 0:8 * 1024].rearrange("p (c n) -> p c n", n=1024)
    SE = W16[:, 0:16 * CAP].rearrange("p (i s) -> p i s", s=CAP)
    LNP = sb("LNP", [128, 2, 1024]); tLNP = Tok("lnp")
    XTt = sb("XT", [128, 8 * NT], BF16); tXT = Tok("xt")
    XT = XTt[:, :].rearrange("p (c t) -> p c t", t=NT)
    X1B = XTt[:, :].rearrange("p (i d) -> p i d", d=1024)
    XR = [sb("XR%d" % i, [128, 1024]) for i in range(2)]; tXR = [Tok(), Tok()]
    TT = [sb("TT%d" % i, [128, 1024]) for i in range(2)]; tTT = [Tok(), Tok()]
    WG = [sb("WG%d" % i, [128, 8, 256], BF16) for i in range(2)]; tWG = [Tok(), Tok()]
    WU = [sb("WU%d" % i, [128, 8, 256], BF16) for i in range(2)]; tWU = [Tok(), Tok()]
    WD = sb("WD", [128, NFC, 1024], BF16); tWD = Tok("wd")
    PP = BIG[:, 0:2048].rearrange("p (c n) -> p c n", n=1024); tPP = tBIG
    RG_ = [[sb("RG%d_%d" % (i, j), [128, 1024], BF16) for j in range(2)] for i in range(1)]; tRG = [[Tok(), Tok()]]
    RG_.append(RG_[0]); tRG.append(tRG[0])
    Of1 = sb("Of1", [128, 16, 8]); BASE = sb("BASE", [128, 16, 8]); IDXF = sb("IDXF", [128, 16, 2]); IDXU = sb("IDXU", [128, 16, 2], mybir.dt.uint32); tIDX = Tok()
    XGt = sb("XG", [128, 8 * CAP], BF16); tXG = Tok("xg")
    XGs = [XGt[:, :].rearrange("p (c s) -> p c s", s=CAP), XTt[:, 0:8 * CAP].rearrange("p (c s) -> p c s", s=CAP)]
    YEs = [XGt[:, 0:NSC * 1024].rearrange("p (s n) -> p s n", n=1024), XTt[:, 0:NSC * 1024].rearrange("p (s n) -> p s n", n=1024)]
    tXGs = [tXG, tXT]
    XG = XGs[0]
    PTt = XGt[:, 0:2 * NT].rearrange("p (c t) -> p c t", t=NT)
    PR = [TT[i][:, 0:256] for i in range(2)]; tPR = tTT
    XTr = sb("XTr", [128, 8, 128], BF16); tXTr = Tok()
    c.SG = [XTr[:, 0:4, :].rearrange("p a b -> p (a b)"), XTr[:, 4:8, :].rearrange("p a b -> p (a b)")]; c.tSG = [Tok(), Tok()]; c.sgrr = 0
    WR = sb("WR", [128, 8, 8], BF16); tWR = Tok()
    RT = sb("RT", [128, 64]); tRT = Tok()
    Af = sb("Af", [128, 16, 8]); Ab = sb("Ab", [128, 16, 8], BF16); Gb = sb("Gb", [128, 16, 8], BF16); tAG = Tok()
    RKM = sb("RKM", [128, 16, 8]); tRKM = Tok()
    GSLs = [sb("GSL%d" % i, [128, NSC, 4]) for i in range(2)]; tGSLs = [Tok(), Tok()]
    G3 = sb("G3", [128, 16, 3], BF16); tG3 = Tok()
    TIDX = sb("TIDX", [128, 8]); TIDU = sb("TIDU", [128, 8], mybir.dt.uint32); tTID = Tok()
    YE = XGt[:, 0:NSC * 1024].rearrange("p (s n) -> p s n", n=1024); tYE = tXG
    xs1 = dram(nc, uname("xs1"), [NT, D], kind="Internal")
    xs2 = dram(nc, uname("xs2"), [NT, D], kind="Internal")
    yed = dram(nc, uname("yed"), [8 * CAP, D], BF16, kind="Internal")
    tyed = Tok("yed")
    txs1, txs2 = Tok("xs1"), Tok("xs2")

    def load_ln(k):
        s.dma("sp", LNP[:], io["lnp"][2 * k:2 * k + 2, :].partition_broadcast(128), w=[tLNP])

    s.dma("sp", c.IDN[:], io["idn"], w=[c.tIDN])
    s.dma("pool", IDNb[:], io["idn"], w=[tIDNb])
    s.dma("pool", UT[:], io["ut"], w=[tUT])
    s.add("pool", lambda e: e.memset(ONESb[:], 1.0), w=[tUT])
    s.dma("sp", IOTA[:], io["iota"], w=[tIOTA])
    s.dma("pool", G3[:, :, 1:3], io["pcol"], w=[tG3])
    load_oT(nc, st, s, io, OT, tBIG, XT, tXT)
    s.dma("pool", WO, io["w_out"].rearrange("(c p) n -> p c n", p=128), w=[tW16])
    s.dma("pool", WR[:], io["router"].rearrange("(c p) n -> p c n", p=128), w=[tWR])
    load_ln(0)
    def route_tile(i, k):
        transpose_rows(s, c, XR[k][:], tXR[k], XTr, tXTr, 0)
        pl, tpl = ps_half(c)
        for dc in range(8):
            s.add("pe", lambda e, pl=pl, dc=dc: e.matmul(pl[:, 0:8], XTr[:, dc, :], WR[:, dc, :], start=(dc == 0), stop=(dc == 7)), r=[tXTr, tWR], w=[tpl])
        LG = RT[:, 0:8]; MX8 = RT[:, 8:16]; OH1 = RT[:, 16:24]; OH2 = RT[:, 24:32]; DD = RT[:, 32:33]; G2 = RT[:, 33:34]; G1 = RT[:, 34:35]; GF = RT[:, 40:48]
        s.add("dve", lambda e, pl=pl: e.tensor_copy(out=LG, in_=pl[:, 0:8]), r=[tpl], w=[tRT])
        s.add("dve", lambda e: e.max(out=MX8, in_=LG), r=[tRT], w=[tRT])
        s.add("dve", lambda e: e.tensor_scalar(OH1, LG, RT[:, 8:9], None, ALU.is_equal), r=[tRT], w=[tRT])
        s.add("dve", lambda e: e.tensor_scalar(OH2, LG, RT[:, 9:10], None, ALU.is_equal), r=[tRT], w=[tRT])
        s.add("dve", lambda e: e.tensor_tensor(out=DD, in0=RT[:, 9:10], in1=RT[:, 8:9], op=ALU.subtract), r=[tRT], w=[tRT])
        s.add("act", lambda e: e.activation(out=G2, in_=DD, func=AF.Sigmoid), r=[tRT], w=[tRT])
        s.add("dve", lambda e: e.tensor_scalar(G1, G2, -1.0, 1.0, ALU.mult, ALU.add), r=[tRT], w=[tRT])
        s.add("dve", lambda e, i=i: e.tensor_copy(out=Of1[:, i, :], in_=OH1), r=[tRT], w=[tAG])
        s.add("dve", lambda e, i=i: e.tensor_tensor(out=Af[:, i, :], in0=OH1, in1=OH2, op=ALU.add), r=[tRT], w=[tAG])
        s.add("dve", lambda e, i=i: e.tensor_copy(out=Ab[:, i, :], in_=Af[:, i, :]), r=[tAG], w=[tAG])
        s.add("dve", lambda e: e.tensor_scalar(GF, OH1, G1, None, ALU.mult), r=[tRT], w=[tRT])
        s.add("dve", lambda e, i=i: e.scalar_tensor_tensor(out=Gb[:, i, :], in0=OH2, scalar=G2, in1=GF, op0=ALU.mult, op1=ALU.add), r=[tRT], w=[tAG])

    for i in range(NT // 128):
        k = i % 2
        s.dma("sp", XR[k][:], io["xres"][i * 128:(i + 1) * 128, :], w=[tXR[k]])
        py, tpy = ps_full(c)
        for nh in range(2):
            for fc in range(8):
                s.add("pe", lambda e, py=py, nh=nh, fc=fc, i=i: e.matmul(
                    py[:, nh * 512:(nh + 1) * 512], OT[:, fc, i * 128:(i + 1) * 128], WO[:, fc, nh * 512:(nh + 1) * 512],
                    start=(fc == 0), stop=(fc == 7)), r=[tBIG, tW16], w=[tpy[nh]])
        s.add("dve", lambda e, py=py, k=k: e.scalar_tensor_tensor(out=TT[k][:], in0=XR[k][:], scalar=ALPHA, in1=py[:], op0=ALU.mult, op1=ALU.add),
              r=[tXR[k], tpy[0], tpy[1]], w=[tTT[k]])
        layernorm_rows(s, c, TT[k][:], tTT[k], XR[k][:], tXR[k], LNP[:, 0, :], LNP[:, 1, :], tLNP)
        s.dma("sp", xs1[i * 128:(i + 1) * 128, :], XR[k][:], r=[tXR[k]], w=[txs1])
        if i >= 1:
            route_tile(i - 1, (i - 1) % 2)
    route_tile(15, 15 % 2)
    pr, tpr = ps_half(c)
    first = True
    for i in range(16):
        for j in range(i + 1):
            s.add("pe", lambda e, pr=pr, i=i, j=j, st_=first: e.matmul(pr[:, i * 8:(i + 1) * 8], (UT if j == i else ONESb)[:], Ab[:, j, :],
                                                                      start=st_, stop=(j == i), skip_group_check=True), r=[tUT, tAG], w=[tpr])
            first = False
    s.add("dve", lambda e, pr=pr: e.scalar_tensor_tensor(out=RKM[:], in0=pr[:, 0:128].rearrange("p (i e) -> p i e", e=8), scalar=1.0, in1=Af[:], op0=ALU.add, op1=ALU.mult),
          r=[tpr, tAG], w=[tRKM])
    s.add("dve", lambda e: e.tensor_scalar(RKM[:], RKM[:], -1.0, None, ALU.add), r=[tRKM], w=[tRKM])
    s.add("dve", lambda e: e.tensor_scalar(RT[:, 48:56], IOTA[:, 0:8], float(CAP), None, ALU.mult), r=[tIOTA, tRT], w=[tRT])
    s.add("dve", lambda e: e.tensor_tensor(out=BASE[:], in0=RKM[:], in1=RT[:, 48:56].unsqueeze(1).to_broadcast([128, 16, 8]), op=ALU.add), r=[tRKM, tRT], w=[tIDX])
    s.add("dve", lambda e: e.tensor_tensor(out=RKM[:], in0=Of1[:], in1=BASE[:], op=ALU.mult), r=[tAG, tIDX, tRKM], w=[tRKM])
    s.add("dve", lambda e: e.reduce_sum(out=IDXF[:, :, 0], in_=RKM[:], axis=AX.X), r=[tRKM], w=[tIDX])
    s.add("dve", lambda e: e.tensor_tensor(out=RKM[:], in0=Af[:], in1=Of1[:], op=ALU.subtract), r=[tAG, tIDX, tRKM], w=[tRKM])
    s.add("dve", lambda e: e.tensor_tensor(out=RKM[:], in0=RKM[:], in1=BASE[:], op=ALU.mult), r=[tIDX, tRKM], w=[tRKM])
    s.add("dve", lambda e: e.reduce_sum(out=IDXF[:, :, 1], in_=RKM[:], axis=AX.X), r=[tRKM], w=[tIDX])
    s.add("dve", lambda e: e.tensor_copy(out=IDXU[:], in_=IDXF[:]), r=[tIDX], w=[tIDX])
    s.add("dve", lambda e: e.scalar_tensor_tensor(out=RKM[:], in0=BASE[:], scalar=1.0, in1=Af[:], op0=ALU.add, op1=ALU.mult), r=[tIDX, tAG, tRKM], w=[tRKM])
    s.add("dve", lambda e: e.tensor_tensor(out=BASE[:], in0=Af[:], in1=RT[:, 48:56].unsqueeze(1).to_broadcast([128, 16, 8]), op=ALU.mult), r=[tAG, tRT, tIDX], w=[tIDX])
    s.add("dve", lambda e: e.tensor_tensor(out=RKM[:], in0=RKM[:], in1=BASE[:], op=ALU.subtract), r=[tIDX, tRKM], w=[tRKM])
    s.add("dve", lambda e: e.tensor_scalar(RKM[:], RKM[:], -1.0, None, ALU.add), r=[tRKM], w=[tRKM])
    load_ln(1)
    groups = [(0, 512), (512, CAP - 512)]

    def build_se(ex_):
        for i in range(16):
            s.add("dve", lambda e, i=i, ex_=ex_: e.tensor_scalar(SE[:, i, :], IOTA[:], RKM[:, i, ex_:ex_ + 1], None, ALU.is_equal), r=[tIOTA, tRKM], w=[tW16])


    LAND = [TT[0], TT[1], XR[0], XR[1]]; tLAND = [tTT[0], tTT[1], tXR[0], tXR[1]]

    def gather_prep(ex):
        s.add("dve", lambda e, ex=ex: e.tensor_copy(out=G3[:, :, 0], in_=Gb[:, :, ex]), r=[tAG, tG3], w=[tG3])
        pgs, tpgs = ps_half(c)
        first = True
        for sc in range(NSC):
            for i in range(16):
                s.add("pe", lambda e, pgs=pgs, sc=sc, i=i, st_=first: e.matmul(pgs[:, sc * 4:sc * 4 + 3], SE[:, i, sc * 128:(sc + 1) * 128], G3[:, i, :],
                                                                              start=st_, stop=(i == 15), skip_group_check=True), r=[tW16, tG3], w=[tpgs])
                first = False
        s.add("dve", lambda e, pgs=pgs: e.tensor_copy(out=GSLs[ex % 2][:], in_=pgs[:, 0:NSC * 4].rearrange("p (s k) -> p s k", k=4)), r=[tpgs], w=[tGSLs[ex % 2]])
        s.add("dve", lambda e: e.scalar_tensor_tensor(out=TIDX[:, 0:NSC], in0=GSLs[ex % 2][:, :, 2], scalar=128.0, in1=GSLs[ex % 2][:, :, 1], op0=ALU.mult, op1=ALU.add),
              r=[tGSLs[ex % 2]], w=[tTID])
        s.add("dve", lambda e: e.tensor_copy(out=TIDU[:, 0:NSC], in_=TIDX[:, 0:NSC]), r=[tTID], w=[tTID])
        for sc in range(min(NSC, 4)):
            s.add("pool", lambda e, sc=sc: e.indirect_dma_start(out=LAND[sc][:], out_offset=None, in_=xs1,
                                                               in_offset=bass.IndirectOffsetOnAxis(ap=TIDU[:, sc:sc + 1], axis=0)),
                  r=[tTID, txs1], w=[tLAND[sc]], dma=True)

    def gather_fin(ex):
        XGd, tXGd = XGs[ex % 2], tXGs[ex % 2]
        for sc in range(NSC):
            if sc >= 4:
                s.add("pool", lambda e, sc=sc: e.indirect_dma_start(out=LAND[sc % 4][:], out_offset=None, in_=xs1,
                                                                   in_offset=bass.IndirectOffsetOnAxis(ap=TIDU[:, sc:sc + 1], axis=0)),
                      r=[tTID, txs1], w=[tLAND[sc % 4]], dma=True)
            transpose_rows(s, c, LAND[sc % 4][:], tLAND[sc % 4], XGd, tXGd, sc * 128)

    build_se(0)
    gather_prep(0)
    gather_fin(0)
    for ex in range(8):
        wg_v = io["mg"][ex].rearrange("(c p) f -> p c f", p=128)
        wu_v = io["mu"][ex].rearrange("(c p) f -> p c f", p=128)
        wd_v = io["md"][ex].rearrange("(c p) n -> p c n", p=128)
        if ex < 7:
            build_se(ex + 1)
        for cb in range(DFF // 256):
            b = cb % 2
            s.dma("pool", WG[b][:], wg_v[:, :, cb * 256:(cb + 1) * 256], w=[tWG[b]])
            s.dma("pool", WU[b][:], wu_v[:, :, cb * 256:(cb + 1) * 256], w=[tWU[b]])
            s.dma("pool", WD[:, 2 * cb:2 * cb + 2, :], wd_v[:, 2 * cb:2 * cb + 2, :], w=[tWD])
            for fl in range(2):
                fc = cb * 2 + fl
                for (g0, gn) in groups:
                    pg, tpg = ps_half(c)
                    pu, tpu = ps_half(c)
                    for dc in range(8):
                        s.add("pe", lambda e, pg=pg, b=b, dc=dc, fl=fl, g0=g0, gn=gn, ex=ex: e.matmul(
                            pg[:, 0:gn], WG[b][:, dc, fl * 128:(fl + 1) * 128], XGs[ex % 2][:, dc, g0:g0 + gn], start=(dc == 0), stop=(dc == 7)),
                            r=[tWG[b], tXGs[ex % 2]], w=[tpg])
                    for dc in range(8):
                        s.add("pe", lambda e, pu=pu, b=b, dc=dc, fl=fl, g0=g0, gn=gn, ex=ex: e.matmul(
                            pu[:, 0:gn], WU[b][:, dc, fl * 128:(fl + 1) * 128], XGs[ex % 2][:, dc, g0:g0 + gn], start=(dc == 0), stop=(dc == 7)),
                            r=[tWU[b], tXGs[ex % 2]], w=[tpu])
                    k = c.sgrr % 2
                    c.sgrr += 1
                    SG, tSG = c.SG[k], c.tSG[k]
                    s.add("act", lambda e, pg=pg, SG=SG, gn=gn: e.activation(out=SG[:, 0:gn], in_=pg[:, 0:gn], func=AF.Silu), r=[tpg], w=[tSG])
                    s.add("dve", lambda e, pu=pu, SG=SG, fc=fc, g0=g0, gn=gn: e.tensor_tensor(
                        out=aT[:, fc, g0:g0 + gn], in0=SG[:, 0:gn], in1=pu[:, 0:gn], op=ALU.mult), r=[tSG, tpu], w=[tBIG])
        if ex < 7:
            gather_prep(ex + 1)
        for sc in range(NSC):
            py, tpy = ps_full(c)
            for nh in range(2):
                for fc in range(NFC):
                    s.add("pe", lambda e, py=py, fc=fc, sc=sc, nh=nh: e.matmul(py[:, nh * 512:(nh + 1) * 512], aT[:, fc, sc * 128:(sc + 1) * 128], WD[:, fc, nh * 512:(nh + 1) * 512],
                                                                            start=(fc == 0), stop=(fc == NFC - 1)), r=[tBIG, tWD], w=[tpy[nh]])
            s.add("dve", lambda e, py=py, sc=sc, ex=ex: e.tensor_scalar(YEs[ex % 2][:, sc, :], py[:], GSLs[ex % 2][:, sc, 0:1], None, ALU.mult),
                  r=[tpy[0], tpy[1], tGSLs[ex % 2]], w=[tXGs[ex % 2]])
        if ex < 7:
            gather_fin(ex + 1)
        s.dma("sp", yed[ex * CAP:(ex + 1) * CAP, :].rearrange("(s p) n -> p s n", p=128), YEs[ex % 2], r=[tXGs[ex % 2]], w=[tyed])
    for i in range(16):
        k = i % 2
        rows = slice(i * 128, (i + 1) * 128)
        for j in range(2):
            s.add("pool", lambda e, i=i, j=j, k=k: e.indirect_dma_start(out=RG_[k][j][:], out_offset=None, in_=yed,
                                                                       in_offset=bass.IndirectOffsetOnAxis(ap=IDXU[:, i, j:j + 1], axis=0)),
                  r=[tIDX, tyed], w=[tRG[k][j]], dma=True)
        s.dma("sp", XR[k][:], xs1[rows, :], r=[txs1], w=[tXR[k]])
        s.add("dve", lambda e, k=k: e.tensor_tensor(out=TT[k][:], in0=RG_[k][0][:], in1=RG_[k][1][:], op=ALU.add), r=[tRG[k][0], tRG[k][1]], w=[tTT[k]])
        s.add("dve", lambda e, k=k: e.scalar_tensor_tensor(out=TT[k][:], in0=XR[k][:], scalar=ALPHA, in1=TT[k][:], op0=ALU.mult, op1=ALU.add),
              r=[tXR[k], tTT[k]], w=[tTT[k]])
        layernorm_rows(s, c, TT[k][:], tTT[k], XR[k][:], tXR[k], LNP[:, 0, :], LNP[:, 1, :], tLNP)
        s.dma("sp", xs2[rows, :], XR[k][:], r=[tXR[k]], w=[txs2])
        transpose_rows(s, c, XR[k][:], tXR[k], XT, tXT, i * 128)
    W16v = W16[:, 0:8 * 1024].rearrange("p (c n) -> p c n", n=1024)
    s.dma("pool", W16v, io["ple_gate"].rearrange("(c p) n -> p c n", p=128), w=[tW16])
    s.dma("pool", PP[:], io["ple_proj"].rearrange("(c p) n -> p c n", p=128), w=[tPP])
    for i in range(NT // 128):
        k = i % 2
        s.dma("sp", PR[k], io["p"][i * 128:(i + 1) * 128, :], w=[tPR[k]])
        transpose_rows(s, c, PR[k], tPR[k], PTt, tXG, i * 128, nchunk=2)
    load_ln(2)
    outs = []
    touts = []
    for i in range(NT // 128):
        k = i % 2
        s.dma("sp", XR[k][:], xs2[i * 128:(i + 1) * 128, :], r=[txs2], w=[tXR[k]])
        pg, tpg = ps_full(c)
        pe_, tpe = ps_full(c)
        for nh in range(2):
            for dc in range(8):
                s.add("pe", lambda e, pg=pg, nh=nh, dc=dc, i=i: e.matmul(
                    pg[:, nh * 512:(nh + 1) * 512], XT[:, dc, i * 128:(i + 1) * 128], W16v[:, dc, nh * 512:(nh + 1) * 512],
                    start=(dc == 0), stop=(dc == 7)), r=[tXT, tW16], w=[tpg[nh]])
            for dc in range(2):
                s.add("pe", lambda e, pe_=pe_, nh=nh, dc=dc, i=i: e.matmul(
                    pe_[:, nh * 512:(nh + 1) * 512], PTt[:, dc, i * 128:(i + 1) * 128], PP[:, dc, nh * 512:(nh + 1) * 512],
                    start=(dc == 0), stop=(dc == 1)), r=[tXG, tPP], w=[tpe[nh]])
        s.add("act", lambda e, pg=pg, k=k: e.activation(out=TT[k][:], in_=pg[:], func=AF.Sigmoid), r=[tpg[0], tpg[1]], w=[tTT[k]])
        s.add("dve", lambda e, pe_=pe_, k=k: e.tensor_tensor(out=TT[k][:], in0=TT[k][:], in1=pe_[:], op=ALU.mult),
              r=[tTT[k], tpe[0], tpe[1]], w=[tTT[k]])
        s.add("dve", lambda e, k=k: e.scalar_tensor_tensor(out=TT[k][:], in0=XR[k][:], scalar=ALPHA, in1=TT[k][:], op0=ALU.mult, op1=ALU.add),
              r=[tXR[k], tTT[k]], w=[tTT[k]])
        layernorm_rows(s, c, TT[k][:], tTT[k], XR[k][:], tXR[k], LNP[:, 0, :], LNP[:, 1, :], tLNP)
        touts.append(Tok())
        outs.append(s.dma("sp", io["out"][i * 128:(i + 1) * 128, :], XR[k][:], r=[tXR[k]], w=[touts[-1]]))
        if "xg" in io and i % 4 == 3:
            j = i // 4
            outs.append(s.add("pool", lambda e, j=j: e.collective_compute("AllGather", ALU.bypass, replica_groups=RG_PAIRS,
                                                                          ins=[io["out"][j * 512:(j + 1) * 512, :].opt()], outs=[io["xg"][j].opt()]),
                              r=touts[-4:], w=[Tok()], dma=True, inc=1))
    return outs

T = 4096
NEG = -30000.0
NQT = 8
SL = [2.0 ** (-i / 2.0) for i in range(1, 17)]
SLOPE_C = SL[0:8]
SLOPE_A = [SL[8], SL[10], SL[12], SL[14]]
SLOPE_B = [SL[9], SL[11], SL[13], SL[15]]
C_KG0, C_KG1, C_KS, C_KW, C_KC, C_KB0, C_KB1 = 0, 128, 256, 320, 384, 448, 576
C_V = 704
C_QA = 1024
C_QB = 1280
C_QC = 1536
C_G = 1792
NW = 1798
M_CAUS, M_WM, M_SM, M_CM = 0, 4, 8, 13
NMASK = 18


def mask_tables():
    j = np.arange(128)[:, None]
    i = np.arange(512)[None, :]
    tabs = []
    for r in range(4):
        tabs.append((-128 * r + i - j) >= 0)
    for o in range(1, 5):
        dd = 128 * o + i - j
        tabs.append((dd >= 0) & (dd < 512))
    for o in range(-3, 2):
        dd = 128 * o + i - j
        tabs.append((dd >= 0) & (dd < 128))
    for m in range(5):
        tabs.append((i - 16 * j) >= (31 - 512 * m))
    return np.stack(tabs)


ALLOWED = mask_tables()


def host_consts(sidx):
    cst = {}
    cst["masks"] = np.where(ALLOWED, 0.0, NEG).astype(np.float32)
    cst["idn"] = np.eye(128, dtype=np.float32)
    cst["i30k"] = (np.eye(128) * 30000.0).astype(np.float32)
    own_a = [SLOPE_A[2 * sidx], SLOPE_A[2 * sidx + 1]]
    own_b = [SLOPE_B[2 * sidx], SLOPE_B[2 * sidx + 1]]
    own_c = SLOPE_C[4 * sidx:4 * sidx + 4]
    sl8 = own_a + own_b + list(own_c)
    p = np.arange(128)[:, None, None]
    oi = np.arange(32)[None, None, :]
    cst["ab"] = (np.array(sl8)[None, :, None] * (p - 128.0 * (oi - 3))).astype(np.float32)
    cc = np.arange(2)[None, None, :, None]
    qt = np.arange(8)[None, None, None, :]
    perm_a = [2 * sidx, 2 * sidx + 1, 2 * (1 - sidx), 2 * (1 - sidx) + 1]
    cst["cb"] = (np.array([SLOPE_A[h] for h in perm_a])[None, :, None, None] * (16.0 * p[:, :, :, None] + 31 + 2048 * cc - 512 * qt)).astype(np.float32).reshape(128, 64)
    n = np.arange(256)[:, None]
    jj = np.arange(64)[None, :]
    ov = np.clip(np.minimum(16 * n + 32, 64 * jj + 64) - np.maximum(16 * n, 64 * jj), 0, None) / 32.0
    ov[255] = 0.0
    cst["ov"] = ov.astype(np.float32).reshape(2, 128, 64)
    t = np.arange(T)
    cst["emat"] = (t[None, :] // 64 == np.arange(64)[:, None]).astype(np.float32)
    r = np.arange(-63, 64)[None, :]
    pp = np.arange(128)[:, None]
    ta = np.where(r <= -2, 1.0, np.where(r == -1, (pp >= 64) * 1.0, 0.0))
    tb = np.where(r <= -2, 0.0, np.where(r == -1, (pp < 64) * 1e9, np.where(r == 0, 1e9, np.where(r == 1, np.where(pp < 64, -1.0, 1e9), -1.0))))
    cst["ta"] = ta.astype(np.float32)
    cst["tb"] = tb.astype(np.float32)
    i512 = np.arange(512)
    ri = np.stack([(-s * i512).astype(np.float32).astype(ml_dtypes.bfloat16).astype(np.float32) for s in own_c])
    cst["ri"] = ri
    g = np.exp(ri.astype(np.float64) + np.array(own_c)[:, None] * i512[None, :])
    cst["gs"] = np.ascontiguousarray(g.reshape(4, 4, 128).transpose(2, 1, 0)).astype(np.float32)
    return cst


def host_w(w_in, sidx):
    W = np.zeros((D, NW), np.float32)
    kva = 256
    kc, vc, ks, vs, kw, vw = [w_in[:, kva + 64 * k: kva + 64 * k + 64] for k in range(6)]
    W[:, C_KG0:C_KG0 + 64] = kc; W[:, C_KG0 + 64:C_KG0 + 128] = kc
    W[:, C_KG1:C_KG1 + 64] = vc; W[:, C_KG1 + 64:C_KG1 + 128] = vc
    W[:, C_KS:C_KS + 64] = ks
    W[:, C_KW:C_KW + 64] = kw
    W[:, C_KC:C_KC + 64] = w_in[:, 1932 + 64 * sidx: 1932 + 64 * sidx + 64]
    for hh in range(2):
        h = 2 * sidx + hh
        base = C_KB0 + 128 * hh
        W[:, base:base + 32] = w_in[:, 908 + 64 * h: 908 + 64 * h + 32]
        W[:, base + 96:base + 128] = w_in[:, 908 + 64 * h + 32: 908 + 64 * h + 64]
        W[:, C_V + 128 + 64 * hh: C_V + 192 + 64 * hh] = w_in[:, 1164 + 64 * h: 1164 + 64 * h + 64]
        W[:, C_QB + 128 * hh: C_QB + 128 * hh + 64] = w_in[:, 652 + 64 * h: 652 + 64 * h + 64]
        W[:, C_QB + 128 * hh + 64: C_QB + 128 * hh + 128] = w_in[:, 652 + 64 * h: 652 + 64 * h + 64]
        W[:, C_G + 3 * hh: C_G + 3 * hh + 3] = w_in[:, 640 + 3 * h: 640 + 3 * h + 3]
    W[:, C_V:C_V + 64] = vs
    W[:, C_V + 64:C_V + 128] = vw
    W[:, C_V + 256:C_V + 320] = w_in[:, 2060 + 64 * sidx: 2060 + 64 * sidx + 64]
    perm_a = [2 * sidx, 2 * sidx + 1, 2 * (1 - sidx), 2 * (1 - sidx) + 1]
    for k_, h in enumerate(perm_a):
        W[:, C_QA + 64 * k_:C_QA + 64 * k_ + 64] = w_in[:, 64 * h:64 * h + 64]
    W[:, C_QC:C_QC + 256] = w_in[:, 1420 + 256 * sidx: 1420 + 256 * sidx + 256]
    return W


def host_small(inp, L, sidx):
    sm = {}
    w1 = inp["cmp_w1"][L]
    sm["w1"] = np.ascontiguousarray(w1.reshape(2, 16, 128, 128).transpose(2, 0, 1, 3)).reshape(128, 2 * 16 * 128)
    sm["w2"] = np.ascontiguousarray(inp["cmp_w2"][L].transpose(1, 0, 2)).reshape(128, 128)
    pos = inp["cmp_pos"][L]
    sm["pos"] = np.ascontiguousarray(pos.reshape(2, 16, 128).transpose(2, 0, 1)).reshape(128, 32)
    sm["dl"] = np.ascontiguousarray(inp["diff_lambda"][L].reshape(1, 128))
    sm["subln"] = np.ascontiguousarray(inp["diff_subln"][L].reshape(1, 64))
    sm["sinks"] = np.ascontiguousarray(inp["sinks"][L][4 * sidx:4 * sidx + 4].reshape(1, 4))
    return sm


A_IN = dict(x=[T, D], w=[D, NW], masks=[NMASK, 128, 512], idn=[128, 128], i30k=[128, 128], ab=[128, 8, 32], cb=[128, 64],
            ov=[2, 128, 64], emat=[64, T], ta=[128, 127], tb=[128, 127], ri=[4, 512], gs=[128, 4, 4],
            w1=[128, 4096], w2=[128, 128], pos=[128, 32], dl=[1, 128], subln=[1, 64], sinks=[1, 4])


def build_A(nc, st, s, c, io, layer):
    lam_init = 0.8 - 0.6 * math.exp(-0.3 * layer)
    sb = lambda name, shape, dt=F32: st.enter_context(nc.sbuf_tensor(uname(name), list(shape), dt))
    PSB = [st.enter_context(nc.psum_tensor(uname("psb%d" % i), [128, 1024], F32)) for i in range(4)]
    TPS = [[Tok("ps%d_%d" % (i, h)) for h in range(2)] for i in range(4)]
    rr = {"s": 0, "a": 0, "f": 0}

    def ps_score():
        k = rr["s"] % 4; rr["s"] += 1
        return PSB[k // 2][:, (k % 2) * 512:(k % 2 + 1) * 512], TPS[k // 2][k % 2]

    def ps_acc():
        k = rr["a"] % 4; rr["a"] += 1
        return PSB[2 + k // 2][:, (k % 2) * 512:(k % 2 + 1) * 512], TPS[2 + k // 2][k % 2]

    def ps_accfull():
        k = rr["f"] % 2; rr["f"] += 1
        rr["a"] = 0
        return PSB[2 + k], TPS[2 + k]

    W = sb("W", [128, 8, NW], BF16); tW = Tok("W")
    IDN = sb("IDN", [128, 128]); tIDN = Tok()
    IDNb = sb("IDNb", [128, 128], BF16); tIDNb = Tok()
    I30K = sb("I30K", [128, 128], BF16); tI30K = Tok()
    MK = sb("MK", [128, NMASK, 512], BF16); tMK = Tok()
    AB = sb("AB", [128, 8, 32]); tAB = Tok()
    CB = sb("CB", [128, 64]); tCB = Tok()
    TA = sb("TA", [128, 127]); TBt = sb("TBt", [128, 127]); tTAB = Tok()
    GS = sb("GS", [128, 4, 4]); tGS = Tok()
    SK = sb("SK", [128, 4, 4]); tSK = Tok()
    W1 = sb("W1", [128, 2, 16, 128], BF16); tW1 = Tok()
    W2 = sb("W2", [128, 2, 64], BF16); tW2 = Tok()
    POS = sb("POS", [128, 2, 16], BF16); tPOS = Tok()
    SM_ = sb("SMALL", [128, 256]); tSM = Tok()
    Kc2 = sb("Kc2", [128, 2, T], BF16); tKc2 = Tok()
    KS = sb("KS", [128, T], BF16); tKS = Tok()
    KW = sb("KW", [64, T], BF16); tKW = Tok()
    KC = sb("KC", [65, T], BF16); tKC = Tok()
    KB = [sb("KB%d" % i, [128, T], BF16) for i in range(2)]; tKB = [Tok(), Tok()]
    VALL = sb("VALL", [128, 32, 5, 65], BF16); tV = Tok()
    XC = sb("XC", [128, 4, 1024]); tXC = Tok()
    XTc = [sb("XTc%d" % i, [128, 8, 512], BF16) for i in range(2)]; tXTc = [Tok(), Tok()]
    HT = sb("HT", [128, 2, 256], BF16); tHT = Tok()
    BP = sb("BP", [128, 2]); tBP = Tok()
    KCT = sb("KCT", [64, 256], BF16); tKCT = Tok()
    RC = sb("RC", [128, 2, 129], BF16); tRC = Tok()
    QA = [[sb("QA%d_%d" % (b, h), [128, 512], BF16) for h in range(4)] for b in range(2)]
    tQA = [[Tok() for h in range(4)] for b in range(2)]
    QB = [[sb("QB%d_%d" % (b, h), [128, 512], BF16) for h in range(2)] for b in range(1)]
    tQB = [[Tok() for h in range(2)] for b in range(1)]
    QB.append(QB[0]); tQB.append(tQB[0])
    QC = [[sb("QC%d_%d" % (b, h), [65, 512], BF16) for h in range(4)] for b in range(1)]
    tQC = [[Tok() for h in range(4)] for b in range(1)]
    QC.append(QC[0]); tQC.append(tQC[0])
    GT = sb("GT", [128, 4, 6]); tGT = Tok()
    PTb = [sb("PT%d" % i, [128, 512], BF16) for i in range(4)]; tPT = [Tok() for _ in range(4)]
    ptrr = [0]
    IMP = sb("IMP", [128, 4, 64]); tIMP = Tok()
    IMP2 = sb("IMP2", [128, 4, 64]); tIMP2 = Tok()
    MX = sb("MX", [128, 4, 16]); tMX = Tok()
    NM = sb("NM", [128, 4, 128], BF16); tNM = Tok()
    REC = sb("REC", [128, 16]); tREC = Tok()
    OCc = sb("OCc", [128, 2, 4, 64]); tOCc = Tok()
    TMP = [sb("TMP%d" % i, [128, 4, 64]) for i in range(2)]; tTMP = [Tok(), Tok()]
    OCH = sb("OCH", [128, 4, 512]); tOCH = Tok()
    OTc = sb("OTc", [128, 4, 512], BF16); tOTc = Tok()

    q = "sp"
    xdeps = []
    if "xh" in io:
        txfs = [Tok("xfull%d" % j) for j in range(4)]
        for j in range(4):
            s.add("pool", lambda e, j=j: e.collective_compute("AllGather", ALU.bypass, replica_groups=[[0, 1], [2, 3], [4, 5], [6, 7]],
                                                              ins=[io["xh"][j * 512:(j + 1) * 512, :].opt()], outs=[io["xg"][j].opt()]),
                  r=[], w=[txfs[j]], dma=True, inc=1)
        xdeps = txfs
    s.dma("pool", W[:], io["w"].rearrange("(c p) n -> p c n", p=128), w=[tW])
    s.dma(q, IDN[:], io["idn"], w=[tIDN])
    s.dma("pool", IDNb[:], io["idn"], w=[tIDNb])
    s.dma("pool", I30K[:], io["i30k"], w=[tI30K])
    for m0 in range(0, NMASK, 4):
        m1 = min(NMASK, m0 + 4)
        s.dma("pool", MK[:, m0:m1, :], io["masks"][m0:m1].rearrange("m p i -> p m i"), w=[tMK])
    s.dma(q, AB[:], io["ab"], w=[tAB])
    s.dma(q, CB[:], io["cb"], w=[tCB])
    s.dma(q, TA[:], io["ta"], w=[tTAB])
    s.dma(q, TBt[:], io["tb"], w=[tTAB])
    s.dma(q, GS[:], io["gs"], w=[tGS])
    s.dma("pool", W1[:], io["w1"].rearrange("p (k c h) -> p k c h", k=2, c=16), w=[tW1])
    s.dma("pool", W2[:], io["w2"].rearrange("p (k d) -> p k d", k=2), w=[tW2])
    s.dma("pool", POS[:], io["pos"].rearrange("p (k c) -> p k c", k=2), w=[tPOS])
    s.dma(q, SM_[:, 0:128], io["dl"].partition_broadcast(128).rearrange("p a b -> p (a b)"), w=[tSM])
    s.dma(q, SM_[:, 128:192], io["subln"].partition_broadcast(128).rearrange("p a b -> p (a b)"), w=[tSM])
    s.dma(q, SM_[:, 192:196], io["sinks"].partition_broadcast(128).rearrange("p a b -> p (a b)"), w=[tSM])
    s.dma("pool", KS[64:128, :], io["emat"], w=[tKS])
    s.dma("pool", RC[:, :, 0:64], io["ov"].rearrange("c p j -> p c j"), w=[tRC])
    for b in range(1):
        for h in range(4):
            s.dma("pool", QC[b][h][64:65, :], io["ri"][h:h + 1, :], w=[tQC[b][h]])
    s.add("pool", lambda e: e.memset(VALL[:, :, :, 64:65], 1.0), w=[tV])
    s.add("pool", lambda e: e.memset(RC[:, :, 128:129], 1.0), w=[tRC])
    s.add("pool", lambda e: e.memset(KC[64:65, :], 1.0), w=[tKC])
    s.add("pool", lambda e: e.memset(NM[:], 0.0), w=[tNM])
    s.add("pool", lambda e: e.memset(HT[:], 0.0), w=[tHT])
    s.add("pool", lambda e: e.memset(KCT[:], 0.0), w=[tKCT])
    s.add("pool", lambda e: e.memset(Kc2[:, :, T - 1:T], 0.0), w=[tKc2])
    s.add("dve", lambda e: e.tensor_tensor(out=SM_[:, 208:240], in0=SM_[:, 0:32], in1=SM_[:, 32:64], op=ALU.mult), r=[tSM], w=[tSM])
    s.add("dve", lambda e: e.reduce_sum(out=SM_[:, 200:201], in_=SM_[:, 208:240], axis=AX.X), r=[tSM], w=[tSM])
    s.add("dve", lambda e: e.tensor_tensor(out=SM_[:, 208:240], in0=SM_[:, 64:96], in1=SM_[:, 96:128], op=ALU.mult), r=[tSM], w=[tSM])
    s.add("dve", lambda e: e.reduce_sum(out=SM_[:, 201:202], in_=SM_[:, 208:240], axis=AX.X), r=[tSM], w=[tSM])
    s.add("act", lambda e: e.activation(out=SM_[:, 202:204], in_=SM_[:, 200:202], func=AF.Exp), r=[tSM], w=[tSM])
    s.add("dve", lambda e: e.tensor_tensor(out=SM_[:, 204:205], in0=SM_[:, 203:204], in1=SM_[:, 202:203], op=ALU.subtract), r=[tSM], w=[tSM])
    s.add("dve", lambda e: e.tensor_scalar(SM_[:, 200:201], SM_[:, 204:205], -lam_init, None, ALU.add), r=[tSM], w=[tSM])
    s.add("dve", lambda e: e.tensor_scalar(SM_[:, 128:192], SM_[:, 128:192], 1.0 - lam_init, None, ALU.mult), r=[tSM], w=[tSM])
    s.add("act", lambda e: e.activation(out=SM_[:, 196:200], in_=SM_[:, 192:196], func=AF.Exp), r=[tSM], w=[tSM])
    for sub in range(4):
        s.add("dve", lambda e, sub=sub: e.tensor_tensor(out=SK[:, sub, :], in0=GS[:, sub, :], in1=SM_[:, 196:200], op=ALU.mult), r=[tSM, tGS], w=[tSK])
    NLAM = SM_[:, 200:201]
    SUBLN = SM_[:, 128:192]

    evrr = [0]

    def evac(out, in_, rtoks, wtoks, scale=None, eng=None):
        if eng is None:
            eng = ("dve", "act")[evrr[0] % 2]; evrr[0] += 1
        if eng == "act":
            if scale is None:
                s.add("act", lambda e: e.activation(out=out, in_=in_, func=AF.Copy), r=rtoks, w=wtoks)
            else:
                s.add("act", lambda e: e.activation(out=out, in_=in_, func=AF.Copy, scale=scale), r=rtoks, w=wtoks)
        else:
            if scale is None:
                s.add("dve", lambda e: e.tensor_copy(out=out, in_=in_), r=rtoks, w=wtoks)
            else:
                s.add("dve", lambda e: e.tensor_scalar(out, in_, scale, None, ALU.mult), r=rtoks, w=wtoks)

    def load_xt(tc, k, eng=None):
        xsrc = io["xchunk"](tc) if "xchunk" in io else io["x"][tc * 512:(tc + 1) * 512, :]
        s.dma("sp", XC[:], xsrc.rearrange("(a p) d -> p a d", p=128), r=([xdeps[tc % 4]] if xdeps else []), w=[tXC])
        for dc in range(8):
            ps, tps = ps_score()
            for sub in range(4):
                s.add("pe", lambda e, ps=ps, sub=sub, dc=dc: e.transpose(ps[:, sub * 128:(sub + 1) * 128], XC[:, sub, dc * 128:(dc + 1) * 128], IDN[:]),
                      r=[tXC, tIDN], w=[tps])
            evac(XTc[k][:, dc, :], ps, [tps], [tXTc[k]], eng=eng)

    def proj_fm(k, col0, m):
        ps, tps = ps_score()
        for dc in range(8):
            s.add("pe", lambda e, ps=ps, dc=dc: e.matmul(ps[0:m, :], W[:, dc, col0:col0 + m], XTc[k][:, dc, :], start=(dc == 0), stop=(dc == 7)),
                  r=[tW, tXTc[k]], w=[tps])
        return ps, tps

    for tc in range(8):
        k = tc % 2
        t0 = tc * 512
        load_xt(tc, k)
        for kv in range(2):
            ps, tps = proj_fm(k, C_KG0 + 128 * kv, 128)
            evac(Kc2[0:64, kv, t0:t0 + 512], ps[0:64, :], [tps], [tKc2])
            if tc == 0:
                evac(Kc2[64:128, kv, 0:511], ps[64:128, 1:512], [tps], [tKc2])
            else:
                evac(Kc2[64:128, kv, t0 - 1:t0 + 511], ps[64:128, :], [tps], [tKc2])
        ps, tps = proj_fm(k, C_KS, 64); evac(KS[0:64, t0:t0 + 512], ps[0:64, :], [tps], [tKS])
        ps, tps = proj_fm(k, C_KW, 64); evac(KW[0:64, t0:t0 + 512], ps[0:64, :], [tps], [tKW])
        ps, tps = proj_fm(k, C_KC, 64); evac(KC[0:64, t0:t0 + 512], ps[0:64, :], [tps], [tKC])
        for hh in range(2):
            ps, tps = proj_fm(k, C_KB0 + 128 * hh, 128); evac(KB[hh][:, t0:t0 + 512], ps, [tps], [tKB[hh]])
        for sub in range(4):
            ps, tps = ps_score()
            for dc in range(8):
                s.add("pe", lambda e, ps=ps, dc=dc, sub=sub, k=k: e.matmul(ps[:, 0:320], XTc[k][:, dc, sub * 128:(sub + 1) * 128], W[:, dc, C_V:C_V + 320],
                                                                      start=(dc == 0), stop=(dc == 7)), r=[tW, tXTc[k]], w=[tps])
            evac(VALL[:, tc * 4 + sub, :, 0:64], ps[:, 0:320].rearrange("p (a b) -> p a b", b=64), [tps], [tV])

    for kv in range(2):
        ps, tps = ps_score()
        for cc in range(16):
            s.add("pe", lambda e, ps=ps, cc=cc, kv=kv: e.matmul(ps[:, 0:1], W1[:, kv, cc, :], POS[:, kv, cc:cc + 1], start=(cc == 0), stop=(cc == 15)),
                  r=[tW1, tPOS], w=[tps])
        evac(BP[:, kv:kv + 1], ps[:, 0:1], [tps], [tBP], eng="dve")
        ps, tps = ps_score()
        for cc in range(16):
            s.add("pe", lambda e, ps=ps, cc=cc, kv=kv: e.matmul(ps[:, 0:255], W1[:, kv, cc, :], Kc2[:, kv, 2 * cc: 2 * cc + 16 * 254 + 1: 16],
                                                                start=(cc == 0), stop=(cc == 15)), r=[tW1, tKc2], w=[tps])
        s.add("act", lambda e, ps=ps, kv=kv: e.activation(out=HT[:, kv, 0:255], in_=ps[:, 0:255], func=AF.Gelu_apprx_tanh, bias=BP[:, kv:kv + 1]),
              r=[tps, tBP], w=[tHT])
    ps, tps = ps_score()
    s.add("pe", lambda e, ps=ps: e.matmul(ps[0:64, 0:256], W2[:, 0, :], HT[:, 0, :], start=True, stop=True), r=[tW2, tHT], w=[tps])
    evac(KCT[:, :], ps[0:64, 0:256], [tps], [tKCT], eng="dve")
    for cc in range(2):
        ps, tps = ps_score()
        s.add("pe", lambda e, ps=ps, cc=cc: e.matmul(ps[:, 0:64], HT[:, 1, cc * 128:(cc + 1) * 128], W2[:, 1, :], start=True, stop=True), r=[tW2, tHT], w=[tps])
        evac(RC[:, cc, 64:128], ps[:, 0:64], [tps], [tRC], eng="dve")

    NPT = 4
    LA = 2

    GP = []
    CAPT = 2

    def gp_flush_one():
        ent = GP.pop(0)
        ent["pv"]()
        if ent["after"] is not None:
            ent["after"]()

    def gp_push(n, pv, after=None):
        GP.append(dict(n=n, pv=pv, after=after))
        while sum(x["n"] for x in GP) > CAPT + n - 1 and len(GP) > 1:
            gp_flush_one()

    def gp_drain():
        while GP:
            gp_flush_one()

    def attn_unit(kbs, lhs_fn, ltoks, rhs, rtoks, bias_fn, vidx, o_acc, to_acc, after=None):
        attn_multi(kbs, [(lhs_fn, ltoks, rhs, rtoks, bias_fn, vidx, o_acc, to_acc)], after=after)

    def attn_multi(kbs, streams, after=None):
        ns = len(streams)
        first = [True] * ns
        last_kb = {}
        for (kb, mi, subs) in kbs:
            for sub in subs:
                last_kb[sub] = kb

        def make_pv(kb, subs, pks):
            def pv():
                for si, (lhs_fn, ltoks, rhs, rtoks, bias_fn, vidx, o_acc, to_acc) in enumerate(streams):
                    pk = pks[si]
                    for sub in subs:
                        s.add("pe", lambda e, pk=pk, sub=sub, kb=kb, st_=first[si], sp_=(last_kb[sub] == kb), o_acc=o_acc, vidx=vidx: e.matmul(
                            o_acc[:, sub * 128:sub * 128 + 65], PTb[pk][:, sub * 128:(sub + 1) * 128], VALL[:, kb, vidx, :], start=st_, stop=sp_,
                            skip_group_check=True),
                            r=[tPT[pk], tV], w=[to_acc])
                        first[si] = False
            return pv

        for idx_, (kb, mi, subs) in enumerate(kbs):
            c0, c1 = min(subs) * 128, (max(subs) + 1) * 128
            tiles = []
            for (lhs_fn, ltoks, rhs, rtoks, bias_fn, vidx, o_acc, to_acc) in streams:
                ps, tps = ps_score()
                s.add("pe", lambda e, ps=ps, kb=kb, mi=mi, c0=c0, c1=c1, lhs_fn=lhs_fn, rhs=rhs: e.matmul(ps[:, c0:c1], lhs_fn(kb), rhs[:, c0:c1], start=True, stop=(mi is None)),
                      r=ltoks + rtoks, w=[tps])
                tiles.append((ps, tps))
            if mi is not None:
                for (ps, tps) in tiles:
                    s.add("pe", lambda e, ps=ps, mi=mi, c0=c0, c1=c1: e.matmul(ps[:, c0:c1], IDNb[:], MK[:, mi, c0:c1], start=False, stop=True), r=[tIDNb, tMK], w=[tps])
            pks = []
            for si, (lhs_fn, ltoks, rhs, rtoks, bias_fn, vidx, o_acc, to_acc) in enumerate(streams):
                ps, tps = tiles[si]
                pk = ptrr[0] % NPT; ptrr[0] += 1
                s.add("act", lambda e, ps=ps, pk=pk, kb=kb, c0=c0, c1=c1, bias_fn=bias_fn: e.activation(out=PTb[pk][:, c0:c1], in_=ps[:, c0:c1], func=AF.Exp, bias=bias_fn(kb)),
                      r=[tps, tAB, tCB], w=[tPT[pk]])
                pks.append(pk)
            gp_push(ns, make_pv(kb, subs, pks), after if idx_ == len(kbs) - 1 else None)

    def kb_list(qt, kind):
        out = []
        lo = {"full": 0, "win": max(0, 4 * qt - 4), "swa": max(0, 4 * qt - 1)}[kind]
        for kb in range(lo, 4 * qt + 4):
            o = 4 * qt - kb
            if kind == "full":
                mi = (M_CAUS - o) if o <= 0 else None
            elif kind == "win":
                mi = (M_CAUS - o) if o <= 0 else (M_WM + o - 1)
            else:
                mi = M_SM + o + 3
            if mi is None:
                subs = [0, 1, 2, 3]
            else:
                subs = [sub for sub in range(4) if ALLOWED[mi][:, sub * 128:(sub + 1) * 128].any()]
                if ALLOWED[mi].all():
                    mi = None
            out.append((kb, mi, subs))
        return out

    def recip_den(o_acc, to_acc, width, col, rec_ap, extra=None):
        den = o_acc[:, 0:4 * width].rearrange("p (a b) -> p a b", b=width)[:, :, col]
        if extra is None:
            s.add("dve", lambda e: e.tensor_scalar(rec_ap, den, 1e-30, None, ALU.max), r=(to_acc if isinstance(to_acc, list) else [to_acc]), w=[tREC])
        else:
            s.add("dve", lambda e: e.tensor_tensor(out=rec_ap, in0=den, in1=extra, op=ALU.add), r=[to_acc, tSK], w=[tREC])
        s.add("dve", lambda e: e.reciprocal(out=rec_ap, in_=rec_ap), r=[tREC], w=[tREC])

    def oview(o_acc, width, c0, n=64):
        return o_acc[:, 0:4 * width].rearrange("p (a b) -> p a b", b=width)[:, :, c0:c0 + n]

    def bc(ap4):
        return ap4.unsqueeze(2).to_broadcast([128, 4, 64])

    outs = []
    toh = []
    def finish_tile(qt):
        for fc in range(4):
            ps, tps = ps_score()
            for sub in range(4):
                s.add("pe", lambda e, ps=ps, sub=sub, fc=fc: e.transpose(ps[:, sub * 128:(sub + 1) * 128], OCH[:, sub, fc * 128:(fc + 1) * 128], IDN[:]),
                      r=[tOCH, tIDN], w=[tps])
            evac(OTc[:, fc, :], ps, [tps], [tOTc], eng="dve")
        if "oTh" in io:
            tq = Tok()
            toh.append(tq)
            outs.append(s.dma("sp", io["oTh"][qt // 4].rearrange("(c p) t -> p c t", p=128)[:, :, (qt % 4) * 512:(qt % 4 + 1) * 512], OTc[:], r=[tOTc], w=[tq]))
            if qt % 4 == 3:
                j = qt // 4
                outs.append(s.add("pool", lambda e, j=j: e.collective_compute("AllGather", ALU.bypass, replica_groups=[[0, 1], [2, 3], [4, 5], [6, 7]],
                                                                              ins=[io["oTh"][j].opt()], outs=[io["oTg"][j].opt()]),
                                  r=toh[-4:], w=[Tok()], dma=True, inc=1))
        else:
            outs.append(s.dma("sp", io["oT"][:, :, qt * 512:(qt + 1) * 512].rearrange("c p t -> p c t"), OTc[:], r=[tOTc]))

    def q_proj(qt):
        k = qt % 2
        b = qt % 2
        load_xt(qt, k, eng="dve")
        for h in range(4):
            ps, tps = proj_fm(k, C_QA + 64 * h, 64)
            evac(QA[b][h][0:64, :], ps[0:64, :], [tps], [tQA[b][h]], scale=0.125, eng="dve")
        for h in range(2):
            ps, tps = proj_fm(k, C_QB + 128 * h, 128)
            evac(QB[b][h][:, :], ps, [tps], [tQB[b][h]], scale=32 ** -0.5, eng="dve")
        for h in range(4):
            ps, tps = proj_fm(k, C_QC + 64 * h, 64)
            evac(QC[b][h][0:64, :], ps[0:64, :], [tps], [tQC[b][h]], scale=0.125, eng="dve")

    q_proj(0)
    for qt in range(NQT):
        k = qt % 2
        b = qt % 2
        ps, tps = ps_score()
        for sub in range(4):
            for dc in range(8):
                s.add("pe", lambda e, ps=ps, dc=dc, sub=sub, k=k: e.matmul(ps[:, sub * 8:sub * 8 + 6], XTc[k][:, dc, sub * 128:(sub + 1) * 128], W[:, dc, C_G:C_G + 6],
                                                                      start=(dc == 0), stop=(dc == 7)), r=[tW, tXTc[k]], w=[tps])
        s.add("act", lambda e, ps=ps: e.activation(out=GT[:], in_=ps[:, 0:32].rearrange("p (a b) -> p a b", b=8)[:, :, 0:6], func=AF.Sigmoid), r=[tps], w=[tGT])

        chunks = [0] if qt < 4 else [0, 1]
        for h in range(4):
            oa, toa = ps_accfull()

            def post_cmp(h=h, oa=oa, toa=toa):
                recip_den(oa, toa, 256, 128, REC[:, 0:4])
                if h == 0:
                    s.add("dve", lambda e: e.tensor_tensor(out=IMP[:], in0=oview(oa, 256, 0), in1=bc(REC[:, 0:4]), op=ALU.mult),
                          r=[toa[0], toa[1], tREC], w=[tIMP])
                else:
                    tm = TMP[h % 2]; ttm = tTMP[h % 2]
                    s.add("dve", lambda e: e.tensor_tensor(out=tm[:], in0=oview(oa, 256, 0), in1=bc(REC[:, 0:4]), op=ALU.mult),
                          r=[toa[0], toa[1], tREC], w=[ttm])
                    s.add("dve", lambda e: e.tensor_tensor(out=IMP[:], in0=IMP[:], in1=tm[:], op=ALU.add), r=[ttm, tIMP], w=[tIMP])
                if h < 2:
                    s.add("dve", lambda e: e.tensor_tensor(out=OCc[:, h, :, :], in0=oview(oa, 256, 64), in1=bc(REC[:, 0:4]), op=ALU.mult),
                          r=[toa[0], toa[1], tREC], w=[tOCc])

            for cc in chunks:
                rel = qt - 4 * cc
                mi = (M_CM + rel) if rel < 5 else None
                ps, tps = ps_score()
                s.add("pe", lambda e, ps=ps, cc=cc, h=h, mi=mi, b=b: e.matmul(ps, KCT[:, cc * 128:(cc + 1) * 128], QA[b][h][0:64, :], start=True, stop=(mi is None)),
                      r=[tKCT, tQA[b][h]], w=[tps])
                if mi is not None:
                    s.add("pe", lambda e, ps=ps, mi=mi: e.matmul(ps, IDNb[:], MK[:, mi, :], start=False, stop=True), r=[tIDNb, tMK], w=[tps])
                pk = ptrr[0] % NPT; ptrr[0] += 1
                ci = h * 16 + cc * 8 + qt
                s.add("act", lambda e, ps=ps, pk=pk, ci=ci: e.activation(out=PTb[pk][:], in_=ps, func=AF.Exp, bias=CB[:, ci:ci + 1]),
                      r=[tps, tCB], w=[tPT[pk]])

                def pv_cmp(oa=oa, toa=toa, pk=pk, cc=cc, first=(cc == chunks[0]), last=(cc == chunks[-1])):
                    for sub in range(4):
                        s.add("pe", lambda e, sub=sub: e.matmul(
                            oa[:, sub * 256:sub * 256 + 129], PTb[pk][:, sub * 128:(sub + 1) * 128], RC[:, cc, :], start=(first and sub % 2 == 0), stop=last,
                            skip_group_check=True),
                            r=[tPT[pk], tRC], w=[toa[sub // 2]])

                gp_push(1, pv_cmp, post_cmp if cc == chunks[-1] else None)
        gp_drain()
        if qt >= 1:
            finish_tile(qt - 1)
        for sub in range(4):
            blk = qt * 4 + sub
            lo = 63 - 2 * blk
            s.add("dve", lambda e, sub=sub, lo=lo: e.tensor_tensor(out=IMP2[:, sub, :], in0=IMP[:, sub, :], in1=TA[:, lo:lo + 64], op=ALU.mult),
                  r=[tIMP, tTAB], w=[tIMP2])
            s.add("dve", lambda e, sub=sub, lo=lo: e.tensor_tensor(out=IMP2[:, sub, :], in0=IMP2[:, sub, :], in1=TBt[:, lo:lo + 64], op=ALU.add),
                  r=[tIMP2, tTAB], w=[tIMP2])
        s.add("dve", lambda e: e.memset(IMP2[:, :, 0:1], 1e9), w=[tIMP2])
        for sub in range(4):
            s.add("dve", lambda e, sub=sub: e.max(out=MX[:, sub, 0:8], in_=IMP2[:, sub, :]), r=[tIMP2], w=[tMX])
            s.add("dve", lambda e, sub=sub: e.match_replace(out=IMP[:, sub, :], in_to_replace=MX[:, sub, 0:8], in_values=IMP2[:, sub, :], imm_value=-1e30),
                  r=[tIMP2, tMX], w=[tIMP])
            s.add("dve", lambda e, sub=sub: e.max(out=MX[:, sub, 8:16], in_=IMP[:, sub, :]), r=[tIMP], w=[tMX])
            s.add("dve", lambda e, sub=sub: e.tensor_scalar(NM[:, sub, 64:128], IMP2[:, sub, :], MX[:, sub, 15:16], 1.0, ALU.is_ge, ALU.subtract),
                  r=[tIMP2, tMX], w=[tNM])
        for own in range(2):
            hs = 2 + own
            feat0 = 128 + 64 * own
            accs = []
            streams = []
            for mp in range(2):
                oa, toa = ps_acc()
                lhs_fn = lambda kb, own=own, mp=mp: KB[own][64 * mp:64 * mp + 64, kb * 128:(kb + 1) * 128]
                rhs = QB[b][own][64 * mp:64 * mp + 64, :]
                bias_fn = lambda kb, hs=hs, qt=qt: AB[:, hs, 4 * qt - kb + 3: 4 * qt - kb + 4]
                streams.append((lhs_fn, [tKB[own]], rhs, [tQB[b][own]], bias_fn, 2 + own, oa, toa))
                accs.append((oa, toa))

            def post_diff(accs=accs, feat0=feat0):
                (o1, to1), (o2, to2) = accs
                r1 = REC[:, 4:8]; r2 = REC[:, 8:12]
                recip_den(o1, to1, 128, 64, r1)
                recip_den(o2, to2, 128, 64, r2)
                s.add("dve", lambda e: e.tensor_scalar(r2, r2, NLAM, None, ALU.mult), r=[tREC, tSM], w=[tREC])
                t1 = TMP[0]; t2 = TMP[1]
                s.add("dve", lambda e: e.tensor_tensor(out=t1[:], in0=oview(o1, 128, 0), in1=bc(r1), op=ALU.mult), r=[to1, tREC], w=[tTMP[0]])
                s.add("dve", lambda e: e.tensor_tensor(out=t2[:], in0=oview(o2, 128, 0), in1=bc(r2), op=ALU.mult), r=[to2, tREC], w=[tTMP[1]])
                s.add("dve", lambda e: e.tensor_tensor(out=t1[:], in0=t1[:], in1=t2[:], op=ALU.add), r=[tTMP[0], tTMP[1]], w=[tTMP[0]])
                s.add("dve", lambda e: e.tensor_tensor(out=t2[:], in0=t1[:], in1=t1[:], op=ALU.mult), r=[tTMP[0]], w=[tTMP[1]])
                ss = REC[:, 12:16]
                s.add("dve", lambda e: e.reduce_sum(out=ss, in_=t2[:], axis=AX.X), r=[tTMP[1]], w=[tREC])
                s.add("dve", lambda e: e.tensor_scalar(ss, ss, 1.0 / 64.0, 1e-5, ALU.mult, ALU.add), r=[tREC], w=[tREC])
                s.add("act", lambda e: e.activation(out=ss, in_=ss, func=AF.Sqrt), r=[tREC], w=[tREC])
                s.add("dve", lambda e: e.reciprocal(out=ss, in_=ss), r=[tREC], w=[tREC])
                s.add("dve", lambda e: e.tensor_tensor(out=t1[:], in0=t1[:], in1=bc(ss), op=ALU.mult), r=[tTMP[0], tREC], w=[tTMP[0]])
                s.add("dve", lambda e: e.tensor_tensor(out=OCH[:, :, feat0:feat0 + 64], in0=t1[:], in1=SUBLN.unsqueeze(1).to_broadcast([128, 4, 64]), op=ALU.mult),
                      r=[tTMP[0], tSM], w=[tOCH])

            attn_multi(kb_list(qt, "full"), streams, after=post_diff)
        for r_ in range(4):
            hs = 4 + r_
            feat0 = 256 + 64 * r_
            oa, toa = ps_acc()
            lhs_fn = lambda kb: KC[0:65, kb * 128:(kb + 1) * 128]
            rhs = QC[b][r_][0:65, :]
            bias_fn = lambda kb, hs=hs, qt=qt: AB[:, hs, 4 * qt - kb + 3: 4 * qt - kb + 4]

            def post_swa(oa=oa, toa=toa, r_=r_, feat0=feat0):
                rec = REC[:, 4:8]
                recip_den(oa, toa, 128, 64, rec, extra=SK[:, :, r_])
                s.add("dve", lambda e: e.tensor_tensor(out=OCH[:, :, feat0:feat0 + 64], in0=oview(oa, 128, 0), in1=bc(rec), op=ALU.mult),
                      r=[toa, tREC], w=[tOCH])

            attn_unit(kb_list(qt, "swa"), lhs_fn, [tKC], rhs, [tQC[b][r_]], bias_fn, 4, oa, toa, after=post_swa)
        ps, tps = ps_score()
        for sub in range(4):
            s.add("pe", lambda e, ps=ps, sub=sub: e.matmul(ps[:, sub * 128:(sub + 1) * 128], NM[:, sub, :], I30K[:], start=True, stop=True),
                  r=[tNM, tI30K], w=[tps])
        for own in range(2):
            h = own
            evac(QA[b][h][64:128, :], ps[64:128, :], [tps], [tQA[b][h]], eng="dve")

        for own in range(2):
            h = own
            hs = own
            feat0 = 64 * own
            s.add("dve", lambda e, own=own, feat0=feat0: e.tensor_tensor(out=OCH[:, :, feat0:feat0 + 64], in0=OCc[:, own, :, :],
                                                                         in1=bc(GT[:, :, 3 * own + 0]), op=ALU.mult), r=[tOCc, tGT], w=[tOCH])
            for br, (kind, lhs_t, ltok, vidx) in enumerate([("full", KS, tKS, 0), ("win", KW, tKW, 1)]):
                oa, toa = ps_acc()
                if kind == "full":
                    lhs_fn = lambda kb: KS[:, kb * 128:(kb + 1) * 128]
                    rhs = QA[b][h][:, :]
                else:
                    lhs_fn = lambda kb: KW[0:64, kb * 128:(kb + 1) * 128]
                    rhs = QA[b][h][0:64, :]
                bias_fn = lambda kb, hs=hs, qt=qt: AB[:, hs, 4 * qt - kb + 3: 4 * qt - kb + 4]

                def post_nsa(oa=oa, toa=toa, own=own, br=br, feat0=feat0):
                    rec = REC[:, 4 + 4 * br: 8 + 4 * br]
                    recip_den(oa, toa, 128, 64, rec)
                    s.add("dve", lambda e: e.tensor_tensor(out=rec, in0=rec, in1=GT[:, :, 3 * own + 1 + br], op=ALU.mult),
                          r=[tREC, tGT], w=[tREC])
                    tm = TMP[br]; ttm = tTMP[br]
                    s.add("dve", lambda e: e.tensor_tensor(out=tm[:], in0=oview(oa, 128, 0), in1=bc(rec), op=ALU.mult),
                          r=[toa, tREC], w=[ttm])
                    s.add("dve", lambda e: e.tensor_tensor(out=OCH[:, :, feat0:feat0 + 64], in0=OCH[:, :, feat0:feat0 + 64], in1=tm[:], op=ALU.add),
                          r=[ttm, tOCH], w=[tOCH])

                attn_unit(kb_list(qt, kind), lhs_fn, [ltok], rhs, [tQA[b][h]], bias_fn, vidx, oa, toa, after=post_nsa)
        if qt + 1 < NQT:
            q_proj(qt + 1)
        gp_drain()
    finish_tile(NQT - 1)
    if io.get("debug"):
        dl_ = [("KC", KC, [65, T], BF16, tKC), ("KW", KW, [64, T], BF16, tKW), ("KS", KS, [128, T], BF16, tKS), ("KB0", KB[0], [128, T], BF16, tKB[0]),
               ("VALL", VALL, [128, 32, 5, 65], BF16, tV), ("KCT", KCT, [64, 256], BF16, tKCT), ("RC", RC, [128, 2, 129], BF16, tRC),
               ("HT", HT, [128, 2, 256], BF16, tHT), ("Kc2", Kc2, [128, 2, T], BF16, tKc2), ("QA0", QA[1][0], [128, 512], BF16, tQA[1][0]),
               ("QB0", QB[1][0], [128, 512], BF16, tQB[1][0]), ("QC0", QC[1][0], [65, 512], BF16, tQC[1][0]), ("GT", GT, [128, 4, 6], F32, tGT),
               ("IMP2", IMP2, [128, 4, 64], F32, tIMP2), ("NM", NM, [128, 4, 128], BF16, tNM), ("OCH", OCH, [128, 4, 512], F32, tOCH),
               ("SMALL", SM_, [128, 256], F32, tSM), ("SK", SK, [128, 4, 4], F32, tSK), ("OCc", OCc, [128, 2, 4, 64], F32, tOCc),
               ("XT", XTc[1], [128, 8, 512], BF16, tXTc[1]), ("W", W, [128, 8, NW], BF16, tW), ("MK", MK, [128, NMASK, 512], BF16, tMK)]
        for (nm, tl, shp, dt_, tk) in dl_:
            dtn = nc.dram_tensor("dbg_" + nm, shp, dt_, kind="ExternalOutput").ap()
            outs.append(s.dma("sp", dtn, tl[:], r=[tk]))
    return outs


from concourse.bass_utils import run_bass_kernel_spmd

B_DENSE_IN = dict(oT=([8, 128, NT], BF16), xres=([NT, D], F32), w_out=([D, D], F32), lnp=([6, D], F32), wg=([D, 2816], F32), wu=([D, 2816], F32),
                  wd=([2816, D], F32), ple_gate=([D, D], F32), ple_proj=([256, D], F32), p=([NT, 256], F32), idn=([128, 128], F32))
B_MOE_IN = dict(oT=([8, 128, NT], BF16), xres=([NT, D], F32), w_out=([D, D], F32), lnp=([6, D], F32), router=([D, 8], F32),
                mg=([8, D, 3584], F32), mu=([8, D, 3584], F32), md=([8, 3584, D], F32), ple_gate=([D, D], F32), ple_proj=([256, D], F32),
                p=([NT, 256], F32), idn=([128, 128], F32), ut=([128, 128], F32), iota=([128, CAP], F32), pcol=([128, 16, 2], F32))


def _prog_A(layer):
    nc = bass.Bass("TRN2", target_bir_lowering=False)
    io = {k: dram(nc, k, shp) for k, shp in A_IN.items()}
    io["oT"] = dram(nc, "oT", [4, 128, T], BF16, kind="ExternalOutput")
    with contextlib.ExitStack() as st:
        s = Sched(nc)
        c = Ctx()
        outs = build_A(nc, st, s, c, io, layer)
        s.emit(final_ops=outs)
    return nc


def _prog_B(moe):
    nc = bass.Bass("TRN2", target_bir_lowering=False)
    spec = B_MOE_IN if moe else B_DENSE_IN
    io = {k: dram(nc, k, shp, dt) for k, (shp, dt) in spec.items()}
    io["out"] = dram(nc, "out", [NT, D], kind="ExternalOutput")
    with contextlib.ExitStack() as st:
        s = Sched(nc)
        c = Ctx()
        setup_psum(nc, st, c)
        outs = (build_B_moe if moe else build_B_dense)(nc, st, s, c, io)
        s.emit(final_ops=outs)
    return nc


def _feat_perm(sidx):
    return np.concatenate([np.arange(128 * sidx, 128 * sidx + 128), 256 + np.arange(128 * sidx, 128 * sidx + 128),
                           512 + np.arange(256 * sidx, 256 * sidx + 256)])


def kernel_unfused(**inputs):
    inp = {k: np.asarray(v) for k, v in inputs.items()}
    x = np.ascontiguousarray(inp["x"], dtype=np.float32)
    nb = x.shape[0]
    cores = list(range(2 * nb))
    idn = np.eye(128, dtype=np.float32)
    ut = (np.arange(128)[:, None] < np.arange(128)[None, :]).astype(np.float32)
    iota = np.tile(np.arange(CAP, dtype=np.float32)[None, :], (128, 1))
    wperm = np.concatenate([_feat_perm(0), _feat_perm(1)])
    consts = [host_consts(0), host_consts(1)]
    for L in range(2):
        packs = [host_w(inp["w_in"][L], s_) for s_ in range(2)]
        smalls = [host_small(inp, L, s_) for s_ in range(2)]
        in_maps = []
        for cid in cores:
            b, s_ = cid // 2, cid % 2
            m = dict(x=np.ascontiguousarray(x[b]), w=packs[s_])
            m.update(consts[s_])
            m.update(smalls[s_])
            in_maps.append(m)
        res = run_bass_kernel_spmd(_prog_A(L), in_maps, core_ids=cores)
        oT = [np.asarray(r["oT"]) for r in res.results]
        moe = (L % 2 == 1)
        lnp = np.stack([inp["ln1_g"][L], inp["ln1_b"][L], inp["ln2_g"][L], inp["ln2_b"][L], inp["ln3_g"][L], inp["ln3_b"][L]]).astype(np.float32)
        w_out = np.ascontiguousarray(inp["w_out"][L][wperm, :])
        in_maps = []
        for cid in cores:
            b, h = cid // 2, cid % 2
            tk = slice(h * NT, (h + 1) * NT)
            oTB = np.ascontiguousarray(np.concatenate([oT[2 * b][:, :, tk], oT[2 * b + 1][:, :, tk]], axis=0))
            m = dict(oT=oTB, xres=np.ascontiguousarray(x[b, tk]), w_out=w_out, lnp=lnp, ple_gate=inp["ple_gate"][L], ple_proj=inp["ple_proj"][L],
                     p=np.ascontiguousarray(inp["p"][L, b, tk]), idn=idn)
            if moe:
                m.update(router=inp["moe_router"][L // 2], mg=inp["moe_w_gate"][L // 2], mu=inp["moe_w_up"][L // 2], md=inp["moe_w_down"][L // 2], ut=ut, iota=iota)
            else:
                m.update(wg=inp["ffn_w_gate"][L // 2], wu=inp["ffn_w_up"][L // 2], wd=inp["ffn_w_down"][L // 2])
            in_maps.append(m)
        res = run_bass_kernel_spmd(_prog_B(moe), in_maps, core_ids=cores)
        xn = np.empty_like(x)
        for cid in cores:
            b, h = cid // 2, cid % 2
            xn[b, h * NT:(h + 1) * NT] = np.asarray(res.results[cid]["out"], dtype=np.float32)
        x = xn
    return x


A_LAYER_KEYS = ("w", "w1", "w2", "pos", "dl", "subln", "sinks")
A_SHARED_KEYS = ("masks", "idn", "i30k", "ab", "cb", "ov", "emat", "ta", "tb", "ri", "gs")
B_COMMON = dict(w_out=([D, D], F32), lnp=([6, D], F32), ple_gate=([D, D], F32), ple_proj=([256, D], F32), p=([NT, 256], F32))


def _prog_fused(nlayers=2):
    PHASE[0] = 0
    Sched.NSCHED = 0
    nc = bass.Bass("TRN2", target_bir_lowering=False)
    ext = {}

    def inp(name, shp, dt=F32):
        ext[name] = dram(nc, name, shp, dt)
        return ext[name]

    x0 = inp("x0", [T, D])
    xres0 = inp("xres0", [NT, D])
    hsel = inp("hsel", [1, 2])
    shared = {k: inp(k, A_IN[k]) for k in A_SHARED_KEYS}
    perA = [{k: inp("%s_%d" % (k, L), A_IN[k]) for k in A_LAYER_KEYS} for L in range(2)]
    perB = [{k: inp("%s_%d" % (k, L), shp, dt) for k, (shp, dt) in B_COMMON.items()} for L in range(2)]
    dense = dict(wg=inp("wg", [D, 2816]), wu=inp("wu", [D, 2816]), wd=inp("wd", [2816, D]))
    moe = dict(router=inp("router", [D, 8]), mg=inp("mg", [8, D, 3584]), mu=inp("mu", [8, D, 3584]), md=inp("md", [8, 3584, D]),
               ut=inp("ut", [128, 128]), iota=inp("iota", [128, CAP]), pcol=inp("pcol", [128, 16, 2]))
    out = dram(nc, "out", [NT, D], kind="ExternalOutput")
    oTown = [dram(nc, "oTown%d" % L, [2, 512, NT], BF16, kind="Internal") for L in range(2)]
    oTg = [dram(nc, "oTg%d" % L, [2, 1024, NT], BF16, kind="Internal") for L in range(2)]
    xh = dram(nc, "xh", [NT, D], kind="Internal")
    xg = dram(nc, "xg", [4, 1024, D], kind="Internal")
    for L in range(nlayers):
        with nc.cleanup_on_exit():
            with contextlib.ExitStack() as st:
                PHASE[0] += 1
                s = Sched(nc)
                c = Ctx()
                io = dict(shared)
                io.update(perA[L])
                io["oTh"] = oTown[L]
                io["oTg"] = oTg[L]
                if L == 0:
                    io["x"] = x0
                else:
                    io["xchunk"] = lambda tc: xg[tc % 4][(tc // 4) * 512:(tc // 4 + 1) * 512, :]
                outs = build_A(nc, st, s, c, io, L)
                s.emit(final_ops=outs)
            nc.all_engine_barrier()
        with nc.cleanup_on_exit():
            with contextlib.ExitStack() as st:
                PHASE[0] += 1
                s = Sched(nc)
                c = Ctx()
                setup_psum(nc, st, c)
                io = dict(perB[L])
                io.update(idn=shared["idn"], hsel=hsel, oTown=oTown[L], oTg=oTg[L])
                io["xres"] = xres0 if L == 0 else xh
                io["out"] = xh if L < nlayers - 1 else out
                if L < nlayers - 1:
                    io["xg"] = xg
                if L % 2 == 0:
                    io.update(dense)
                    outs = build_B_dense(nc, st, s, c, io)
                else:
                    io.update(moe)
                    outs = build_B_moe(nc, st, s, c, io)
                s.emit(final_ops=outs)
            nc.all_engine_barrier()
    return nc


def kernel(**inputs):
    inp = {k: np.asarray(v) for k, v in inputs.items()}
    x = np.ascontiguousarray(inp["x"], dtype=np.float32)
    nb = x.shape[0]
    cores = list(range(2 * nb))
    idn = np.eye(128, dtype=np.float32)
    ut = (np.arange(128)[:, None] < np.arange(128)[None, :]).astype(np.float32)
    iota = np.tile(np.arange(CAP, dtype=np.float32)[None, :], (128, 1))
    pcol = np.stack([np.tile(np.arange(128, dtype=np.float32)[:, None], (1, 16)), np.tile(np.arange(16, dtype=np.float32)[None, :], (128, 1))], axis=-1)
    wperm = np.concatenate([_feat_perm(0), _feat_perm(1)])
    consts = [host_consts(0), host_consts(1)]
    packs = [[host_w(inp["w_in"][L], s_) for s_ in range(2)] for L in range(2)]
    smalls = [[host_small(inp, L, s_) for s_ in range(2)] for L in range(2)]
    lnps = [np.stack([inp["ln1_g"][L], inp["ln1_b"][L], inp["ln2_g"][L], inp["ln2_b"][L], inp["ln3_g"][L], inp["ln3_b"][L]]).astype(np.float32) for L in range(2)]
    w_outs = [np.ascontiguousarray(inp["w_out"][L][wperm, :]) for L in range(2)]
    in_maps = []
    for cid in cores:
        b, s_ = cid // 2, cid % 2
        tk = slice(s_ * NT, (s_ + 1) * NT)
        hs = np.zeros((1, 2), np.float32)
        hs[0, s_] = 1.0
        m = dict(x0=np.ascontiguousarray(x[b]), xres0=np.ascontiguousarray(x[b, tk]), hsel=hs)
        for k in A_SHARED_KEYS:
            m[k] = consts[s_][k]
        for L in range(2):
            m["w_%d" % L] = packs[L][s_]
            for k in A_LAYER_KEYS[1:]:
                m["%s_%d" % (k, L)] = smalls[L][s_][k]
            m["w_out_%d" % L] = w_outs[L]
            m["lnp_%d" % L] = lnps[L]
            m["ple_gate_%d" % L] = inp["ple_gate"][L]
            m["ple_proj_%d" % L] = inp["ple_proj"][L]
            m["p_%d" % L] = np.ascontiguousarray(inp["p"][L, b, tk])
        m.update(wg=inp["ffn_w_gate"][0], wu=inp["ffn_w_up"][0], wd=inp["ffn_w_down"][0], router=inp["moe_router"][0],
                 mg=inp["moe_w_gate"][0], mu=inp["moe_w_up"][0], md=inp["moe_w_down"][0], ut=ut, iota=iota, pcol=pcol)
        in_maps.append(m)
    res = run_bass_kernel_spmd(_prog_fused(), in_maps, core_ids=cores)
    out = np.empty_like(x)
    for cid in cores:
        b, s_ = cid // 2, cid % 2
        out[b, s_ * NT:(s_ + 1) * NT] = np.asarray(res.results[cid]["out"], dtype=np.float32)
    return out
```

```python
import contextlib, math
import numpy as np
import ml_dtypes
import concourse.bass as bass
import concourse.mybir as mybir

F32 = mybir.dt.float32
BF16 = mybir.dt.bfloat16
AF = mybir.ActivationFunctionType
ALU = mybir.AluOpType
AX = mybir.AxisListType

PHASE = [0]


def uname(name):
    return "%s_%d" % (name, PHASE[0])


ENGS = ("pe", "act", "dve", "pool", "sp")


class Tok:
    __slots__ = ("name", "w", "rs")

    def __init__(self, name=""):
        self.name = name
        self.w = None
        self.rs = []


class Op:
    __slots__ = ("eng", "fn", "deps", "dma", "sig", "needs", "gi", "inc")

    def __init__(self, eng, fn, dma):
        self.eng = eng
        self.fn = fn
        self.deps = set()
        self.dma = dma
        self.sig = None
        self.needs = False
        self.gi = 0
        self.inc = 16


class Sched:
    NDMA = 12
    NSCHED = 0

    def __init__(self, nc, same_engine_sync=True):
        self.nc = nc
        self.ops = []
        self.same = same_engine_sync

    def add(self, eng, fn, r=(), w=(), dma=False, inc=16):
        op = Op(eng, fn, dma)
        op.inc = inc
        op.gi = len(self.ops)
        for t in r:
            if t.w is not None:
                op.deps.add(t.w)
        for t in w:
            if t.w is not None:
                op.deps.add(t.w)
            for x in t.rs:
                op.deps.add(x)
        for t in r:
            t.rs.append(op)
        for t in w:
            t.w = op
            t.rs = []
        op.deps.discard(op)
        self.ops.append(op)
        return op

    def dma(self, q, out, in_, r=(), w=(), **kw):
        return self.add(q, lambda e: e.dma_start(out=out, in_=in_, **kw), r, w, dma=True)

    def emit(self, final_ops=()):
        nc = self.nc
        ops = self.ops
        for op in ops:
            for d in op.deps:
                if d.dma:
                    continue
                if d.eng != op.eng or (self.same and d.eng != "pe"):
                    d.needs = True
        for op in final_ops:
            op.needs = True
        cnt = {e: 0 for e in ENGS}
        dcnt = {e: [0] * self.NDMA for e in ENGS}
        drr = {e: 0 for e in ENGS}
        prev_dma_wait = {}
        ncoll = 0
        coll_keys = []
        for op in ops:
            if op.dma and op.inc != 16:
                ncoll += 1
                prev_dma_wait[op] = (op.eng, 0, 0)
                op.sig = (("k", op.eng, ncoll), op.inc)
                coll_keys.append(op.sig[0])
            elif op.dma:
                k = drr[op.eng]
                drr[op.eng] = (k + 1) % self.NDMA
                prev_dma_wait[op] = (op.eng, k, dcnt[op.eng][k])
                dcnt[op.eng][k] += op.inc
                op.sig = (("d", op.eng, k), dcnt[op.eng][k])
            elif op.needs:
                cnt[op.eng] += 1
                op.sig = (("c", op.eng), cnt[op.eng])
        per = {e: [o for o in ops if o.eng == e] for e in ENGS}
        used = [e for e in ENGS if per[e]]
        import contextlib
        with contextlib.ExitStack() as st:
            sems = {}
            for e in ENGS:
                if cnt[e] > 0:
                    sems[("c", e)] = nc.alloc_semaphore(name="c_%s_%d" % (e, Sched.NSCHED))
                for k in range(self.NDMA):
                    if dcnt[e][k] > 0:
                        sems[("d", e, k)] = nc.alloc_semaphore(name="d_%s_%d_%d" % (e, k, Sched.NSCHED))
            for key in coll_keys:
                sems[key] = nc.alloc_semaphore(name="k_%s_%d_%d" % (key[1], key[2], Sched.NSCHED))
            Sched.NSCHED += 1
            block = st.enter_context(nc.Block())

            def run(ename, eng):
                waited = {}
                for op in per[ename]:
                    need = {}
                    for d in op.deps:
                        if d.sig is None:
                            continue
                        if (not d.dma) and d.eng == ename and (ename == "pe" or not self.same):
                            continue
                        key, val = d.sig
                        if need.get(key, 0) < val:
                            need[key] = val
                    if op.dma:
                        _, k, v = prev_dma_wait[op]
                        if v > 0:
                            key = ("d", ename, k)
                            if need.get(key, 0) < v:
                                need[key] = v
                    for key, val in need.items():
                        if waited.get(key, 0) < val:
                            eng.wait_ge(sems[key], val)
                            waited[key] = val
                    ins = op.fn(eng)
                    if op.sig is not None:
                        key, val = op.sig
                        ins.then_inc(sems[key], op.inc if op.dma else 1)
                if ename == "sp":
                    for op in final_ops:
                        key, val = op.sig
                        if waited.get(key, 0) < val:
                            eng.wait_ge(sems[key], val)
                            waited[key] = val

            if per["sp"] or final_ops:
                block.sync(lambda e: run("sp", e))
            if per["pe"]:
                block.tensor(lambda e: run("pe", e))
            if per["act"]:
                block.scalar(lambda e: run("act", e))
            if per["dve"]:
                block.vector(lambda e: run("dve", e))
            if per["pool"]:
                block.gpsimd(lambda e: run("pool", e))
        return {e: len(per[e]) for e in ENGS}

NT = 2048
D = 1024
ALPHA = 4 ** 0.25
EPS = 1e-5


def dram(nc, name, shape, dt=F32, kind="ExternalInput"):
    return nc.dram_tensor(name, list(shape), dt, kind=kind).ap()


class Ctx:
    pass


def setup_psum(nc, st, c):
    c.PSB = [st.enter_context(nc.psum_tensor(uname("psb%d" % i), [128, 1024], F32)) for i in range(4)]
    c.TPS = [[Tok("ps%d_%d" % (i, h)) for h in range(2)] for i in range(4)]
    c.psrr = 0


def ps_full(c):
    i = c.psrr % 4
    c.psrr += 1
    return c.PSB[i], c.TPS[i]


def ps_half(c):
    if not hasattr(c, "hrr"):
        c.hrr = 0
    k = c.hrr % 8
    c.hrr += 1
    return c.PSB[k // 2][:, (k % 2) * 512:(k % 2 + 1) * 512], c.TPS[k // 2][k % 2]


RG_PAIRS = [[0, 1], [2, 3], [4, 5], [6, 7]]


def load_oT(nc, st, s, io, OT, tOT, H1, tH1):
    if "oTown" not in io:
        s.dma("sp", OT, io["oT"].rearrange("c p t -> p c t"), w=[tOT])
        return
    HS = st.enter_context(nc.sbuf_tensor(uname("HS"), [128, 2], F32)); tHS = Tok()
    s.dma("sp", HS[:], io["hsel"].partition_broadcast(128).rearrange("p a b -> p (a b)"), w=[tHS])
    for fc in range(8):
        s.dma("sp", OT[:, fc, :], io["oTg"][0][fc * 128:(fc + 1) * 128, :], w=[tOT])
        s.dma("sp", H1[:, fc, :], io["oTg"][1][fc * 128:(fc + 1) * 128, :], w=[tH1])
    for fc in range(8):
        s.add("dve", lambda e, fc=fc: e.tensor_scalar(H1[:, fc, :], H1[:, fc, :], HS[:, 1:2], None, ALU.mult), r=[tHS, tH1], w=[tH1])
        s.add("dve", lambda e, fc=fc: e.scalar_tensor_tensor(out=OT[:, fc, :], in0=OT[:, fc, :], scalar=HS[:, 0:1], in1=H1[:, fc, :], op0=ALU.mult, op1=ALU.add),
              r=[tHS, tH1, tOT], w=[tOT])


def layernorm_rows(s, c, src, tsrc, dst, tdst, G, Bt, tG):
    ST, tST = c.ST, c.tST
    s.add("dve", lambda e: e.bn_stats(out=ST[:, 0:6], in_=src[:, 0:512]), r=[tsrc], w=[tST])
    s.add("dve", lambda e: e.bn_stats(out=ST[:, 6:12], in_=src[:, 512:1024]), r=[tsrc], w=[tST])
    s.add("dve", lambda e: e.bn_aggr(out=ST[:, 12:14], in_=ST[:, 0:12]), r=[tST], w=[tST])
    s.add("act", lambda e: e.activation(out=ST[:, 15:16], in_=ST[:, 13:14], func=AF.Sqrt, bias=c.EPSC[:, 0:1]), r=[tST, c.tEPSC], w=[tST])
    s.add("dve", lambda e: e.reciprocal(out=ST[:, 14:15], in_=ST[:, 15:16]), r=[tST], w=[tST])
    s.add("dve", lambda e: e.tensor_scalar(dst, src, ST[:, 12:13], ST[:, 14:15], ALU.subtract, ALU.mult), r=[tsrc, tST], w=[tdst])
    s.add("dve", lambda e: e.tensor_tensor(out=dst, in0=dst, in1=G, op=ALU.mult), r=[tdst, tG], w=[tdst])
    s.add("dve", lambda e: e.tensor_tensor(out=dst, in0=dst, in1=Bt, op=ALU.add), r=[tdst, tG], w=[tdst])


def ln_ops(s, c, src, tsrc, dst, tdst, G, Bt, tG, ST, tST):
    return [
        lambda: s.add("dve", lambda e: e.bn_stats(out=ST[:, 0:6], in_=src[:, 0:512]), r=[tsrc], w=[tST]),
        lambda: s.add("dve", lambda e: e.bn_stats(out=ST[:, 6:12], in_=src[:, 512:1024]), r=[tsrc], w=[tST]),
        lambda: s.add("dve", lambda e: e.bn_aggr(out=ST[:, 12:14], in_=ST[:, 0:12]), r=[tST], w=[tST]),
        lambda: s.add("act", lambda e: e.activation(out=ST[:, 15:16], in_=ST[:, 13:14], func=AF.Sqrt, bias=c.EPSC[:, 0:1]), r=[tST, c.tEPSC], w=[tST]),
        lambda: s.add("dve", lambda e: e.reciprocal(out=ST[:, 14:15], in_=ST[:, 15:16]), r=[tST], w=[tST]),
        lambda: s.add("dve", lambda e: e.tensor_scalar(dst, src, ST[:, 12:13], ST[:, 14:15], ALU.subtract, ALU.mult), r=[tsrc, tST], w=[tdst]),
        lambda: s.add("dve", lambda e: e.tensor_tensor(out=dst, in0=dst, in1=G, op=ALU.mult), r=[tdst, tG], w=[tdst]),
        lambda: s.add("dve", lambda e: e.tensor_tensor(out=dst, in0=dst, in1=Bt, op=ALU.add), r=[tdst, tG], w=[tdst]),
    ]


def interleave(chains):
    for j in range(max(len(ch) for ch in chains)):
        for ch in chains:
            if j < len(ch):
                ch[j]()


def transpose_rows(s, c, src, tsrc, XT, tXT, col0, nchunk=8):
    for g0 in range(0, nchunk, 4):
        n = min(4, nchunk - g0)
        ps, tps = ps_half(c)
        for j in range(n):
            dc = g0 + j
            s.add("pe", lambda e, ps=ps, j=j, dc=dc: e.transpose(ps[:, j * 128:(j + 1) * 128], src[:, dc * 128:(dc + 1) * 128], c.IDN[:]),
                  r=[tsrc, c.tIDN], w=[tps])
        s.add("act", lambda e, ps=ps, g0=g0, n=n: e.activation(
            out=XT[:, g0:g0 + n, col0:col0 + 128], in_=ps[:, 0:n * 128].rearrange("p (a b) -> p a b", b=128), func=AF.Copy),
            r=[tps], w=[tXT])


def expert_ffn(s, c, XS, tXS, C, wg, wu, wd, dff, aT, taT, WG, tWG, WU, tWU, WD, tWD, out_cb, wd_preloaded=False):
    nfc = dff // 128
    ncb = dff // 256
    wg_v = wg.rearrange("(c p) f -> p c f", p=128)
    wu_v = wu.rearrange("(c p) f -> p c f", p=128)
    wd_v = wd.rearrange("(c p) n -> p c n", p=128)
    groups = [(g0, min(512, C - g0)) for g0 in range(0, C, 512)]
    for cb in range(ncb):
        b = cb % 2
        s.dma("pool", WG[b][:], wg_v[:, :, cb * 256:(cb + 1) * 256], w=[tWG[b]])
        s.dma("pool", WU[b][:], wu_v[:, :, cb * 256:(cb + 1) * 256], w=[tWU[b]])
        if not wd_preloaded:
            s.dma("pool", WD[:, 2 * cb:2 * cb + 2, :], wd_v[:, 2 * cb:2 * cb + 2, :], w=[tWD])
        for fl in range(2):
            fc = cb * 2 + fl
            for (g0, gn) in groups:
                pg, tpg = ps_half(c)
                pu, tpu = ps_half(c)
                for dc in range(8):
                    s.add("pe", lambda e, pg=pg, b=b, dc=dc, fl=fl, g0=g0, gn=gn: e.matmul(
                        pg[:, 0:gn], WG[b][:, dc, fl * 128:(fl + 1) * 128], XS[:, dc, g0:g0 + gn], start=(dc == 0), stop=(dc == 7)),
                        r=[tWG[b], tXS], w=[tpg])
                for dc in range(8):
                    s.add("pe", lambda e, pu=pu, b=b, dc=dc, fl=fl, g0=g0, gn=gn: e.matmul(
                        pu[:, 0:gn], WU[b][:, dc, fl * 128:(fl + 1) * 128], XS[:, dc, g0:g0 + gn], start=(dc == 0), stop=(dc == 7)),
                        r=[tWU[b], tXS], w=[tpu])
                k = c.sgrr % 2
                c.sgrr += 1
                SG, tSG = c.SG[k], c.tSG[k]
                s.add("act", lambda e, pg=pg, SG=SG, gn=gn: e.activation(out=SG[:, 0:gn], in_=pg[:, 0:gn], func=AF.Silu),
                      r=[tpg], w=[tSG])
                s.add("dve", lambda e, pu=pu, SG=SG, fc=fc, g0=g0, gn=gn: e.tensor_tensor(
                    out=aT[:, fc, g0:g0 + gn], in0=SG[:, 0:gn], in1=pu[:, 0:gn], op=ALU.mult),
                    r=[tSG, tpu], w=[taT])
    for sc in range(C // 128):
        py, tpy = ps_full(c)
        for nh in range(2):
            for fc in range(nfc):
                s.add("pe", lambda e, py=py, nh=nh, fc=fc, sc=sc: e.matmul(
                    py[:, nh * 512:(nh + 1) * 512], aT[:, fc, sc * 128:(sc + 1) * 128], WD[:, fc, nh * 512:(nh + 1) * 512],
                    start=(fc == 0), stop=(fc == nfc - 1)),
                    r=[taT, tWD], w=[tpy[nh]])
        out_cb(sc, py, tpy)


def build_B_dense(nc, st, s, c, io):
    DFF = 2816
    NFC = DFF // 128
    sb = lambda name, shape, dt=F32: st.enter_context(nc.sbuf_tensor(uname(name), list(shape), dt))
    c.IDN = sb("IDN", [128, 128]); c.tIDN = Tok("idn")
    c.ST = sb("ST", [128, 16]); c.tST = Tok("st")
    c.EPSC = sb("EPSC", [128, 1]); c.tEPSC = Tok("eps")
    s.add("dve", lambda e: e.memset(c.EPSC[:], EPS), w=[c.tEPSC])
    c.SG = [sb("SG%d" % i, [128, 512], BF16) for i in range(2)]; c.tSG = [Tok(), Tok()]; c.sgrr = 0
    BIG = sb("BIG", [128, NFC * 1024], BF16)
    OT = BIG[:, 0:8 * NT].rearrange("p (c t) -> p c t", t=NT); tBIG = Tok("big")
    aT = BIG[:, 0:NFC * 1024].rearrange("p (c t) -> p c t", t=1024)
    W16 = sb("W16", [128, 8, 1024], BF16); tW16 = Tok("w16")
    LNP = sb("LNP", [128, 2, 1024]); tLNP = Tok("lnp")
    XT = sb("XT", [128, 8, NT], BF16); tXT = Tok("xt")
    XR = [sb("XR%d" % i, [128, 1024]) for i in range(4)]; tXR = [Tok() for _ in range(4)]
    ST2 = [c.ST, sb("STb", [128, 16])]; tST2 = [c.tST, Tok("stb")]
    TT = [sb("TT%d" % i, [128, 1024]) for i in range(2)]; tTT = [Tok(), Tok()]
    WG = [sb("WG%d" % i, [128, 8, 256], BF16) for i in range(2)]; tWG = [Tok(), Tok()]
    WU = [sb("WU%d" % i, [128, 8, 256], BF16) for i in range(2)]; tWU = [Tok(), Tok()]
    WD = sb("WD", [128, NFC, 1024], BF16); tWD = Tok("wd")
    PP = sb("PP", [128, 2, 1024], BF16); tPP = Tok("pp")
    PTt = sb("PTt", [128, 2, NT], BF16); tPTt = Tok("ptt")
    PR = [sb("PR%d" % i, [128, 256]) for i in range(2)]; tPR = [Tok(), Tok()]
    xs1 = dram(nc, uname("xs1"), [NT, D], kind="Internal")
    xs2 = dram(nc, uname("xs2"), [NT, D], kind="Internal")
    txs1, txs2 = Tok("xs1"), Tok("xs2")

    def load_ln(k):
        s.dma("sp", LNP[:], io["lnp"][2 * k:2 * k + 2, :].partition_broadcast(128), w=[tLNP])

    s.dma("sp", c.IDN[:], io["idn"], w=[c.tIDN])
    load_oT(nc, st, s, io, OT, tBIG, XT, tXT)
    s.dma("pool", W16[:], io["w_out"].rearrange("(c p) n -> p c n", p=128), w=[tW16])
    load_ln(0)
    for ip in range(0, NT // 128, 2):
        chains = []
        for i in (ip, ip + 1):
            k = i % 2
            k4 = i % 4
            s.dma("sp", XR[k4][:], io["xres"][i * 128:(i + 1) * 128, :], w=[tXR[k4]])
            py, tpy = ps_full(c)
            for nh in range(2):
                for fc in range(8):
                    s.add("pe", lambda e, py=py, nh=nh, fc=fc, i=i: e.matmul(
                        py[:, nh * 512:(nh + 1) * 512], OT[:, fc, i * 128:(i + 1) * 128], W16[:, fc, nh * 512:(nh + 1) * 512],
                        start=(fc == 0), stop=(fc == 7)), r=[tBIG, tW16], w=[tpy[nh]])
            ops = [lambda py=py, tpy=tpy, k=k, k4=k4: s.add("dve", lambda e: e.scalar_tensor_tensor(out=TT[k][:], in0=XR[k4][:], scalar=ALPHA, in1=py[:], op0=ALU.mult, op1=ALU.add),
                                                            r=[tXR[k4], tpy[0], tpy[1]], w=[tTT[k]])]
            ops += ln_ops(s, c, TT[k][:], tTT[k], XR[k4][:], tXR[k4], LNP[:, 0, :], LNP[:, 1, :], tLNP, ST2[k], tST2[k])
            ops.append(lambda i=i, k4=k4: s.dma("sp", xs1[i * 128:(i + 1) * 128, :], XR[k4][:], r=[tXR[k4]], w=[txs1]))
            chains.append(ops)
        interleave(chains)
        if ip >= 2:
            for i in (ip - 2, ip - 1):
                transpose_rows(s, c, XR[i % 4][:], tXR[i % 4], XT, tXT, i * 128)
    for i in (NT // 128 - 2, NT // 128 - 1):
        transpose_rows(s, c, XR[i % 4][:], tXR[i % 4], XT, tXT, i * 128)
    load_ln(1)
    s.dma("pool", W16[:], io["ple_gate"].rearrange("(c p) n -> p c n", p=128), w=[tW16])
    s.dma("pool", PP[:], io["ple_proj"].rearrange("(c p) n -> p c n", p=128), w=[tPP])
    for grp in range(NT // 1024):
        def out_cb(sc, py, tpy, grp=grp):
            i = grp * 8 + sc
            k = i % 2
            s.dma("sp", XR[k][:], xs1[i * 128:(i + 1) * 128, :], r=[txs1], w=[tXR[k]])
            s.add("dve", lambda e: e.scalar_tensor_tensor(out=TT[k][:], in0=XR[k][:], scalar=ALPHA, in1=py[:], op0=ALU.mult, op1=ALU.add),
                  r=[tXR[k], tpy[0], tpy[1]], w=[tTT[k]])
            layernorm_rows(s, c, TT[k][:], tTT[k], XR[k][:], tXR[k], LNP[:, 0, :], LNP[:, 1, :], tLNP)
            s.dma("sp", xs2[i * 128:(i + 1) * 128, :], XR[k][:], r=[tXR[k]], w=[txs2])

        expert_ffn(s, c, XT[:, :, grp * 1024:(grp + 1) * 1024], tXT, 1024, io["wg"], io["wu"], io["wd"], DFF,
                   aT, tBIG, WG, tWG, WU, tWU, WD, tWD, out_cb, wd_preloaded=(grp > 0))
    for i in range(NT // 128):
        k = i % 2
        s.dma("sp", XR[k][:], xs2[i * 128:(i + 1) * 128, :], r=[txs2], w=[tXR[k]])
        transpose_rows(s, c, XR[k][:], tXR[k], XT, tXT, i * 128)
        s.dma("sp", PR[k][:], io["p"][i * 128:(i + 1) * 128, :], w=[tPR[k]])
        transpose_rows(s, c, PR[k][:], tPR[k], PTt, tPTt, i * 128, nchunk=2)
    load_ln(2)
    outs = []
    touts = []
    chains = []
    for i in range(NT // 128):
        k = i % 2
        s.dma("sp", XR[k][:], xs2[i * 128:(i + 1) * 128, :], r=[txs2], w=[tXR[k]])
        pg, tpg = ps_full(c)
        pe_, tpe = ps_full(c)
        for nh in range(2):
            for dc in range(8):
                s.add("pe", lambda e, pg=pg, nh=nh, dc=dc, i=i: e.matmul(
                    pg[:, nh * 512:(nh + 1) * 512], XT[:, dc, i * 128:(i + 1) * 128], W16[:, dc, nh * 512:(nh + 1) * 512],
                    start=(dc == 0), stop=(dc == 7)), r=[tXT, tW16], w=[tpg[nh]])
            for dc in range(2):
                s.add("pe", lambda e, pe_=pe_, nh=nh, dc=dc, i=i: e.matmul(
                    pe_[:, nh * 512:(nh + 1) * 512], PTt[:, dc, i * 128:(i + 1) * 128], PP[:, dc, nh * 512:(nh + 1) * 512],
                    start=(dc == 0), stop=(dc == 1)), r=[tPTt, tPP], w=[tpe[nh]])
        ops = [
            lambda pg=pg, tpg=tpg, k=k: s.add("act", lambda e: e.activation(out=TT[k][:], in_=pg[:], func=AF.Sigmoid), r=[tpg[0], tpg[1]], w=[tTT[k]]),
            lambda pe_=pe_, tpe=tpe, k=k: s.add("dve", lambda e: e.tensor_tensor(out=TT[k][:], in0=TT[k][:], in1=pe_[:], op=ALU.mult),
                                               r=[tTT[k], tpe[0], tpe[1]], w=[tTT[k]]),
            lambda k=k: s.add("dve", lambda e: e.scalar_tensor_tensor(out=TT[k][:], in0=XR[k][:], scalar=ALPHA, in1=TT[k][:], op0=ALU.mult, op1=ALU.add),
                              r=[tXR[k], tTT[k]], w=[tTT[k]]),
        ]
        ops += ln_ops(s, c, TT[k][:], tTT[k], XR[k][:], tXR[k], LNP[:, 0, :], LNP[:, 1, :], tLNP, ST2[k], tST2[k])

        def store_out(i=i, k=k):
            touts.append(Tok())
            outs.append(s.dma("sp", io["out"][i * 128:(i + 1) * 128, :], XR[k][:], r=[tXR[k]], w=[touts[-1]]))
        ops.append(store_out)
        if "xg" in io and i % 4 == 3:
            def xchg(j=i // 4):
                outs.append(s.add("pool", lambda e: e.collective_compute("AllGather", ALU.bypass, replica_groups=RG_PAIRS,
                                                                         ins=[io["out"][j * 512:(j + 1) * 512, :].opt()], outs=[io["xg"][j].opt()]),
                                  r=touts[-4:], w=[Tok()], dma=True, inc=1))
            ops.append(xchg)
        chains.append(ops)
        if i % 2 == 1:
            interleave(chains)
            chains = []
    return outs


CAP = 640
NSC = CAP // 128


def build_B_moe(nc, st, s, c, io):
    DFF = 3584
    NFC = DFF // 128
    sb = lambda name, shape, dt=F32: st.enter_context(nc.sbuf_tensor(uname(name), list(shape), dt))
    c.IDN = sb("IDN", [128, 128]); c.tIDN = Tok("idn")
    IDNb = sb("IDNb", [128, 128], BF16); tIDNb = Tok()
    UT = sb("UT", [128, 128], BF16); ONESb = sb("ONESb", [128, 128], BF16); tUT = Tok()
    IOTA = sb("IOTA", [128, CAP]); tIOTA = Tok()
    c.ST = sb("ST", [128, 16]); c.tST = Tok("st")
    c.EPSC = sb("EPSC", [128, 1]); c.tEPSC = Tok("eps")
    ST2 = [c.ST, sb("STb", [128, 16])]; tST2 = [c.tST, Tok("stb")]
    s.add("dve", lambda e: e.memset(c.EPSC[:], EPS), w=[c.tEPSC])
    BIG = sb("BIG", [128, NFC * CAP], BF16); tBIG = Tok("big")
    OT = BIG[:, 0:8 * NT].rearrange("p (c t) -> p c t", t=NT)
    aT = BIG[:, 0:NFC * CAP].rearrange("p (c t) -> p c t", t=CAP)
    W16 = sb("W16", [128, 16 * CAP], BF16); tW16 = Tok("w16")
    WO = W16[:, 0:8 * 1024].rearrange("p (c n) -> p c n", n=1024)
    SE = W16[:, 0:16 * CAP].rearrange("p (i s) -> p i s", s=CAP)
    LNP = sb("LNP", [128, 2, 1024]); tLNP = Tok("lnp")
    XTt = sb("XT", [128, 8 * NT], BF16); tXT = Tok("xt")
    XT = XTt[:, :].rearrange("p (c t) -> p c t", t=NT)
    X1B = XTt[:, :].rearrange("p (i d) -> p i d", d=1024)
    XR = [sb("XR%d" % i, [128, 1024]) for i in range(2)]; tXR = [Tok(), Tok()]
    TT = [sb("TT%d" % i, [128, 1024]) for i in range(2)]; tTT = [Tok(), Tok()]
    WG = [sb("WG%d" % i, [128, 8, 256], BF16) for i in range(2)]; tWG = [Tok(), Tok()]
    WU = [sb("WU%d" % i, [128, 8, 256], BF16) for i in range(2)]; tWU = [Tok(), Tok()]
    WD = sb("WD", [128, NFC, 1024], BF16); tWD = Tok("wd")
    PP = BIG[:, 0:2048].rearrange("p (c n) -> p c n", n=1024); tPP = tBIG
    RG_ = [[sb("RG%d_%d" % (i, j), [128, 1024], BF16) for j in range(2)] for i in range(1)]; tRG = [[Tok(), Tok()]]
    RG_.append(RG_[0]); tRG.append(tRG[0])
    Of1 = sb("Of1", [128, 16, 8]); BASE = sb("BASE", [128, 16, 8]); IDXF = sb("IDXF", [128, 16, 2]); IDXU = sb("IDXU", [128, 16, 2], mybir.dt.uint32); tIDX = Tok()
    XGt = sb("XG", [128, 8 * CAP], BF16); tXG = Tok("xg")
    XGs = [XGt[:, :].rearrange("p (c s) -> p c s", s=CAP), XTt[:, 0:8 * CAP].rearrange("p (c s) -> p c s", s=CAP)]
    YEs = [XGt[:, 0:NSC * 1024].rearrange("p (s n) -> p s n", n=1024), XTt[:, 0:NSC * 1024].rearrange("p (s n) -> p s n", n=1024)]
    tXGs = [tXG, tXT]
    XG = XGs[0]
    PTt = XGt[:, 0:2 * NT].rearrange("p (c t) -> p c t", t=NT)
    PR = [TT[i][:, 0:256] for i in range(2)]; tPR = tTT
    XTr = sb("XTr", [128, 8, 128], BF16); tXTr = Tok()
    c.SG = [XTr[:, 0:4, :].rearrange("p a b -> p (a b)"), XTr[:, 4:8, :].rearrange("p a b -> p (a b)")]; c.tSG = [Tok(), Tok()]; c.sgrr = 0
    WR = sb("WR", [128, 8, 8], BF16); tWR = Tok()
    RT = sb("RT", [128, 64]); tRT = Tok()
    Af = sb("Af", [128, 16, 8]); Ab = sb("Ab", [128, 16, 8], BF16); Gb = sb("Gb", [128, 16, 8], BF16); tAG = Tok()
    RKM = sb("RKM", [128, 16, 8]); tRKM = Tok()
    GSLs = [sb("GSL%d" % i, [128, NSC, 4]) for i in range(2)]; tGSLs = [Tok(), Tok()]
    G3 = sb("G3", [128, 16, 3], BF16); tG3 = Tok()
    TIDX = sb("TIDX", [128, 8]); TIDU = sb("TIDU", [128, 8], mybir.dt.uint32); tTID = Tok()
    YE = XGt[:, 0:NSC * 1024].rearrange("p (s n) -> p s n", n=1024); tYE = tXG
    xs1 = dram(nc, uname("xs1"), [NT, D], kind="Internal")
    xs2 = dram(nc, uname("xs2"), [NT, D], kind="Internal")
    yed = dram(nc, uname("yed"), [8 * CAP, D], BF16, kind="Internal")
    tyed = Tok("yed")
    txs1, txs2 = Tok("xs1"), Tok("xs2")

    def load_ln(k):
        s.dma("sp", LNP[:], io["lnp"][2 * k:2 * k + 2, :].partition_broadcast(128), w=[tLNP])

    s.dma("sp", c.IDN[:], io["idn"], w=[c.tIDN])
    s.dma("pool", IDNb[:], io["idn"], w=[tIDNb])
    s.dma("pool", UT[:], io["ut"], w=[tUT])
    s.add("pool", lambda e: e.memset(ONESb[:], 1.0), w=[tUT])
    s.dma("sp", IOTA[:], io["iota"], w=[tIOTA])
    s.dma("pool", G3[:, :, 1:3], io["pcol"], w=[tG3])
    load_oT(nc, st, s, io, OT, tBIG, XT, tXT)
    s.dma("pool", WO, io["w_out"].rearrange("(c p) n -> p c n", p=128), w=[tW16])
    s.dma("pool", WR[:], io["router"].rearrange("(c p) n -> p c n", p=128), w=[tWR])
    load_ln(0)
    def route_tile(i, k):
        transpose_rows(s, c, XR[k][:], tXR[k], XTr, tXTr, 0)
        pl, tpl = ps_half(c)
        for dc in range(8):
            s.add("pe", lambda e, pl=pl, dc=dc: e.matmul(pl[:, 0:8], XTr[:, dc, :], WR[:, dc, :], start=(dc == 0), stop=(dc == 7)), r=[tXTr, tWR], w=[tpl])
        LG = RT[:, 0:8]; MX8 = RT[:, 8:16]; OH1 = RT[:, 16:24]; OH2 = RT[:, 24:32]; DD = RT[:, 32:33]; G2 = RT[:, 33:34]; G1 = RT[:, 34:35]; GF = RT[:, 40:48]
        s.add("dve", lambda e, pl=pl: e.tensor_copy(out=LG, in_=pl[:, 0:8]), r=[tpl], w=[tRT])
        s.add("dve", lambda e: e.max(out=MX8, in_=LG), r=[tRT], w=[tRT])
        s.add("dve", lambda e: e.tensor_scalar(OH1, LG, RT[:, 8:9], None, ALU.is_equal), r=[tRT], w=[tRT])
        s.add("dve", lambda e: e.tensor_scalar(OH2, LG, RT[:, 9:10], None, ALU.is_equal), r=[tRT], w=[tRT])
        s.add("dve", lambda e: e.tensor_tensor(out=DD, in0=RT[:, 9:10], in1=RT[:, 8:9], op=ALU.subtract), r=[tRT], w=[tRT])
        s.add("act", lambda e: e.activation(out=G2, in_=DD, func=AF.Sigmoid), r=[tRT], w=[tRT])
        s.add("dve", lambda e: e.tensor_scalar(G1, G2, -1.0, 1.0, ALU.mult, ALU.add), r=[tRT], w=[tRT])
        s.add("dve", lambda e, i=i: e.tensor_copy(out=Of1[:, i, :], in_=OH1), r=[tRT], w=[tAG])
        s.add("dve", lambda e, i=i: e.tensor_tensor(out=Af[:, i, :], in0=OH1, in1=OH2, op=ALU.add), r=[tRT], w=[tAG])
        s.add("dve", lambda e, i=i: e.tensor_copy(out=Ab[:, i, :], in_=Af[:, i, :]), r=[tAG], w=[tAG])
        s.add("dve", lambda e: e.tensor_scalar(GF, OH1, G1, None, ALU.mult), r=[tRT], w=[tRT])
        s.add("dve", lambda e, i=i: e.scalar_tensor_tensor(out=Gb[:, i, :], in0=OH2, scalar=G2, in1=GF, op0=ALU.mult, op1=ALU.add), r=[tRT], w=[tAG])

    for i in range(NT // 128):
        k = i % 2
        s.dma("sp", XR[k][:], io["xres"][i * 128:(i + 1) * 128, :], w=[tXR[k]])
        py, tpy = ps_full(c)
        for nh in range(2):
            for fc in range(8):
                s.add("pe", lambda e, py=py, nh=nh, fc=fc, i=i: e.matmul(
                    py[:, nh * 512:(nh + 1) * 512], OT[:, fc, i * 128:(i + 1) * 128], WO[:, fc, nh * 512:(nh + 1) * 512],
                    start=(fc == 0), stop=(fc == 7)), r=[tBIG, tW16], w=[tpy[nh]])
        s.add("dve", lambda e, py=py, k=k: e.scalar_tensor_tensor(out=TT[k][:], in0=XR[k][:], scalar=ALPHA, in1=py[:], op0=ALU.mult, op1=ALU.add),
              r=[tXR[k], tpy[0], tpy[1]], w=[tTT[k]])
        layernorm_rows(s, c, TT[k][:], tTT[k], XR[k][:], tXR[k], LNP[:, 0, :], LNP[:, 1, :], tLNP)
        s.dma("sp", xs1[i * 128:(i + 1) * 128, :], XR[k][:], r=[tXR[k]], w=[txs1])
        if i >= 1:
            route_tile(i - 1, (i - 1) % 2)
    route_tile(15, 15 % 2)
    pr, tpr = ps_half(c)
    first = True
    for i in range(16):
        for j in range(i + 1):
            s.add("pe", lambda e, pr=pr, i=i, j=j, st_=first: e.matmul(pr[:, i * 8:(i + 1) * 8], (UT if j == i else ONESb)[:], Ab[:, j, :],
                                                                      start=st_, stop=(j == i), skip_group_check=True), r=[tUT, tAG], w=[tpr])
            first = False
    s.add("dve", lambda e, pr=pr: e.scalar_tensor_tensor(out=RKM[:], in0=pr[:, 0:128].rearrange("p (i e) -> p i e", e=8), scalar=1.0, in1=Af[:], op0=ALU.add, op1=ALU.mult),
          r=[tpr, tAG], w=[tRKM])
    s.add("dve", lambda e: e.tensor_scalar(RKM[:], RKM[:], -1.0, None, ALU.add), r=[tRKM], w=[tRKM])
    s.add("dve", lambda e: e.tensor_scalar(RT[:, 48:56], IOTA[:, 0:8], float(CAP), None, ALU.mult), r=[tIOTA, tRT], w=[tRT])
    s.add("dve", lambda e: e.tensor_tensor(out=BASE[:], in0=RKM[:], in1=RT[:, 48:56].unsqueeze(1).to_broadcast([128, 16, 8]), op=ALU.add), r=[tRKM, tRT], w=[tIDX])
    s.add("dve", lambda e: e.tensor_tensor(out=RKM[:], in0=Of1[:], in1=BASE[:], op=ALU.mult), r=[tAG, tIDX, tRKM], w=[tRKM])
    s.add("dve", lambda e: e.reduce_sum(out=IDXF[:, :, 0], in_=RKM[:], axis=AX.X), r=[tRKM], w=[tIDX])
    s.add("dve", lambda e: e.tensor_tensor(out=RKM[:], in0=Af[:], in1=Of1[:], op=ALU.subtract), r=[tAG, tIDX, tRKM], w=[tRKM])
    s.add("dve", lambda e: e.tensor_tensor(out=RKM[:], in0=RKM[:], in1=BASE[:], op=ALU.mult), r=[tIDX, tRKM], w=[tRKM])
    s.add("dve", lambda e: e.reduce_sum(out=IDXF[:, :, 1], in_=RKM[:], axis=AX.X), r=[tRKM], w=[tIDX])
    s.add("dve", lambda e: e.tensor_copy(out=IDXU[:], in_=IDXF[:]), r=[tIDX], w=[tIDX])
    s.add("dve", lambda e: e.scalar_tensor_tensor(out=RKM[:], in0=BASE[:], scalar=1.0, in1=Af[:], op0=ALU.add, op1=ALU.mult), r=[tIDX, tAG, tRKM], w=[tRKM])
    s.add("dve", lambda e: e.tensor_tensor(out=BASE[:], in0=Af[:], in1=RT[:, 48:56].unsqueeze(1).to_broadcast([128, 16, 8]), op=ALU.mult), r=[tAG, tRT, tIDX], w=[tIDX])
    s.add("dve", lambda e: e.tensor_tensor(out=RKM[:], in0=RKM[:], in1=BASE[:], op=ALU.subtract), r=[tIDX, tRKM], w=[tRKM])
    s.add("dve", lambda e: e.tensor_scalar(RKM[:], RKM[:], -1.0, None, ALU.add), r=[tRKM], w=[tRKM])
    load_ln(1)
    groups = [(0, 512), (512, CAP - 512)]

    def build_se(ex_):
        for i in range(16):
            s.add("dve", lambda e, i=i, ex_=ex_: e.tensor_scalar(SE[:, i, :], IOTA[:], RKM[:, i, ex_:ex_ + 1], None, ALU.is_equal), r=[tIOTA, tRKM], w=[tW16])


    LAND = [TT[0], TT[1], XR[0], XR[1]]; tLAND = [tTT[0], tTT[1], tXR[0], tXR[1]]

    def gather_prep(ex):
        s.add("dve", lambda e, ex=ex: e.tensor_copy(out=G3[:, :, 0], in_=Gb[:, :, ex]), r=[tAG, tG3], w=[tG3])
        pgs, tpgs = ps_half(c)
        first = True
        for sc in range(NSC):
            for i in range(16):
                s.add("pe", lambda e, pgs=pgs, sc=sc, i=i, st_=first: e.matmul(pgs[:, sc * 4:sc * 4 + 3], SE[:, i, sc * 128:(sc + 1) * 128], G3[:, i, :],
                                                                              start=st_, stop=(i == 15), skip_group_check=True), r=[tW16, tG3], w=[tpgs])
                first = False
        s.add("dve", lambda e, pgs=pgs: e.tensor_copy(out=GSLs[ex % 2][:], in_=pgs[:, 0:NSC * 4].rearrange("p (s k) -> p s k", k=4)), r=[tpgs], w=[tGSLs[ex % 2]])
        s.add("dve", lambda e: e.scalar_tensor_tensor(out=TIDX[:, 0:NSC], in0=GSLs[ex % 2][:, :, 2], scalar=128.0, in1=GSLs[ex % 2][:, :, 1], op0=ALU.mult, op1=ALU.add),
              r=[tGSLs[ex % 2]], w=[tTID])
        s.add("dve", lambda e: e.tensor_copy(out=TIDU[:, 0:NSC], in_=TIDX[:, 0:NSC]), r=[tTID], w=[tTID])
        for sc in range(min(NSC, 4)):
            s.add("pool", lambda e, sc=sc: e.indirect_dma_start(out=LAND[sc][:], out_offset=None, in_=xs1,
                                                               in_offset=bass.IndirectOffsetOnAxis(ap=TIDU[:, sc:sc + 1], axis=0)),
                  r=[tTID, txs1], w=[tLAND[sc]], dma=True)

    def gather_fin(ex):
        XGd, tXGd = XGs[ex % 2], tXGs[ex % 2]
        for sc in range(NSC):
            if sc >= 4:
                s.add("pool", lambda e, sc=sc: e.indirect_dma_start(out=LAND[sc % 4][:], out_offset=None, in_=xs1,
                                                                   in_offset=bass.IndirectOffsetOnAxis(ap=TIDU[:, sc:sc + 1], axis=0)),
                      r=[tTID, txs1], w=[tLAND[sc % 4]], dma=True)
            transpose_rows(s, c, LAND[sc % 4][:], tLAND[sc % 4], XGd, tXGd, sc * 128)

    build_se(0)
    gather_prep(0)
    gather_fin(0)
    for ex in range(8):
        wg_v = io["mg"][ex].rearrange("(c p) f -> p c f", p=128)
        wu_v = io["mu"][ex].rearrange("(c p) f -> p c f", p=128)
        wd_v = io["md"][ex].rearrange("(c p) n -> p c n", p=128)
        if ex < 7:
            build_se(ex + 1)
        for cb in range(DFF // 256):
            b = cb % 2
            s.dma("pool", WG[b][:], wg_v[:, :, cb * 256:(cb + 1) * 256], w=[tWG[b]])
            s.dma("pool", WU[b][:], wu_v[:, :, cb * 256:(cb + 1) * 256], w=[tWU[b]])
            s.dma("pool", WD[:, 2 * cb:2 * cb + 2, :], wd_v[:, 2 * cb:2 * cb + 2, :], w=[tWD])
            for fl in range(2):
                fc = cb * 2 + fl
                for (g0, gn) in groups:
                    pg, tpg = ps_half(c)
                    pu, tpu = ps_half(c)
                    for dc in range(8):
                        s.add("pe", lambda e, pg=pg, b=b, dc=dc, fl=fl, g0=g0, gn=gn, ex=ex: e.matmul(
                            pg[:, 0:gn], WG[b][:, dc, fl * 128:(fl + 1) * 128], XGs[ex % 2][:, dc, g0:g0 + gn], start=(dc == 0), stop=(dc == 7)),
                            r=[tWG[b], tXGs[ex % 2]], w=[tpg])
                    for dc in range(8):
                        s.add("pe", lambda e, pu=pu, b=b, dc=dc, fl=fl, g0=g0, gn=gn, ex=ex: e.matmul(
                            pu[:, 0:gn], WU[b][:, dc, fl * 128:(fl + 1) * 128], XGs[ex % 2][:, dc, g0:g0 + gn], start=(dc == 0), stop=(dc == 7)),
                            r=[tWU[b], tXGs[ex % 2]], w=[tpu])
                    k = c.sgrr % 2
                    c.sgrr += 1
                    SG, tSG = c.SG[k], c.tSG[k]
                    s.add("act", lambda e, pg=pg, SG=SG, gn=gn: e.activation(out=SG[:, 0:gn], in_=pg[:, 0:gn], func=AF.Silu), r=[tpg], w=[tSG])
                    s.add("dve", lambda e, pu=pu, SG=SG, fc=fc, g0=g0, gn=gn: e.tensor_tensor(
                        out=aT[:, fc, g0:g0 + gn], in0=SG[:, 0:gn], in1=pu[:, 0:gn], op=ALU.mult), r=[tSG, tpu], w=[tBIG])
        if ex < 7:
            gather_prep(ex + 1)
        for sc in range(NSC):
            py, tpy = ps_full(c)
            for nh in range(2):
                for fc in range(NFC):
                    s.add("pe", lambda e, py=py, fc=fc, sc=sc, nh=nh: e.matmul(py[:, nh * 512:(nh + 1) * 512], aT[:, fc, sc * 128:(sc + 1) * 128], WD[:, fc, nh * 512:(nh + 1) * 512],
                                                                            start=(fc == 0), stop=(fc == NFC - 1)), r=[tBIG, tWD], w=[tpy[nh]])
            s.add("dve", lambda e, py=py, sc=sc, ex=ex: e.tensor_scalar(YEs[ex % 2][:, sc, :], py[:], GSLs[ex % 2][:, sc, 0:1], None, ALU.mult),
                  r=[tpy[0], tpy[1], tGSLs[ex % 2]], w=[tXGs[ex % 2]])
        if ex < 7:
            gather_fin(ex + 1)
        s.dma("sp", yed[ex * CAP:(ex + 1) * CAP, :].rearrange("(s p) n -> p s n", p=128), YEs[ex % 2], r=[tXGs[ex % 2]], w=[tyed])
    for i in range(16):
        k = i % 2
        rows = slice(i * 128, (i + 1) * 128)
        for j in range(2):
            s.add("pool", lambda e, i=i, j=j, k=k: e.indirect_dma_start(out=RG_[k][j][:], out_offset=None, in_=yed,
                                                                       in_offset=bass.IndirectOffsetOnAxis(ap=IDXU[:, i, j:j + 1], axis=0)),
                  r=[tIDX, tyed], w=[tRG[k][j]], dma=True)
        s.dma("sp", XR[k][:], xs1[rows, :], r=[txs1], w=[tXR[k]])
        s.add("dve", lambda e, k=k: e.tensor_tensor(out=TT[k][:], in0=RG_[k][0][:], in1=RG_[k][1][:], op=ALU.add), r=[tRG[k][0], tRG[k][1]], w=[tTT[k]])
        s.add("dve", lambda e, k=k: e.scalar_tensor_tensor(out=TT[k][:], in0=XR[k][:], scalar=ALPHA, in1=TT[k][:], op0=ALU.mult, op1=ALU.add),
              r=[tXR[k], tTT[k]], w=[tTT[k]])
        layernorm_rows(s, c, TT[k][:], tTT[k], XR[k][:], tXR[k], LNP[:, 0, :], LNP[:, 1, :], tLNP)
        s.dma("sp", xs2[rows, :], XR[k][:], r=[tXR[k]], w=[txs2])
        transpose_rows(s, c, XR[k][:], tXR[k], XT, tXT, i * 128)
    W16v = W16[:, 0:8 * 1024].rearrange("p (c n) -> p c n", n=1024)
    s.dma("pool", W16v, io["ple_gate"].rearrange("(c p) n -> p c n", p=128), w=[tW16])
    s.dma("pool", PP[:], io["ple_proj"].rearrange("(c p) n -> p c n", p=128), w=[tPP])
    for i in range(NT // 128):
        k = i % 2
        s.dma("sp", PR[k], io["p"][i * 128:(i + 1) * 128, :], w=[tPR[k]])
        transpose_rows(s, c, PR[k], tPR[k], PTt, tXG, i * 128, nchunk=2)
    load_ln(2)
    outs = []
    touts = []
    chains = []
    for i in range(NT // 128):
        k = i % 2
        s.dma("sp", XR[k][:], xs2[i * 128:(i + 1) * 128, :], r=[txs2], w=[tXR[k]])
        pg, tpg = ps_full(c)
        pe_, tpe = ps_full(c)
        for nh in range(2):
            for dc in range(8):
                s.add("pe", lambda e, pg=pg, nh=nh, dc=dc, i=i: e.matmul(
                    pg[:, nh * 512:(nh + 1) * 512], XT[:, dc, i * 128:(i + 1) * 128], W16v[:, dc, nh * 512:(nh + 1) * 512],
                    start=(dc == 0), stop=(dc == 7)), r=[tXT, tW16], w=[tpg[nh]])
            for dc in range(2):
                s.add("pe", lambda e, pe_=pe_, nh=nh, dc=dc, i=i: e.matmul(
                    pe_[:, nh * 512:(nh + 1) * 512], PTt[:, dc, i * 128:(i + 1) * 128], PP[:, dc, nh * 512:(nh + 1) * 512],
                    start=(dc == 0), stop=(dc == 1)), r=[tXG, tPP], w=[tpe[nh]])
        ops = [
            lambda pg=pg, tpg=tpg, k=k: s.add("act", lambda e: e.activation(out=TT[k][:], in_=pg[:], func=AF.Sigmoid), r=[tpg[0], tpg[1]], w=[tTT[k]]),
            lambda pe_=pe_, tpe=tpe, k=k: s.add("dve", lambda e: e.tensor_tensor(out=TT[k][:], in0=TT[k][:], in1=pe_[:], op=ALU.mult),
                                               r=[tTT[k], tpe[0], tpe[1]], w=[tTT[k]]),
            lambda k=k: s.add("dve", lambda e: e.scalar_tensor_tensor(out=TT[k][:], in0=XR[k][:], scalar=ALPHA, in1=TT[k][:], op0=ALU.mult, op1=ALU.add),
                              r=[tXR[k], tTT[k]], w=[tTT[k]]),
        ]
        ops += ln_ops(s, c, TT[k][:], tTT[k], XR[k][:], tXR[k], LNP[:, 0, :], LNP[:, 1, :], tLNP, ST2[k], tST2[k])

        def store_out(i=i, k=k):
            touts.append(Tok())
            outs.append(s.dma("sp", io["out"][i * 128:(i + 1) * 128, :], XR[k][:], r=[tXR[k]], w=[touts[-1]]))
        ops.append(store_out)
        if "xg" in io and i % 4 == 3:
            def xchg(j=i // 4):
                outs.append(s.add("pool", lambda e: e.collective_compute("AllGather", ALU.bypass, replica_groups=RG_PAIRS,
                                                                         ins=[io["out"][j * 512:(j + 1) * 512, :].opt()], outs=[io["xg"][j].opt()]),
                                  r=touts[-4:], w=[Tok()], dma=True, inc=1))
            ops.append(xchg)
        chains.append(ops)
        if i % 2 == 1:
            interleave(chains)
            chains = []
    return outs

T = 4096
NEG = -30000.0
NQT = 8
SL = [2.0 ** (-i / 2.0) for i in range(1, 17)]
SLOPE_C = SL[0:8]
SLOPE_A = [SL[8], SL[10], SL[12], SL[14]]
SLOPE_B = [SL[9], SL[11], SL[13], SL[15]]
C_KG0, C_KG1, C_KS, C_KW, C_KC, C_KB0, C_KB1 = 0, 128, 256, 320, 384, 448, 576
C_V = 704
C_QA = 1024
C_QB = 1280
C_QC = 1536
C_G = 1792
NW = 1798
M_CAUS, M_WM, M_SM, M_CM = 0, 4, 8, 13
NMASK = 18


def mask_tables():
    j = np.arange(128)[:, None]
    i = np.arange(512)[None, :]
    tabs = []
    for r in range(4):
        tabs.append((-128 * r + i - j) >= 0)
    for o in range(1, 5):
        dd = 128 * o + i - j
        tabs.append((dd >= 0) & (dd < 512))
    for o in range(-3, 2):
        dd = 128 * o + i - j
        tabs.append((dd >= 0) & (dd < 128))
    for m in range(5):
        tabs.append((i - 16 * j) >= (31 - 512 * m))
    return np.stack(tabs)


ALLOWED = mask_tables()


def host_consts(sidx):
    cst = {}
    cst["masks"] = np.where(ALLOWED, 0.0, NEG).astype(np.float32)
    cst["idn"] = np.eye(128, dtype=np.float32)
    cst["i30k"] = (np.eye(128) * 30000.0).astype(np.float32)
    own_a = [SLOPE_A[2 * sidx], SLOPE_A[2 * sidx + 1]]
    own_b = [SLOPE_B[2 * sidx], SLOPE_B[2 * sidx + 1]]
    own_c = SLOPE_C[4 * sidx:4 * sidx + 4]
    sl8 = own_a + own_b + list(own_c)
    p = np.arange(128)[:, None, None]
    oi = np.arange(32)[None, None, :]
    cst["ab"] = (np.array(sl8)[None, :, None] * (p - 128.0 * (oi - 3))).astype(np.float32)
    cc = np.arange(2)[None, None, :, None]
    qt = np.arange(8)[None, None, None, :]
    perm_a = [2 * sidx, 2 * sidx + 1, 2 * (1 - sidx), 2 * (1 - sidx) + 1]
    cst["cb"] = (np.array([SLOPE_A[h] for h in perm_a])[None, :, None, None] * (16.0 * p[:, :, :, None] + 31 + 2048 * cc - 512 * qt)).astype(np.float32).reshape(128, 64)
    n = np.arange(256)[:, None]
    jj = np.arange(64)[None, :]
    ov = np.clip(np.minimum(16 * n + 32, 64 * jj + 64) - np.maximum(16 * n, 64 * jj), 0, None) / 32.0
    ov[255] = 0.0
    cst["ov"] = ov.astype(np.float32).reshape(2, 128, 64)
    t = np.arange(T)
    cst["emat"] = (t[None, :] // 64 == np.arange(64)[:, None]).astype(np.float32)
    r = np.arange(-63, 64)[None, :]
    pp = np.arange(128)[:, None]
    ta = np.where(r <= -2, 1.0, np.where(r == -1, (pp >= 64) * 1.0, 0.0))
    tb = np.where(r <= -2, 0.0, np.where(r == -1, (pp < 64) * 1e9, np.where(r == 0, 1e9, np.where(r == 1, np.where(pp < 64, -1.0, 1e9), -1.0))))
    cst["ta"] = ta.astype(np.float32)
    cst["tb"] = tb.astype(np.float32)
    i512 = np.arange(512)
    ri = np.stack([(-s * i512).astype(np.float32).astype(ml_dtypes.bfloat16).astype(np.float32) for s in own_c])
    cst["ri"] = ri
    g = np.exp(ri.astype(np.float64) + np.array(own_c)[:, None] * i512[None, :])
    cst["gs"] = np.ascontiguousarray(g.reshape(4, 4, 128).transpose(2, 1, 0)).astype(np.float32)
    return cst


def host_w(w_in, sidx):
    W = np.zeros((D, NW), np.float32)
    kva = 256
    kc, vc, ks, vs, kw, vw = [w_in[:, kva + 64 * k: kva + 64 * k + 64] for k in range(6)]
    W[:, C_KG0:C_KG0 + 64] = kc; W[:, C_KG0 + 64:C_KG0 + 128] = kc
    W[:, C_KG1:C_KG1 + 64] = vc; W[:, C_KG1 + 64:C_KG1 + 128] = vc
    W[:, C_KS:C_KS + 64] = ks
    W[:, C_KW:C_KW + 64] = kw
    W[:, C_KC:C_KC + 64] = w_in[:, 1932 + 64 * sidx: 1932 + 64 * sidx + 64]
    for hh in range(2):
        h = 2 * sidx + hh
        base = C_KB0 + 128 * hh
        W[:, base:base + 32] = w_in[:, 908 + 64 * h: 908 + 64 * h + 32]
        W[:, base + 96:base + 128] = w_in[:, 908 + 64 * h + 32: 908 + 64 * h + 64]
        W[:, C_V + 128 + 64 * hh: C_V + 192 + 64 * hh] = w_in[:, 1164 + 64 * h: 1164 + 64 * h + 64]
        W[:, C_QB + 128 * hh: C_QB + 128 * hh + 64] = w_in[:, 652 + 64 * h: 652 + 64 * h + 64]
        W[:, C_QB + 128 * hh + 64: C_QB + 128 * hh + 128] = w_in[:, 652 + 64 * h: 652 + 64 * h + 64]
        W[:, C_G + 3 * hh: C_G + 3 * hh + 3] = w_in[:, 640 + 3 * h: 640 + 3 * h + 3]
    W[:, C_V:C_V + 64] = vs
    W[:, C_V + 64:C_V + 128] = vw
    W[:, C_V + 256:C_V + 320] = w_in[:, 2060 + 64 * sidx: 2060 + 64 * sidx + 64]
    perm_a = [2 * sidx, 2 * sidx + 1, 2 * (1 - sidx), 2 * (1 - sidx) + 1]
    for k_, h in enumerate(perm_a):
        W[:, C_QA + 64 * k_:C_QA + 64 * k_ + 64] = w_in[:, 64 * h:64 * h + 64]
    W[:, C_QC:C_QC + 256] = w_in[:, 1420 + 256 * sidx: 1420 + 256 * sidx + 256]
    return W


def host_small(inp, L, sidx):
    sm = {}
    w1 = inp["cmp_w1"][L]
    sm["w1"] = np.ascontiguousarray(w1.reshape(2, 16, 128, 128).transpose(2, 0, 1, 3)).reshape(128, 2 * 16 * 128)
    sm["w2"] = np.ascontiguousarray(inp["cmp_w2"][L].transpose(1, 0, 2)).reshape(128, 128)
    pos = inp["cmp_pos"][L]
    sm["pos"] = np.ascontiguousarray(pos.reshape(2, 16, 128).transpose(2, 0, 1)).reshape(128, 32)
    sm["dl"] = np.ascontiguousarray(inp["diff_lambda"][L].reshape(1, 128))
    sm["subln"] = np.ascontiguousarray(inp["diff_subln"][L].reshape(1, 64))
    sm["sinks"] = np.ascontiguousarray(inp["sinks"][L][4 * sidx:4 * sidx + 4].reshape(1, 4))
    return sm


A_IN = dict(x=[T, D], w=[D, NW], masks=[NMASK, 128, 512], idn=[128, 128], i30k=[128, 128], ab=[128, 8, 32], cb=[128, 64],
            ov=[2, 128, 64], emat=[64, T], ta=[128, 127], tb=[128, 127], ri=[4, 512], gs=[128, 4, 4],
            w1=[128, 4096], w2=[128, 128], pos=[128, 32], dl=[1, 128], subln=[1, 64], sinks=[1, 4])


def build_A(nc, st, s, c, io, layer):
    lam_init = 0.8 - 0.6 * math.exp(-0.3 * layer)
    sb = lambda name, shape, dt=F32: st.enter_context(nc.sbuf_tensor(uname(name), list(shape), dt))
    PSB = [st.enter_context(nc.psum_tensor(uname("psb%d" % i), [128, 1024], F32)) for i in range(4)]
    TPS = [[Tok("ps%d_%d" % (i, h)) for h in range(2)] for i in range(4)]
    rr = {"s": 0, "a": 0, "f": 0}

    def ps_score():
        k = rr["s"] % 4; rr["s"] += 1
        return PSB[k // 2][:, (k % 2) * 512:(k % 2 + 1) * 512], TPS[k // 2][k % 2]

    def ps_acc():
        k = rr["a"] % 4; rr["a"] += 1
        return PSB[2 + k // 2][:, (k % 2) * 512:(k % 2 + 1) * 512], TPS[2 + k // 2][k % 2]

    def ps_accfull():
        k = rr["f"] % 2; rr["f"] += 1
        rr["a"] = 0
        return PSB[2 + k], TPS[2 + k]

    W = sb("W", [128, 8, NW], BF16); tW = Tok("W")
    IDN = sb("IDN", [128, 128]); tIDN = Tok()
    IDNb = sb("IDNb", [128, 128], BF16); tIDNb = Tok()
    I30K = sb("I30K", [128, 128], BF16); tI30K = Tok()
    MK = sb("MK", [128, NMASK, 512], BF16); tMK = Tok()
    AB = sb("AB", [128, 8, 32]); tAB = Tok()
    CB = sb("CB", [128, 64]); tCB = Tok()
    TA = sb("TA", [128, 127]); TBt = sb("TBt", [128, 127]); tTAB = Tok()
    GS = sb("GS", [128, 4, 4]); tGS = Tok()
    SK = sb("SK", [128, 4, 4]); tSK = Tok()
    W1 = sb("W1", [128, 2, 16, 128], BF16); tW1 = Tok()
    W2 = sb("W2", [128, 2, 64], BF16); tW2 = Tok()
    POS = sb("POS", [128, 2, 16], BF16); tPOS = Tok()
    SM_ = sb("SMALL", [128, 256]); tSM = Tok()
    Kc2 = sb("Kc2", [128, 2, T], BF16); tKc2 = Tok()
    KS = sb("KS", [128, T], BF16); tKS = Tok()
    KW = sb("KW", [64, T], BF16); tKW = Tok()
    KC = sb("KC", [65, T], BF16); tKC = Tok()
    KB = [sb("KB%d" % i, [128, T], BF16) for i in range(2)]; tKB = [Tok(), Tok()]
    VALL = sb("VALL", [128, 32, 5, 65], BF16); tV = Tok()
    XC = sb("XC", [128, 4, 1024]); tXC = Tok()
    XTc = [sb("XTc%d" % i, [128, 8, 512], BF16) for i in range(2)]; tXTc = [Tok(), Tok()]
    HT = sb("HT", [128, 2, 256], BF16); tHT = Tok()
    BP = sb("BP", [128, 2]); tBP = Tok()
    KCT = sb("KCT", [64, 256], BF16); tKCT = Tok()
    RC = sb("RC", [128, 2, 129], BF16); tRC = Tok()
    QA = [[sb("QA%d_%d" % (b, h), [128, 512], BF16) for h in range(4)] for b in range(2)]
    tQA = [[Tok() for h in range(4)] for b in range(2)]
    QB = [[sb("QB%d_%d" % (b, h), [128, 512], BF16) for h in range(2)] for b in range(1)]
    tQB = [[Tok() for h in range(2)] for b in range(1)]
    QB.append(QB[0]); tQB.append(tQB[0])
    QC = [[sb("QC%d_%d" % (b, h), [65, 512], BF16) for h in range(4)] for b in range(1)]
    tQC = [[Tok() for h in range(4)] for b in range(1)]
    QC.append(QC[0]); tQC.append(tQC[0])
    GT = sb("GT", [128, 4, 6]); tGT = Tok()
    PTb = [sb("PT%d" % i, [128, 512], BF16) for i in range(4)]; tPT = [Tok() for _ in range(4)]
    ptrr = [0]
    IMP = sb("IMP", [128, 4, 64]); tIMP = Tok()
    IMP2 = sb("IMP2", [128, 4, 64]); tIMP2 = Tok()
    MX = sb("MX", [128, 4, 16]); tMX = Tok()
    NM = sb("NM", [128, 4, 128], BF16); tNM = Tok()
    REC = sb("REC", [128, 16]); tREC = Tok()
    OCc = sb("OCc", [128, 2, 4, 64]); tOCc = Tok()
    TMP = [sb("TMP%d" % i, [128, 4, 64]) for i in range(2)]; tTMP = [Tok(), Tok()]
    OCH = sb("OCH", [128, 4, 512]); tOCH = Tok()
    OTc = sb("OTc", [128, 4, 512], BF16); tOTc = Tok()

    q = "sp"
    xdeps = []
    if "xh" in io:
        txfs = [Tok("xfull%d" % j) for j in range(4)]
        for j in range(4):
            s.add("pool", lambda e, j=j: e.collective_compute("AllGather", ALU.bypass, replica_groups=[[0, 1], [2, 3], [4, 5], [6, 7]],
                                                              ins=[io["xh"][j * 512:(j + 1) * 512, :].opt()], outs=[io["xg"][j].opt()]),
                  r=[], w=[txfs[j]], dma=True, inc=1)
        xdeps = txfs
    s.dma("pool", W[:], io["w"].rearrange("(c p) n -> p c n", p=128), w=[tW])
    s.dma(q, IDN[:], io["idn"], w=[tIDN])
    s.dma("pool", IDNb[:], io["idn"], w=[tIDNb])
    s.dma("pool", I30K[:], io["i30k"], w=[tI30K])
    for m0 in range(0, NMASK, 4):
        m1 = min(NMASK, m0 + 4)
        s.dma("pool", MK[:, m0:m1, :], io["masks"][m0:m1].rearrange("m p i -> p m i"), w=[tMK])
    s.dma(q, AB[:], io["ab"], w=[tAB])
    s.dma(q, CB[:], io["cb"], w=[tCB])
    s.dma(q, TA[:], io["ta"], w=[tTAB])
    s.dma(q, TBt[:], io["tb"], w=[tTAB])
    s.dma(q, GS[:], io["gs"], w=[tGS])
    s.dma("pool", W1[:], io["w1"].rearrange("p (k c h) -> p k c h", k=2, c=16), w=[tW1])
    s.dma("pool", W2[:], io["w2"].rearrange("p (k d) -> p k d", k=2), w=[tW2])
    s.dma("pool", POS[:], io["pos"].rearrange("p (k c) -> p k c", k=2), w=[tPOS])
    s.dma(q, SM_[:, 0:128], io["dl"].partition_broadcast(128).rearrange("p a b -> p (a b)"), w=[tSM])
    s.dma(q, SM_[:, 128:192], io["subln"].partition_broadcast(128).rearrange("p a b -> p (a b)"), w=[tSM])
    s.dma(q, SM_[:, 192:196], io["sinks"].partition_broadcast(128).rearrange("p a b -> p (a b)"), w=[tSM])
    s.dma("pool", KS[64:128, :], io["emat"], w=[tKS])
    s.dma("pool", RC[:, :, 0:64], io["ov"].rearrange("c p j -> p c j"), w=[tRC])
    for b in range(1):
        for h in range(4):
            s.dma("pool", QC[b][h][64:65, :], io["ri"][h:h + 1, :], w=[tQC[b][h]])
    s.add("pool", lambda e: e.memset(VALL[:, :, :, 64:65], 1.0), w=[tV])
    s.add("pool", lambda e: e.memset(RC[:, :, 128:129], 1.0), w=[tRC])
    s.add("pool", lambda e: e.memset(KC[64:65, :], 1.0), w=[tKC])
    s.add("pool", lambda e: e.memset(NM[:], 0.0), w=[tNM])
    s.add("pool", lambda e: e.memset(HT[:], 0.0), w=[tHT])
    s.add("pool", lambda e: e.memset(KCT[:], 0.0), w=[tKCT])
    s.add("pool", lambda e: e.memset(Kc2[:, :, T - 1:T], 0.0), w=[tKc2])
    s.add("dve", lambda e: e.tensor_tensor(out=SM_[:, 208:240], in0=SM_[:, 0:32], in1=SM_[:, 32:64], op=ALU.mult), r=[tSM], w=[tSM])
    s.add("dve", lambda e: e.reduce_sum(out=SM_[:, 200:201], in_=SM_[:, 208:240], axis=AX.X), r=[tSM], w=[tSM])
    s.add("dve", lambda e: e.tensor_tensor(out=SM_[:, 208:240], in0=SM_[:, 64:96], in1=SM_[:, 96:128], op=ALU.mult), r=[tSM], w=[tSM])
    s.add("dve", lambda e: e.reduce_sum(out=SM_[:, 201:202], in_=SM_[:, 208:240], axis=AX.X), r=[tSM], w=[tSM])
    s.add("act", lambda e: e.activation(out=SM_[:, 202:204], in_=SM_[:, 200:202], func=AF.Exp), r=[tSM], w=[tSM])
    s.add("dve", lambda e: e.tensor_tensor(out=SM_[:, 204:205], in0=SM_[:, 203:204], in1=SM_[:, 202:203], op=ALU.subtract), r=[tSM], w=[tSM])
    s.add("dve", lambda e: e.tensor_scalar(SM_[:, 200:201], SM_[:, 204:205], -lam_init, None, ALU.add), r=[tSM], w=[tSM])
    s.add("dve", lambda e: e.tensor_scalar(SM_[:, 128:192], SM_[:, 128:192], 1.0 - lam_init, None, ALU.mult), r=[tSM], w=[tSM])
    s.add("act", lambda e: e.activation(out=SM_[:, 196:200], in_=SM_[:, 192:196], func=AF.Exp), r=[tSM], w=[tSM])
    for sub in range(4):
        s.add("dve", lambda e, sub=sub: e.tensor_tensor(out=SK[:, sub, :], in0=GS[:, sub, :], in1=SM_[:, 196:200], op=ALU.mult), r=[tSM, tGS], w=[tSK])
    NLAM = SM_[:, 200:201]
    SUBLN = SM_[:, 128:192]

    evrr = [0]

    def evac(out, in_, rtoks, wtoks, scale=None, eng=None):
        if eng is None:
            eng = ("dve", "act")[evrr[0] % 2]; evrr[0] += 1
        if eng == "act":
            if scale is None:
                s.add("act", lambda e: e.activation(out=out, in_=in_, func=AF.Copy), r=rtoks, w=wtoks)
            else:
                s.add("act", lambda e: e.activation(out=out, in_=in_, func=AF.Copy, scale=scale), r=rtoks, w=wtoks)
        else:
            if scale is None:
                s.add("dve", lambda e: e.tensor_copy(out=out, in_=in_), r=rtoks, w=wtoks)
            else:
                s.add("dve", lambda e: e.tensor_scalar(out, in_, scale, None, ALU.mult), r=rtoks, w=wtoks)

    def load_xt(tc, k, eng=None):
        xsrc = io["xchunk"](tc) if "xchunk" in io else io["x"][tc * 512:(tc + 1) * 512, :]
        s.dma("sp", XC[:], xsrc.rearrange("(a p) d -> p a d", p=128), r=([xdeps[tc % 4]] if xdeps else []), w=[tXC])
        for dc in range(8):
            ps, tps = ps_score()
            for sub in range(4):
                s.add("pe", lambda e, ps=ps, sub=sub, dc=dc: e.transpose(ps[:, sub * 128:(sub + 1) * 128], XC[:, sub, dc * 128:(dc + 1) * 128], IDN[:]),
                      r=[tXC, tIDN], w=[tps])
            evac(XTc[k][:, dc, :], ps, [tps], [tXTc[k]], eng=eng)

    def proj_fm(k, col0, m):
        ps, tps = ps_score()
        for dc in range(8):
            s.add("pe", lambda e, ps=ps, dc=dc: e.matmul(ps[0:m, :], W[:, dc, col0:col0 + m], XTc[k][:, dc, :], start=(dc == 0), stop=(dc == 7)),
                  r=[tW, tXTc[k]], w=[tps])
        return ps, tps

    for tc in range(8):
        k = tc % 2
        t0 = tc * 512
        load_xt(tc, k)
        for kv in range(2):
            ps, tps = proj_fm(k, C_KG0 + 128 * kv, 128)
            evac(Kc2[0:64, kv, t0:t0 + 512], ps[0:64, :], [tps], [tKc2])
            if tc == 0:
                evac(Kc2[64:128, kv, 0:511], ps[64:128, 1:512], [tps], [tKc2])
            else:
                evac(Kc2[64:128, kv, t0 - 1:t0 + 511], ps[64:128, :], [tps], [tKc2])
        ps, tps = proj_fm(k, C_KS, 64); evac(KS[0:64, t0:t0 + 512], ps[0:64, :], [tps], [tKS])
        ps, tps = proj_fm(k, C_KW, 64); evac(KW[0:64, t0:t0 + 512], ps[0:64, :], [tps], [tKW])
        ps, tps = proj_fm(k, C_KC, 64); evac(KC[0:64, t0:t0 + 512], ps[0:64, :], [tps], [tKC])
        for hh in range(2):
            ps, tps = proj_fm(k, C_KB0 + 128 * hh, 128); evac(KB[hh][:, t0:t0 + 512], ps, [tps], [tKB[hh]])
        for sub in range(4):
            ps, tps = ps_score()
            for dc in range(8):
                s.add("pe", lambda e, ps=ps, dc=dc, sub=sub, k=k: e.matmul(ps[:, 0:320], XTc[k][:, dc, sub * 128:(sub + 1) * 128], W[:, dc, C_V:C_V + 320],
                                                                      start=(dc == 0), stop=(dc == 7)), r=[tW, tXTc[k]], w=[tps])
            evac(VALL[:, tc * 4 + sub, :, 0:64], ps[:, 0:320].rearrange("p (a b) -> p a b", b=64), [tps], [tV])

    for kv in range(2):
        ps, tps = ps_score()
        for cc in range(16):
            s.add("pe", lambda e, ps=ps, cc=cc, kv=kv: e.matmul(ps[:, 0:1], W1[:, kv, cc, :], POS[:, kv, cc:cc + 1], start=(cc == 0), stop=(cc == 15)),
                  r=[tW1, tPOS], w=[tps])
        evac(BP[:, kv:kv + 1], ps[:, 0:1], [tps], [tBP], eng="dve")
        ps, tps = ps_score()
        for cc in range(16):
            s.add("pe", lambda e, ps=ps, cc=cc, kv=kv: e.matmul(ps[:, 0:255], W1[:, kv, cc, :], Kc2[:, kv, 2 * cc: 2 * cc + 16 * 254 + 1: 16],
                                                                start=(cc == 0), stop=(cc == 15)), r=[tW1, tKc2], w=[tps])
        s.add("act", lambda e, ps=ps, kv=kv: e.activation(out=HT[:, kv, 0:255], in_=ps[:, 0:255], func=AF.Gelu_apprx_tanh, bias=BP[:, kv:kv + 1]),
              r=[tps, tBP], w=[tHT])
    ps, tps = ps_score()
    s.add("pe", lambda e, ps=ps: e.matmul(ps[0:64, 0:256], W2[:, 0, :], HT[:, 0, :], start=True, stop=True), r=[tW2, tHT], w=[tps])
    evac(KCT[:, :], ps[0:64, 0:256], [tps], [tKCT], eng="dve")
    for cc in range(2):
        ps, tps = ps_score()
        s.add("pe", lambda e, ps=ps, cc=cc: e.matmul(ps[:, 0:64], HT[:, 1, cc * 128:(cc + 1) * 128], W2[:, 1, :], start=True, stop=True), r=[tW2, tHT], w=[tps])
        evac(RC[:, cc, 64:128], ps[:, 0:64], [tps], [tRC], eng="dve")

    NPT = 4
    LA = 2

    GP = []
    CAPT = 2

    def gp_flush_one():
        ent = GP.pop(0)
        ent["pv"]()
        if ent["after"] is not None:
            ent["after"]()

    def gp_push(n, pv, after=None):
        GP.append(dict(n=n, pv=pv, after=after))
        while sum(x["n"] for x in GP) > CAPT + n - 1 and len(GP) > 1:
            gp_flush_one()

    def gp_drain():
        while GP:
            gp_flush_one()

    def attn_unit(kbs, lhs_fn, ltoks, rhs, rtoks, bias_fn, vidx, o_acc, to_acc, after=None):
        attn_multi(kbs, [(lhs_fn, ltoks, rhs, rtoks, bias_fn, vidx, o_acc, to_acc)], after=after)

    def attn_multi(kbs, streams, after=None):
        ns = len(streams)
        first = [True] * ns
        last_kb = {}
        for (kb, mi, subs) in kbs:
            for sub in subs:
                last_kb[sub] = kb

        def make_pv(kb, subs, pks):
            def pv():
                for si, (lhs_fn, ltoks, rhs, rtoks, bias_fn, vidx, o_acc, to_acc) in enumerate(streams):
                    pk = pks[si]
                    for sub in subs:
                        s.add("pe", lambda e, pk=pk, sub=sub, kb=kb, st_=first[si], sp_=(last_kb[sub] == kb), o_acc=o_acc, vidx=vidx: e.matmul(
                            o_acc[:, sub * 128:sub * 128 + 65], PTb[pk][:, sub * 128:(sub + 1) * 128], VALL[:, kb, vidx, :], start=st_, stop=sp_,
                            skip_group_check=True),
                            r=[tPT[pk], tV], w=[to_acc])
                        first[si] = False
            return pv

        for idx_, (kb, mi, subs) in enumerate(kbs):
            c0, c1 = min(subs) * 128, (max(subs) + 1) * 128
            tiles = []
            for (lhs_fn, ltoks, rhs, rtoks, bias_fn, vidx, o_acc, to_acc) in streams:
                ps, tps = ps_score()
                s.add("pe", lambda e, ps=ps, kb=kb, mi=mi, c0=c0, c1=c1, lhs_fn=lhs_fn, rhs=rhs: e.matmul(ps[:, c0:c1], lhs_fn(kb), rhs[:, c0:c1], start=True, stop=(mi is None)),
                      r=ltoks + rtoks, w=[tps])
                tiles.append((ps, tps))
            if mi is not None:
                for (ps, tps) in tiles:
                    s.add("pe", lambda e, ps=ps, mi=mi, c0=c0, c1=c1: e.matmul(ps[:, c0:c1], IDNb[:], MK[:, mi, c0:c1], start=False, stop=True), r=[tIDNb, tMK], w=[tps])
            pks = []
            for si, (lhs_fn, ltoks, rhs, rtoks, bias_fn, vidx, o_acc, to_acc) in enumerate(streams):
                ps, tps = tiles[si]
                pk = ptrr[0] % NPT; ptrr[0] += 1
                s.add("act", lambda e, ps=ps, pk=pk, kb=kb, c0=c0, c1=c1, bias_fn=bias_fn: e.activation(out=PTb[pk][:, c0:c1], in_=ps[:, c0:c1], func=AF.Exp, bias=bias_fn(kb)),
                      r=[tps, tAB, tCB], w=[tPT[pk]])
                pks.append(pk)
            gp_push(ns, make_pv(kb, subs, pks), after if idx_ == len(kbs) - 1 else None)

    def kb_list(qt, kind):
        out = []
        lo = {"full": 0, "win": max(0, 4 * qt - 4), "swa": max(0, 4 * qt - 1)}[kind]
        for kb in range(lo, 4 * qt + 4):
            o = 4 * qt - kb
            if kind == "full":
                mi = (M_CAUS - o) if o <= 0 else None
            elif kind == "win":
                mi = (M_CAUS - o) if o <= 0 else (M_WM + o - 1)
            else:
                mi = M_SM + o + 3
            if mi is None:
                subs = [0, 1, 2, 3]
            else:
                subs = [sub for sub in range(4) if ALLOWED[mi][:, sub * 128:(sub + 1) * 128].any()]
                if ALLOWED[mi].all():
                    mi = None
            out.append((kb, mi, subs))
        return out

    def recip_den(o_acc, to_acc, width, col, rec_ap, extra=None):
        den = o_acc[:, 0:4 * width].rearrange("p (a b) -> p a b", b=width)[:, :, col]
        if extra is None:
            s.add("dve", lambda e: e.tensor_scalar(rec_ap, den, 1e-30, None, ALU.max), r=(to_acc if isinstance(to_acc, list) else [to_acc]), w=[tREC])
        else:
            s.add("dve", lambda e: e.tensor_tensor(out=rec_ap, in0=den, in1=extra, op=ALU.add), r=[to_acc, tSK], w=[tREC])
        s.add("dve", lambda e: e.reciprocal(out=rec_ap, in_=rec_ap), r=[tREC], w=[tREC])

    def oview(o_acc, width, c0, n=64):
        return o_acc[:, 0:4 * width].rearrange("p (a b) -> p a b", b=width)[:, :, c0:c0 + n]

    def bc(ap4):
        return ap4.unsqueeze(2).to_broadcast([128, 4, 64])

    outs = []
    toh = []
    def finish_tile(qt):
        for fc in range(4):
            ps, tps = ps_score()
            for sub in range(4):
                s.add("pe", lambda e, ps=ps, sub=sub, fc=fc: e.transpose(ps[:, sub * 128:(sub + 1) * 128], OCH[:, sub, fc * 128:(fc + 1) * 128], IDN[:]),
                      r=[tOCH, tIDN], w=[tps])
            evac(OTc[:, fc, :], ps, [tps], [tOTc], eng="dve")
        if "oTh" in io:
            tq = Tok()
            toh.append(tq)
            outs.append(s.dma("sp", io["oTh"][qt // 4].rearrange("(c p) t -> p c t", p=128)[:, :, (qt % 4) * 512:(qt % 4 + 1) * 512], OTc[:], r=[tOTc], w=[tq]))
            if qt % 4 == 3:
                j = qt // 4
                outs.append(s.add("pool", lambda e, j=j: e.collective_compute("AllGather", ALU.bypass, replica_groups=[[0, 1], [2, 3], [4, 5], [6, 7]],
                                                                              ins=[io["oTh"][j].opt()], outs=[io["oTg"][j].opt()]),
                                  r=toh[-4:], w=[Tok()], dma=True, inc=1))
        else:
            outs.append(s.dma("sp", io["oT"][:, :, qt * 512:(qt + 1) * 512].rearrange("c p t -> p c t"), OTc[:], r=[tOTc]))

    def q_proj(qt):
        k = qt % 2
        b = qt % 2
        load_xt(qt, k, eng="dve")
        for h in range(4):
            ps, tps = proj_fm(k, C_QA + 64 * h, 64)
            evac(QA[b][h][0:64, :], ps[0:64, :], [tps], [tQA[b][h]], scale=0.125, eng="dve")
        for h in range(2):
            ps, tps = proj_fm(k, C_QB + 128 * h, 128)
            evac(QB[b][h][:, :], ps, [tps], [tQB[b][h]], scale=32 ** -0.5, eng="dve")
        for h in range(4):
            ps, tps = proj_fm(k, C_QC + 64 * h, 64)
            evac(QC[b][h][0:64, :], ps[0:64, :], [tps], [tQC[b][h]], scale=0.125, eng="dve")

    q_proj(0)
    for qt in range(NQT):
        k = qt % 2
        b = qt % 2
        ps, tps = ps_score()
        for sub in range(4):
            for dc in range(8):
                s.add("pe", lambda e, ps=ps, dc=dc, sub=sub, k=k: e.matmul(ps[:, sub * 8:sub * 8 + 6], XTc[k][:, dc, sub * 128:(sub + 1) * 128], W[:, dc, C_G:C_G + 6],
                                                                      start=(dc == 0), stop=(dc == 7)), r=[tW, tXTc[k]], w=[tps])
        s.add("act", lambda e, ps=ps: e.activation(out=GT[:], in_=ps[:, 0:32].rearrange("p (a b) -> p a b", b=8)[:, :, 0:6], func=AF.Sigmoid), r=[tps], w=[tGT])

        chunks = [0] if qt < 4 else [0, 1]
        for h in range(4):
            oa, toa = ps_accfull()

            def post_cmp(h=h, oa=oa, toa=toa):
                recip_den(oa, toa, 256, 128, REC[:, 0:4])
                if h == 0:
                    s.add("dve", lambda e: e.tensor_tensor(out=IMP[:], in0=oview(oa, 256, 0), in1=bc(REC[:, 0:4]), op=ALU.mult),
                          r=[toa[0], toa[1], tREC], w=[tIMP])
                else:
                    tm = TMP[h % 2]; ttm = tTMP[h % 2]
                    s.add("dve", lambda e: e.tensor_tensor(out=tm[:], in0=oview(oa, 256, 0), in1=bc(REC[:, 0:4]), op=ALU.mult),
                          r=[toa[0], toa[1], tREC], w=[ttm])
                    s.add("dve", lambda e: e.tensor_tensor(out=IMP[:], in0=IMP[:], in1=tm[:], op=ALU.add), r=[ttm, tIMP], w=[tIMP])
                if h < 2:
                    s.add("dve", lambda e: e.tensor_tensor(out=OCc[:, h, :, :], in0=oview(oa, 256, 64), in1=bc(REC[:, 0:4]), op=ALU.mult),
                          r=[toa[0], toa[1], tREC], w=[tOCc])

            for cc in chunks:
                rel = qt - 4 * cc
                mi = (M_CM + rel) if rel < 5 else None
                ps, tps = ps_score()
                s.add("pe", lambda e, ps=ps, cc=cc, h=h, mi=mi, b=b: e.matmul(ps, KCT[:, cc * 128:(cc + 1) * 128], QA[b][h][0:64, :], start=True, stop=(mi is None)),
                      r=[tKCT, tQA[b][h]], w=[tps])
                if mi is not None:
                    s.add("pe", lambda e, ps=ps, mi=mi: e.matmul(ps, IDNb[:], MK[:, mi, :], start=False, stop=True), r=[tIDNb, tMK], w=[tps])
                pk = ptrr[0] % NPT; ptrr[0] += 1
                ci = h * 16 + cc * 8 + qt
                s.add("act", lambda e, ps=ps, pk=pk, ci=ci: e.activation(out=PTb[pk][:], in_=ps, func=AF.Exp, bias=CB[:, ci:ci + 1]),
                      r=[tps, tCB], w=[tPT[pk]])

                def pv_cmp(oa=oa, toa=toa, pk=pk, cc=cc, first=(cc == chunks[0]), last=(cc == chunks[-1])):
                    for sub in range(4):
                        s.add("pe", lambda e, sub=sub: e.matmul(
                            oa[:, sub * 256:sub * 256 + 129], PTb[pk][:, sub * 128:(sub + 1) * 128], RC[:, cc, :], start=(first and sub % 2 == 0), stop=last,
                            skip_group_check=True),
                            r=[tPT[pk], tRC], w=[toa[sub // 2]])

                gp_push(1, pv_cmp, post_cmp if cc == chunks[-1] else None)
        gp_drain()
        if qt >= 1:
            finish_tile(qt - 1)
        for sub in range(4):
            blk = qt * 4 + sub
            lo = 63 - 2 * blk
            s.add("dve", lambda e, sub=sub, lo=lo: e.tensor_tensor(out=IMP2[:, sub, :], in0=IMP[:, sub, :], in1=TA[:, lo:lo + 64], op=ALU.mult),
                  r=[tIMP, tTAB], w=[tIMP2])
            s.add("dve", lambda e, sub=sub, lo=lo: e.tensor_tensor(out=IMP2[:, sub, :], in0=IMP2[:, sub, :], in1=TBt[:, lo:lo + 64], op=ALU.add),
                  r=[tIMP2, tTAB], w=[tIMP2])
        s.add("dve", lambda e: e.memset(IMP2[:, :, 0:1], 1e9), w=[tIMP2])
        for sub in range(4):
            s.add("dve", lambda e, sub=sub: e.max(out=MX[:, sub, 0:8], in_=IMP2[:, sub, :]), r=[tIMP2], w=[tMX])
            s.add("dve", lambda e, sub=sub: e.match_replace(out=IMP[:, sub, :], in_to_replace=MX[:, sub, 0:8], in_values=IMP2[:, sub, :], imm_value=-1e30),
                  r=[tIMP2, tMX], w=[tIMP])
            s.add("dve", lambda e, sub=sub: e.max(out=MX[:, sub, 8:16], in_=IMP[:, sub, :]), r=[tIMP], w=[tMX])
            s.add("dve", lambda e, sub=sub: e.tensor_scalar(NM[:, sub, 64:128], IMP2[:, sub, :], MX[:, sub, 15:16], 1.0, ALU.is_ge, ALU.subtract),
                  r=[tIMP2, tMX], w=[tNM])
        for own in range(2):
            hs = 2 + own
            feat0 = 128 + 64 * own
            accs = []
            streams = []
            for mp in range(2):
                oa, toa = ps_acc()
                lhs_fn = lambda kb, own=own, mp=mp: KB[own][64 * mp:64 * mp + 64, kb * 128:(kb + 1) * 128]
                rhs = QB[b][own][64 * mp:64 * mp + 64, :]
                bias_fn = lambda kb, hs=hs, qt=qt: AB[:, hs, 4 * qt - kb + 3: 4 * qt - kb + 4]
                streams.append((lhs_fn, [tKB[own]], rhs, [tQB[b][own]], bias_fn, 2 + own, oa, toa))
                accs.append((oa, toa))

            def post_diff(accs=accs, feat0=feat0):
                (o1, to1), (o2, to2) = accs
                r1 = REC[:, 4:8]; r2 = REC[:, 8:12]
                recip_den(o1, to1, 128, 64, r1)
                recip_den(o2, to2, 128, 64, r2)
                s.add("dve", lambda e: e.tensor_scalar(r2, r2, NLAM, None, ALU.mult), r=[tREC, tSM], w=[tREC])
                t1 = TMP[0]; t2 = TMP[1]
                s.add("dve", lambda e: e.tensor_tensor(out=t1[:], in0=oview(o1, 128, 0), in1=bc(r1), op=ALU.mult), r=[to1, tREC], w=[tTMP[0]])
                s.add("dve", lambda e: e.tensor_tensor(out=t2[:], in0=oview(o2, 128, 0), in1=bc(r2), op=ALU.mult), r=[to2, tREC], w=[tTMP[1]])
                s.add("dve", lambda e: e.tensor_tensor(out=t1[:], in0=t1[:], in1=t2[:], op=ALU.add), r=[tTMP[0], tTMP[1]], w=[tTMP[0]])
                s.add("dve", lambda e: e.tensor_tensor(out=t2[:], in0=t1[:], in1=t1[:], op=ALU.mult), r=[tTMP[0]], w=[tTMP[1]])
                ss = REC[:, 12:16]
                s.add("dve", lambda e: e.reduce_sum(out=ss, in_=t2[:], axis=AX.X), r=[tTMP[1]], w=[tREC])
                s.add("dve", lambda e: e.tensor_scalar(ss, ss, 1.0 / 64.0, 1e-5, ALU.mult, ALU.add), r=[tREC], w=[tREC])
                s.add("act", lambda e: e.activation(out=ss, in_=ss, func=AF.Sqrt), r=[tREC], w=[tREC])
                s.add("dve", lambda e: e.reciprocal(out=ss, in_=ss), r=[tREC], w=[tREC])
                s.add("dve", lambda e: e.tensor_tensor(out=t1[:], in0=t1[:], in1=bc(ss), op=ALU.mult), r=[tTMP[0], tREC], w=[tTMP[0]])
                s.add("dve", lambda e: e.tensor_tensor(out=OCH[:, :, feat0:feat0 + 64], in0=t1[:], in1=SUBLN.unsqueeze(1).to_broadcast([128, 4, 64]), op=ALU.mult),
                      r=[tTMP[0], tSM], w=[tOCH])

            attn_multi(kb_list(qt, "full"), streams, after=post_diff)
        for r_ in range(4):
            hs = 4 + r_
            feat0 = 256 + 64 * r_
            oa, toa = ps_acc()
            lhs_fn = lambda kb: KC[0:65, kb * 128:(kb + 1) * 128]
            rhs = QC[b][r_][0:65, :]
            bias_fn = lambda kb, hs=hs, qt=qt: AB[:, hs, 4 * qt - kb + 3: 4 * qt - kb + 4]

            def post_swa(oa=oa, toa=toa, r_=r_, feat0=feat0):
                rec = REC[:, 4:8]
                recip_den(oa, toa, 128, 64, rec, extra=SK[:, :, r_])
                s.add("dve", lambda e: e.tensor_tensor(out=OCH[:, :, feat0:feat0 + 64], in0=oview(oa, 128, 0), in1=bc(rec), op=ALU.mult),
                      r=[toa, tREC], w=[tOCH])

            attn_unit(kb_list(qt, "swa"), lhs_fn, [tKC], rhs, [tQC[b][r_]], bias_fn, 4, oa, toa, after=post_swa)
        ps, tps = ps_score()
        for sub in range(4):
            s.add("pe", lambda e, ps=ps, sub=sub: e.matmul(ps[:, sub * 128:(sub + 1) * 128], NM[:, sub, :], I30K[:], start=True, stop=True),
                  r=[tNM, tI30K], w=[tps])
        for own in range(2):
            h = own
            evac(QA[b][h][64:128, :], ps[64:128, :], [tps], [tQA[b][h]], eng="dve")

        for own in range(2):
            h = own
            hs = own
            feat0 = 64 * own
            s.add("dve", lambda e, own=own, feat0=feat0: e.tensor_tensor(out=OCH[:, :, feat0:feat0 + 64], in0=OCc[:, own, :, :],
                                                                         in1=bc(GT[:, :, 3 * own + 0]), op=ALU.mult), r=[tOCc, tGT], w=[tOCH])
            for br, (kind, lhs_t, ltok, vidx) in enumerate([("full", KS, tKS, 0), ("win", KW, tKW, 1)]):
                oa, toa = ps_acc()
                if kind == "full":
                    lhs_fn = lambda kb: KS[:, kb * 128:(kb + 1) * 128]
                    rhs = QA[b][h][:, :]
                else:
                    lhs_fn = lambda kb: KW[0:64, kb * 128:(kb + 1) * 128]
                    rhs = QA[b][h][0:64, :]
                bias_fn = lambda kb, hs=hs, qt=qt: AB[:, hs, 4 * qt - kb + 3: 4 * qt - kb + 4]

                def post_nsa(oa=oa, toa=toa, own=own, br=br, feat0=feat0):
                    rec = REC[:, 4 + 4 * br: 8 + 4 * br]
                    recip_den(oa, toa, 128, 64, rec)
                    s.add("dve", lambda e: e.tensor_tensor(out=rec, in0=rec, in1=GT[:, :, 3 * own + 1 + br], op=ALU.mult),
                          r=[tREC, tGT], w=[tREC])
                    tm = TMP[br]; ttm = tTMP[br]
                    s.add("dve", lambda e: e.tensor_tensor(out=tm[:], in0=oview(oa, 128, 0), in1=bc(rec), op=ALU.mult),
                          r=[toa, tREC], w=[ttm])
                    s.add("dve", lambda e: e.tensor_tensor(out=OCH[:, :, feat0:feat0 + 64], in0=OCH[:, :, feat0:feat0 + 64], in1=tm[:], op=ALU.add),
                          r=[ttm, tOCH], w=[tOCH])

                attn_unit(kb_list(qt, kind), lhs_fn, [ltok], rhs, [tQA[b][h]], bias_fn, vidx, oa, toa, after=post_nsa)
        if qt + 1 < NQT:
            q_proj(qt + 1)
        gp_drain()
    finish_tile(NQT - 1)
    if io.get("debug"):
        dl_ = [("KC", KC, [65, T], BF16, tKC), ("KW", KW, [64, T], BF16, tKW), ("KS", KS, [128, T], BF16, tKS), ("KB0", KB[0], [128, T], BF16, tKB[0]),
               ("VALL", VALL, [128, 32, 5, 65], BF16, tV), ("KCT", KCT, [64, 256], BF16, tKCT), ("RC", RC, [128, 2, 129], BF16, tRC),
               ("HT", HT, [128, 2, 256], BF16, tHT), ("Kc2", Kc2, [128, 2, T], BF16, tKc2), ("QA0", QA[1][0], [128, 512], BF16, tQA[1][0]),
               ("QB0", QB[1][0], [128, 512], BF16, tQB[1][0]), ("QC0", QC[1][0], [65, 512], BF16, tQC[1][0]), ("GT", GT, [128, 4, 6], F32, tGT),
               ("IMP2", IMP2, [128, 4, 64], F32, tIMP2), ("NM", NM, [128, 4, 128], BF16, tNM), ("OCH", OCH, [128, 4, 512], F32, tOCH),
               ("SMALL", SM_, [128, 256], F32, tSM), ("SK", SK, [128, 4, 4], F32, tSK), ("OCc", OCc, [128, 2, 4, 64], F32, tOCc),
               ("XT", XTc[1], [128, 8, 512], BF16, tXTc[1]), ("W", W, [128, 8, NW], BF16, tW), ("MK", MK, [128, NMASK, 512], BF16, tMK)]
        for (nm, tl, shp, dt_, tk) in dl_:
            dtn = nc.dram_tensor("dbg_" + nm, shp, dt_, kind="ExternalOutput").ap()
            outs.append(s.dma("sp", dtn, tl[:], r=[tk]))
    return outs


from concourse.bass_utils import run_bass_kernel_spmd

B_DENSE_IN = dict(oT=([8, 128, NT], BF16), xres=([NT, D], F32), w_out=([D, D], F32), lnp=([6, D], F32), wg=([D, 2816], F32), wu=([D, 2816], F32),
                  wd=([2816, D], F32), ple_gate=([D, D], F32), ple_proj=([256, D], F32), p=([NT, 256], F32), idn=([128, 128], F32))
B_MOE_IN = dict(oT=([8, 128, NT], BF16), xres=([NT, D], F32), w_out=([D, D], F32), lnp=([6, D], F32), router=([D, 8], F32),
                mg=([8, D, 3584], F32), mu=([8, D, 3584], F32), md=([8, 3584, D], F32), ple_gate=([D, D], F32), ple_proj=([256, D], F32),
                p=([NT, 256], F32), idn=([128, 128], F32), ut=([128, 128], F32), iota=([128, CAP], F32), pcol=([128, 16, 2], F32))


def _prog_A(layer):
    nc = bass.Bass("TRN2", target_bir_lowering=False)
    io = {k: dram(nc, k, shp) for k, shp in A_IN.items()}
    io["oT"] = dram(nc, "oT", [4, 128, T], BF16, kind="ExternalOutput")
    with contextlib.ExitStack() as st:
        s = Sched(nc)
        c = Ctx()
        outs = build_A(nc, st, s, c, io, layer)
        s.emit(final_ops=outs)
    return nc


def _prog_B(moe):
    nc = bass.Bass("TRN2", target_bir_lowering=False)
    spec = B_MOE_IN if moe else B_DENSE_IN
    io = {k: dram(nc, k, shp, dt) for k, (shp, dt) in spec.items()}
    io["out"] = dram(nc, "out", [NT, D], kind="ExternalOutput")
    with contextlib.ExitStack() as st:
        s = Sched(nc)
        c = Ctx()
        setup_psum(nc, st, c)
        outs = (build_B_moe if moe else build_B_dense)(nc, st, s, c, io)
        s.emit(final_ops=outs)
    return nc


def _feat_perm(sidx):
    return np.concatenate([np.arange(128 * sidx, 128 * sidx + 128), 256 + np.arange(128 * sidx, 128 * sidx + 128),
                           512 + np.arange(256 * sidx, 256 * sidx + 256)])


def kernel_unfused(**inputs):
    inp = {k: np.asarray(v) for k, v in inputs.items()}
    x = np.ascontiguousarray(inp["x"], dtype=np.float32)
    nb = x.shape[0]
    cores = list(range(2 * nb))
    idn = np.eye(128, dtype=np.float32)
    ut = (np.arange(128)[:, None] < np.arange(128)[None, :]).astype(np.float32)
    iota = np.tile(np.arange(CAP, dtype=np.float32)[None, :], (128, 1))
    wperm = np.concatenate([_feat_perm(0), _feat_perm(1)])
    consts = [host_consts(0), host_consts(1)]
    for L in range(2):
        packs = [host_w(inp["w_in"][L], s_) for s_ in range(2)]
        smalls = [host_small(inp, L, s_) for s_ in range(2)]
        in_maps = []
        for cid in cores:
            b, s_ = cid // 2, cid % 2
            m = dict(x=np.ascontiguousarray(x[b]), w=packs[s_])
            m.update(consts[s_])
            m.update(smalls[s_])
            in_maps.append(m)
        res = run_bass_kernel_spmd(_prog_A(L), in_maps, core_ids=cores)
        oT = [np.asarray(r["oT"]) for r in res.results]
        moe = (L % 2 == 1)
        lnp = np.stack([inp["ln1_g"][L], inp["ln1_b"][L], inp["ln2_g"][L], inp["ln2_b"][L], inp["ln3_g"][L], inp["ln3_b"][L]]).astype(np.float32)
        w_out = np.ascontiguousarray(inp["w_out"][L][wperm, :])
        in_maps = []
        for cid in cores:
            b, h = cid // 2, cid % 2
            tk = slice(h * NT, (h + 1) * NT)
            oTB = np.ascontiguousarray(np.concatenate([oT[2 * b][:, :, tk], oT[2 * b + 1][:, :, tk]], axis=0))
            m = dict(oT=oTB, xres=np.ascontiguousarray(x[b, tk]), w_out=w_out, lnp=lnp, ple_gate=inp["ple_gate"][L], ple_proj=inp["ple_proj"][L],
                     p=np.ascontiguousarray(inp["p"][L, b, tk]), idn=idn)
            if moe:
                m.update(router=inp["moe_router"][L // 2], mg=inp["moe_w_gate"][L // 2], mu=inp["moe_w_up"][L // 2], md=inp["moe_w_down"][L // 2], ut=ut, iota=iota)
            else:
                m.update(wg=inp["ffn_w_gate"][L // 2], wu=inp["ffn_w_up"][L // 2], wd=inp["ffn_w_down"][L // 2])
            in_maps.append(m)
        res = run_bass_kernel_spmd(_prog_B(moe), in_maps, core_ids=cores)
        xn = np.empty_like(x)
        for cid in cores:
            b, h = cid // 2, cid % 2
            xn[b, h * NT:(h + 1) * NT] = np.asarray(res.results[cid]["out"], dtype=np.float32)
        x = xn
    return x


A_LAYER_KEYS = ("w", "w1", "w2", "pos", "dl", "subln", "sinks")
A_SHARED_KEYS = ("masks", "idn", "i30k", "ab", "cb", "ov", "emat", "ta", "tb", "ri", "gs")
B_COMMON = dict(w_out=([D, D], F32), lnp=([6, D], F32), ple_gate=([D, D], F32), ple_proj=([256, D], F32), p=([NT, 256], F32))


def _prog_fused(nlayers=2):
    PHASE[0] = 0
    Sched.NSCHED = 0
    nc = bass.Bass("TRN2", target_bir_lowering=False)
    ext = {}

    def inp(name, shp, dt=F32):
        ext[name] = dram(nc, name, shp, dt)
        return ext[name]

    x0 = inp("x0", [T, D])
    xres0 = inp("xres0", [NT, D])
    hsel = inp("hsel", [1, 2])
    shared = {k: inp(k, A_IN[k]) for k in A_SHARED_KEYS}
    perA = [{k: inp("%s_%d" % (k, L), A_IN[k]) for k in A_LAYER_KEYS} for L in range(2)]
    perB = [{k: inp("%s_%d" % (k, L), shp, dt) for k, (shp, dt) in B_COMMON.items()} for L in range(2)]
    dense = dict(wg=inp("wg", [D, 2816]), wu=inp("wu", [D, 2816]), wd=inp("wd", [2816, D]))
    moe = dict(router=inp("router", [D, 8]), mg=inp("mg", [8, D, 3584]), mu=inp("mu", [8, D, 3584]), md=inp("md", [8, 3584, D]),
               ut=inp("ut", [128, 128]), iota=inp("iota", [128, CAP]), pcol=inp("pcol", [128, 16, 2]))
    out = dram(nc, "out", [NT, D], kind="ExternalOutput")
    oTown = [dram(nc, "oTown%d" % L, [2, 512, NT], BF16, kind="Internal") for L in range(2)]
    oTg = [dram(nc, "oTg%d" % L, [2, 1024, NT], BF16, kind="Internal") for L in range(2)]
    xh = dram(nc, "xh", [NT, D], kind="Internal")
    xg = dram(nc, "xg", [4, 1024, D], kind="Internal")
    for L in range(nlayers):
        with nc.cleanup_on_exit():
            with contextlib.ExitStack() as st:
                PHASE[0] += 1
                s = Sched(nc)
                c = Ctx()
                io = dict(shared)
                io.update(perA[L])
                io["oTh"] = oTown[L]
                io["oTg"] = oTg[L]
                if L == 0:
                    io["x"] = x0
                else:
                    io["xchunk"] = lambda tc: xg[tc % 4][(tc // 4) * 512:(tc // 4 + 1) * 512, :]
                outs = build_A(nc, st, s, c, io, L)
                s.emit(final_ops=outs)
            nc.all_engine_barrier()
        with nc.cleanup_on_exit():
            with contextlib.ExitStack() as st:
                PHASE[0] += 1
                s = Sched(nc)
                c = Ctx()
                setup_psum(nc, st, c)
                io = dict(perB[L])
                io.update(idn=shared["idn"], hsel=hsel, oTown=oTown[L], oTg=oTg[L])
                io["xres"] = xres0 if L == 0 else xh
                io["out"] = xh if L < nlayers - 1 else out
                if L < nlayers - 1:
                    io["xg"] = xg
                if L % 2 == 0:
                    io.update(dense)
                    outs = build_B_dense(nc, st, s, c, io)
                else:
                    io.update(moe)
                    outs = build_B_moe(nc, st, s, c, io)
                s.emit(final_ops=outs)
            nc.all_engine_barrier()
    return nc


def kernel(**inputs):
    inp = {k: np.asarray(v) for k, v in inputs.items()}
    x = np.ascontiguousarray(inp["x"], dtype=np.float32)
    nb = x.shape[0]
    cores = list(range(2 * nb))
    idn = np.eye(128, dtype=np.float32)
    ut = (np.arange(128)[:, None] < np.arange(128)[None, :]).astype(np.float32)
    iota = np.tile(np.arange(CAP, dtype=np.float32)[None, :], (128, 1))
    pcol = np.stack([np.tile(np.arange(128, dtype=np.float32)[:, None], (1, 16)), np.tile(np.arange(16, dtype=np.float32)[None, :], (128, 1))], axis=-1)
    wperm = np.concatenate([_feat_perm(0), _feat_perm(1)])
    consts = [host_consts(0), host_consts(1)]
    packs = [[host_w(inp["w_in"][L], s_) for s_ in range(2)] for L in range(2)]
    smalls = [[host_small(inp, L, s_) for s_ in range(2)] for L in range(2)]
    lnps = [np.stack([inp["ln1_g"][L], inp["ln1_b"][L], inp["ln2_g"][L], inp["ln2_b"][L], inp["ln3_g"][L], inp["ln3_b"][L]]).astype(np.float32) for L in range(2)]
    w_outs = [np.ascontiguousarray(inp["w_out"][L][wperm, :]) for L in range(2)]
    in_maps = []
    for cid in cores:
        b, s_ = cid // 2, cid % 2
        tk = slice(s_ * NT, (s_ + 1) * NT)
        hs = np.zeros((1, 2), np.float32)
        hs[0, s_] = 1.0
        m = dict(x0=np.ascontiguousarray(x[b]), xres0=np.ascontiguousarray(x[b, tk]), hsel=hs)
        for k in A_SHARED_KEYS:
            m[k] = consts[s_][k]
        for L in range(2):
            m["w_%d" % L] = packs[L][s_]
            for k in A_LAYER_KEYS[1:]:
                m["%s_%d" % (k, L)] = smalls[L][s_][k]
            m["w_out_%d" % L] = w_outs[L]
            m["lnp_%d" % L] = lnps[L]
            m["ple_gate_%d" % L] = inp["ple_gate"][L]
            m["ple_proj_%d" % L] = inp["ple_proj"][L]
            m["p_%d" % L] = np.ascontiguousarray(inp["p"][L, b, tk])
        m.update(wg=inp["ffn_w_gate"][0], wu=inp["ffn_w_up"][0], wd=inp["ffn_w_down"][0], router=inp["moe_router"][0],
                 mg=inp["moe_w_gate"][0], mu=inp["moe_w_up"][0], md=inp["moe_w_down"][0], ut=ut, iota=iota, pcol=pcol)
        in_maps.append(m)
    res = run_bass_kernel_spmd(_prog_fused(), in_maps, core_ids=cores)
    out = np.empty_like(x)
    for cid in cores:
        b, s_ = cid // 2, cid % 2
        out[b, s_ * NT:(s_ + 1) * NT] = np.asarray(res.results[cid]["out"], dtype=np.float32)
    return out
```

```python
import contextlib, math
import numpy as np
import ml_dtypes
import concourse.bass as bass
import concourse.mybir as mybir

F32 = mybir.dt.float32
BF16 = mybir.dt.bfloat16
AF = mybir.ActivationFunctionType
ALU = mybir.AluOpType
AX = mybir.AxisListType

PHASE = [0]


def uname(name):
    return "%s_%d" % (name, PHASE[0])


ENGS = ("pe", "act", "dve", "pool", "sp")


class Tok:
    __slots__ = ("name", "w", "rs")

    def __init__(self, name=""):
        self.name = name
        self.w = None
        self.rs = []


class Op:
    __slots__ = ("eng", "fn", "deps", "dma", "sig", "needs", "gi", "inc")

    def __init__(self, eng, fn, dma):
        self.eng = eng
        self.fn = fn
        self.deps = set()
        self.dma = dma
        self.sig = None
        self.needs = False
        self.gi = 0
        self.inc = 16


class Sched:
    NDMA = 12
    NSCHED = 0

    def __init__(self, nc, same_engine_sync=True):
        self.nc = nc
        self.ops = []
        self.same = same_engine_sync

    def add(self, eng, fn, r=(), w=(), dma=False, inc=16):
        op = Op(eng, fn, dma)
        op.inc = inc
        op.gi = len(self.ops)
        for t in r:
            if t.w is not None:
                op.deps.add(t.w)
        for t in w:
            if t.w is not None:
                op.deps.add(t.w)
            for x in t.rs:
                op.deps.add(x)
        for t in r:
            t.rs.append(op)
        for t in w:
            t.w = op
            t.rs = []
        op.deps.discard(op)
        self.ops.append(op)
        return op

    def dma(self, q, out, in_, r=(), w=(), **kw):
        return self.add(q, lambda e: e.dma_start(out=out, in_=in_, **kw), r, w, dma=True)

    def emit(self, final_ops=()):
        nc = self.nc
        ops = self.ops
        for op in ops:
            for d in op.deps:
                if d.dma:
                    continue
                if d.eng != op.eng or (self.same and d.eng != "pe"):
                    d.needs = True
        for op in final_ops:
            op.needs = True
        cnt = {e: 0 for e in ENGS}
        dcnt = {e: [0] * self.NDMA for e in ENGS}
        drr = {e: 0 for e in ENGS}
        prev_dma_wait = {}
        ncoll = 0
        coll_keys = []
        for op in ops:
            if op.dma and op.inc != 16:
                ncoll += 1
                prev_dma_wait[op] = (op.eng, 0, 0)
                op.sig = (("k", op.eng, ncoll), op.inc)
                coll_keys.append(op.sig[0])
            elif op.dma:
                k = drr[op.eng]
                drr[op.eng] = (k + 1) % self.NDMA
                prev_dma_wait[op] = (op.eng, k, dcnt[op.eng][k])
                dcnt[op.eng][k] += op.inc
                op.sig = (("d", op.eng, k), dcnt[op.eng][k])
            elif op.needs:
                cnt[op.eng] += 1
                op.sig = (("c", op.eng), cnt[op.eng])
        per = {e: [o for o in ops if o.eng == e] for e in ENGS}
        used = [e for e in ENGS if per[e]]
        import contextlib
        with contextlib.ExitStack() as st:
            sems = {}
            for e in ENGS:
                if cnt[e] > 0:
                    sems[("c", e)] = nc.alloc_semaphore(name="c_%s_%d" % (e, Sched.NSCHED))
                for k in range(self.NDMA):
                    if dcnt[e][k] > 0:
                        sems[("d", e, k)] = nc.alloc_semaphore(name="d_%s_%d_%d" % (e, k, Sched.NSCHED))
            for key in coll_keys:
                sems[key] = nc.alloc_semaphore(name="k_%s_%d_%d" % (key[1], key[2], Sched.NSCHED))
            Sched.NSCHED += 1
            block = st.enter_context(nc.Block())

            def run(ename, eng):
                waited = {}
                for op in per[ename]:
                    need = {}
                    for d in op.deps:
                        if d.sig is None:
                            continue
                        if (not d.dma) and d.eng == ename and (ename == "pe" or not self.same):
                            continue
                        key, val = d.sig
                        if need.get(key, 0) < val:
                            need[key] = val
                    if op.dma:
                        _, k, v = prev_dma_wait[op]
                        if v > 0:
                            key = ("d", ename, k)
                            if need.get(key, 0) < v:
                                need[key] = v
                    for key, val in need.items():
                        if waited.get(key, 0) < val:
                            eng.wait_ge(sems[key], val)
                            waited[key] = val
                    ins = op.fn(eng)
                    if op.sig is not None:
                        key, val = op.sig
                        ins.then_inc(sems[key], op.inc if op.dma else 1)
                if ename == "sp":
                    for op in final_ops:
                        key, val = op.sig
                        if waited.get(key, 0) < val:
                            eng.wait_ge(sems[key], val)
                            waited[key] = val

            if per["sp"] or final_ops:
                block.sync(lambda e: run("sp", e))
            if per["pe"]:
                block.tensor(lambda e: run("pe", e))
            if per["act"]:
                block.scalar(lambda e: run("act", e))
            if per["dve"]:
                block.vector(lambda e: run("dve", e))
            if per["pool"]:
                block.gpsimd(lambda e: run("pool", e))
        return {e: len(per[e]) for e in ENGS}

NT = 2048
D = 1024
ALPHA = 4 ** 0.25
EPS = 1e-5


def dram(nc, name, shape, dt=F32, kind="ExternalInput"):
    return nc.dram_tensor(name, list(shape), dt, kind=kind).ap()


class Ctx:
    pass


def setup_psum(nc, st, c):
    c.PSB = [st.enter_context(nc.psum_tensor(uname("psb%d" % i), [128, 1024], F32)) for i in range(4)]
    c.TPS = [[Tok("ps%d_%d" % (i, h)) for h in range(2)] for i in range(4)]
    c.psrr = 0


def ps_full(c):
    i = c.psrr % 4
    c.psrr += 1
    return c.PSB[i], c.TPS[i]


def ps_half(c):
    if not hasattr(c, "hrr"):
        c.hrr = 0
    k = c.hrr % 8
    c.hrr += 1
    return c.PSB[k // 2][:, (k % 2) * 512:(k % 2 + 1) * 512], c.TPS[k // 2][k % 2]


RG_PAIRS = [[0, 1], [2, 3], [4, 5], [6, 7]]


def load_oT(nc, st, s, io, OT, tOT, H1, tH1):
    if "oTown" not in io:
        s.dma("sp", OT, io["oT"].rearrange("c p t -> p c t"), w=[tOT])
        return
    HS = st.enter_context(nc.sbuf_tensor(uname("HS"), [128, 2], F32)); tHS = Tok()
    s.dma("sp", HS[:], io["hsel"].partition_broadcast(128).rearrange("p a b -> p (a b)"), w=[tHS])
    for fc in range(8):
        s.dma("sp", OT[:, fc, :], io["oTg"][0][fc * 128:(fc + 1) * 128, :], w=[tOT])
        s.dma("sp", H1[:, fc, :], io["oTg"][1][fc * 128:(fc + 1) * 128, :], w=[tH1])
    for fc in range(8):
        s.add("dve", lambda e, fc=fc: e.tensor_scalar(H1[:, fc, :], H1[:, fc, :], HS[:, 1:2], None, ALU.mult), r=[tHS, tH1], w=[tH1])
        s.add("dve", lambda e, fc=fc: e.scalar_tensor_tensor(out=OT[:, fc, :], in0=OT[:, fc, :], scalar=HS[:, 0:1], in1=H1[:, fc, :], op0=ALU.mult, op1=ALU.add),
              r=[tHS, tH1, tOT], w=[tOT])


def layernorm_rows(s, c, src, tsrc, dst, tdst, G, Bt, tG):
    ST, tST = c.ST, c.tST
    s.add("dve", lambda e: e.bn_stats(out=ST[:, 0:6], in_=src[:, 0:512]), r=[tsrc], w=[tST])
    s.add("dve", lambda e: e.bn_stats(out=ST[:, 6:12], in_=src[:, 512:1024]), r=[tsrc], w=[tST])
    s.add("dve", lambda e: e.bn_aggr(out=ST[:, 12:14], in_=ST[:, 0:12]), r=[tST], w=[tST])
    s.add("act", lambda e: e.activation(out=ST[:, 15:16], in_=ST[:, 13:14], func=AF.Sqrt, bias=c.EPSC[:, 0:1]), r=[tST, c.tEPSC], w=[tST])
    s.add("dve", lambda e: e.reciprocal(out=ST[:, 14:15], in_=ST[:, 15:16]), r=[tST], w=[tST])
    s.add("dve", lambda e: e.tensor_scalar(dst, src, ST[:, 12:13], ST[:, 14:15], ALU.subtract, ALU.mult), r=[tsrc, tST], w=[tdst])
    s.add("dve", lambda e: e.tensor_tensor(out=dst, in0=dst, in1=G, op=ALU.mult), r=[tdst, tG], w=[tdst])
    s.add("dve", lambda e: e.tensor_tensor(out=dst, in0=dst, in1=Bt, op=ALU.add), r=[tdst, tG], w=[tdst])


def ln_ops(s, c, src, tsrc, dst, tdst, G, Bt, tG, ST, tST):
    return [
        lambda: s.add("dve", lambda e: e.bn_stats(out=ST[:, 0:6], in_=src[:, 0:512]), r=[tsrc], w=[tST]),
        lambda: s.add("dve", lambda e: e.bn_stats(out=ST[:, 6:12], in_=src[:, 512:1024]), r=[tsrc], w=[tST]),
        lambda: s.add("dve", lambda e: e.bn_aggr(out=ST[:, 12:14], in_=ST[:, 0:12]), r=[tST], w=[tST]),
        lambda: s.add("act", lambda e: e.activation(out=ST[:, 15:16], in_=ST[:, 13:14], func=AF.Sqrt, bias=c.EPSC[:, 0:1]), r=[tST, c.tEPSC], w=[tST]),
        lambda: s.add("dve", lambda e: e.reciprocal(out=ST[:, 14:15], in_=ST[:, 15:16]), r=[tST], w=[tST]),
        lambda: s.add("dve", lambda e: e.tensor_scalar(dst, src, ST[:, 12:13], ST[:, 14:15], ALU.subtract, ALU.mult), r=[tsrc, tST], w=[tdst]),
        lambda: s.add("dve", lambda e: e.tensor_tensor(out=dst, in0=dst, in1=G, op=ALU.mult), r=[tdst, tG], w=[tdst]),
        lambda: s.add("dve", lambda e: e.tensor_tensor(out=dst, in0=dst, in1=Bt, op=ALU.add), r=[tdst, tG], w=[tdst]),
    ]


def interleave(chains):
    for j in range(max(len(ch) for ch in chains)):
        for ch in chains:
            if j < len(ch):
                ch[j]()


def transpose_rows(s, c, src, tsrc, XT, tXT, col0, nchunk=8):
    for g0 in range(0, nchunk, 4):
        n = min(4, nchunk - g0)
        ps, tps = ps_half(c)
        for j in range(n):
            dc = g0 + j
            s.add("pe", lambda e, ps=ps, j=j, dc=dc: e.transpose(ps[:, j * 128:(j + 1) * 128], src[:, dc * 128:(dc + 1) * 128], c.IDN[:]),
                  r=[tsrc, c.tIDN], w=[tps])
        s.add("act", lambda e, ps=ps, g0=g0, n=n: e.activation(
            out=XT[:, g0:g0 + n, col0:col0 + 128], in_=ps[:, 0:n * 128].rearrange("p (a b) -> p a b", b=128), func=AF.Copy),
            r=[tps], w=[tXT])


def expert_ffn(s, c, XS, tXS, C, wg, wu, wd, dff, aT, taT, WG, tWG, WU, tWU, WD, tWD, out_cb, wd_preloaded=False):
    nfc = dff // 128
    ncb = dff // 256
    wg_v = wg.rearrange("(c p) f -> p c f", p=128)
    wu_v = wu.rearrange("(c p) f -> p c f", p=128)
    wd_v = wd.rearrange("(c p) n -> p c n", p=128)
    groups = [(g0, min(512, C - g0)) for g0 in range(0, C, 512)]
    for cb in range(ncb):
        b = cb % 2
        s.dma("pool", WG[b][:], wg_v[:, :, cb * 256:(cb + 1) * 256], w=[tWG[b]])
        s.dma("pool", WU[b][:], wu_v[:, :, cb * 256:(cb + 1) * 256], w=[tWU[b]])
        if not wd_preloaded:
            s.dma("pool", WD[:, 2 * cb:2 * cb + 2, :], wd_v[:, 2 * cb:2 * cb + 2, :], w=[tWD])
        for fl in range(2):
            fc = cb * 2 + fl
            for (g0, gn) in groups:
                pg, tpg = ps_half(c)
                pu, tpu = ps_half(c)
                for dc in range(8):
                    s.add("pe", lambda e, pg=pg, b=b, dc=dc, fl=fl, g0=g0, gn=gn: e.matmul(
                        pg[:, 0:gn], WG[b][:, dc, fl * 128:(fl + 1) * 128], XS[:, dc, g0:g0 + gn], start=(dc == 0), stop=(dc == 7)),
                        r=[tWG[b], tXS], w=[tpg])
                for dc in range(8):
                    s.add("pe", lambda e, pu=pu, b=b, dc=dc, fl=fl, g0=g0, gn=gn: e.matmul(
                        pu[:, 0:gn], WU[b][:, dc, fl * 128:(fl + 1) * 128], XS[:, dc, g0:g0 + gn], start=(dc == 0), stop=(dc == 7)),
                        r=[tWU[b], tXS], w=[tpu])
                k = c.sgrr % 2
                c.sgrr += 1
                SG, tSG = c.SG[k], c.tSG[k]
                s.add("act", lambda e, pg=pg, SG=SG, gn=gn: e.activation(out=SG[:, 0:gn], in_=pg[:, 0:gn], func=AF.Silu),
                      r=[tpg], w=[tSG])
                s.add("dve", lambda e, pu=pu, SG=SG, fc=fc, g0=g0, gn=gn: e.tensor_tensor(
                    out=aT[:, fc, g0:g0 + gn], in0=SG[:, 0:gn], in1=pu[:, 0:gn], op=ALU.mult),
                    r=[tSG, tpu], w=[taT])
    for sc in range(C // 128):
        py, tpy = ps_full(c)
        for nh in range(2):
            for fc in range(nfc):
                s.add("pe", lambda e, py=py, nh=nh, fc=fc, sc=sc: e.matmul(
                    py[:, nh * 512:(nh + 1) * 512], aT[:, fc, sc * 128:(sc + 1) * 128], WD[:, fc, nh * 512:(nh + 1) * 512],
                    start=(fc == 0), stop=(fc == nfc - 1)),
                    r=[taT, tWD], w=[tpy[nh]])
        out_cb(sc, py, tpy)


def build_B_dense(nc, st, s, c, io):
    DFF = 2816
    NFC = DFF // 128
    sb = lambda name, shape, dt=F32: st.enter_context(nc.sbuf_tensor(uname(name), list(shape), dt))
    c.IDN = sb("IDN", [128, 128]); c.tIDN = Tok("idn")
    c.ST = sb("ST", [128, 16]); c.tST = Tok("st")
    c.EPSC = sb("EPSC", [128, 1]); c.tEPSC = Tok("eps")
    s.add("dve", lambda e: e.memset(c.EPSC[:], EPS), w=[c.tEPSC])
    c.SG = [sb("SG%d" % i, [128, 512], BF16) for i in range(2)]; c.tSG = [Tok(), Tok()]; c.sgrr = 0
    BIG = sb("BIG", [128, NFC * 1024], BF16)
    OT = BIG[:, 0:8 * NT].rearrange("p (c t) -> p c t", t=NT); tBIG = Tok("big")
    aT = BIG[:, 0:NFC * 1024].rearrange("p (c t) -> p c t", t=1024)
    W16 = sb("W16", [128, 8, 1024], BF16); tW16 = Tok("w16")
    LNP = sb("LNP", [128, 2, 1024]); tLNP = Tok("lnp")
    XT = sb("XT", [128, 8, NT], BF16); tXT = Tok("xt")
    XR = [sb("XR%d" % i, [128, 1024]) for i in range(4)]; tXR = [Tok() for _ in range(4)]
    ST2 = [c.ST, sb("STb", [128, 16])]; tST2 = [c.tST, Tok("stb")]
    TT = [sb("TT%d" % i, [128, 1024]) for i in range(2)]; tTT = [Tok(), Tok()]
    WG = [sb("WG%d" % i, [128, 8, 256], BF16) for i in range(2)]; tWG = [Tok(), Tok()]
    WU = [sb("WU%d" % i, [128, 8, 256], BF16) for i in range(2)]; tWU = [Tok(), Tok()]
    WD = sb("WD", [128, NFC, 1024], BF16); tWD = Tok("wd")
    PP = sb("PP", [128, 2, 1024], BF16); tPP = Tok("pp")
    PTt = sb("PTt", [128, 2, NT], BF16); tPTt = Tok("ptt")
    PR = [sb("PR%d" % i, [128, 256]) for i in range(2)]; tPR = [Tok(), Tok()]
    xs1 = dram(nc, uname("xs1"), [NT, D], kind="Internal")
    xs2 = dram(nc, uname("xs2"), [NT, D], kind="Internal")
    txs1, txs2 = Tok("xs1"), Tok("xs2")

    def load_ln(k):
        s.dma("sp", LNP[:], io["lnp"][2 * k:2 * k + 2, :].partition_broadcast(128), w=[tLNP])

    s.dma("sp", c.IDN[:], io["idn"], w=[c.tIDN])
    load_oT(nc, st, s, io, OT, tBIG, XT, tXT)
    s.dma("pool", W16[:], io["w_out"].rearrange("(c p) n -> p c n", p=128), w=[tW16])
    load_ln(0)
    for ip in range(0, NT // 128, 2):
        chains = []
        for i in (ip, ip + 1):
            k = i % 2
            k4 = i % 4
            s.dma("sp", XR[k4][:], io["xres"][i * 128:(i + 1) * 128, :], w=[tXR[k4]])
            py, tpy = ps_full(c)
            for nh in range(2):
                for fc in range(8):
                    s.add("pe", lambda e, py=py, nh=nh, fc=fc, i=i: e.matmul(
                        py[:, nh * 512:(nh + 1) * 512], OT[:, fc, i * 128:(i + 1) * 128], W16[:, fc, nh * 512:(nh + 1) * 512],
                        start=(fc == 0), stop=(fc == 7)), r=[tBIG, tW16], w=[tpy[nh]])
            ops = [lambda py=py, tpy=tpy, k=k, k4=k4: s.add("dve", lambda e: e.scalar_tensor_tensor(out=TT[k][:], in0=XR[k4][:], scalar=ALPHA, in1=py[:], op0=ALU.mult, op1=ALU.add),
                                                            r=[tXR[k4], tpy[0], tpy[1]], w=[tTT[k]])]
            ops += ln_ops(s, c, TT[k][:], tTT[k], XR[k4][:], tXR[k4], LNP[:, 0, :], LNP[:, 1, :], tLNP, ST2[k], tST2[k])
            ops.append(lambda i=i, k4=k4: s.dma("sp", xs1[i * 128:(i + 1) * 128, :], XR[k4][:], r=[tXR[k4]], w=[txs1]))
            chains.append(ops)
        interleave(chains)
        if ip >= 2:
            for i in (ip - 2, ip - 1):
                transpose_rows(s, c, XR[i % 4][:], tXR[i % 4], XT, tXT, i * 128)
    for i in (NT // 128 - 2, NT // 128 - 1):
        transpose_rows(s, c, XR[i % 4][:], tXR[i % 4], XT, tXT, i * 128)
    load_ln(1)
    s.dma("pool", W16[:], io["ple_gate"].rearrange("(c p) n -> p c n", p=128), w=[tW16])
    s.dma("pool", PP[:], io["ple_proj"].rearrange("(c p) n -> p c n", p=128), w=[tPP])
    for grp in range(NT // 1024):
        def out_cb(sc, py, tpy, grp=grp):
            i = grp * 8 + sc
            k = i % 2
            s.dma("sp", XR[k][:], xs1[i * 128:(i + 1) * 128, :], r=[txs1], w=[tXR[k]])
            s.add("dve", lambda e: e.scalar_tensor_tensor(out=TT[k][:], in0=XR[k][:], scalar=ALPHA, in1=py[:], op0=ALU.mult, op1=ALU.add),
                  r=[tXR[k], tpy[0], tpy[1]], w=[tTT[k]])
            layernorm_rows(s, c, TT[k][:], tTT[k], XR[k][:], tXR[k], LNP[:, 0, :], LNP[:, 1, :], tLNP)
            s.dma("sp", xs2[i * 128:(i + 1) * 128, :], XR[k][:], r=[tXR[k]], w=[txs2])

        expert_ffn(s, c, XT[:, :, grp * 1024:(grp + 1) * 1024], tXT, 1024, io["wg"], io["wu"], io["wd"], DFF,
                   aT, tBIG, WG, tWG, WU, tWU, WD, tWD, out_cb, wd_preloaded=(grp > 0))
    for i in range(NT // 128):
        k = i % 2
        s.dma("sp", XR[k][:], xs2[i * 128:(i + 1) * 128, :], r=[txs2], w=[tXR[k]])
        transpose_rows(s, c, XR[k][:], tXR[k], XT, tXT, i * 128)
        s.dma("sp", PR[k][:], io["p"][i * 128:(i + 1) * 128, :], w=[tPR[k]])
        transpose_rows(s, c, PR[k][:], tPR[k], PTt, tPTt, i * 128, nchunk=2)
    load_ln(2)
    outs = []
    touts = []
    chains = []
    for i in range(NT // 128):
        k = i % 2
        s.dma("sp", XR[k][:], xs2[i * 128:(i + 1) * 128, :], r=[txs2], w=[tXR[k]])
        pg, tpg = ps_full(c)
        pe_, tpe = ps_full(c)
        for nh in range(2):
            for dc in range(8):
                s.add("pe", lambda e, pg=pg, nh=nh, dc=dc, i=i: e.matmul(
                    pg[:, nh * 512:(nh + 1) * 512], XT[:, dc, i * 128:(i + 1) * 128], W16[:, dc, nh * 512:(nh + 1) * 512],
                    start=(dc == 0), stop=(dc == 7)), r=[tXT, tW16], w=[tpg[nh]])
            for dc in range(2):
                s.add("pe", lambda e, pe_=pe_, nh=nh, dc=dc, i=i: e.matmul(
                    pe_[:, nh * 512:(nh + 1) * 512], PTt[:, dc, i * 128:(i + 1) * 128], PP[:, dc, nh * 512:(nh + 1) * 512],
                    start=(dc == 0), stop=(dc == 1)), r=[tPTt, tPP], w=[tpe[nh]])
        ops = [
            lambda pg=pg, tpg=tpg, k=k: s.add("act", lambda e: e.activation(out=TT[k][:], in_=pg[:], func=AF.Sigmoid), r=[tpg[0], tpg[1]], w=[tTT[k]]),
            lambda pe_=pe_, tpe=tpe, k=k: s.add("dve", lambda e: e.tensor_tensor(out=TT[k][:], in0=TT[k][:], in1=pe_[:], op=ALU.mult),
                                               r=[tTT[k], tpe[0], tpe[1]], w=[tTT[k]]),
            lambda k=k: s.add("dve", lambda e: e.scalar_tensor_tensor(out=TT[k][:], in0=XR[k][:], scalar=ALPHA, in1=TT[k][:], op0=ALU.mult, op1=ALU.add),
                              r=[tXR[k], tTT[k]], w=[tTT[k]]),
        ]
        ops += ln_ops(s, c, TT[k][:], tTT[k], XR[k][:], tXR[k], LNP[:, 0, :], LNP[:, 1, :], tLNP, ST2[k], tST2[k])

        def store_out(i=i, k=k):
            touts.append(Tok())
            outs.append(s.dma("sp", io["out"][i * 128:(i + 1) * 128, :], XR[k][:], r=[tXR[k]], w=[touts[-1]]))
        ops.append(store_out)
        if "xg" in io and i % 4 == 3:
            def xchg(j=i // 4):
                outs.append(s.add("pool", lambda e: e.collective_compute("AllGather", ALU.bypass, replica_groups=RG_PAIRS,
                                                                         ins=[io["out"][j * 512:(j + 1) * 512, :].opt()], outs=[io["xg"][j].opt()]),
                                  r=touts[-4:], w=[Tok()], dma=True, inc=1))
            ops.append(xchg)
        chains.append(ops)
        if i % 2 == 1:
            interleave(chains)
            chains = []
    return outs


CAP = 640
NSC = CAP // 128


def build_B_moe(nc, st, s, c, io):
    DFF = 3584
    NFC = DFF // 128
    sb = lambda name, shape, dt=F32: st.enter_context(nc.sbuf_tensor(uname(name), list(shape), dt))
    c.IDN = sb("IDN", [128, 128]); c.tIDN = Tok("idn")
    IDNb = sb("IDNb", [128, 128], BF16); tIDNb = Tok()
    UT = sb("UT", [128, 128], BF16); ONESb = sb("ONESb", [128, 128], BF16); tUT = Tok()
    IOTA = sb("IOTA", [128, CAP]); tIOTA = Tok()
    c.ST = sb("ST", [128, 16]); c.tST = Tok("st")
    c.EPSC = sb("EPSC", [128, 1]); c.tEPSC = Tok("eps")
    ST2 = [c.ST, sb("STb", [128, 16])]; tST2 = [c.tST, Tok("stb")]
    s.add("dve", lambda e: e.memset(c.EPSC[:], EPS), w=[c.tEPSC])
    BIG = sb("BIG", [128, NFC * CAP], BF16); tBIG = Tok("big")
    OT = BIG[:, 0:8 * NT].rearrange("p (c t) -> p c t", t=NT)
    aT = BIG[:, 0:NFC * CAP].rearrange("p (c t) -> p c t", t=CAP)
    W16 = sb("W16", [128, 16 * CAP], BF16); tW16 = Tok("w16")
    WO = W16[:, 0:8 * 1024].rearrange("p (c n) -> p c n", n=1024)
    SE = W16[:, 0:16 * CAP].rearrange("p (i s) -> p i s", s=CAP)
    LNP = sb("LNP", [128, 2, 1024]); tLNP = Tok("lnp")
    XTt = sb("XT", [128, 8 * NT], BF16); tXT = Tok("xt")
    XT = XTt[:, :].rearrange("p (c t) -> p c t", t=NT)
    X1B = XTt[:, :].rearrange("p (i d) -> p i d", d=1024)
    XR = [sb("XR%d" % i, [128, 1024]) for i in range(2)]; tXR = [Tok(), Tok()]
    TT = [sb("TT%d" % i, [128, 1024]) for i in range(2)]; tTT = [Tok(), Tok()]
    WG = [sb("WG%d" % i, [128, 8, 256], BF16) for i in range(2)]; tWG = [Tok(), Tok()]
    WU = [sb("WU%d" % i, [128, 8, 256], BF16) for i in range(2)]; tWU = [Tok(), Tok()]
    WD = sb("WD", [128, NFC, 1024], BF16); tWD = Tok("wd")
    PP = BIG[:, 0:2048].rearrange("p (c n) -> p c n", n=1024); tPP = tBIG
    Of1 = sb("Of1", [128, 16, 8]); BASE = sb("BASE", [128, 16, 8]); IDXF = sb("IDXF", [128, 16, 2]); IDXU = sb("IDXU", [128, 16, 2], mybir.dt.uint32); tIDX = Tok()
    XGt = sb("XG", [128, 8 * CAP], BF16); tXG = Tok("xg")
    XGs = [XGt[:, :].rearrange("p (c s) -> p c s", s=CAP), XTt[:, 0:8 * CAP].rearrange("p (c s) -> p c s", s=CAP)]
    YEs = [XGt[:, 0:NSC * 1024].rearrange("p (s n) -> p s n", n=1024), XTt[:, 0:NSC * 1024].rearrange("p (s n) -> p s n", n=1024)]
    tXGs = [tXG, tXT]
    XG = XGs[0]
    PTt = XGt[:, 0:2 * NT].rearrange("p (c t) -> p c t", t=NT)
    PR = [TT[i][:, 0:256] for i in range(2)]; tPR = tTT
    XTr = sb("XTr", [128, 8, 128], BF16); tXTr = Tok()
    c.SG = [XTr[:, 0:4, :].rearrange("p a b -> p (a b)"), XTr[:, 4:8, :].rearrange("p a b -> p (a b)")]; c.tSG = [Tok(), Tok()]; c.sgrr = 0
    WR = sb("WR", [128, 8, 8], BF16); tWR = Tok()
    RT = sb("RT", [128, 64]); tRT = Tok()
    Af = sb("Af", [128, 16, 8]); Ab = sb("Ab", [128, 16, 8], BF16); Gb = sb("Gb", [128, 16, 8], BF16); tAG = Tok()
    RKM = sb("RKM", [128, 16, 8]); tRKM = Tok()
    GSLs = [sb("GSL%d" % i, [128, NSC, 4]) for i in range(2)]; tGSLs = [Tok(), Tok()]
    G3 = sb("G3", [128, 16, 3], BF16); tG3 = Tok()
    TIDX = sb("TIDX", [128, 8]); TIDU = sb("TIDU", [128, 8], mybir.dt.uint32); tTID = Tok()
    YE = XGt[:, 0:NSC * 1024].rearrange("p (s n) -> p s n", n=1024); tYE = tXG
    xs1 = dram(nc, uname("xs1"), [NT, D], kind="Internal")
    xs2 = dram(nc, uname("xs2"), [NT, D], kind="Internal")
    yed = dram(nc, uname("yed"), [8 * CAP, D], BF16, kind="Internal")
    tyed = Tok("yed")
    txs1, txs2 = Tok("xs1"), Tok("xs2")

    def load_ln(k):
        s.dma("sp", LNP[:], io["lnp"][2 * k:2 * k + 2, :].partition_broadcast(128), w=[tLNP])

    s.dma("sp", c.IDN[:], io["idn"], w=[c.tIDN])
    s.dma("pool", IDNb[:], io["idn"], w=[tIDNb])
    s.dma("pool", UT[:], io["ut"], w=[tUT])
    s.add("pool", lambda e: e.memset(ONESb[:], 1.0), w=[tUT])
    s.dma("sp", IOTA[:], io["iota"], w=[tIOTA])
    s.dma("pool", G3[:, :, 1:3], io["pcol"], w=[tG3])
    load_oT(nc, st, s, io, OT, tBIG, XT, tXT)
    s.dma("pool", WO, io["w_out"].rearrange("(c p) n -> p c n", p=128), w=[tW16])
    s.dma("pool", WR[:], io["router"].rearrange("(c p) n -> p c n", p=128), w=[tWR])
    load_ln(0)
    def route_tile(i, k):
        transpose_rows(s, c, XR[k][:], tXR[k], XTr, tXTr, 0)
        pl, tpl = ps_half(c)
        for dc in range(8):
            s.add("pe", lambda e, pl=pl, dc=dc: e.matmul(pl[:, 0:8], XTr[:, dc, :], WR[:, dc, :], start=(dc == 0), stop=(dc == 7)), r=[tXTr, tWR], w=[tpl])
        LG = RT[:, 0:8]; MX8 = RT[:, 8:16]; OH1 = RT[:, 16:24]; OH2 = RT[:, 24:32]; DD = RT[:, 32:33]; G2 = RT[:, 33:34]; G1 = RT[:, 34:35]; GF = RT[:, 40:48]
        s.add("dve", lambda e, pl=pl: e.tensor_copy(out=LG, in_=pl[:, 0:8]), r=[tpl], w=[tRT])
        s.add("dve", lambda e: e.max(out=MX8, in_=LG), r=[tRT], w=[tRT])
        s.add("dve", lambda e: e.tensor_scalar(OH1, LG, RT[:, 8:9], None, ALU.is_equal), r=[tRT], w=[tRT])
        s.add("dve", lambda e: e.tensor_scalar(OH2, LG, RT[:, 9:10], None, ALU.is_equal), r=[tRT], w=[tRT])
        s.add("dve", lambda e: e.tensor_tensor(out=DD, in0=RT[:, 9:10], in1=RT[:, 8:9], op=ALU.subtract), r=[tRT], w=[tRT])
        s.add("act", lambda e: e.activation(out=G2, in_=DD, func=AF.Sigmoid), r=[tRT], w=[tRT])
        s.add("dve", lambda e: e.tensor_scalar(G1, G2, -1.0, 1.0, ALU.mult, ALU.add), r=[tRT], w=[tRT])
        s.add("dve", lambda e, i=i: e.tensor_copy(out=Of1[:, i, :], in_=OH1), r=[tRT], w=[tAG])
        s.add("dve", lambda e, i=i: e.tensor_tensor(out=Af[:, i, :], in0=OH1, in1=OH2, op=ALU.add), r=[tRT], w=[tAG])
        s.add("dve", lambda e, i=i: e.tensor_copy(out=Ab[:, i, :], in_=Af[:, i, :]), r=[tAG], w=[tAG])
        s.add("dve", lambda e: e.tensor_scalar(GF, OH1, G1, None, ALU.mult), r=[tRT], w=[tRT])
        s.add("dve", lambda e, i=i: e.scalar_tensor_tensor(out=Gb[:, i, :], in0=OH2, scalar=G2, in1=GF, op0=ALU.mult, op1=ALU.add), r=[tRT], w=[tAG])

    for i in range(NT // 128):
        k = i % 2
        s.dma("sp", XR[k][:], io["xres"][i * 128:(i + 1) * 128, :], w=[tXR[k]])
        py, tpy = ps_full(c)
        for nh in range(2):
            for fc in range(8):
                s.add("pe", lambda e, py=py, nh=nh, fc=fc, i=i: e.matmul(
                    py[:, nh * 512:(nh + 1) * 512], OT[:, fc, i * 128:(i + 1) * 128], WO[:, fc, nh * 512:(nh + 1) * 512],
                    start=(fc == 0), stop=(fc == 7)), r=[tBIG, tW16], w=[tpy[nh]])
        s.add("dve", lambda e, py=py, k=k: e.scalar_tensor_tensor(out=TT[k][:], in0=XR[k][:], scalar=ALPHA, in1=py[:], op0=ALU.mult, op1=ALU.add),
              r=[tXR[k], tpy[0], tpy[1]], w=[tTT[k]])
        layernorm_rows(s, c, TT[k][:], tTT[k], XR[k][:], tXR[k], LNP[:, 0, :], LNP[:, 1, :], tLNP)
        s.dma("sp", xs1[i * 128:(i + 1) * 128, :], XR[k][:], r=[tXR[k]], w=[txs1])
        if i >= 1:
            route_tile(i - 1, (i - 1) % 2)
    route_tile(15, 15 % 2)
    pr, tpr = ps_half(c)
    first = True
    for i in range(16):
        for j in range(i + 1):
            s.add("pe", lambda e, pr=pr, i=i, j=j, st_=first: e.matmul(pr[:, i * 8:(i + 1) * 8], (UT if j == i else ONESb)[:], Ab[:, j, :],
                                                                      start=st_, stop=(j == i), skip_group_check=True), r=[tUT, tAG], w=[tpr])
            first = False
    s.add("dve", lambda e, pr=pr: e.scalar_tensor_tensor(out=RKM[:], in0=pr[:, 0:128].rearrange("p (i e) -> p i e", e=8), scalar=1.0, in1=Af[:], op0=ALU.add, op1=ALU.mult),
          r=[tpr, tAG], w=[tRKM])
    s.add("dve", lambda e: e.tensor_scalar(RKM[:], RKM[:], -1.0, None, ALU.add), r=[tRKM], w=[tRKM])
    s.add("dve", lambda e: e.tensor_scalar(RT[:, 48:56], IOTA[:, 0:8], float(CAP), None, ALU.mult), r=[tIOTA, tRT], w=[tRT])
    s.add("dve", lambda e: e.tensor_tensor(out=BASE[:], in0=RKM[:], in1=RT[:, 48:56].unsqueeze(1).to_broadcast([128, 16, 8]), op=ALU.add), r=[tRKM, tRT], w=[tIDX])
    s.add("dve", lambda e: e.tensor_tensor(out=RKM[:], in0=Of1[:], in1=BASE[:], op=ALU.mult), r=[tAG, tIDX, tRKM], w=[tRKM])
    s.add("dve", lambda e: e.reduce_sum(out=IDXF[:, :, 0], in_=RKM[:], axis=AX.X), r=[tRKM], w=[tIDX])
    s.add("dve", lambda e: e.tensor_tensor(out=RKM[:], in0=Af[:], in1=Of1[:], op=ALU.subtract), r=[tAG, tIDX, tRKM], w=[tRKM])
    s.add("dve", lambda e: e.tensor_tensor(out=RKM[:], in0=RKM[:], in1=BASE[:], op=ALU.mult), r=[tIDX, tRKM], w=[tRKM])
    s.add("dve", lambda e: e.reduce_sum(out=IDXF[:, :, 1], in_=RKM[:], axis=AX.X), r=[tRKM], w=[tIDX])
    s.add("dve", lambda e: e.tensor_copy(out=IDXU[:], in_=IDXF[:]), r=[tIDX], w=[tIDX])
    s.add("dve", lambda e: e.scalar_tensor_tensor(out=RKM[:], in0=BASE[:], scalar=1.0, in1=Af[:], op0=ALU.add, op1=ALU.mult), r=[tIDX, tAG, tRKM], w=[tRKM])
    s.add("dve", lambda e: e.tensor_tensor(out=BASE[:], in0=Af[:], in1=RT[:, 48:56].unsqueeze(1).to_broadcast([128, 16, 8]), op=ALU.mult), r=[tAG, tRT, tIDX], w=[tIDX])
    s.add("dve", lambda e: e.tensor_tensor(out=RKM[:], in0=RKM[:], in1=BASE[:], op=ALU.subtract), r=[tIDX, tRKM], w=[tRKM])
    s.add("dve", lambda e: e.tensor_scalar(RKM[:], RKM[:], -1.0, None, ALU.add), r=[tRKM], w=[tRKM])
    load_ln(1)
    groups = [(0, 512), (512, CAP - 512)]

    def build_se(ex_):
        for i in range(16):
            s.add("dve", lambda e, i=i, ex_=ex_: e.tensor_scalar(SE[:, i, :], IOTA[:], RKM[:, i, ex_:ex_ + 1], None, ALU.is_equal), r=[tIOTA, tRKM], w=[tW16])


    LAND = [TT[0], TT[1], XR[0], XR[1]]; tLAND = [tTT[0], tTT[1], tXR[0], tXR[1]]

    def gather_prep(ex):
        s.add("dve", lambda e, ex=ex: e.tensor_copy(out=G3[:, :, 0], in_=Gb[:, :, ex]), r=[tAG, tG3], w=[tG3])
        pgs, tpgs = ps_half(c)
        first = True
        for sc in range(NSC):
            for i in range(16):
                s.add("pe", lambda e, pgs=pgs, sc=sc, i=i, st_=first: e.matmul(pgs[:, sc * 4:sc * 4 + 3], SE[:, i, sc * 128:(sc + 1) * 128], G3[:, i, :],
                                                                              start=st_, stop=(i == 15), skip_group_check=True), r=[tW16, tG3], w=[tpgs])
                first = False
        s.add("dve", lambda e, pgs=pgs: e.tensor_copy(out=GSLs[ex % 2][:], in_=pgs[:, 0:NSC * 4].rearrange("p (s k) -> p s k", k=4)), r=[tpgs], w=[tGSLs[ex % 2]])
        s.add("dve", lambda e: e.scalar_tensor_tensor(out=TIDX[:, 0:NSC], in0=GSLs[ex % 2][:, :, 2], scalar=128.0, in1=GSLs[ex % 2][:, :, 1], op0=ALU.mult, op1=ALU.add),
              r=[tGSLs[ex % 2]], w=[tTID])
        s.add("dve", lambda e: e.tensor_copy(out=TIDU[:, 0:NSC], in_=TIDX[:, 0:NSC]), r=[tTID], w=[tTID])
        for sc in range(min(NSC, 4)):
            s.add("pool", lambda e, sc=sc: e.indirect_dma_start(out=LAND[sc][:], out_offset=None, in_=xs1,
                                                               in_offset=bass.IndirectOffsetOnAxis(ap=TIDU[:, sc:sc + 1], axis=0)),
                  r=[tTID, txs1], w=[tLAND[sc]], dma=True)

    def gather_fin(ex):
        XGd, tXGd = XGs[ex % 2], tXGs[ex % 2]
        for sc in range(NSC):
            if sc >= 4:
                s.add("pool", lambda e, sc=sc: e.indirect_dma_start(out=LAND[sc % 4][:], out_offset=None, in_=xs1,
                                                                   in_offset=bass.IndirectOffsetOnAxis(ap=TIDU[:, sc:sc + 1], axis=0)),
                      r=[tTID, txs1], w=[tLAND[sc % 4]], dma=True)
            transpose_rows(s, c, LAND[sc % 4][:], tLAND[sc % 4], XGd, tXGd, sc * 128)

    build_se(0)
    gather_prep(0)
    gather_fin(0)
    for ex in range(8):
        wg_v = io["mg"][ex].rearrange("(c p) f -> p c f", p=128)
        wu_v = io["mu"][ex].rearrange("(c p) f -> p c f", p=128)
        wd_v = io["md"][ex].rearrange("(c p) n -> p c n", p=128)
        if ex < 7:
            build_se(ex + 1)
        for cb in range(DFF // 256):
            b = cb % 2
            s.dma("pool", WG[b][:], wg_v[:, :, cb * 256:(cb + 1) * 256], w=[tWG[b]])
            s.dma("pool", WU[b][:], wu_v[:, :, cb * 256:(cb + 1) * 256], w=[tWU[b]])
            s.dma("pool", WD[:, 2 * cb:2 * cb + 2, :], wd_v[:, 2 * cb:2 * cb + 2, :], w=[tWD])
            for fl in range(2):
                fc = cb * 2 + fl
                for (g0, gn) in groups:
                    pg, tpg = ps_half(c)
                    pu, tpu = ps_half(c)
                    for dc in range(8):
                        s.add("pe", lambda e, pg=pg, b=b, dc=dc, fl=fl, g0=g0, gn=gn, ex=ex: e.matmul(
                            pg[:, 0:gn], WG[b][:, dc, fl * 128:(fl + 1) * 128], XGs[ex % 2][:, dc, g0:g0 + gn], start=(dc == 0), stop=(dc == 7)),
                            r=[tWG[b], tXGs[ex % 2]], w=[tpg])
                    for dc in range(8):
                        s.add("pe", lambda e, pu=pu, b=b, dc=dc, fl=fl, g0=g0, gn=gn, ex=ex: e.matmul(
                            pu[:, 0:gn], WU[b][:, dc, fl * 128:(fl + 1) * 128], XGs[ex % 2][:, dc, g0:g0 + gn], start=(dc == 0), stop=(dc == 7)),
                            r=[tWU[b], tXGs[ex % 2]], w=[tpu])
                    k = c.sgrr % 2
                    c.sgrr += 1
                    SG, tSG = c.SG[k], c.tSG[k]
                    s.add("act", lambda e, pg=pg, SG=SG, gn=gn: e.activation(out=SG[:, 0:gn], in_=pg[:, 0:gn], func=AF.Silu), r=[tpg], w=[tSG])
                    s.add("dve", lambda e, pu=pu, SG=SG, fc=fc, g0=g0, gn=gn: e.tensor_tensor(
                        out=aT[:, fc, g0:g0 + gn], in0=SG[:, 0:gn], in1=pu[:, 0:gn], op=ALU.mult), r=[tSG, tpu], w=[tBIG])
        if ex < 7:
            gather_prep(ex + 1)
        for sc in range(NSC):
            py, tpy = ps_full(c)
            for nh in range(2):
                for fc in range(NFC):
                    s.add("pe", lambda e, py=py, fc=fc, sc=sc, nh=nh: e.matmul(py[:, nh * 512:(nh + 1) * 512], aT[:, fc, sc * 128:(sc + 1) * 128], WD[:, fc, nh * 512:(nh + 1) * 512],
                                                                            start=(fc == 0), stop=(fc == NFC - 1)), r=[tBIG, tWD], w=[tpy[nh]])
            s.add("dve", lambda e, py=py, sc=sc, ex=ex: e.tensor_scalar(YEs[ex % 2][:, sc, :], py[:], GSLs[ex % 2][:, sc, 0:1], None, ALU.mult),
                  r=[tpy[0], tpy[1], tGSLs[ex % 2]], w=[tXGs[ex % 2]])
        if ex < 7:
            gather_fin(ex + 1)
        s.dma("sp", yed[ex * CAP:(ex + 1) * CAP, :].rearrange("(s p) n -> p s n", p=128), YEs[ex % 2], r=[tXGs[ex % 2]], w=[tyed])
    RG4 = [[WG[k][:, 0:4, :].rearrange("p a b -> p (a b)"), WG[k][:, 4:8, :].rearrange("p a b -> p (a b)")] for k in range(2)]
    tRG4 = [[Tok(), Tok()], [Tok(), Tok()]]
    chains = []
    for i in range(16):
        k = i % 2
        rows = slice(i * 128, (i + 1) * 128)
        for j in range(2):
            s.add("pool", lambda e, i=i, j=j, k=k: e.indirect_dma_start(out=RG4[k][j], out_offset=None, in_=yed,
                                                                       in_offset=bass.IndirectOffsetOnAxis(ap=IDXU[:, i, j:j + 1], axis=0)),
                  r=[tIDX, tyed, tWG[k]], w=[tRG4[k][j]], dma=True)
        s.dma("sp", XR[k][:], xs1[rows, :], r=[txs1], w=[tXR[k]])
        ops = [
            lambda k=k: s.add("dve", lambda e: e.tensor_tensor(out=TT[k][:], in0=RG4[k][0], in1=RG4[k][1], op=ALU.add), r=[tRG4[k][0], tRG4[k][1]], w=[tTT[k]]),
            lambda k=k: s.add("dve", lambda e: e.scalar_tensor_tensor(out=TT[k][:], in0=XR[k][:], scalar=ALPHA, in1=TT[k][:], op0=ALU.mult, op1=ALU.add),
                              r=[tXR[k], tTT[k]], w=[tTT[k]]),
        ]
        ops += ln_ops(s, c, TT[k][:], tTT[k], XR[k][:], tXR[k], LNP[:, 0, :], LNP[:, 1, :], tLNP, ST2[k], tST2[k])
        ops.append(lambda k=k, rows=rows: s.dma("sp", xs2[rows, :], XR[k][:], r=[tXR[k]], w=[txs2]))
        ops.append(lambda k=k, i=i: transpose_rows(s, c, XR[k][:], tXR[k], XT, tXT, i * 128))
        chains.append(ops)
        if i % 2 == 1:
            interleave(chains)
            chains = []
    W16v = W16[:, 0:8 * 1024].rearrange("p (c n) -> p c n", n=1024)
    s.dma("pool", W16v, io["ple_gate"].rearrange("(c p) n -> p c n", p=128), w=[tW16])
    s.dma("pool", PP[:], io["ple_proj"].rearrange("(c p) n -> p c n", p=128), w=[tPP])
    for i in range(NT // 128):
        k = i % 2
        s.dma("sp", PR[k], io["p"][i * 128:(i + 1) * 128, :], w=[tPR[k]])
        transpose_rows(s, c, PR[k], tPR[k], PTt, tXG, i * 128, nchunk=2)
    load_ln(2)
    outs = []
    touts = []
    chains = []
    for i in range(NT // 128):
        k = i % 2
        s.dma("sp", XR[k][:], xs2[i * 128:(i + 1) * 128, :], r=[txs2], w=[tXR[k]])
        pg, tpg = ps_full(c)
        pe_, tpe = ps_full(c)
        for nh in range(2):
            for dc in range(8):
                s.add("pe", lambda e, pg=pg, nh=nh, dc=dc, i=i: e.matmul(
                    pg[:, nh * 512:(nh + 1) * 512], XT[:, dc, i * 128:(i + 1) * 128], W16v[:, dc, nh * 512:(nh + 1) * 512],
                    start=(dc == 0), stop=(dc == 7)), r=[tXT, tW16], w=[tpg[nh]])
            for dc in range(2):
                s.add("pe", lambda e, pe_=pe_, nh=nh, dc=dc, i=i: e.matmul(
                    pe_[:, nh * 512:(nh + 1) * 512], PTt[:, dc, i * 128:(i + 1) * 128], PP[:, dc, nh * 512:(nh + 1) * 512],
                    start=(dc == 0), stop=(dc == 1)), r=[tXG, tPP], w=[tpe[nh]])
        ops = [
            lambda pg=pg, tpg=tpg, k=k: s.add("act", lambda e: e.activation(out=TT[k][:], in_=pg[:], func=AF.Sigmoid), r=[tpg[0], tpg[1]], w=[tTT[k]]),
            lambda pe_=pe_, tpe=tpe, k=k: s.add("dve", lambda e: e.tensor_tensor(out=TT[k][:], in0=TT[k][:], in1=pe_[:], op=ALU.mult),
                                               r=[tTT[k], tpe[0], tpe[1]], w=[tTT[k]]),
            lambda k=k: s.add("dve", lambda e: e.scalar_tensor_tensor(out=TT[k][:], in0=XR[k][:], scalar=ALPHA, in1=TT[k][:], op0=ALU.mult, op1=ALU.add),
                              r=[tXR[k], tTT[k]], w=[tTT[k]]),
        ]
        ops += ln_ops(s, c, TT[k][:], tTT[k], XR[k][:], tXR[k], LNP[:, 0, :], LNP[:, 1, :], tLNP, ST2[k], tST2[k])

        def store_out(i=i, k=k):
            touts.append(Tok())
            outs.append(s.dma("sp", io["out"][i * 128:(i + 1) * 128, :], XR[k][:], r=[tXR[k]], w=[touts[-1]]))
        ops.append(store_out)
        if "xg" in io and i % 4 == 3:
            def xchg(j=i // 4):
                outs.append(s.add("pool", lambda e: e.collective_compute("AllGather", ALU.bypass, replica_groups=RG_PAIRS,
                                                                         ins=[io["out"][j * 512:(j + 1) * 512, :].opt()], outs=[io["xg"][j].opt()]),
                                  r=touts[-4:], w=[Tok()], dma=True, inc=1))
            ops.append(xchg)
        chains.append(ops)
        if i % 2 == 1:
            interleave(chains)
            chains = []
    return outs

T = 4096
NEG = -30000.0
NQT = 8
SL = [2.0 ** (-i / 2.0) for i in range(1, 17)]
SLOPE_C = SL[0:8]
SLOPE_A = [SL[8], SL[10], SL[12], SL[14]]
SLOPE_B = [SL[9], SL[11], SL[13], SL[15]]
C_KG0, C_KG1, C_KS, C_KW, C_KC, C_KB0, C_KB1 = 0, 128, 256, 320, 384, 448, 576
C_V = 704
C_QA = 1024
C_QB = 1280
C_QC = 1536
C_G = 1792
NW = 1798
M_CAUS, M_WM, M_SM, M_CM = 0, 4, 8, 13
NMASK = 18


def mask_tables():
    j = np.arange(128)[:, None]
    i = np.arange(512)[None, :]
    tabs = []
    for r in range(4):
        tabs.append((-128 * r + i - j) >= 0)
    for o in range(1, 5):
        dd = 128 * o + i - j
        tabs.append((dd >= 0) & (dd < 512))
    for o in range(-3, 2):
        dd = 128 * o + i - j
        tabs.append((dd >= 0) & (dd < 128))
    for m in range(5):
        tabs.append((i - 16 * j) >= (31 - 512 * m))
    return np.stack(tabs)


ALLOWED = mask_tables()


def host_consts(sidx):
    cst = {}
    cst["masks"] = np.where(ALLOWED, 0.0, NEG).astype(np.float32)
    cst["idn"] = np.eye(128, dtype=np.float32)
    cst["i30k"] = (np.eye(128) * 30000.0).astype(np.float32)
    own_a = [SLOPE_A[2 * sidx], SLOPE_A[2 * sidx + 1]]
    own_b = [SLOPE_B[2 * sidx], SLOPE_B[2 * sidx + 1]]
    own_c = SLOPE_C[4 * sidx:4 * sidx + 4]
    sl8 = own_a + own_b + list(own_c)
    p = np.arange(128)[:, None, None]
    oi = np.arange(32)[None, None, :]
    cst["ab"] = (np.array(sl8)[None, :, None] * (p - 128.0 * (oi - 3))).astype(np.float32)
    cc = np.arange(2)[None, None, :, None]
    qt = np.arange(8)[None, None, None, :]
    perm_a = [2 * sidx, 2 * sidx + 1, 2 * (1 - sidx), 2 * (1 - sidx) + 1]
    cst["cb"] = (np.array([SLOPE_A[h] for h in perm_a])[None, :, None, None] * (16.0 * p[:, :, :, None] + 31 + 2048 * cc - 512 * qt)).astype(np.float32).reshape(128, 64)
    n = np.arange(256)[:, None]
    jj = np.arange(64)[None, :]
    ov = np.clip(np.minimum(16 * n + 32, 64 * jj + 64) - np.maximum(16 * n, 64 * jj), 0, None) / 32.0
    ov[255] = 0.0
    cst["ov"] = ov.astype(np.float32).reshape(2, 128, 64)
    t = np.arange(T)
    cst["emat"] = (t[None, :] // 64 == np.arange(64)[:, None]).astype(np.float32)
    r = np.arange(-63, 64)[None, :]
    pp = np.arange(128)[:, None]
    ta = np.where(r <= -2, 1.0, np.where(r == -1, (pp >= 64) * 1.0, 0.0))
    tb = np.where(r <= -2, 0.0, np.where(r == -1, (pp < 64) * 1e9, np.where(r == 0, 1e9, np.where(r == 1, np.where(pp < 64, -1.0, 1e9), -1.0))))
    cst["ta"] = ta.astype(np.float32)
    cst["tb"] = tb.astype(np.float32)
    i512 = np.arange(512)
    ri = np.stack([(-s * i512).astype(np.float32).astype(ml_dtypes.bfloat16).astype(np.float32) for s in own_c])
    cst["ri"] = ri
    g = np.exp(ri.astype(np.float64) + np.array(own_c)[:, None] * i512[None, :])
    cst["gs"] = np.ascontiguousarray(g.reshape(4, 4, 128).transpose(2, 1, 0)).astype(np.float32)
    return cst


def host_w(w_in, sidx):
    W = np.zeros((D, NW), np.float32)
    kva = 256
    kc, vc, ks, vs, kw, vw = [w_in[:, kva + 64 * k: kva + 64 * k + 64] for k in range(6)]
    W[:, C_KG0:C_KG0 + 64] = kc; W[:, C_KG0 + 64:C_KG0 + 128] = kc
    W[:, C_KG1:C_KG1 + 64] = vc; W[:, C_KG1 + 64:C_KG1 + 128] = vc
    W[:, C_KS:C_KS + 64] = ks
    W[:, C_KW:C_KW + 64] = kw
    W[:, C_KC:C_KC + 64] = w_in[:, 1932 + 64 * sidx: 1932 + 64 * sidx + 64]
    for hh in range(2):
        h = 2 * sidx + hh
        base = C_KB0 + 128 * hh
        W[:, base:base + 32] = w_in[:, 908 + 64 * h: 908 + 64 * h + 32]
        W[:, base + 96:base + 128] = w_in[:, 908 + 64 * h + 32: 908 + 64 * h + 64]
        W[:, C_V + 128 + 64 * hh: C_V + 192 + 64 * hh] = w_in[:, 1164 + 64 * h: 1164 + 64 * h + 64]
        W[:, C_QB + 128 * hh: C_QB + 128 * hh + 64] = w_in[:, 652 + 64 * h: 652 + 64 * h + 64]
        W[:, C_QB + 128 * hh + 64: C_QB + 128 * hh + 128] = w_in[:, 652 + 64 * h: 652 + 64 * h + 64]
        W[:, C_G + 3 * hh: C_G + 3 * hh + 3] = w_in[:, 640 + 3 * h: 640 + 3 * h + 3]
    W[:, C_V:C_V + 64] = vs
    W[:, C_V + 64:C_V + 128] = vw
    W[:, C_V + 256:C_V + 320] = w_in[:, 2060 + 64 * sidx: 2060 + 64 * sidx + 64]
    perm_a = [2 * sidx, 2 * sidx + 1, 2 * (1 - sidx), 2 * (1 - sidx) + 1]
    for k_, h in enumerate(perm_a):
        W[:, C_QA + 64 * k_:C_QA + 64 * k_ + 64] = w_in[:, 64 * h:64 * h + 64]
    W[:, C_QC:C_QC + 256] = w_in[:, 1420 + 256 * sidx: 1420 + 256 * sidx + 256]
    return W


def host_small(inp, L, sidx):
    sm = {}
    w1 = inp["cmp_w1"][L]
    sm["w1"] = np.ascontiguousarray(w1.reshape(2, 16, 128, 128).transpose(2, 0, 1, 3)).reshape(128, 2 * 16 * 128)
    sm["w2"] = np.ascontiguousarray(inp["cmp_w2"][L].transpose(1, 0, 2)).reshape(128, 128)
    pos = inp["cmp_pos"][L]
    sm["pos"] = np.ascontiguousarray(pos.reshape(2, 16, 128).transpose(2, 0, 1)).reshape(128, 32)
    sm["dl"] = np.ascontiguousarray(inp["diff_lambda"][L].reshape(1, 128))
    sm["subln"] = np.ascontiguousarray(inp["diff_subln"][L].reshape(1, 64))
    sm["sinks"] = np.ascontiguousarray(inp["sinks"][L][4 * sidx:4 * sidx + 4].reshape(1, 4))
    return sm


A_IN = dict(x=[T, D], w=[D, NW], masks=[NMASK, 128, 512], idn=[128, 128], i30k=[128, 128], ab=[128, 8, 32], cb=[128, 64],
            ov=[2, 128, 64], emat=[64, T], ta=[128, 127], tb=[128, 127], ri=[4, 512], gs=[128, 4, 4],
            w1=[128, 4096], w2=[128, 128], pos=[128, 32], dl=[1, 128], subln=[1, 64], sinks=[1, 4])


def build_A(nc, st, s, c, io, layer):
    lam_init = 0.8 - 0.6 * math.exp(-0.3 * layer)
    sb = lambda name, shape, dt=F32: st.enter_context(nc.sbuf_tensor(uname(name), list(shape), dt))
    PSB = [st.enter_context(nc.psum_tensor(uname("psb%d" % i), [128, 1024], F32)) for i in range(4)]
    TPS = [[Tok("ps%d_%d" % (i, h)) for h in range(2)] for i in range(4)]
    rr = {"s": 0, "a": 0, "f": 0}

    def ps_score():
        k = rr["s"] % 4; rr["s"] += 1
        return PSB[k // 2][:, (k % 2) * 512:(k % 2 + 1) * 512], TPS[k // 2][k % 2]

    def ps_acc():
        k = rr["a"] % 4; rr["a"] += 1
        return PSB[2 + k // 2][:, (k % 2) * 512:(k % 2 + 1) * 512], TPS[2 + k // 2][k % 2]

    def ps_accfull():
        k = rr["f"] % 2; rr["f"] += 1
        rr["a"] = 0
        return PSB[2 + k], TPS[2 + k]

    W = sb("W", [128, 8, NW], BF16); tW = Tok("W")
    IDN = sb("IDN", [128, 128]); tIDN = Tok()
    IDNb = sb("IDNb", [128, 128], BF16); tIDNb = Tok()
    I30K = sb("I30K", [128, 128], BF16); tI30K = Tok()
    MK = sb("MK", [128, NMASK, 512], BF16); tMK = Tok()
    AB = sb("AB", [128, 8, 32]); tAB = Tok()
    CB = sb("CB", [128, 64]); tCB = Tok()
    TA = sb("TA", [128, 127]); TBt = sb("TBt", [128, 127]); tTAB = Tok()
    GS = sb("GS", [128, 4, 4]); tGS = Tok()
    SK = sb("SK", [128, 4, 4]); tSK = Tok()
    W1 = sb("W1", [128, 2, 16, 128], BF16); tW1 = Tok()
    W2 = sb("W2", [128, 2, 64], BF16); tW2 = Tok()
    POS = sb("POS", [128, 2, 16], BF16); tPOS = Tok()
    SM_ = sb("SMALL", [128, 256]); tSM = Tok()
    Kc2 = sb("Kc2", [128, 2, T], BF16); tKc2 = Tok()
    KS = sb("KS", [128, T], BF16); tKS = Tok()
    KW = sb("KW", [64, T], BF16); tKW = Tok()
    KC = sb("KC", [65, T], BF16); tKC = Tok()
    KB = [sb("KB%d" % i, [128, T], BF16) for i in range(2)]; tKB = [Tok(), Tok()]
    VALL = sb("VALL", [128, 32, 5, 65], BF16); tV = Tok()
    XC = sb("XC", [128, 4, 1024]); tXC = Tok()
    XTc = [sb("XTc%d" % i, [128, 8, 512], BF16) for i in range(2)]; tXTc = [Tok(), Tok()]
    HT = sb("HT", [128, 2, 256], BF16); tHT = Tok()
    BP = sb("BP", [128, 2]); tBP = Tok()
    KCT = sb("KCT", [64, 256], BF16); tKCT = Tok()
    RC = sb("RC", [128, 2, 129], BF16); tRC = Tok()
    QA = [[sb("QA%d_%d" % (b, h), [128, 512], BF16) for h in range(4)] for b in range(2)]
    tQA = [[Tok() for h in range(4)] for b in range(2)]
    QB = [[sb("QB%d_%d" % (b, h), [128, 512], BF16) for h in range(2)] for b in range(1)]
    tQB = [[Tok() for h in range(2)] for b in range(1)]
    QB.append(QB[0]); tQB.append(tQB[0])
    QC = [[sb("QC%d_%d" % (b, h), [65, 512], BF16) for h in range(4)] for b in range(1)]
    tQC = [[Tok() for h in range(4)] for b in range(1)]
    QC.append(QC[0]); tQC.append(tQC[0])
    GT = sb("GT", [128, 4, 6]); tGT = Tok()
    PTb = [sb("PT%d" % i, [128, 512], BF16) for i in range(4)]; tPT = [Tok() for _ in range(4)]
    ptrr = [0]
    IMP = sb("IMP", [128, 4, 64]); tIMP = Tok()
    IMP2 = sb("IMP2", [128, 4, 64]); tIMP2 = Tok()
    MX = sb("MX", [128, 4, 16]); tMX = Tok()
    NM = sb("NM", [128, 4, 128], BF16); tNM = Tok()
    REC = sb("REC", [128, 16]); tREC = Tok()
    OCc = sb("OCc", [128, 2, 4, 64]); tOCc = Tok()
    TMP = [sb("TMP%d" % i, [128, 4, 64]) for i in range(2)]; tTMP = [Tok(), Tok()]
    OCH = sb("OCH", [128, 4, 512]); tOCH = Tok()
    OTc = sb("OTc", [128, 4, 512], BF16); tOTc = Tok()

    q = "sp"
    xdeps = []
    if "xh" in io:
        txfs = [Tok("xfull%d" % j) for j in range(4)]
        for j in range(4):
            s.add("pool", lambda e, j=j: e.collective_compute("AllGather", ALU.bypass, replica_groups=[[0, 1], [2, 3], [4, 5], [6, 7]],
                                                              ins=[io["xh"][j * 512:(j + 1) * 512, :].opt()], outs=[io["xg"][j].opt()]),
                  r=[], w=[txfs[j]], dma=True, inc=1)
        xdeps = txfs
    s.dma("pool", W[:], io["w"].rearrange("(c p) n -> p c n", p=128), w=[tW])
    s.dma(q, IDN[:], io["idn"], w=[tIDN])
    s.dma("pool", IDNb[:], io["idn"], w=[tIDNb])
    s.dma("pool", I30K[:], io["i30k"], w=[tI30K])
    for m0 in range(0, NMASK, 4):
        m1 = min(NMASK, m0 + 4)
        s.dma("pool", MK[:, m0:m1, :], io["masks"][m0:m1].rearrange("m p i -> p m i"), w=[tMK])
    s.dma(q, AB[:], io["ab"], w=[tAB])
    s.dma(q, CB[:], io["cb"], w=[tCB])
    s.dma(q, TA[:], io["ta"], w=[tTAB])
    s.dma(q, TBt[:], io["tb"], w=[tTAB])
    s.dma(q, GS[:], io["gs"], w=[tGS])
    s.dma("pool", W1[:], io["w1"].rearrange("p (k c h) -> p k c h", k=2, c=16), w=[tW1])
    s.dma("pool", W2[:], io["w2"].rearrange("p (k d) -> p k d", k=2), w=[tW2])
    s.dma("pool", POS[:], io["pos"].rearrange("p (k c) -> p k c", k=2), w=[tPOS])
    s.dma(q, SM_[:, 0:128], io["dl"].partition_broadcast(128).rearrange("p a b -> p (a b)"), w=[tSM])
    s.dma(q, SM_[:, 128:192], io["subln"].partition_broadcast(128).rearrange("p a b -> p (a b)"), w=[tSM])
    s.dma(q, SM_[:, 192:196], io["sinks"].partition_broadcast(128).rearrange("p a b -> p (a b)"), w=[tSM])
    s.dma("pool", KS[64:128, :], io["emat"], w=[tKS])
    s.dma("pool", RC[:, :, 0:64], io["ov"].rearrange("c p j -> p c j"), w=[tRC])
    for b in range(1):
        for h in range(4):
            s.dma("pool", QC[b][h][64:65, :], io["ri"][h:h + 1, :], w=[tQC[b][h]])
    s.add("pool", lambda e: e.memset(VALL[:, :, :, 64:65], 1.0), w=[tV])
    s.add("pool", lambda e: e.memset(RC[:, :, 128:129], 1.0), w=[tRC])
    s.add("pool", lambda e: e.memset(KC[64:65, :], 1.0), w=[tKC])
    s.add("pool", lambda e: e.memset(NM[:], 0.0), w=[tNM])
    s.add("pool", lambda e: e.memset(HT[:], 0.0), w=[tHT])
    s.add("pool", lambda e: e.memset(KCT[:], 0.0), w=[tKCT])
    s.add("pool", lambda e: e.memset(Kc2[:, :, T - 1:T], 0.0), w=[tKc2])
    s.add("dve", lambda e: e.tensor_tensor(out=SM_[:, 208:240], in0=SM_[:, 0:32], in1=SM_[:, 32:64], op=ALU.mult), r=[tSM], w=[tSM])
    s.add("dve", lambda e: e.reduce_sum(out=SM_[:, 200:201], in_=SM_[:, 208:240], axis=AX.X), r=[tSM], w=[tSM])
    s.add("dve", lambda e: e.tensor_tensor(out=SM_[:, 208:240], in0=SM_[:, 64:96], in1=SM_[:, 96:128], op=ALU.mult), r=[tSM], w=[tSM])
    s.add("dve", lambda e: e.reduce_sum(out=SM_[:, 201:202], in_=SM_[:, 208:240], axis=AX.X), r=[tSM], w=[tSM])
    s.add("act", lambda e: e.activation(out=SM_[:, 202:204], in_=SM_[:, 200:202], func=AF.Exp), r=[tSM], w=[tSM])
    s.add("dve", lambda e: e.tensor_tensor(out=SM_[:, 204:205], in0=SM_[:, 203:204], in1=SM_[:, 202:203], op=ALU.subtract), r=[tSM], w=[tSM])
    s.add("dve", lambda e: e.tensor_scalar(SM_[:, 200:201], SM_[:, 204:205], -lam_init, None, ALU.add), r=[tSM], w=[tSM])
    s.add("dve", lambda e: e.tensor_scalar(SM_[:, 128:192], SM_[:, 128:192], 1.0 - lam_init, None, ALU.mult), r=[tSM], w=[tSM])
    s.add("act", lambda e: e.activation(out=SM_[:, 196:200], in_=SM_[:, 192:196], func=AF.Exp), r=[tSM], w=[tSM])
    for sub in range(4):
        s.add("dve", lambda e, sub=sub: e.tensor_tensor(out=SK[:, sub, :], in0=GS[:, sub, :], in1=SM_[:, 196:200], op=ALU.mult), r=[tSM, tGS], w=[tSK])
    NLAM = SM_[:, 200:201]
    SUBLN = SM_[:, 128:192]

    evrr = [0]

    def evac(out, in_, rtoks, wtoks, scale=None, eng=None):
        if eng is None:
            eng = ("dve", "act")[evrr[0] % 2]; evrr[0] += 1
        if eng == "act":
            if scale is None:
                s.add("act", lambda e: e.activation(out=out, in_=in_, func=AF.Copy), r=rtoks, w=wtoks)
            else:
                s.add("act", lambda e: e.activation(out=out, in_=in_, func=AF.Copy, scale=scale), r=rtoks, w=wtoks)
        else:
            if scale is None:
                s.add("dve", lambda e: e.tensor_copy(out=out, in_=in_), r=rtoks, w=wtoks)
            else:
                s.add("dve", lambda e: e.tensor_scalar(out, in_, scale, None, ALU.mult), r=rtoks, w=wtoks)

    def load_xt(tc, k, eng=None):
        xsrc = io["xchunk"](tc) if "xchunk" in io else io["x"][tc * 512:(tc + 1) * 512, :]
        s.dma("sp", XC[:], xsrc.rearrange("(a p) d -> p a d", p=128), r=([xdeps[tc % 4]] if xdeps else []), w=[tXC])
        for dc in range(8):
            ps, tps = ps_score()
            for sub in range(4):
                s.add("pe", lambda e, ps=ps, sub=sub, dc=dc: e.transpose(ps[:, sub * 128:(sub + 1) * 128], XC[:, sub, dc * 128:(dc + 1) * 128], IDN[:]),
                      r=[tXC, tIDN], w=[tps])
            evac(XTc[k][:, dc, :], ps, [tps], [tXTc[k]], eng=eng)

    def proj_fm(k, col0, m):
        ps, tps = ps_score()
        for dc in range(8):
            s.add("pe", lambda e, ps=ps, dc=dc: e.matmul(ps[0:m, :], W[:, dc, col0:col0 + m], XTc[k][:, dc, :], start=(dc == 0), stop=(dc == 7)),
                  r=[tW, tXTc[k]], w=[tps])
        return ps, tps

    for tc in range(8):
        k = tc % 2
        t0 = tc * 512
        load_xt(tc, k)
        for kv in range(2):
            ps, tps = proj_fm(k, C_KG0 + 128 * kv, 128)
            evac(Kc2[0:64, kv, t0:t0 + 512], ps[0:64, :], [tps], [tKc2])
            if tc == 0:
                evac(Kc2[64:128, kv, 0:511], ps[64:128, 1:512], [tps], [tKc2])
            else:
                evac(Kc2[64:128, kv, t0 - 1:t0 + 511], ps[64:128, :], [tps], [tKc2])
        ps, tps = proj_fm(k, C_KS, 64); evac(KS[0:64, t0:t0 + 512], ps[0:64, :], [tps], [tKS])
        ps, tps = proj_fm(k, C_KW, 64); evac(KW[0:64, t0:t0 + 512], ps[0:64, :], [tps], [tKW])
        ps, tps = proj_fm(k, C_KC, 64); evac(KC[0:64, t0:t0 + 512], ps[0:64, :], [tps], [tKC])
        for hh in range(2):
            ps, tps = proj_fm(k, C_KB0 + 128 * hh, 128); evac(KB[hh][:, t0:t0 + 512], ps, [tps], [tKB[hh]])
        for sub in range(4):
            ps, tps = ps_score()
            for dc in range(8):
                s.add("pe", lambda e, ps=ps, dc=dc, sub=sub, k=k: e.matmul(ps[:, 0:320], XTc[k][:, dc, sub * 128:(sub + 1) * 128], W[:, dc, C_V:C_V + 320],
                                                                      start=(dc == 0), stop=(dc == 7)), r=[tW, tXTc[k]], w=[tps])
            evac(VALL[:, tc * 4 + sub, :, 0:64], ps[:, 0:320].rearrange("p (a b) -> p a b", b=64), [tps], [tV])

    for kv in range(2):
        ps, tps = ps_score()
        for cc in range(16):
            s.add("pe", lambda e, ps=ps, cc=cc, kv=kv: e.matmul(ps[:, 0:1], W1[:, kv, cc, :], POS[:, kv, cc:cc + 1], start=(cc == 0), stop=(cc == 15)),
                  r=[tW1, tPOS], w=[tps])
        evac(BP[:, kv:kv + 1], ps[:, 0:1], [tps], [tBP], eng="dve")
        ps, tps = ps_score()
        for cc in range(16):
            s.add("pe", lambda e, ps=ps, cc=cc, kv=kv: e.matmul(ps[:, 0:255], W1[:, kv, cc, :], Kc2[:, kv, 2 * cc: 2 * cc + 16 * 254 + 1: 16],
                                                                start=(cc == 0), stop=(cc == 15)), r=[tW1, tKc2], w=[tps])
        s.add("act", lambda e, ps=ps, kv=kv: e.activation(out=HT[:, kv, 0:255], in_=ps[:, 0:255], func=AF.Gelu_apprx_tanh, bias=BP[:, kv:kv + 1]),
              r=[tps, tBP], w=[tHT])
    ps, tps = ps_score()
    s.add("pe", lambda e, ps=ps: e.matmul(ps[0:64, 0:256], W2[:, 0, :], HT[:, 0, :], start=True, stop=True), r=[tW2, tHT], w=[tps])
    evac(KCT[:, :], ps[0:64, 0:256], [tps], [tKCT], eng="dve")
    for cc in range(2):
        ps, tps = ps_score()
        s.add("pe", lambda e, ps=ps, cc=cc: e.matmul(ps[:, 0:64], HT[:, 1, cc * 128:(cc + 1) * 128], W2[:, 1, :], start=True, stop=True), r=[tW2, tHT], w=[tps])
        evac(RC[:, cc, 64:128], ps[:, 0:64], [tps], [tRC], eng="dve")

    NPT = 4
    LA = 2

    GP = []
    CAPT = 2

    def gp_flush_one():
        ent = GP.pop(0)
        ent["pv"]()
        if ent["after"] is not None:
            ent["after"]()

    def gp_push(n, pv, after=None):
        GP.append(dict(n=n, pv=pv, after=after))
        while sum(x["n"] for x in GP) > CAPT + n - 1 and len(GP) > 1:
            gp_flush_one()

    def gp_drain():
        while GP:
            gp_flush_one()

    def attn_unit(kbs, lhs_fn, ltoks, rhs, rtoks, bias_fn, vidx, o_acc, to_acc, after=None):
        attn_multi(kbs, [(lhs_fn, ltoks, rhs, rtoks, bias_fn, vidx, o_acc, to_acc)], after=after)

    def attn_multi(kbs, streams, after=None):
        ns = len(streams)
        first = [True] * ns
        last_kb = {}
        for (kb, mi, subs) in kbs:
            for sub in subs:
                last_kb[sub] = kb

        def make_pv(kb, subs, pks):
            def pv():
                for si, (lhs_fn, ltoks, rhs, rtoks, bias_fn, vidx, o_acc, to_acc) in enumerate(streams):
                    pk = pks[si]
                    for sub in subs:
                        s.add("pe", lambda e, pk=pk, sub=sub, kb=kb, st_=first[si], sp_=(last_kb[sub] == kb), o_acc=o_acc, vidx=vidx: e.matmul(
                            o_acc[:, sub * 128:sub * 128 + 65], PTb[pk][:, sub * 128:(sub + 1) * 128], VALL[:, kb, vidx, :], start=st_, stop=sp_,
                            skip_group_check=True),
                            r=[tPT[pk], tV], w=[to_acc])
                        first[si] = False
            return pv

        for idx_, (kb, mi, subs) in enumerate(kbs):
            c0, c1 = min(subs) * 128, (max(subs) + 1) * 128
            tiles = []
            for (lhs_fn, ltoks, rhs, rtoks, bias_fn, vidx, o_acc, to_acc) in streams:
                ps, tps = ps_score()
                s.add("pe", lambda e, ps=ps, kb=kb, mi=mi, c0=c0, c1=c1, lhs_fn=lhs_fn, rhs=rhs: e.matmul(ps[:, c0:c1], lhs_fn(kb), rhs[:, c0:c1], start=True, stop=(mi is None)),
                      r=ltoks + rtoks, w=[tps])
                tiles.append((ps, tps))
            if mi is not None:
                for (ps, tps) in tiles:
                    s.add("pe", lambda e, ps=ps, mi=mi, c0=c0, c1=c1: e.matmul(ps[:, c0:c1], IDNb[:], MK[:, mi, c0:c1], start=False, stop=True), r=[tIDNb, tMK], w=[tps])
            pks = []
            for si, (lhs_fn, ltoks, rhs, rtoks, bias_fn, vidx, o_acc, to_acc) in enumerate(streams):
                ps, tps = tiles[si]
                pk = ptrr[0] % NPT; ptrr[0] += 1
                s.add("act", lambda e, ps=ps, pk=pk, kb=kb, c0=c0, c1=c1, bias_fn=bias_fn: e.activation(out=PTb[pk][:, c0:c1], in_=ps[:, c0:c1], func=AF.Exp, bias=bias_fn(kb)),
                      r=[tps, tAB, tCB], w=[tPT[pk]])
                pks.append(pk)
            gp_push(ns, make_pv(kb, subs, pks), after if idx_ == len(kbs) - 1 else None)

    def kb_list(qt, kind):
        out = []
        lo = {"full": 0, "win": max(0, 4 * qt - 4), "swa": max(0, 4 * qt - 1)}[kind]
        for kb in range(lo, 4 * qt + 4):
            o = 4 * qt - kb
            if kind == "full":
                mi = (M_CAUS - o) if o <= 0 else None
            elif kind == "win":
                mi = (M_CAUS - o) if o <= 0 else (M_WM + o - 1)
            else:
                mi = M_SM + o + 3
            if mi is None:
                subs = [0, 1, 2, 3]
            else:
                subs = [sub for sub in range(4) if ALLOWED[mi][:, sub * 128:(sub + 1) * 128].any()]
                if ALLOWED[mi].all():
                    mi = None
            out.append((kb, mi, subs))
        return out

    def recip_den(o_acc, to_acc, width, col, rec_ap, extra=None):
        den = o_acc[:, 0:4 * width].rearrange("p (a b) -> p a b", b=width)[:, :, col]
        if extra is None:
            s.add("dve", lambda e: e.tensor_scalar(rec_ap, den, 1e-30, None, ALU.max), r=(to_acc if isinstance(to_acc, list) else [to_acc]), w=[tREC])
        else:
            s.add("dve", lambda e: e.tensor_tensor(out=rec_ap, in0=den, in1=extra, op=ALU.add), r=[to_acc, tSK], w=[tREC])
        s.add("dve", lambda e: e.reciprocal(out=rec_ap, in_=rec_ap), r=[tREC], w=[tREC])

    def oview(o_acc, width, c0, n=64):
        return o_acc[:, 0:4 * width].rearrange("p (a b) -> p a b", b=width)[:, :, c0:c0 + n]

    def bc(ap4):
        return ap4.unsqueeze(2).to_broadcast([128, 4, 64])

    outs = []
    toh = []
    def finish_tile(qt):
        for fc in range(4):
            ps, tps = ps_score()
            for sub in range(4):
                s.add("pe", lambda e, ps=ps, sub=sub, fc=fc: e.transpose(ps[:, sub * 128:(sub + 1) * 128], OCH[:, sub, fc * 128:(fc + 1) * 128], IDN[:]),
                      r=[tOCH, tIDN], w=[tps])
            evac(OTc[:, fc, :], ps, [tps], [tOTc], eng="dve")
        if "oTh" in io:
            tq = Tok()
            toh.append(tq)
            outs.append(s.dma("sp", io["oTh"][qt // 4].rearrange("(c p) t -> p c t", p=128)[:, :, (qt % 4) * 512:(qt % 4 + 1) * 512], OTc[:], r=[tOTc], w=[tq]))
            if qt % 4 == 3:
                j = qt // 4
                outs.append(s.add("pool", lambda e, j=j: e.collective_compute("AllGather", ALU.bypass, replica_groups=[[0, 1], [2, 3], [4, 5], [6, 7]],
                                                                              ins=[io["oTh"][j].opt()], outs=[io["oTg"][j].opt()]),
                                  r=toh[-4:], w=[Tok()], dma=True, inc=1))
        else:
            outs.append(s.dma("sp", io["oT"][:, :, qt * 512:(qt + 1) * 512].rearrange("c p t -> p c t"), OTc[:], r=[tOTc]))

    def q_proj(qt):
        k = qt % 2
        b = qt % 2
        load_xt(qt, k, eng="dve")
        for h in range(4):
            ps, tps = proj_fm(k, C_QA + 64 * h, 64)
            evac(QA[b][h][0:64, :], ps[0:64, :], [tps], [tQA[b][h]], scale=0.125, eng="dve")
        for h in range(2):
            ps, tps = proj_fm(k, C_QB + 128 * h, 128)
            evac(QB[b][h][:, :], ps, [tps], [tQB[b][h]], scale=32 ** -0.5, eng="dve")
        for h in range(4):
            ps, tps = proj_fm(k, C_QC + 64 * h, 64)
            evac(QC[b][h][0:64, :], ps[0:64, :], [tps], [tQC[b][h]], scale=0.125, eng="dve")

    q_proj(0)
    for qt in range(NQT):
        k = qt % 2
        b = qt % 2
        ps, tps = ps_score()
        for sub in range(4):
            for dc in range(8):
                s.add("pe", lambda e, ps=ps, dc=dc, sub=sub, k=k: e.matmul(ps[:, sub * 8:sub * 8 + 6], XTc[k][:, dc, sub * 128:(sub + 1) * 128], W[:, dc, C_G:C_G + 6],
                                                                      start=(dc == 0), stop=(dc == 7)), r=[tW, tXTc[k]], w=[tps])
        s.add("act", lambda e, ps=ps: e.activation(out=GT[:], in_=ps[:, 0:32].rearrange("p (a b) -> p a b", b=8)[:, :, 0:6], func=AF.Sigmoid), r=[tps], w=[tGT])

        chunks = [0] if qt < 4 else [0, 1]
        for h in range(4):
            oa, toa = ps_accfull()

            def post_cmp(h=h, oa=oa, toa=toa):
                recip_den(oa, toa, 256, 128, REC[:, 0:4])
                if h == 0:
                    s.add("dve", lambda e: e.tensor_tensor(out=IMP[:], in0=oview(oa, 256, 0), in1=bc(REC[:, 0:4]), op=ALU.mult),
                          r=[toa[0], toa[1], tREC], w=[tIMP])
                else:
                    tm = TMP[h % 2]; ttm = tTMP[h % 2]
                    s.add("dve", lambda e: e.tensor_tensor(out=tm[:], in0=oview(oa, 256, 0), in1=bc(REC[:, 0:4]), op=ALU.mult),
                          r=[toa[0], toa[1], tREC], w=[ttm])
                    s.add("dve", lambda e: e.tensor_tensor(out=IMP[:], in0=IMP[:], in1=tm[:], op=ALU.add), r=[ttm, tIMP], w=[tIMP])
                if h < 2:
                    s.add("dve", lambda e: e.tensor_tensor(out=OCc[:, h, :, :], in0=oview(oa, 256, 64), in1=bc(REC[:, 0:4]), op=ALU.mult),
                          r=[toa[0], toa[1], tREC], w=[tOCc])

            for cc in chunks:
                rel = qt - 4 * cc
                mi = (M_CM + rel) if rel < 5 else None
                ps, tps = ps_score()
                s.add("pe", lambda e, ps=ps, cc=cc, h=h, mi=mi, b=b: e.matmul(ps, KCT[:, cc * 128:(cc + 1) * 128], QA[b][h][0:64, :], start=True, stop=(mi is None)),
                      r=[tKCT, tQA[b][h]], w=[tps])
                if mi is not None:
                    s.add("pe", lambda e, ps=ps, mi=mi: e.matmul(ps, IDNb[:], MK[:, mi, :], start=False, stop=True), r=[tIDNb, tMK], w=[tps])
                pk = ptrr[0] % NPT; ptrr[0] += 1
                ci = h * 16 + cc * 8 + qt
                s.add("act", lambda e, ps=ps, pk=pk, ci=ci: e.activation(out=PTb[pk][:], in_=ps, func=AF.Exp, bias=CB[:, ci:ci + 1]),
                      r=[tps, tCB], w=[tPT[pk]])

                def pv_cmp(oa=oa, toa=toa, pk=pk, cc=cc, first=(cc == chunks[0]), last=(cc == chunks[-1])):
                    for sub in range(4):
                        s.add("pe", lambda e, sub=sub: e.matmul(
                            oa[:, sub * 256:sub * 256 + 129], PTb[pk][:, sub * 128:(sub + 1) * 128], RC[:, cc, :], start=(first and sub % 2 == 0), stop=last,
                            skip_group_check=True),
                            r=[tPT[pk], tRC], w=[toa[sub // 2]])

                gp_push(1, pv_cmp, post_cmp if cc == chunks[-1] else None)
        gp_drain()
        if qt >= 1:
            finish_tile(qt - 1)
        for sub in range(4):
            blk = qt * 4 + sub
            lo = 63 - 2 * blk
            s.add("dve", lambda e, sub=sub, lo=lo: e.tensor_tensor(out=IMP2[:, sub, :], in0=IMP[:, sub, :], in1=TA[:, lo:lo + 64], op=ALU.mult),
                  r=[tIMP, tTAB], w=[tIMP2])
            s.add("dve", lambda e, sub=sub, lo=lo: e.tensor_tensor(out=IMP2[:, sub, :], in0=IMP2[:, sub, :], in1=TBt[:, lo:lo + 64], op=ALU.add),
                  r=[tIMP2, tTAB], w=[tIMP2])
        s.add("dve", lambda e: e.memset(IMP2[:, :, 0:1], 1e9), w=[tIMP2])
        for sub in range(4):
            s.add("dve", lambda e, sub=sub: e.max(out=MX[:, sub, 0:8], in_=IMP2[:, sub, :]), r=[tIMP2], w=[tMX])
            s.add("dve", lambda e, sub=sub: e.match_replace(out=IMP[:, sub, :], in_to_replace=MX[:, sub, 0:8], in_values=IMP2[:, sub, :], imm_value=-1e30),
                  r=[tIMP2, tMX], w=[tIMP])
            s.add("dve", lambda e, sub=sub: e.max(out=MX[:, sub, 8:16], in_=IMP[:, sub, :]), r=[tIMP], w=[tMX])
            s.add("dve", lambda e, sub=sub: e.tensor_scalar(NM[:, sub, 64:128], IMP2[:, sub, :], MX[:, sub, 15:16], 1.0, ALU.is_ge, ALU.subtract),
                  r=[tIMP2, tMX], w=[tNM])
        for own in range(2):
            hs = 2 + own
            feat0 = 128 + 64 * own
            accs = []
            streams = []
            for mp in range(2):
                oa, toa = ps_acc()
                lhs_fn = lambda kb, own=own, mp=mp: KB[own][64 * mp:64 * mp + 64, kb * 128:(kb + 1) * 128]
                rhs = QB[b][own][64 * mp:64 * mp + 64, :]
                bias_fn = lambda kb, hs=hs, qt=qt: AB[:, hs, 4 * qt - kb + 3: 4 * qt - kb + 4]
                streams.append((lhs_fn, [tKB[own]], rhs, [tQB[b][own]], bias_fn, 2 + own, oa, toa))
                accs.append((oa, toa))

            def post_diff(accs=accs, feat0=feat0):
                (o1, to1), (o2, to2) = accs
                r1 = REC[:, 4:8]; r2 = REC[:, 8:12]
                recip_den(o1, to1, 128, 64, r1)
                recip_den(o2, to2, 128, 64, r2)
                s.add("dve", lambda e: e.tensor_scalar(r2, r2, NLAM, None, ALU.mult), r=[tREC, tSM], w=[tREC])
                t1 = TMP[0]; t2 = TMP[1]
                s.add("dve", lambda e: e.tensor_tensor(out=t1[:], in0=oview(o1, 128, 0), in1=bc(r1), op=ALU.mult), r=[to1, tREC], w=[tTMP[0]])
                s.add("dve", lambda e: e.tensor_tensor(out=t2[:], in0=oview(o2, 128, 0), in1=bc(r2), op=ALU.mult), r=[to2, tREC], w=[tTMP[1]])
                s.add("dve", lambda e: e.tensor_tensor(out=t1[:], in0=t1[:], in1=t2[:], op=ALU.add), r=[tTMP[0], tTMP[1]], w=[tTMP[0]])
                s.add("dve", lambda e: e.tensor_tensor(out=t2[:], in0=t1[:], in1=t1[:], op=ALU.mult), r=[tTMP[0]], w=[tTMP[1]])
                ss = REC[:, 12:16]
                s.add("dve", lambda e: e.reduce_sum(out=ss, in_=t2[:], axis=AX.X), r=[tTMP[1]], w=[tREC])
                s.add("dve", lambda e: e.tensor_scalar(ss, ss, 1.0 / 64.0, 1e-5, ALU.mult, ALU.add), r=[tREC], w=[tREC])
                s.add("act", lambda e: e.activation(out=ss, in_=ss, func=AF.Sqrt), r=[tREC], w=[tREC])
                s.add("dve", lambda e: e.reciprocal(out=ss, in_=ss), r=[tREC], w=[tREC])
                s.add("dve", lambda e: e.tensor_tensor(out=t1[:], in0=t1[:], in1=bc(ss), op=ALU.mult), r=[tTMP[0], tREC], w=[tTMP[0]])
                s.add("dve", lambda e: e.tensor_tensor(out=OCH[:, :, feat0:feat0 + 64], in0=t1[:], in1=SUBLN.unsqueeze(1).to_broadcast([128, 4, 64]), op=ALU.mult),
                      r=[tTMP[0], tSM], w=[tOCH])

            attn_multi(kb_list(qt, "full"), streams, after=post_diff)
        for r_ in range(4):
            hs = 4 + r_
            feat0 = 256 + 64 * r_
            oa, toa = ps_acc()
            lhs_fn = lambda kb: KC[0:65, kb * 128:(kb + 1) * 128]
            rhs = QC[b][r_][0:65, :]
            bias_fn = lambda kb, hs=hs, qt=qt: AB[:, hs, 4 * qt - kb + 3: 4 * qt - kb + 4]

            def post_swa(oa=oa, toa=toa, r_=r_, feat0=feat0):
                rec = REC[:, 4:8]
                recip_den(oa, toa, 128, 64, rec, extra=SK[:, :, r_])
                s.add("dve", lambda e: e.tensor_tensor(out=OCH[:, :, feat0:feat0 + 64], in0=oview(oa, 128, 0), in1=bc(rec), op=ALU.mult),
                      r=[toa, tREC], w=[tOCH])

            attn_unit(kb_list(qt, "swa"), lhs_fn, [tKC], rhs, [tQC[b][r_]], bias_fn, 4, oa, toa, after=post_swa)
        ps, tps = ps_score()
        for sub in range(4):
            s.add("pe", lambda e, ps=ps, sub=sub: e.matmul(ps[:, sub * 128:(sub + 1) * 128], NM[:, sub, :], I30K[:], start=True, stop=True),
                  r=[tNM, tI30K], w=[tps])
        for own in range(2):
            h = own
            evac(QA[b][h][64:128, :], ps[64:128, :], [tps], [tQA[b][h]], eng="dve")

        for own in range(2):
            h = own
            hs = own
            feat0 = 64 * own
            s.add("dve", lambda e, own=own, feat0=feat0: e.tensor_tensor(out=OCH[:, :, feat0:feat0 + 64], in0=OCc[:, own, :, :],
                                                                         in1=bc(GT[:, :, 3 * own + 0]), op=ALU.mult), r=[tOCc, tGT], w=[tOCH])
            for br, (kind, lhs_t, ltok, vidx) in enumerate([("full", KS, tKS, 0), ("win", KW, tKW, 1)]):
                oa, toa = ps_acc()
                if kind == "full":
                    lhs_fn = lambda kb: KS[:, kb * 128:(kb + 1) * 128]
                    rhs = QA[b][h][:, :]
                else:
                    lhs_fn = lambda kb: KW[0:64, kb * 128:(kb + 1) * 128]
                    rhs = QA[b][h][0:64, :]
                bias_fn = lambda kb, hs=hs, qt=qt: AB[:, hs, 4 * qt - kb + 3: 4 * qt - kb + 4]

                def post_nsa(oa=oa, toa=toa, own=own, br=br, feat0=feat0):
                    rec = REC[:, 4 + 4 * br: 8 + 4 * br]
                    recip_den(oa, toa, 128, 64, rec)
                    s.add("dve", lambda e: e.tensor_tensor(out=rec, in0=rec, in1=GT[:, :, 3 * own + 1 + br], op=ALU.mult),
                          r=[tREC, tGT], w=[tREC])
                    tm = TMP[br]; ttm = tTMP[br]
                    s.add("dve", lambda e: e.tensor_tensor(out=tm[:], in0=oview(oa, 128, 0), in1=bc(rec), op=ALU.mult),
                          r=[toa, tREC], w=[ttm])
                    s.add("dve", lambda e: e.tensor_tensor(out=OCH[:, :, feat0:feat0 + 64], in0=OCH[:, :, feat0:feat0 + 64], in1=tm[:], op=ALU.add),
                          r=[ttm, tOCH], w=[tOCH])

                attn_unit(kb_list(qt, kind), lhs_fn, [ltok], rhs, [tQA[b][h]], bias_fn, vidx, oa, toa, after=post_nsa)
        if qt + 1 < NQT:
            q_proj(qt + 1)
        gp_drain()
    finish_tile(NQT - 1)
    if io.get("debug"):
        dl_ = [("KC", KC, [65, T], BF16, tKC), ("KW", KW, [64, T], BF16, tKW), ("KS", KS, [128, T], BF16, tKS), ("KB0", KB[0], [128, T], BF16, tKB[0]),
               ("VALL", VALL, [128, 32, 5, 65], BF16, tV), ("KCT", KCT, [64, 256], BF16, tKCT), ("RC", RC, [128, 2, 129], BF16, tRC),
               ("HT", HT, [128, 2, 256], BF16, tHT), ("Kc2", Kc2, [128, 2, T], BF16, tKc2), ("QA0", QA[1][0], [128, 512], BF16, tQA[1][0]),
               ("QB0", QB[1][0], [128, 512], BF16, tQB[1][0]), ("QC0", QC[1][0], [65, 512], BF16, tQC[1][0]), ("GT", GT, [128, 4, 6], F32, tGT),
               ("IMP2", IMP2, [128, 4, 64], F32, tIMP2), ("NM", NM, [128, 4, 128], BF16, tNM), ("OCH", OCH, [128, 4, 512], F32, tOCH),
               ("SMALL", SM_, [128, 256], F32, tSM), ("SK", SK, [128, 4, 4], F32, tSK), ("OCc", OCc, [128, 2, 4, 64], F32, tOCc),
               ("XT", XTc[1], [128, 8, 512], BF16, tXTc[1]), ("W", W, [128, 8, NW], BF16, tW), ("MK", MK, [128, NMASK, 512], BF16, tMK)]
        for (nm, tl, shp, dt_, tk) in dl_:
            dtn = nc.dram_tensor("dbg_" + nm, shp, dt_, kind="ExternalOutput").ap()
            outs.append(s.dma("sp", dtn, tl[:], r=[tk]))
    return outs


from concourse.bass_utils import run_bass_kernel_spmd

B_DENSE_IN = dict(oT=([8, 128, NT], BF16), xres=([NT, D], F32), w_out=([D, D], F32), lnp=([6, D], F32), wg=([D, 2816], F32), wu=([D, 2816], F32),
                  wd=([2816, D], F32), ple_gate=([D, D], F32), ple_proj=([256, D], F32), p=([NT, 256], F32), idn=([128, 128], F32))
B_MOE_IN = dict(oT=([8, 128, NT], BF16), xres=([NT, D], F32), w_out=([D, D], F32), lnp=([6, D], F32), router=([D, 8], F32),
                mg=([8, D, 3584], F32), mu=([8, D, 3584], F32), md=([8, 3584, D], F32), ple_gate=([D, D], F32), ple_proj=([256, D], F32),
                p=([NT, 256], F32), idn=([128, 128], F32), ut=([128, 128], F32), iota=([128, CAP], F32), pcol=([128, 16, 2], F32))


def _prog_A(layer):
    nc = bass.Bass("TRN2", target_bir_lowering=False)
    io = {k: dram(nc, k, shp) for k, shp in A_IN.items()}
    io["oT"] = dram(nc, "oT", [4, 128, T], BF16, kind="ExternalOutput")
    with contextlib.ExitStack() as st:
        s = Sched(nc)
        c = Ctx()
        outs = build_A(nc, st, s, c, io, layer)
        s.emit(final_ops=outs)
    return nc


def _prog_B(moe):
    nc = bass.Bass("TRN2", target_bir_lowering=False)
    spec = B_MOE_IN if moe else B_DENSE_IN
    io = {k: dram(nc, k, shp, dt) for k, (shp, dt) in spec.items()}
    io["out"] = dram(nc, "out", [NT, D], kind="ExternalOutput")
    with contextlib.ExitStack() as st:
        s = Sched(nc)
        c = Ctx()
        setup_psum(nc, st, c)
        outs = (build_B_moe if moe else build_B_dense)(nc, st, s, c, io)
        s.emit(final_ops=outs)
    return nc


def _feat_perm(sidx):
    return np.concatenate([np.arange(128 * sidx, 128 * sidx + 128), 256 + np.arange(128 * sidx, 128 * sidx + 128),
                           512 + np.arange(256 * sidx, 256 * sidx + 256)])


def kernel_unfused(**inputs):
    inp = {k: np.asarray(v) for k, v in inputs.items()}
    x = np.ascontiguousarray(inp["x"], dtype=np.float32)
    nb = x.shape[0]
    cores = list(range(2 * nb))
    idn = np.eye(128, dtype=np.float32)
    ut = (np.arange(128)[:, None] < np.arange(128)[None, :]).astype(np.float32)
    iota = np.tile(np.arange(CAP, dtype=np.float32)[None, :], (128, 1))
    wperm = np.concatenate([_feat_perm(0), _feat_perm(1)])
    consts = [host_consts(0), host_consts(1)]
    for L in range(2):
        packs = [host_w(inp["w_in"][L], s_) for s_ in range(2)]
        smalls = [host_small(inp, L, s_) for s_ in range(2)]
        in_maps = []
        for cid in cores:
            b, s_ = cid // 2, cid % 2
            m = dict(x=np.ascontiguousarray(x[b]), w=packs[s_])
            m.update(consts[s_])
            m.update(smalls[s_])
            in_maps.append(m)
        res = run_bass_kernel_spmd(_prog_A(L), in_maps, core_ids=cores)
        oT = [np.asarray(r["oT"]) for r in res.results]
        moe = (L % 2 == 1)
        lnp = np.stack([inp["ln1_g"][L], inp["ln1_b"][L], inp["ln2_g"][L], inp["ln2_b"][L], inp["ln3_g"][L], inp["ln3_b"][L]]).astype(np.float32)
        w_out = np.ascontiguousarray(inp["w_out"][L][wperm, :])
        in_maps = []
        for cid in cores:
            b, h = cid // 2, cid % 2
            tk = slice(h * NT, (h + 1) * NT)
            oTB = np.ascontiguousarray(np.concatenate([oT[2 * b][:, :, tk], oT[2 * b + 1][:, :, tk]], axis=0))
            m = dict(oT=oTB, xres=np.ascontiguousarray(x[b, tk]), w_out=w_out, lnp=lnp, ple_gate=inp["ple_gate"][L], ple_proj=inp["ple_proj"][L],
                     p=np.ascontiguousarray(inp["p"][L, b, tk]), idn=idn)
            if moe:
                m.update(router=inp["moe_router"][L // 2], mg=inp["moe_w_gate"][L // 2], mu=inp["moe_w_up"][L // 2], md=inp["moe_w_down"][L // 2], ut=ut, iota=iota)
            else:
                m.update(wg=inp["ffn_w_gate"][L // 2], wu=inp["ffn_w_up"][L // 2], wd=inp["ffn_w_down"][L // 2])
            in_maps.append(m)
        res = run_bass_kernel_spmd(_prog_B(moe), in_maps, core_ids=cores)
        xn = np.empty_like(x)
        for cid in cores:
            b, h = cid // 2, cid % 2
            xn[b, h * NT:(h + 1) * NT] = np.asarray(res.results[cid]["out"], dtype=np.float32)
        x = xn
    return x


A_LAYER_KEYS = ("w", "w1", "w2", "pos", "dl", "subln", "sinks")
A_SHARED_KEYS = ("masks", "idn", "i30k", "ab", "cb", "ov", "emat", "ta", "tb", "ri", "gs")
B_COMMON = dict(w_out=([D, D], F32), lnp=([6, D], F32), ple_gate=([D, D], F32), ple_proj=([256, D], F32), p=([NT, 256], F32))


def _prog_fused(nlayers=2):
    PHASE[0] = 0
    Sched.NSCHED = 0
    nc = bass.Bass("TRN2", target_bir_lowering=False)
    ext = {}

    def inp(name, shp, dt=F32):
        ext[name] = dram(nc, name, shp, dt)
        return ext[name]

    x0 = inp("x0", [T, D])
    xres0 = inp("xres0", [NT, D])
    hsel = inp("hsel", [1, 2])
    shared = {k: inp(k, A_IN[k]) for k in A_SHARED_KEYS}
    perA = [{k: inp("%s_%d" % (k, L), A_IN[k]) for k in A_LAYER_KEYS} for L in range(2)]
    perB = [{k: inp("%s_%d" % (k, L), shp, dt) for k, (shp, dt) in B_COMMON.items()} for L in range(2)]
    dense = dict(wg=inp("wg", [D, 2816]), wu=inp("wu", [D, 2816]), wd=inp("wd", [2816, D]))
    moe = dict(router=inp("router", [D, 8]), mg=inp("mg", [8, D, 3584]), mu=inp("mu", [8, D, 3584]), md=inp("md", [8, 3584, D]),
               ut=inp("ut", [128, 128]), iota=inp("iota", [128, CAP]), pcol=inp("pcol", [128, 16, 2]))
    out = dram(nc, "out", [NT, D], kind="ExternalOutput")
    oTown = [dram(nc, "oTown%d" % L, [2, 512, NT], BF16, kind="Internal") for L in range(2)]
    oTg = [dram(nc, "oTg%d" % L, [2, 1024, NT], BF16, kind="Internal") for L in range(2)]
    xh = dram(nc, "xh", [NT, D], kind="Internal")
    xg = dram(nc, "xg", [4, 1024, D], kind="Internal")
    for L in range(nlayers):
        with nc.cleanup_on_exit():
            with contextlib.ExitStack() as st:
                PHASE[0] += 1
                s = Sched(nc)
                c = Ctx()
                io = dict(shared)
                io.update(perA[L])
                io["oTh"] = oTown[L]
                io["oTg"] = oTg[L]
                if L == 0:
                    io["x"] = x0
                else:
                    io["xchunk"] = lambda tc: xg[tc % 4][(tc // 4) * 512:(tc // 4 + 1) * 512, :]
                outs = build_A(nc, st, s, c, io, L)
                s.emit(final_ops=outs)
            nc.all_engine_barrier()
        with nc.cleanup_on_exit():
            with contextlib.ExitStack() as st:
                PHASE[0] += 1
                s = Sched(nc)
                c = Ctx()
                setup_psum(nc, st, c)
                io = dict(perB[L])
                io.update(idn=shared["idn"], hsel=hsel, oTown=oTown[L], oTg=oTg[L])
                io["xres"] = xres0 if L == 0 else xh
                io["out"] = xh if L < nlayers - 1 else out
                if L < nlayers - 1:
                    io["xg"] = xg
                if L % 2 == 0:
                    io.update(dense)
                    outs = build_B_dense(nc, st, s, c, io)
                else:
                    io.update(moe)
                    outs = build_B_moe(nc, st, s, c, io)
                s.emit(final_ops=outs)
            nc.all_engine_barrier()
    return nc


def kernel(**inputs):
    inp = {k: np.asarray(v) for k, v in inputs.items()}
    x = np.ascontiguousarray(inp["x"], dtype=np.float32)
    nb = x.shape[0]
    cores = list(range(2 * nb))
    idn = np.eye(128, dtype=np.float32)
    ut = (np.arange(128)[:, None] < np.arange(128)[None, :]).astype(np.float32)
    iota = np.tile(np.arange(CAP, dtype=np.float32)[None, :], (128, 1))
    pcol = np.stack([np.tile(np.arange(128, dtype=np.float32)[:, None], (1, 16)), np.tile(np.arange(16, dtype=np.float32)[None, :], (128, 1))], axis=-1)
    wperm = np.concatenate([_feat_perm(0), _feat_perm(1)])
    consts = [host_consts(0), host_consts(1)]
    packs = [[host_w(inp["w_in"][L], s_) for s_ in range(2)] for L in range(2)]
    smalls = [[host_small(inp, L, s_) for s_ in range(2)] for L in range(2)]
    lnps = [np.stack([inp["ln1_g"][L], inp["ln1_b"][L], inp["ln2_g"][L], inp["ln2_b"][L], inp["ln3_g"][L], inp["ln3_b"][L]]).astype(np.float32) for L in range(2)]
    w_outs = [np.ascontiguousarray(inp["w_out"][L][wperm, :]) for L in range(2)]
    in_maps = []
    for cid in cores:
        b, s_ = cid // 2, cid % 2
        tk = slice(s_ * NT, (s_ + 1) * NT)
        hs = np.zeros((1, 2), np.float32)
        hs[0, s_] = 1.0
        m = dict(x0=np.ascontiguousarray(x[b]), xres0=np.ascontiguousarray(x[b, tk]), hsel=hs)
        for k in A_SHARED_KEYS:
            m[k] = consts[s_][k]
        for L in range(2):
            m["w_%d" % L] = packs[L][s_]
            for k in A_LAYER_KEYS[1:]:
                m["%s_%d" % (k, L)] = smalls[L][s_][k]
            m["w_out_%d" % L] = w_outs[L]
            m["lnp_%d" % L] = lnps[L]
            m["ple_gate_%d" % L] = inp["ple_gate"][L]
            m["ple_proj_%d" % L] = inp["ple_proj"][L]
            m["p_%d" % L] = np.ascontiguousarray(inp["p"][L, b, tk])
        m.update(wg=inp["ffn_w_gate"][0], wu=inp["ffn_w_up"][0], wd=inp["ffn_w_down"][0], router=inp["moe_router"][0],
                 mg=inp["moe_w_gate"][0], mu=inp["moe_w_up"][0], md=inp["moe_w_down"][0], ut=ut, iota=iota, pcol=pcol)
        in_maps.append(m)
    res = run_bass_kernel_spmd(_prog_fused(), in_maps, core_ids=cores)
    out = np.empty_like(x)
    for cid in cores:
        b, s_ = cid // 2, cid % 2
        out[b, s_ * NT:(s_ + 1) * NT] = np.asarray(res.results[cid]["out"], dtype=np.float32)
    return out
```

```python
import contextlib, math
import numpy as np
import ml_dtypes
import concourse.bass as bass
import concourse.mybir as mybir

F32 = mybir.dt.float32
BF16 = mybir.dt.bfloat16
AF = mybir.ActivationFunctionType
ALU = mybir.AluOpType
AX = mybir.AxisListType

PHASE = [0]


def uname(name):
    return "%s_%d" % (name, PHASE[0])


ENGS = ("pe", "act", "dve", "pool", "sp")


class Tok:
    __slots__ = ("name", "w", "rs")

    def __init__(self, name=""):
        self.name = name
        self.w = None
        self.rs = []


class Op:
    __slots__ = ("eng", "fn", "deps", "dma", "sig", "needs", "gi", "inc")

    def __init__(self, eng, fn, dma):
        self.eng = eng
        self.fn = fn
        self.deps = set()
        self.dma = dma
        self.sig = None
        self.needs = False
        self.gi = 0
        self.inc = 16


class Sched:
    NDMA = 12
    NSCHED = 0

    def __init__(self, nc, same_engine_sync=True):
        self.nc = nc
        self.ops = []
        self.same = same_engine_sync

    def add(self, eng, fn, r=(), w=(), dma=False, inc=16):
        op = Op(eng, fn, dma)
        op.inc = inc
        op.gi = len(self.ops)
        for t in r:
            if t.w is not None:
                op.deps.add(t.w)
        for t in w:
            if t.w is not None:
                op.deps.add(t.w)
            for x in t.rs:
                op.deps.add(x)
        for t in r:
            t.rs.append(op)
        for t in w:
            t.w = op
            t.rs = []
        op.deps.discard(op)
        self.ops.append(op)
        return op

    def dma(self, q, out, in_, r=(), w=(), **kw):
        return self.add(q, lambda e: e.dma_start(out=out, in_=in_, **kw), r, w, dma=True)

    def emit(self, final_ops=()):
        nc = self.nc
        ops = self.ops
        for op in ops:
            for d in op.deps:
                if d.dma:
                    continue
                if d.eng != op.eng or (self.same and d.eng != "pe"):
                    d.needs = True
        for op in final_ops:
            op.needs = True
        cnt = {e: 0 for e in ENGS}
        dcnt = {e: [0] * self.NDMA for e in ENGS}
        drr = {e: 0 for e in ENGS}
        prev_dma_wait = {}
        ncoll = 0
        coll_keys = []
        for op in ops:
            if op.dma and op.inc != 16:
                ncoll += 1
                prev_dma_wait[op] = (op.eng, 0, 0)
                op.sig = (("k", op.eng, ncoll), op.inc)
                coll_keys.append(op.sig[0])
            elif op.dma:
                k = drr[op.eng]
                drr[op.eng] = (k + 1) % self.NDMA
                prev_dma_wait[op] = (op.eng, k, dcnt[op.eng][k])
                dcnt[op.eng][k] += op.inc
                op.sig = (("d", op.eng, k), dcnt[op.eng][k])
            elif op.needs:
                cnt[op.eng] += 1
                op.sig = (("c", op.eng), cnt[op.eng])
        per = {e: [o for o in ops if o.eng == e] for e in ENGS}
        used = [e for e in ENGS if per[e]]
        import contextlib
        with contextlib.ExitStack() as st:
            sems = {}
            for e in ENGS:
                if cnt[e] > 0:
                    sems[("c", e)] = nc.alloc_semaphore(name="c_%s_%d" % (e, Sched.NSCHED))
                for k in range(self.NDMA):
                    if dcnt[e][k] > 0:
                        sems[("d", e, k)] = nc.alloc_semaphore(name="d_%s_%d_%d" % (e, k, Sched.NSCHED))
            for key in coll_keys:
                sems[key] = nc.alloc_semaphore(name="k_%s_%d_%d" % (key[1], key[2], Sched.NSCHED))
            Sched.NSCHED += 1
            block = st.enter_context(nc.Block())

            def run(ename, eng):
                waited = {}
                for op in per[ename]:
                    need = {}
                    for d in op.deps:
                        if d.sig is None:
                            continue
                        if (not d.dma) and d.eng == ename and (ename == "pe" or not self.same):
                            continue
                        key, val = d.sig
                        if need.get(key, 0) < val:
                            need[key] = val
                    if op.dma:
                        _, k, v = prev_dma_wait[op]
                        if v > 0:
                            key = ("d", ename, k)
                            if need.get(key, 0) < v:
                                need[key] = v
                    for key, val in need.items():
                        if waited.get(key, 0) < val:
                            eng.wait_ge(sems[key], val)
                            waited[key] = val
                    ins = op.fn(eng)
                    if op.sig is not None:
                        key, val = op.sig
                        ins.then_inc(sems[key], op.inc if op.dma else 1)
                if ename == "sp":
                    for op in final_ops:
                        key, val = op.sig
                        if waited.get(key, 0) < val:
                            eng.wait_ge(sems[key], val)
                            waited[key] = val

            if per["sp"] or final_ops:
                block.sync(lambda e: run("sp", e))
            if per["pe"]:
                block.tensor(lambda e: run("pe", e))
            if per["act"]:
                block.scalar(lambda e: run("act", e))
            if per["dve"]:
                block.vector(lambda e: run("dve", e))
            if per["pool"]:
                block.gpsimd(lambda e: run("pool", e))
        return {e: len(per[e]) for e in ENGS}

NT = 2048
D = 1024
ALPHA = 4 ** 0.25
EPS = 1e-5


def dram(nc, name, shape, dt=F32, kind="ExternalInput"):
    return nc.dram_tensor(name, list(shape), dt, kind=kind).ap()


class Ctx:
    pass


def setup_psum(nc, st, c):
    c.PSB = [st.enter_context(nc.psum_tensor(uname("psb%d" % i), [128, 1024], F32)) for i in range(4)]
    c.TPS = [[Tok("ps%d_%d" % (i, h)) for h in range(2)] for i in range(4)]
    c.psrr = 0


def ps_full(c):
    i = c.psrr % 4
    c.psrr += 1
    return c.PSB[i], c.TPS[i]


def ps_half(c):
    if not hasattr(c, "hrr"):
        c.hrr = 0
    k = c.hrr % 8
    c.hrr += 1
    return c.PSB[k // 2][:, (k % 2) * 512:(k % 2 + 1) * 512], c.TPS[k // 2][k % 2]


RG_PAIRS = [[0, 1], [2, 3], [4, 5], [6, 7]]


def load_oT(nc, st, s, io, OT, tOT, H1, tH1):
    if "oTown" not in io:
        s.dma("sp", OT, io["oT"].rearrange("c p t -> p c t"), w=[tOT])
        return
    HS = st.enter_context(nc.sbuf_tensor(uname("HS"), [128, 2], F32)); tHS = Tok()
    s.dma("sp", HS[:], io["hsel"].partition_broadcast(128).rearrange("p a b -> p (a b)"), w=[tHS])
    for fc in range(8):
        s.dma("sp", OT[:, fc, :], io["oTg"][0][fc * 128:(fc + 1) * 128, :], w=[tOT])
        s.dma("sp", H1[:, fc, :], io["oTg"][1][fc * 128:(fc + 1) * 128, :], w=[tH1])
    for fc in range(8):
        s.add("dve", lambda e, fc=fc: e.tensor_scalar(H1[:, fc, :], H1[:, fc, :], HS[:, 1:2], None, ALU.mult), r=[tHS, tH1], w=[tH1])
        s.add("dve", lambda e, fc=fc: e.scalar_tensor_tensor(out=OT[:, fc, :], in0=OT[:, fc, :], scalar=HS[:, 0:1], in1=H1[:, fc, :], op0=ALU.mult, op1=ALU.add),
              r=[tHS, tH1, tOT], w=[tOT])


def layernorm_rows(s, c, src, tsrc, dst, tdst, G, Bt, tG):
    ST, tST = c.ST, c.tST
    s.add("dve", lambda e: e.bn_stats(out=ST[:, 0:6], in_=src[:, 0:512]), r=[tsrc], w=[tST])
    s.add("dve", lambda e: e.bn_stats(out=ST[:, 6:12], in_=src[:, 512:1024]), r=[tsrc], w=[tST])
    s.add("dve", lambda e: e.bn_aggr(out=ST[:, 12:14], in_=ST[:, 0:12]), r=[tST], w=[tST])
    s.add("act", lambda e: e.activation(out=ST[:, 15:16], in_=ST[:, 13:14], func=AF.Sqrt, bias=c.EPSC[:, 0:1]), r=[tST, c.tEPSC], w=[tST])
    s.add("dve", lambda e: e.reciprocal(out=ST[:, 14:15], in_=ST[:, 15:16]), r=[tST], w=[tST])
    s.add("dve", lambda e: e.tensor_scalar(dst, src, ST[:, 12:13], ST[:, 14:15], ALU.subtract, ALU.mult), r=[tsrc, tST], w=[tdst])
    s.add("dve", lambda e: e.tensor_tensor(out=dst, in0=dst, in1=G, op=ALU.mult), r=[tdst, tG], w=[tdst])
    s.add("dve", lambda e: e.tensor_tensor(out=dst, in0=dst, in1=Bt, op=ALU.add), r=[tdst, tG], w=[tdst])


def ln_ops(s, c, src, tsrc, dst, tdst, G, Bt, tG, ST, tST):
    return [
        lambda: s.add("dve", lambda e: e.bn_stats(out=ST[:, 0:6], in_=src[:, 0:512]), r=[tsrc], w=[tST]),
        lambda: s.add("dve", lambda e: e.bn_stats(out=ST[:, 6:12], in_=src[:, 512:1024]), r=[tsrc], w=[tST]),
        lambda: s.add("dve", lambda e: e.bn_aggr(out=ST[:, 12:14], in_=ST[:, 0:12]), r=[tST], w=[tST]),
        lambda: s.add("act", lambda e: e.activation(out=ST[:, 15:16], in_=ST[:, 13:14], func=AF.Sqrt, bias=c.EPSC[:, 0:1]), r=[tST, c.tEPSC], w=[tST]),
        lambda: s.add("dve", lambda e: e.reciprocal(out=ST[:, 14:15], in_=ST[:, 15:16]), r=[tST], w=[tST]),
        lambda: s.add("dve", lambda e: e.tensor_scalar(dst, src, ST[:, 12:13], ST[:, 14:15], ALU.subtract, ALU.mult), r=[tsrc, tST], w=[tdst]),
        lambda: s.add("dve", lambda e: e.tensor_tensor(out=dst, in0=dst, in1=G, op=ALU.mult), r=[tdst, tG], w=[tdst]),
        lambda: s.add("dve", lambda e: e.tensor_tensor(out=dst, in0=dst, in1=Bt, op=ALU.add), r=[tdst, tG], w=[tdst]),
    ]


def interleave(chains):
    for j in range(max(len(ch) for ch in chains)):
        for ch in chains:
            if j < len(ch):
                ch[j]()


def transpose_rows(s, c, src, tsrc, XT, tXT, col0, nchunk=8):
    for g0 in range(0, nchunk, 4):
        n = min(4, nchunk - g0)
        ps, tps = ps_half(c)
        for j in range(n):
            dc = g0 + j
            s.add("pe", lambda e, ps=ps, j=j, dc=dc: e.transpose(ps[:, j * 128:(j + 1) * 128], src[:, dc * 128:(dc + 1) * 128], c.IDN[:]),
                  r=[tsrc, c.tIDN], w=[tps])
        s.add("act", lambda e, ps=ps, g0=g0, n=n: e.activation(
            out=XT[:, g0:g0 + n, col0:col0 + 128], in_=ps[:, 0:n * 128].rearrange("p (a b) -> p a b", b=128), func=AF.Copy),
            r=[tps], w=[tXT])


def expert_ffn(s, c, XS, tXS, C, wg, wu, wd, dff, aT, taT, WG, tWG, WU, tWU, WD, tWD, out_cb, wd_preloaded=False):
    nfc = dff // 128
    ncb = dff // 256
    wg_v = wg.rearrange("(c p) f -> p c f", p=128)
    wu_v = wu.rearrange("(c p) f -> p c f", p=128)
    wd_v = wd.rearrange("(c p) n -> p c n", p=128)
    groups = [(g0, min(512, C - g0)) for g0 in range(0, C, 512)]
    for cb in range(ncb):
        b = cb % 2
        s.dma("pool", WG[b][:], wg_v[:, :, cb * 256:(cb + 1) * 256], w=[tWG[b]])
        s.dma("pool", WU[b][:], wu_v[:, :, cb * 256:(cb + 1) * 256], w=[tWU[b]])
        if not wd_preloaded:
            s.dma("pool", WD[:, 2 * cb:2 * cb + 2, :], wd_v[:, 2 * cb:2 * cb + 2, :], w=[tWD])
        for fl in range(2):
            fc = cb * 2 + fl
            for (g0, gn) in groups:
                pg, tpg = ps_half(c)
                pu, tpu = ps_half(c)
                for dc in range(8):
                    s.add("pe", lambda e, pg=pg, b=b, dc=dc, fl=fl, g0=g0, gn=gn: e.matmul(
                        pg[:, 0:gn], WG[b][:, dc, fl * 128:(fl + 1) * 128], XS[:, dc, g0:g0 + gn], start=(dc == 0), stop=(dc == 7)),
                        r=[tWG[b], tXS], w=[tpg])
                for dc in range(8):
                    s.add("pe", lambda e, pu=pu, b=b, dc=dc, fl=fl, g0=g0, gn=gn: e.matmul(
                        pu[:, 0:gn], WU[b][:, dc, fl * 128:(fl + 1) * 128], XS[:, dc, g0:g0 + gn], start=(dc == 0), stop=(dc == 7)),
                        r=[tWU[b], tXS], w=[tpu])
                k = c.sgrr % 2
                c.sgrr += 1
                SG, tSG = c.SG[k], c.tSG[k]
                s.add("act", lambda e, pg=pg, SG=SG, gn=gn: e.activation(out=SG[:, 0:gn], in_=pg[:, 0:gn], func=AF.Silu),
                      r=[tpg], w=[tSG])
                s.add("dve", lambda e, pu=pu, SG=SG, fc=fc, g0=g0, gn=gn: e.tensor_tensor(
                    out=aT[:, fc, g0:g0 + gn], in0=SG[:, 0:gn], in1=pu[:, 0:gn], op=ALU.mult),
                    r=[tSG, tpu], w=[taT])
    for sc in range(C // 128):
        py, tpy = ps_full(c)
        for nh in range(2):
            for fc in range(nfc):
                s.add("pe", lambda e, py=py, nh=nh, fc=fc, sc=sc: e.matmul(
                    py[:, nh * 512:(nh + 1) * 512], aT[:, fc, sc * 128:(sc + 1) * 128], WD[:, fc, nh * 512:(nh + 1) * 512],
                    start=(fc == 0), stop=(fc == nfc - 1)),
                    r=[taT, tWD], w=[tpy[nh]])
        out_cb(sc, py, tpy)


def build_B_dense(nc, st, s, c, io):
    DFF = 2816
    NFC = DFF // 128
    sb = lambda name, shape, dt=F32: st.enter_context(nc.sbuf_tensor(uname(name), list(shape), dt))
    c.IDN = sb("IDN", [128, 128]); c.tIDN = Tok("idn")
    c.ST = sb("ST", [128, 16]); c.tST = Tok("st")
    c.EPSC = sb("EPSC", [128, 1]); c.tEPSC = Tok("eps")
    s.add("dve", lambda e: e.memset(c.EPSC[:], EPS), w=[c.tEPSC])
    c.SG = [sb("SG%d" % i, [128, 512], BF16) for i in range(2)]; c.tSG = [Tok(), Tok()]; c.sgrr = 0
    BIG = sb("BIG", [128, NFC * 1024], BF16)
    OT = BIG[:, 0:8 * NT].rearrange("p (c t) -> p c t", t=NT); tBIG = Tok("big")
    aT = BIG[:, 0:NFC * 1024].rearrange("p (c t) -> p c t", t=1024)
    W16 = sb("W16", [128, 8, 1024], BF16); tW16 = Tok("w16")
    LNP = sb("LNP", [128, 2, 1024]); tLNP = Tok("lnp")
    XT = sb("XT", [128, 8, NT], BF16); tXT = Tok("xt")
    XR = [sb("XR%d" % i, [128, 1024]) for i in range(4)]; tXR = [Tok() for _ in range(4)]
    ST2 = [c.ST, sb("STb", [128, 16])]; tST2 = [c.tST, Tok("stb")]
    TT = [sb("TT%d" % i, [128, 1024]) for i in range(2)]; tTT = [Tok(), Tok()]
    WG = [sb("WG%d" % i, [128, 8, 256], BF16) for i in range(2)]; tWG = [Tok(), Tok()]
    WU = [sb("WU%d" % i, [128, 8, 256], BF16) for i in range(2)]; tWU = [Tok(), Tok()]
    WD = sb("WD", [128, NFC, 1024], BF16); tWD = Tok("wd")
    PP = sb("PP", [128, 2, 1024], BF16); tPP = Tok("pp")
    PTt = sb("PTt", [128, 2, NT], BF16); tPTt = Tok("ptt")
    PR = [sb("PR%d" % i, [128, 256]) for i in range(2)]; tPR = [Tok(), Tok()]
    xs1 = dram(nc, uname("xs1"), [NT, D], kind="Internal")
    xs2 = dram(nc, uname("xs2"), [NT, D], kind="Internal")
    txs1, txs2 = Tok("xs1"), Tok("xs2")

    def load_ln(k):
        s.dma("sp", LNP[:], io["lnp"][2 * k:2 * k + 2, :].partition_broadcast(128), w=[tLNP])

    s.dma("sp", c.IDN[:], io["idn"], w=[c.tIDN])
    load_oT(nc, st, s, io, OT, tBIG, XT, tXT)
    s.dma("pool", W16[:], io["w_out"].rearrange("(c p) n -> p c n", p=128), w=[tW16])
    load_ln(0)
    for ip in range(0, NT // 128, 2):
        chains = []
        for i in (ip, ip + 1):
            k = i % 2
            k4 = i % 4
            s.dma("sp", XR[k4][:], io["xres"][i * 128:(i + 1) * 128, :], w=[tXR[k4]])
            py, tpy = ps_full(c)
            for nh in range(2):
                for fc in range(8):
                    s.add("pe", lambda e, py=py, nh=nh, fc=fc, i=i: e.matmul(
                        py[:, nh * 512:(nh + 1) * 512], OT[:, fc, i * 128:(i + 1) * 128], W16[:, fc, nh * 512:(nh + 1) * 512],
                        start=(fc == 0), stop=(fc == 7)), r=[tBIG, tW16], w=[tpy[nh]])
            ops = [lambda py=py, tpy=tpy, k=k, k4=k4: s.add("dve", lambda e: e.scalar_tensor_tensor(out=TT[k][:], in0=XR[k4][:], scalar=ALPHA, in1=py[:], op0=ALU.mult, op1=ALU.add),
                                                            r=[tXR[k4], tpy[0], tpy[1]], w=[tTT[k]])]
            ops += ln_ops(s, c, TT[k][:], tTT[k], XR[k4][:], tXR[k4], LNP[:, 0, :], LNP[:, 1, :], tLNP, ST2[k], tST2[k])
            ops.append(lambda i=i, k4=k4: s.dma("sp", xs1[i * 128:(i + 1) * 128, :], XR[k4][:], r=[tXR[k4]], w=[txs1]))
            chains.append(ops)
        interleave(chains)
        if ip >= 2:
            for i in (ip - 2, ip - 1):
                transpose_rows(s, c, XR[i % 4][:], tXR[i % 4], XT, tXT, i * 128)
    for i in (NT // 128 - 2, NT // 128 - 1):
        transpose_rows(s, c, XR[i % 4][:], tXR[i % 4], XT, tXT, i * 128)
    load_ln(1)
    s.dma("pool", W16[:], io["ple_gate"].rearrange("(c p) n -> p c n", p=128), w=[tW16])
    s.dma("pool", PP[:], io["ple_proj"].rearrange("(c p) n -> p c n", p=128), w=[tPP])
    for grp in range(NT // 1024):
        cb_chains = []

        def out_cb(sc, py, tpy, grp=grp):
            i = grp * 8 + sc
            k = i % 2
            s.dma("sp", XR[k][:], xs1[i * 128:(i + 1) * 128, :], r=[txs1], w=[tXR[k]])
            ops = [lambda: s.add("dve", lambda e: e.scalar_tensor_tensor(out=TT[k][:], in0=XR[k][:], scalar=ALPHA, in1=py[:], op0=ALU.mult, op1=ALU.add),
                                 r=[tXR[k], tpy[0], tpy[1]], w=[tTT[k]])]
            ops += ln_ops(s, c, TT[k][:], tTT[k], XR[k][:], tXR[k], LNP[:, 0, :], LNP[:, 1, :], tLNP, ST2[k], tST2[k])
            ops.append(lambda: s.dma("sp", xs2[i * 128:(i + 1) * 128, :], XR[k][:], r=[tXR[k]], w=[txs2]))
            cb_chains.append(ops)
            if sc % 2 == 1:
                interleave(cb_chains)
                del cb_chains[:]

        expert_ffn(s, c, XT[:, :, grp * 1024:(grp + 1) * 1024], tXT, 1024, io["wg"], io["wu"], io["wd"], DFF,
                   aT, tBIG, WG, tWG, WU, tWU, WD, tWD, out_cb, wd_preloaded=(grp > 0))
    for i in range(NT // 128):
        k = i % 2
        s.dma("sp", XR[k][:], xs2[i * 128:(i + 1) * 128, :], r=[txs2], w=[tXR[k]])
        transpose_rows(s, c, XR[k][:], tXR[k], XT, tXT, i * 128)
        s.dma("sp", PR[k][:], io["p"][i * 128:(i + 1) * 128, :], w=[tPR[k]])
        transpose_rows(s, c, PR[k][:], tPR[k], PTt, tPTt, i * 128, nchunk=2)
    load_ln(2)
    outs = []
    touts = []
    chains = []
    for i in range(NT // 128):
        k = i % 2
        s.dma("sp", XR[k][:], xs2[i * 128:(i + 1) * 128, :], r=[txs2], w=[tXR[k]])
        pg, tpg = ps_full(c)
        pe_, tpe = ps_full(c)
        for nh in range(2):
            for dc in range(8):
                s.add("pe", lambda e, pg=pg, nh=nh, dc=dc, i=i: e.matmul(
                    pg[:, nh * 512:(nh + 1) * 512], XT[:, dc, i * 128:(i + 1) * 128], W16[:, dc, nh * 512:(nh + 1) * 512],
                    start=(dc == 0), stop=(dc == 7)), r=[tXT, tW16], w=[tpg[nh]])
            for dc in range(2):
                s.add("pe", lambda e, pe_=pe_, nh=nh, dc=dc, i=i: e.matmul(
                    pe_[:, nh * 512:(nh + 1) * 512], PTt[:, dc, i * 128:(i + 1) * 128], PP[:, dc, nh * 512:(nh + 1) * 512],
                    start=(dc == 0), stop=(dc == 1)), r=[tPTt, tPP], w=[tpe[nh]])
        ops = [
            lambda pg=pg, tpg=tpg, k=k: s.add("act", lambda e: e.activation(out=TT[k][:], in_=pg[:], func=AF.Sigmoid), r=[tpg[0], tpg[1]], w=[tTT[k]]),
            lambda pe_=pe_, tpe=tpe, k=k: s.add("dve", lambda e: e.tensor_tensor(out=TT[k][:], in0=TT[k][:], in1=pe_[:], op=ALU.mult),
                                               r=[tTT[k], tpe[0], tpe[1]], w=[tTT[k]]),
            lambda k=k: s.add("dve", lambda e: e.scalar_tensor_tensor(out=TT[k][:], in0=XR[k][:], scalar=ALPHA, in1=TT[k][:], op0=ALU.mult, op1=ALU.add),
                              r=[tXR[k], tTT[k]], w=[tTT[k]]),
        ]
        ops += ln_ops(s, c, TT[k][:], tTT[k], XR[k][:], tXR[k], LNP[:, 0, :], LNP[:, 1, :], tLNP, ST2[k], tST2[k])

        def store_out(i=i, k=k):
            touts.append(Tok())
            outs.append(s.dma("sp", io["out"][i * 128:(i + 1) * 128, :], XR[k][:], r=[tXR[k]], w=[touts[-1]]))
        ops.append(store_out)
        if "xg" in io and i % 4 == 3:
            def xchg(j=i // 4):
                outs.append(s.add("pool", lambda e: e.collective_compute("AllGather", ALU.bypass, replica_groups=RG_PAIRS,
                                                                         ins=[io["out"][j * 512:(j + 1) * 512, :].opt()], outs=[io["xg"][j].opt()]),
                                  r=touts[-4:], w=[Tok()], dma=True, inc=1))
            ops.append(xchg)
        chains.append(ops)
        if i % 2 == 1:
            interleave(chains)
            chains = []
    return outs


CAP = 640
NSC = CAP // 128


def build_B_moe(nc, st, s, c, io):
    DFF = 3584
    NFC = DFF // 128
    sb = lambda name, shape, dt=F32: st.enter_context(nc.sbuf_tensor(uname(name), list(shape), dt))
    c.IDN = sb("IDN", [128, 128]); c.tIDN = Tok("idn")
    IDNb = sb("IDNb", [128, 128], BF16); tIDNb = Tok()
    UT = sb("UT", [128, 128], BF16); ONESb = sb("ONESb", [128, 128], BF16); tUT = Tok()
    IOTA = sb("IOTA", [128, CAP]); tIOTA = Tok()
    c.ST = sb("ST", [128, 16]); c.tST = Tok("st")
    c.EPSC = sb("EPSC", [128, 1]); c.tEPSC = Tok("eps")
    ST2 = [c.ST, sb("STb", [128, 16])]; tST2 = [c.tST, Tok("stb")]
    s.add("dve", lambda e: e.memset(c.EPSC[:], EPS), w=[c.tEPSC])
    BIG = sb("BIG", [128, NFC * CAP], BF16); tBIG = Tok("big")
    OT = BIG[:, 0:8 * NT].rearrange("p (c t) -> p c t", t=NT)
    aT = BIG[:, 0:NFC * CAP].rearrange("p (c t) -> p c t", t=CAP)
    W16 = sb("W16", [128, 16 * CAP], BF16); tW16 = Tok("w16")
    WO = W16[:, 0:8 * 1024].rearrange("p (c n) -> p c n", n=1024)
    SE = W16[:, 0:16 * CAP].rearrange("p (i s) -> p i s", s=CAP)
    LNP = sb("LNP", [128, 2, 1024]); tLNP = Tok("lnp")
    XTt = sb("XT", [128, 8 * NT], BF16); tXT = Tok("xt")
    XT = XTt[:, :].rearrange("p (c t) -> p c t", t=NT)
    X1B = XTt[:, :].rearrange("p (i d) -> p i d", d=1024)
    XR = [sb("XR%d" % i, [128, 1024]) for i in range(2)]; tXR = [Tok(), Tok()]
    TT = [sb("TT%d" % i, [128, 1024]) for i in range(2)]; tTT = [Tok(), Tok()]
    WG = [sb("WG%d" % i, [128, 8, 256], BF16) for i in range(2)]; tWG = [Tok(), Tok()]
    WU = [sb("WU%d" % i, [128, 8, 256], BF16) for i in range(2)]; tWU = [Tok(), Tok()]
    WD = sb("WD", [128, NFC, 1024], BF16); tWD = Tok("wd")
    PP = BIG[:, 0:2048].rearrange("p (c n) -> p c n", n=1024); tPP = tBIG
    Of1 = sb("Of1", [128, 16, 8]); BASE = sb("BASE", [128, 16, 8]); IDXF = sb("IDXF", [128, 16, 2]); IDXU = sb("IDXU", [128, 16, 2], mybir.dt.uint32); tIDX = Tok()
    XGt = sb("XG", [128, 8 * CAP], BF16); tXG = Tok("xg")
    XGs = [XGt[:, :].rearrange("p (c s) -> p c s", s=CAP), XTt[:, 0:8 * CAP].rearrange("p (c s) -> p c s", s=CAP)]
    YEs = [XGt[:, 0:NSC * 1024].rearrange("p (s n) -> p s n", n=1024), XTt[:, 0:NSC * 1024].rearrange("p (s n) -> p s n", n=1024)]
    tXGs = [tXG, tXT]
    XG = XGs[0]
    PTt = XGt[:, 0:2 * NT].rearrange("p (c t) -> p c t", t=NT)
    PR = [TT[i][:, 0:256] for i in range(2)]; tPR = tTT
    XTr = sb("XTr", [128, 8, 128], BF16); tXTr = Tok()
    c.SG = [XTr[:, 0:4, :].rearrange("p a b -> p (a b)"), XTr[:, 4:8, :].rearrange("p a b -> p (a b)")]; c.tSG = [Tok(), Tok()]; c.sgrr = 0
    WR = sb("WR", [128, 8, 8], BF16); tWR = Tok()
    RT = sb("RT", [128, 64]); tRT = Tok()
    Af = sb("Af", [128, 16, 8]); Ab = sb("Ab", [128, 16, 8], BF16); Gb = sb("Gb", [128, 16, 8], BF16); tAG = Tok()
    RKM = sb("RKM", [128, 16, 8]); tRKM = Tok()
    GSLs = [sb("GSL%d" % i, [128, NSC, 4]) for i in range(2)]; tGSLs = [Tok(), Tok()]
    G3 = sb("G3", [128, 16, 3], BF16); tG3 = Tok()
    TIDX = sb("TIDX", [128, 8]); TIDU = sb("TIDU", [128, 8], mybir.dt.uint32); tTID = Tok()
    YE = XGt[:, 0:NSC * 1024].rearrange("p (s n) -> p s n", n=1024); tYE = tXG
    xs1 = dram(nc, uname("xs1"), [NT, D], kind="Internal")
    xs2 = dram(nc, uname("xs2"), [NT, D], kind="Internal")
    yed = dram(nc, uname("yed"), [8 * CAP, D], BF16, kind="Internal")
    tyed = Tok("yed")
    txs1, txs2 = Tok("xs1"), Tok("xs2")

    def load_ln(k):
        s.dma("sp", LNP[:], io["lnp"][2 * k:2 * k + 2, :].partition_broadcast(128), w=[tLNP])

    s.dma("sp", c.IDN[:], io["idn"], w=[c.tIDN])
    s.dma("pool", IDNb[:], io["idn"], w=[tIDNb])
    s.dma("pool", UT[:], io["ut"], w=[tUT])
    s.add("pool", lambda e: e.memset(ONESb[:], 1.0), w=[tUT])
    s.dma("sp", IOTA[:], io["iota"], w=[tIOTA])
    s.dma("pool", G3[:, :, 1:3], io["pcol"], w=[tG3])
    load_oT(nc, st, s, io, OT, tBIG, XT, tXT)
    s.dma("pool", WO, io["w_out"].rearrange("(c p) n -> p c n", p=128), w=[tW16])
    s.dma("pool", WR[:], io["router"].rearrange("(c p) n -> p c n", p=128), w=[tWR])
    load_ln(0)
    def route_tile(i, k):
        transpose_rows(s, c, XR[k][:], tXR[k], XTr, tXTr, 0)
        pl, tpl = ps_half(c)
        for dc in range(8):
            s.add("pe", lambda e, pl=pl, dc=dc: e.matmul(pl[:, 0:8], XTr[:, dc, :], WR[:, dc, :], start=(dc == 0), stop=(dc == 7)), r=[tXTr, tWR], w=[tpl])
        LG = RT[:, 0:8]; MX8 = RT[:, 8:16]; OH1 = RT[:, 16:24]; OH2 = RT[:, 24:32]; DD = RT[:, 32:33]; G2 = RT[:, 33:34]; G1 = RT[:, 34:35]; GF = RT[:, 40:48]
        s.add("dve", lambda e, pl=pl: e.tensor_copy(out=LG, in_=pl[:, 0:8]), r=[tpl], w=[tRT])
        s.add("dve", lambda e: e.max(out=MX8, in_=LG), r=[tRT], w=[tRT])
        s.add("dve", lambda e: e.tensor_scalar(OH1, LG, RT[:, 8:9], None, ALU.is_equal), r=[tRT], w=[tRT])
        s.add("dve", lambda e: e.tensor_scalar(OH2, LG, RT[:, 9:10], None, ALU.is_equal), r=[tRT], w=[tRT])
        s.add("dve", lambda e: e.tensor_tensor(out=DD, in0=RT[:, 9:10], in1=RT[:, 8:9], op=ALU.subtract), r=[tRT], w=[tRT])
        s.add("act", lambda e: e.activation(out=G2, in_=DD, func=AF.Sigmoid), r=[tRT], w=[tRT])
        s.add("dve", lambda e: e.tensor_scalar(G1, G2, -1.0, 1.0, ALU.mult, ALU.add), r=[tRT], w=[tRT])
        s.add("dve", lambda e, i=i: e.tensor_copy(out=Of1[:, i, :], in_=OH1), r=[tRT], w=[tAG])
        s.add("dve", lambda e, i=i: e.tensor_tensor(out=Af[:, i, :], in0=OH1, in1=OH2, op=ALU.add), r=[tRT], w=[tAG])
        s.add("dve", lambda e, i=i: e.tensor_copy(out=Ab[:, i, :], in_=Af[:, i, :]), r=[tAG], w=[tAG])
        s.add("dve", lambda e: e.tensor_scalar(GF, OH1, G1, None, ALU.mult), r=[tRT], w=[tRT])
        s.add("dve", lambda e, i=i: e.scalar_tensor_tensor(out=Gb[:, i, :], in0=OH2, scalar=G2, in1=GF, op0=ALU.mult, op1=ALU.add), r=[tRT], w=[tAG])

    for i in range(NT // 128):
        k = i % 2
        s.dma("sp", XR[k][:], io["xres"][i * 128:(i + 1) * 128, :], w=[tXR[k]])
        py, tpy = ps_full(c)
        for nh in range(2):
            for fc in range(8):
                s.add("pe", lambda e, py=py, nh=nh, fc=fc, i=i: e.matmul(
                    py[:, nh * 512:(nh + 1) * 512], OT[:, fc, i * 128:(i + 1) * 128], WO[:, fc, nh * 512:(nh + 1) * 512],
                    start=(fc == 0), stop=(fc == 7)), r=[tBIG, tW16], w=[tpy[nh]])
        s.add("dve", lambda e, py=py, k=k: e.scalar_tensor_tensor(out=TT[k][:], in0=XR[k][:], scalar=ALPHA, in1=py[:], op0=ALU.mult, op1=ALU.add),
              r=[tXR[k], tpy[0], tpy[1]], w=[tTT[k]])
        layernorm_rows(s, c, TT[k][:], tTT[k], XR[k][:], tXR[k], LNP[:, 0, :], LNP[:, 1, :], tLNP)
        s.dma("sp", xs1[i * 128:(i + 1) * 128, :], XR[k][:], r=[tXR[k]], w=[txs1])
        if i >= 1:
            route_tile(i - 1, (i - 1) % 2)
    route_tile(15, 15 % 2)
    pr, tpr = ps_half(c)
    first = True
    for i in range(16):
        for j in range(i + 1):
            s.add("pe", lambda e, pr=pr, i=i, j=j, st_=first: e.matmul(pr[:, i * 8:(i + 1) * 8], (UT if j == i else ONESb)[:], Ab[:, j, :],
                                                                      start=st_, stop=(j == i), skip_group_check=True), r=[tUT, tAG], w=[tpr])
            first = False
    s.add("dve", lambda e, pr=pr: e.scalar_tensor_tensor(out=RKM[:], in0=pr[:, 0:128].rearrange("p (i e) -> p i e", e=8), scalar=1.0, in1=Af[:], op0=ALU.add, op1=ALU.mult),
          r=[tpr, tAG], w=[tRKM])
    s.add("dve", lambda e: e.tensor_scalar(RKM[:], RKM[:], -1.0, None, ALU.add), r=[tRKM], w=[tRKM])
    s.add("dve", lambda e: e.tensor_scalar(RT[:, 48:56], IOTA[:, 0:8], float(CAP), None, ALU.mult), r=[tIOTA, tRT], w=[tRT])
    s.add("dve", lambda e: e.tensor_tensor(out=BASE[:], in0=RKM[:], in1=RT[:, 48:56].unsqueeze(1).to_broadcast([128, 16, 8]), op=ALU.add), r=[tRKM, tRT], w=[tIDX])
    s.add("dve", lambda e: e.tensor_tensor(out=RKM[:], in0=Of1[:], in1=BASE[:], op=ALU.mult), r=[tAG, tIDX, tRKM], w=[tRKM])
    s.add("dve", lambda e: e.reduce_sum(out=IDXF[:, :, 0], in_=RKM[:], axis=AX.X), r=[tRKM], w=[tIDX])
    s.add("dve", lambda e: e.tensor_tensor(out=RKM[:], in0=Af[:], in1=Of1[:], op=ALU.subtract), r=[tAG, tIDX, tRKM], w=[tRKM])
    s.add("dve", lambda e: e.tensor_tensor(out=RKM[:], in0=RKM[:], in1=BASE[:], op=ALU.mult), r=[tIDX, tRKM], w=[tRKM])
    s.add("dve", lambda e: e.reduce_sum(out=IDXF[:, :, 1], in_=RKM[:], axis=AX.X), r=[tRKM], w=[tIDX])
    s.add("dve", lambda e: e.tensor_copy(out=IDXU[:], in_=IDXF[:]), r=[tIDX], w=[tIDX])
    s.add("dve", lambda e: e.scalar_tensor_tensor(out=RKM[:], in0=BASE[:], scalar=1.0, in1=Af[:], op0=ALU.add, op1=ALU.mult), r=[tIDX, tAG, tRKM], w=[tRKM])
    s.add("dve", lambda e: e.tensor_tensor(out=BASE[:], in0=Af[:], in1=RT[:, 48:56].unsqueeze(1).to_broadcast([128, 16, 8]), op=ALU.mult), r=[tAG, tRT, tIDX], w=[tIDX])
    s.add("dve", lambda e: e.tensor_tensor(out=RKM[:], in0=RKM[:], in1=BASE[:], op=ALU.subtract), r=[tIDX, tRKM], w=[tRKM])
    s.add("dve", lambda e: e.tensor_scalar(RKM[:], RKM[:], -1.0, None, ALU.add), r=[tRKM], w=[tRKM])
    load_ln(1)
    groups = [(0, 512), (512, CAP - 512)]

    def build_se(ex_):
        for i in range(16):
            s.add("dve", lambda e, i=i, ex_=ex_: e.tensor_scalar(SE[:, i, :], IOTA[:], RKM[:, i, ex_:ex_ + 1], None, ALU.is_equal), r=[tIOTA, tRKM], w=[tW16])


    LAND = [TT[0], TT[1], XR[0], XR[1]]; tLAND = [tTT[0], tTT[1], tXR[0], tXR[1]]

    def gather_prep(ex):
        s.add("dve", lambda e, ex=ex: e.tensor_copy(out=G3[:, :, 0], in_=Gb[:, :, ex]), r=[tAG, tG3], w=[tG3])
        pgs, tpgs = ps_half(c)
        first = True
        for sc in range(NSC):
            for i in range(16):
                s.add("pe", lambda e, pgs=pgs, sc=sc, i=i, st_=first: e.matmul(pgs[:, sc * 4:sc * 4 + 3], SE[:, i, sc * 128:(sc + 1) * 128], G3[:, i, :],
                                                                              start=st_, stop=(i == 15), skip_group_check=True), r=[tW16, tG3], w=[tpgs])
                first = False
        s.add("dve", lambda e, pgs=pgs: e.tensor_copy(out=GSLs[ex % 2][:], in_=pgs[:, 0:NSC * 4].rearrange("p (s k) -> p s k", k=4)), r=[tpgs], w=[tGSLs[ex % 2]])
        s.add("dve", lambda e: e.scalar_tensor_tensor(out=TIDX[:, 0:NSC], in0=GSLs[ex % 2][:, :, 2], scalar=128.0, in1=GSLs[ex % 2][:, :, 1], op0=ALU.mult, op1=ALU.add),
              r=[tGSLs[ex % 2]], w=[tTID])
        s.add("dve", lambda e: e.tensor_copy(out=TIDU[:, 0:NSC], in_=TIDX[:, 0:NSC]), r=[tTID], w=[tTID])
        for sc in range(min(NSC, 4)):
            s.add("pool", lambda e, sc=sc: e.indirect_dma_start(out=LAND[sc][:], out_offset=None, in_=xs1,
                                                               in_offset=bass.IndirectOffsetOnAxis(ap=TIDU[:, sc:sc + 1], axis=0)),
                  r=[tTID, txs1], w=[tLAND[sc]], dma=True)

    def gather_fin(ex):
        XGd, tXGd = XGs[ex % 2], tXGs[ex % 2]
        for sc in range(NSC):
            if sc >= 4:
                s.add("pool", lambda e, sc=sc: e.indirect_dma_start(out=LAND[sc % 4][:], out_offset=None, in_=xs1,
                                                                   in_offset=bass.IndirectOffsetOnAxis(ap=TIDU[:, sc:sc + 1], axis=0)),
                      r=[tTID, txs1], w=[tLAND[sc % 4]], dma=True)
            transpose_rows(s, c, LAND[sc % 4][:], tLAND[sc % 4], XGd, tXGd, sc * 128)

    build_se(0)
    gather_prep(0)
    gather_fin(0)
    for ex in range(8):
        wg_v = io["mg"][ex].rearrange("(c p) f -> p c f", p=128)
        wu_v = io["mu"][ex].rearrange("(c p) f -> p c f", p=128)
        wd_v = io["md"][ex].rearrange("(c p) n -> p c n", p=128)
        if ex < 7:
            build_se(ex + 1)
        for cb in range(DFF // 256):
            b = cb % 2
            s.dma("pool", WG[b][:], wg_v[:, :, cb * 256:(cb + 1) * 256], w=[tWG[b]])
            s.dma("pool", WU[b][:], wu_v[:, :, cb * 256:(cb + 1) * 256], w=[tWU[b]])
            s.dma("pool", WD[:, 2 * cb:2 * cb + 2, :], wd_v[:, 2 * cb:2 * cb + 2, :], w=[tWD])
            for fl in range(2):
                fc = cb * 2 + fl
                for (g0, gn) in groups:
                    pg, tpg = ps_half(c)
                    pu, tpu = ps_half(c)
                    for dc in range(8):
                        s.add("pe", lambda e, pg=pg, b=b, dc=dc, fl=fl, g0=g0, gn=gn, ex=ex: e.matmul(
                            pg[:, 0:gn], WG[b][:, dc, fl * 128:(fl + 1) * 128], XGs[ex % 2][:, dc, g0:g0 + gn], start=(dc == 0), stop=(dc == 7)),
                            r=[tWG[b], tXGs[ex % 2]], w=[tpg])
                    for dc in range(8):
                        s.add("pe", lambda e, pu=pu, b=b, dc=dc, fl=fl, g0=g0, gn=gn, ex=ex: e.matmul(
                            pu[:, 0:gn], WU[b][:, dc, fl * 128:(fl + 1) * 128], XGs[ex % 2][:, dc, g0:g0 + gn], start=(dc == 0), stop=(dc == 7)),
                            r=[tWU[b], tXGs[ex % 2]], w=[tpu])
                    k = c.sgrr % 2
                    c.sgrr += 1
                    SG, tSG = c.SG[k], c.tSG[k]
                    s.add("act", lambda e, pg=pg, SG=SG, gn=gn: e.activation(out=SG[:, 0:gn], in_=pg[:, 0:gn], func=AF.Silu), r=[tpg], w=[tSG])
                    s.add("dve", lambda e, pu=pu, SG=SG, fc=fc, g0=g0, gn=gn: e.tensor_tensor(
                        out=aT[:, fc, g0:g0 + gn], in0=SG[:, 0:gn], in1=pu[:, 0:gn], op=ALU.mult), r=[tSG, tpu], w=[tBIG])
        if ex < 7:
            gather_prep(ex + 1)
        for sc in range(NSC):
            py, tpy = ps_full(c)
            for nh in range(2):
                for fc in range(NFC):
                    s.add("pe", lambda e, py=py, fc=fc, sc=sc, nh=nh: e.matmul(py[:, nh * 512:(nh + 1) * 512], aT[:, fc, sc * 128:(sc + 1) * 128], WD[:, fc, nh * 512:(nh + 1) * 512],
                                                                            start=(fc == 0), stop=(fc == NFC - 1)), r=[tBIG, tWD], w=[tpy[nh]])
            s.add("dve", lambda e, py=py, sc=sc, ex=ex: e.tensor_scalar(YEs[ex % 2][:, sc, :], py[:], GSLs[ex % 2][:, sc, 0:1], None, ALU.mult),
                  r=[tpy[0], tpy[1], tGSLs[ex % 2]], w=[tXGs[ex % 2]])
        if ex < 7:
            gather_fin(ex + 1)
        s.dma("sp", yed[ex * CAP:(ex + 1) * CAP, :].rearrange("(s p) n -> p s n", p=128), YEs[ex % 2], r=[tXGs[ex % 2]], w=[tyed])
    RG4 = [[WG[k][:, 0:4, :].rearrange("p a b -> p (a b)"), WG[k][:, 4:8, :].rearrange("p a b -> p (a b)")] for k in range(2)]
    tRG4 = [[Tok(), Tok()], [Tok(), Tok()]]
    chains = []
    for i in range(16):
        k = i % 2
        rows = slice(i * 128, (i + 1) * 128)
        for j in range(2):
            s.add("pool", lambda e, i=i, j=j, k=k: e.indirect_dma_start(out=RG4[k][j], out_offset=None, in_=yed,
                                                                       in_offset=bass.IndirectOffsetOnAxis(ap=IDXU[:, i, j:j + 1], axis=0)),
                  r=[tIDX, tyed, tWG[k]], w=[tRG4[k][j]], dma=True)
        s.dma("sp", XR[k][:], xs1[rows, :], r=[txs1], w=[tXR[k]])
        ops = [
            lambda k=k: s.add("dve", lambda e: e.tensor_tensor(out=TT[k][:], in0=RG4[k][0], in1=RG4[k][1], op=ALU.add), r=[tRG4[k][0], tRG4[k][1]], w=[tTT[k]]),
            lambda k=k: s.add("dve", lambda e: e.scalar_tensor_tensor(out=TT[k][:], in0=XR[k][:], scalar=ALPHA, in1=TT[k][:], op0=ALU.mult, op1=ALU.add),
                              r=[tXR[k], tTT[k]], w=[tTT[k]]),
        ]
        ops += ln_ops(s, c, TT[k][:], tTT[k], XR[k][:], tXR[k], LNP[:, 0, :], LNP[:, 1, :], tLNP, ST2[k], tST2[k])
        ops.append(lambda k=k, rows=rows: s.dma("sp", xs2[rows, :], XR[k][:], r=[tXR[k]], w=[txs2]))
        ops.append(lambda k=k, i=i: transpose_rows(s, c, XR[k][:], tXR[k], XT, tXT, i * 128))
        chains.append(ops)
        if i % 2 == 1:
            interleave(chains)
            chains = []
    W16v = W16[:, 0:8 * 1024].rearrange("p (c n) -> p c n", n=1024)
    s.dma("pool", W16v, io["ple_gate"].rearrange("(c p) n -> p c n", p=128), w=[tW16])
    s.dma("pool", PP[:], io["ple_proj"].rearrange("(c p) n -> p c n", p=128), w=[tPP])
    for i in range(NT // 128):
        k = i % 2
        s.dma("sp", PR[k], io["p"][i * 128:(i + 1) * 128, :], w=[tPR[k]])
        transpose_rows(s, c, PR[k], tPR[k], PTt, tXG, i * 128, nchunk=2)
    load_ln(2)
    outs = []
    touts = []
    chains = []
    for i in range(NT // 128):
        k = i % 2
        s.dma("sp", XR[k][:], xs2[i * 128:(i + 1) * 128, :], r=[txs2], w=[tXR[k]])
        pg, tpg = ps_full(c)
        pe_, tpe = ps_full(c)
        for nh in range(2):
            for dc in range(8):
                s.add("pe", lambda e, pg=pg, nh=nh, dc=dc, i=i: e.matmul(
                    pg[:, nh * 512:(nh + 1) * 512], XT[:, dc, i * 128:(i + 1) * 128], W16v[:, dc, nh * 512:(nh + 1) * 512],
                    start=(dc == 0), stop=(dc == 7)), r=[tXT, tW16], w=[tpg[nh]])
            for dc in range(2):
                s.add("pe", lambda e, pe_=pe_, nh=nh, dc=dc, i=i: e.matmul(
                    pe_[:, nh * 512:(nh + 1) * 512], PTt[:, dc, i * 128:(i + 1) * 128], PP[:, dc, nh * 512:(nh + 1) * 512],
                    start=(dc == 0), stop=(dc == 1)), r=[tXG, tPP], w=[tpe[nh]])
        ops = [
            lambda pg=pg, tpg=tpg, k=k: s.add("act", lambda e: e.activation(out=TT[k][:], in_=pg[:], func=AF.Sigmoid), r=[tpg[0], tpg[1]], w=[tTT[k]]),
            lambda pe_=pe_, tpe=tpe, k=k: s.add("dve", lambda e: e.tensor_tensor(out=TT[k][:], in0=TT[k][:], in1=pe_[:], op=ALU.mult),
                                               r=[tTT[k], tpe[0], tpe[1]], w=[tTT[k]]),
            lambda k=k: s.add("dve", lambda e: e.scalar_tensor_tensor(out=TT[k][:], in0=XR[k][:], scalar=ALPHA, in1=TT[k][:], op0=ALU.mult, op1=ALU.add),
                              r=[tXR[k], tTT[k]], w=[tTT[k]]),
        ]
        ops += ln_ops(s, c, TT[k][:], tTT[k], XR[k][:], tXR[k], LNP[:, 0, :], LNP[:, 1, :], tLNP, ST2[k], tST2[k])

        def store_out(i=i, k=k):
            touts.append(Tok())
            outs.append(s.dma("sp", io["out"][i * 128:(i + 1) * 128, :], XR[k][:], r=[tXR[k]], w=[touts[-1]]))
        ops.append(store_out)
        if "xg" in io and i % 4 == 3:
            def xchg(j=i // 4):
                outs.append(s.add("pool", lambda e: e.collective_compute("AllGather", ALU.bypass, replica_groups=RG_PAIRS,
                                                                         ins=[io["out"][j * 512:(j + 1) * 512, :].opt()], outs=[io["xg"][j].opt()]),
                                  r=touts[-4:], w=[Tok()], dma=True, inc=1))
            ops.append(xchg)
        chains.append(ops)
        if i % 2 == 1:
            interleave(chains)
            chains = []
    return outs

T = 4096
NEG = -30000.0
NQT = 8
SL = [2.0 ** (-i / 2.0) for i in range(1, 17)]
SLOPE_C = SL[0:8]
SLOPE_A = [SL[8], SL[10], SL[12], SL[14]]
SLOPE_B = [SL[9], SL[11], SL[13], SL[15]]
C_KG0, C_KG1, C_KS, C_KW, C_KC, C_KB0, C_KB1 = 0, 128, 256, 320, 384, 448, 576
C_V = 704
C_QA = 1024
C_QB = 1280
C_QC = 1536
C_G = 1792
NW = 1798
M_CAUS, M_WM, M_SM, M_CM = 0, 4, 8, 13
NMASK = 18


def mask_tables():
    j = np.arange(128)[:, None]
    i = np.arange(512)[None, :]
    tabs = []
    for r in range(4):
        tabs.append((-128 * r + i - j) >= 0)
    for o in range(1, 5):
        dd = 128 * o + i - j
        tabs.append((dd >= 0) & (dd < 512))
    for o in range(-3, 2):
        dd = 128 * o + i - j
        tabs.append((dd >= 0) & (dd < 128))
    for m in range(5):
        tabs.append((i - 16 * j) >= (31 - 512 * m))
    return np.stack(tabs)


ALLOWED = mask_tables()


def host_consts(sidx):
    cst = {}
    cst["masks"] = np.where(ALLOWED, 0.0, NEG).astype(np.float32)
    cst["idn"] = np.eye(128, dtype=np.float32)
    cst["i30k"] = (np.eye(128) * 30000.0).astype(np.float32)
    own_a = [SLOPE_A[2 * sidx], SLOPE_A[2 * sidx + 1]]
    own_b = [SLOPE_B[2 * sidx], SLOPE_B[2 * sidx + 1]]
    own_c = SLOPE_C[4 * sidx:4 * sidx + 4]
    sl8 = own_a + own_b + list(own_c)
    p = np.arange(128)[:, None, None]
    oi = np.arange(32)[None, None, :]
    cst["ab"] = (np.array(sl8)[None, :, None] * (p - 128.0 * (oi - 3))).astype(np.float32)
    cc = np.arange(2)[None, None, :, None]
    qt = np.arange(8)[None, None, None, :]
    perm_a = [2 * sidx, 2 * sidx + 1, 2 * (1 - sidx), 2 * (1 - sidx) + 1]
    cst["cb"] = (np.array([SLOPE_A[h] for h in perm_a])[None, :, None, None] * (16.0 * p[:, :, :, None] + 31 + 2048 * cc - 512 * qt)).astype(np.float32).reshape(128, 64)
    n = np.arange(256)[:, None]
    jj = np.arange(64)[None, :]
    ov = np.clip(np.minimum(16 * n + 32, 64 * jj + 64) - np.maximum(16 * n, 64 * jj), 0, None) / 32.0
    ov[255] = 0.0
    cst["ov"] = ov.astype(np.float32).reshape(2, 128, 64)
    t = np.arange(T)
    cst["emat"] = (t[None, :] // 64 == np.arange(64)[:, None]).astype(np.float32)
    r = np.arange(-63, 64)[None, :]
    pp = np.arange(128)[:, None]
    ta = np.where(r <= -2, 1.0, np.where(r == -1, (pp >= 64) * 1.0, 0.0))
    tb = np.where(r <= -2, 0.0, np.where(r == -1, (pp < 64) * 1e9, np.where(r == 0, 1e9, np.where(r == 1, np.where(pp < 64, -1.0, 1e9), -1.0))))
    cst["ta"] = ta.astype(np.float32)
    cst["tb"] = tb.astype(np.float32)
    i512 = np.arange(512)
    ri = np.stack([(-s * i512).astype(np.float32).astype(ml_dtypes.bfloat16).astype(np.float32) for s in own_c])
    cst["ri"] = ri
    g = np.exp(ri.astype(np.float64) + np.array(own_c)[:, None] * i512[None, :])
    cst["gs"] = np.ascontiguousarray(g.reshape(4, 4, 128).transpose(2, 1, 0)).astype(np.float32)
    return cst


def host_w(w_in, sidx):
    W = np.zeros((D, NW), np.float32)
    kva = 256
    kc, vc, ks, vs, kw, vw = [w_in[:, kva + 64 * k: kva + 64 * k + 64] for k in range(6)]
    W[:, C_KG0:C_KG0 + 64] = kc; W[:, C_KG0 + 64:C_KG0 + 128] = kc
    W[:, C_KG1:C_KG1 + 64] = vc; W[:, C_KG1 + 64:C_KG1 + 128] = vc
    W[:, C_KS:C_KS + 64] = ks
    W[:, C_KW:C_KW + 64] = kw
    W[:, C_KC:C_KC + 64] = w_in[:, 1932 + 64 * sidx: 1932 + 64 * sidx + 64]
    for hh in range(2):
        h = 2 * sidx + hh
        base = C_KB0 + 128 * hh
        W[:, base:base + 32] = w_in[:, 908 + 64 * h: 908 + 64 * h + 32]
        W[:, base + 96:base + 128] = w_in[:, 908 + 64 * h + 32: 908 + 64 * h + 64]
        W[:, C_V + 128 + 64 * hh: C_V + 192 + 64 * hh] = w_in[:, 1164 + 64 * h: 1164 + 64 * h + 64]
        W[:, C_QB + 128 * hh: C_QB + 128 * hh + 64] = w_in[:, 652 + 64 * h: 652 + 64 * h + 64]
        W[:, C_QB + 128 * hh + 64: C_QB + 128 * hh + 128] = w_in[:, 652 + 64 * h: 652 + 64 * h + 64]
        W[:, C_G + 3 * hh: C_G + 3 * hh + 3] = w_in[:, 640 + 3 * h: 640 + 3 * h + 3]
    W[:, C_V:C_V + 64] = vs
    W[:, C_V + 64:C_V + 128] = vw
    W[:, C_V + 256:C_V + 320] = w_in[:, 2060 + 64 * sidx: 2060 + 64 * sidx + 64]
    perm_a = [2 * sidx, 2 * sidx + 1, 2 * (1 - sidx), 2 * (1 - sidx) + 1]
    for k_, h in enumerate(perm_a):
        W[:, C_QA + 64 * k_:C_QA + 64 * k_ + 64] = w_in[:, 64 * h:64 * h + 64]
    W[:, C_QC:C_QC + 256] = w_in[:, 1420 + 256 * sidx: 1420 + 256 * sidx + 256]
    return W


def host_small(inp, L, sidx):
    sm = {}
    w1 = inp["cmp_w1"][L]
    sm["w1"] = np.ascontiguousarray(w1.reshape(2, 16, 128, 128).transpose(2, 0, 1, 3)).reshape(128, 2 * 16 * 128)
    sm["w2"] = np.ascontiguousarray(inp["cmp_w2"][L].transpose(1, 0, 2)).reshape(128, 128)
    pos = inp["cmp_pos"][L]
    sm["pos"] = np.ascontiguousarray(pos.reshape(2, 16, 128).transpose(2, 0, 1)).reshape(128, 32)
    sm["dl"] = np.ascontiguousarray(inp["diff_lambda"][L].reshape(1, 128))
    sm["subln"] = np.ascontiguousarray(inp["diff_subln"][L].reshape(1, 64))
    sm["sinks"] = np.ascontiguousarray(inp["sinks"][L][4 * sidx:4 * sidx + 4].reshape(1, 4))
    return sm


A_IN = dict(x=[T, D], w=[D, NW], masks=[NMASK, 128, 512], idn=[128, 128], i30k=[128, 128], ab=[128, 8, 32], cb=[128, 64],
            ov=[2, 128, 64], emat=[64, T], ta=[128, 127], tb=[128, 127], ri=[4, 512], gs=[128, 4, 4],
            w1=[128, 4096], w2=[128, 128], pos=[128, 32], dl=[1, 128], subln=[1, 64], sinks=[1, 4])


def build_A(nc, st, s, c, io, layer):
    lam_init = 0.8 - 0.6 * math.exp(-0.3 * layer)
    sb = lambda name, shape, dt=F32: st.enter_context(nc.sbuf_tensor(uname(name), list(shape), dt))
    PSB = [st.enter_context(nc.psum_tensor(uname("psb%d" % i), [128, 1024], F32)) for i in range(4)]
    TPS = [[Tok("ps%d_%d" % (i, h)) for h in range(2)] for i in range(4)]
    rr = {"s": 0, "a": 0, "f": 0}

    def ps_score():
        k = rr["s"] % 4; rr["s"] += 1
        return PSB[k // 2][:, (k % 2) * 512:(k % 2 + 1) * 512], TPS[k // 2][k % 2]

    def ps_acc():
        k = rr["a"] % 4; rr["a"] += 1
        return PSB[2 + k // 2][:, (k % 2) * 512:(k % 2 + 1) * 512], TPS[2 + k // 2][k % 2]

    def ps_accfull():
        k = rr["f"] % 2; rr["f"] += 1
        rr["a"] = 0
        return PSB[2 + k], TPS[2 + k]

    W = sb("W", [128, 8, NW], BF16); tW = Tok("W")
    IDN = sb("IDN", [128, 128]); tIDN = Tok()
    IDNb = sb("IDNb", [128, 128], BF16); tIDNb = Tok()
    I30K = sb("I30K", [128, 128], BF16); tI30K = Tok()
    MK = sb("MK", [128, NMASK, 512], BF16); tMK = Tok()
    AB = sb("AB", [128, 8, 32]); tAB = Tok()
    CB = sb("CB", [128, 64]); tCB = Tok()
    TA = sb("TA", [128, 127]); TBt = sb("TBt", [128, 127]); tTAB = Tok()
    GS = sb("GS", [128, 4, 4]); tGS = Tok()
    SK = sb("SK", [128, 4, 4]); tSK = Tok()
    W1 = sb("W1", [128, 2, 16, 128], BF16); tW1 = Tok()
    W2 = sb("W2", [128, 2, 64], BF16); tW2 = Tok()
    POS = sb("POS", [128, 2, 16], BF16); tPOS = Tok()
    SM_ = sb("SMALL", [128, 256]); tSM = Tok()
    Kc2 = sb("Kc2", [128, 2, T], BF16); tKc2 = Tok()
    KS = sb("KS", [128, T], BF16); tKS = Tok()
    KW = sb("KW", [64, T], BF16); tKW = Tok()
    KC = sb("KC", [65, T], BF16); tKC = Tok()
    KB = [sb("KB%d" % i, [128, T], BF16) for i in range(2)]; tKB = [Tok(), Tok()]
    VALL = sb("VALL", [128, 32, 5, 65], BF16); tV = Tok()
    XC = sb("XC", [128, 4, 1024]); tXC = Tok()
    XTc = [sb("XTc%d" % i, [128, 8, 512], BF16) for i in range(2)]; tXTc = [Tok(), Tok()]
    HT = sb("HT", [128, 2, 256], BF16); tHT = Tok()
    BP = sb("BP", [128, 2]); tBP = Tok()
    KCT = sb("KCT", [64, 256], BF16); tKCT = Tok()
    RC = sb("RC", [128, 2, 129], BF16); tRC = Tok()
    QA = [[sb("QA%d_%d" % (b, h), [128, 512], BF16) for h in range(4)] for b in range(2)]
    tQA = [[Tok() for h in range(4)] for b in range(2)]
    QB = [[sb("QB%d_%d" % (b, h), [128, 512], BF16) for h in range(2)] for b in range(1)]
    tQB = [[Tok() for h in range(2)] for b in range(1)]
    QB.append(QB[0]); tQB.append(tQB[0])
    QC = [[sb("QC%d_%d" % (b, h), [65, 512], BF16) for h in range(4)] for b in range(1)]
    tQC = [[Tok() for h in range(4)] for b in range(1)]
    QC.append(QC[0]); tQC.append(tQC[0])
    GT = sb("GT", [128, 4, 6]); tGT = Tok()
    PTb = [sb("PT%d" % i, [128, 512], BF16) for i in range(4)]; tPT = [Tok() for _ in range(4)]
    ptrr = [0]
    IMP = sb("IMP", [128, 4, 64]); tIMP = Tok()
    IMP2 = sb("IMP2", [128, 4, 64]); tIMP2 = Tok()
    MX = sb("MX", [128, 4, 16]); tMX = Tok()
    NM = sb("NM", [128, 4, 128], BF16); tNM = Tok()
    REC = sb("REC", [128, 16]); tREC = Tok()
    OCc = sb("OCc", [128, 2, 4, 64]); tOCc = Tok()
    TMP = [sb("TMP%d" % i, [128, 4, 64]) for i in range(2)]; tTMP = [Tok(), Tok()]
    OCH = sb("OCH", [128, 4, 512]); tOCH = Tok()
    OTc = sb("OTc", [128, 4, 512], BF16); tOTc = Tok()

    q = "sp"
    xdeps = []
    if "xh" in io:
        txfs = [Tok("xfull%d" % j) for j in range(4)]
        for j in range(4):
            s.add("pool", lambda e, j=j: e.collective_compute("AllGather", ALU.bypass, replica_groups=[[0, 1], [2, 3], [4, 5], [6, 7]],
                                                              ins=[io["xh"][j * 512:(j + 1) * 512, :].opt()], outs=[io["xg"][j].opt()]),
                  r=[], w=[txfs[j]], dma=True, inc=1)
        xdeps = txfs
    s.dma("pool", W[:], io["w"].rearrange("(c p) n -> p c n", p=128), w=[tW])
    s.dma(q, IDN[:], io["idn"], w=[tIDN])
    s.dma("pool", IDNb[:], io["idn"], w=[tIDNb])
    s.dma("pool", I30K[:], io["i30k"], w=[tI30K])
    for m0 in range(0, NMASK, 4):
        m1 = min(NMASK, m0 + 4)
        s.dma("pool", MK[:, m0:m1, :], io["masks"][m0:m1].rearrange("m p i -> p m i"), w=[tMK])
    s.dma(q, AB[:], io["ab"], w=[tAB])
    s.dma(q, CB[:], io["cb"], w=[tCB])
    s.dma(q, TA[:], io["ta"], w=[tTAB])
    s.dma(q, TBt[:], io["tb"], w=[tTAB])
    s.dma(q, GS[:], io["gs"], w=[tGS])
    s.dma("pool", W1[:], io["w1"].rearrange("p (k c h) -> p k c h", k=2, c=16), w=[tW1])
    s.dma("pool", W2[:], io["w2"].rearrange("p (k d) -> p k d", k=2), w=[tW2])
    s.dma("pool", POS[:], io["pos"].rearrange("p (k c) -> p k c", k=2), w=[tPOS])
    s.dma(q, SM_[:, 0:128], io["dl"].partition_broadcast(128).rearrange("p a b -> p (a b)"), w=[tSM])
    s.dma(q, SM_[:, 128:192], io["subln"].partition_broadcast(128).rearrange("p a b -> p (a b)"), w=[tSM])
    s.dma(q, SM_[:, 192:196], io["sinks"].partition_broadcast(128).rearrange("p a b -> p (a b)"), w=[tSM])
    s.dma("pool", KS[64:128, :], io["emat"], w=[tKS])
    s.dma("pool", RC[:, :, 0:64], io["ov"].rearrange("c p j -> p c j"), w=[tRC])
    for b in range(1):
        for h in range(4):
            s.dma("pool", QC[b][h][64:65, :], io["ri"][h:h + 1, :], w=[tQC[b][h]])
    s.add("pool", lambda e: e.memset(VALL[:, :, :, 64:65], 1.0), w=[tV])
    s.add("pool", lambda e: e.memset(RC[:, :, 128:129], 1.0), w=[tRC])
    s.add("pool", lambda e: e.memset(KC[64:65, :], 1.0), w=[tKC])
    s.add("pool", lambda e: e.memset(NM[:], 0.0), w=[tNM])
    s.add("pool", lambda e: e.memset(HT[:], 0.0), w=[tHT])
    s.add("pool", lambda e: e.memset(KCT[:], 0.0), w=[tKCT])
    s.add("pool", lambda e: e.memset(Kc2[:, :, T - 1:T], 0.0), w=[tKc2])
    s.add("dve", lambda e: e.tensor_tensor(out=SM_[:, 208:240], in0=SM_[:, 0:32], in1=SM_[:, 32:64], op=ALU.mult), r=[tSM], w=[tSM])
    s.add("dve", lambda e: e.reduce_sum(out=SM_[:, 200:201], in_=SM_[:, 208:240], axis=AX.X), r=[tSM], w=[tSM])
    s.add("dve", lambda e: e.tensor_tensor(out=SM_[:, 208:240], in0=SM_[:, 64:96], in1=SM_[:, 96:128], op=ALU.mult), r=[tSM], w=[tSM])
    s.add("dve", lambda e: e.reduce_sum(out=SM_[:, 201:202], in_=SM_[:, 208:240], axis=AX.X), r=[tSM], w=[tSM])
    s.add("act", lambda e: e.activation(out=SM_[:, 202:204], in_=SM_[:, 200:202], func=AF.Exp), r=[tSM], w=[tSM])
    s.add("dve", lambda e: e.tensor_tensor(out=SM_[:, 204:205], in0=SM_[:, 203:204], in1=SM_[:, 202:203], op=ALU.subtract), r=[tSM], w=[tSM])
    s.add("dve", lambda e: e.tensor_scalar(SM_[:, 200:201], SM_[:, 204:205], -lam_init, None, ALU.add), r=[tSM], w=[tSM])
    s.add("dve", lambda e: e.tensor_scalar(SM_[:, 128:192], SM_[:, 128:192], 1.0 - lam_init, None, ALU.mult), r=[tSM], w=[tSM])
    s.add("act", lambda e: e.activation(out=SM_[:, 196:200], in_=SM_[:, 192:196], func=AF.Exp), r=[tSM], w=[tSM])
    for sub in range(4):
        s.add("dve", lambda e, sub=sub: e.tensor_tensor(out=SK[:, sub, :], in0=GS[:, sub, :], in1=SM_[:, 196:200], op=ALU.mult), r=[tSM, tGS], w=[tSK])
    NLAM = SM_[:, 200:201]
    SUBLN = SM_[:, 128:192]

    evrr = [0]

    def evac(out, in_, rtoks, wtoks, scale=None, eng=None):
        if eng is None:
            eng = ("dve", "act")[evrr[0] % 2]; evrr[0] += 1
        if eng == "act":
            if scale is None:
                s.add("act", lambda e: e.activation(out=out, in_=in_, func=AF.Copy), r=rtoks, w=wtoks)
            else:
                s.add("act", lambda e: e.activation(out=out, in_=in_, func=AF.Copy, scale=scale), r=rtoks, w=wtoks)
        else:
            if scale is None:
                s.add("dve", lambda e: e.tensor_copy(out=out, in_=in_), r=rtoks, w=wtoks)
            else:
                s.add("dve", lambda e: e.tensor_scalar(out, in_, scale, None, ALU.mult), r=rtoks, w=wtoks)

    def load_xt(tc, k, eng=None):
        xsrc = io["xchunk"](tc) if "xchunk" in io else io["x"][tc * 512:(tc + 1) * 512, :]
        s.dma("sp", XC[:], xsrc.rearrange("(a p) d -> p a d", p=128), r=([xdeps[tc % 4]] if xdeps else []), w=[tXC])
        for dc in range(8):
            ps, tps = ps_score()
            for sub in range(4):
                s.add("pe", lambda e, ps=ps, sub=sub, dc=dc: e.transpose(ps[:, sub * 128:(sub + 1) * 128], XC[:, sub, dc * 128:(dc + 1) * 128], IDN[:]),
                      r=[tXC, tIDN], w=[tps])
            evac(XTc[k][:, dc, :], ps, [tps], [tXTc[k]], eng=eng)

    def proj_fm(k, col0, m):
        ps, tps = ps_score()
        for dc in range(8):
            s.add("pe", lambda e, ps=ps, dc=dc: e.matmul(ps[0:m, :], W[:, dc, col0:col0 + m], XTc[k][:, dc, :], start=(dc == 0), stop=(dc == 7)),
                  r=[tW, tXTc[k]], w=[tps])
        return ps, tps

    for tc in range(8):
        k = tc % 2
        t0 = tc * 512
        load_xt(tc, k)
        for kv in range(2):
            ps, tps = proj_fm(k, C_KG0 + 128 * kv, 128)
            evac(Kc2[0:64, kv, t0:t0 + 512], ps[0:64, :], [tps], [tKc2])
            if tc == 0:
                evac(Kc2[64:128, kv, 0:511], ps[64:128, 1:512], [tps], [tKc2])
            else:
                evac(Kc2[64:128, kv, t0 - 1:t0 + 511], ps[64:128, :], [tps], [tKc2])
        ps, tps = proj_fm(k, C_KS, 64); evac(KS[0:64, t0:t0 + 512], ps[0:64, :], [tps], [tKS])
        ps, tps = proj_fm(k, C_KW, 64); evac(KW[0:64, t0:t0 + 512], ps[0:64, :], [tps], [tKW])
        ps, tps = proj_fm(k, C_KC, 64); evac(KC[0:64, t0:t0 + 512], ps[0:64, :], [tps], [tKC])
        for hh in range(2):
            ps, tps = proj_fm(k, C_KB0 + 128 * hh, 128); evac(KB[hh][:, t0:t0 + 512], ps, [tps], [tKB[hh]])
        for sub in range(4):
            ps, tps = ps_score()
            for dc in range(8):
                s.add("pe", lambda e, ps=ps, dc=dc, sub=sub, k=k: e.matmul(ps[:, 0:320], XTc[k][:, dc, sub * 128:(sub + 1) * 128], W[:, dc, C_V:C_V + 320],
                                                                      start=(dc == 0), stop=(dc == 7)), r=[tW, tXTc[k]], w=[tps])
            evac(VALL[:, tc * 4 + sub, :, 0:64], ps[:, 0:320].rearrange("p (a b) -> p a b", b=64), [tps], [tV])

    for kv in range(2):
        ps, tps = ps_score()
        for cc in range(16):
            s.add("pe", lambda e, ps=ps, cc=cc, kv=kv: e.matmul(ps[:, 0:1], W1[:, kv, cc, :], POS[:, kv, cc:cc + 1], start=(cc == 0), stop=(cc == 15)),
                  r=[tW1, tPOS], w=[tps])
        evac(BP[:, kv:kv + 1], ps[:, 0:1], [tps], [tBP], eng="dve")
        ps, tps = ps_score()
        for cc in range(16):
            s.add("pe", lambda e, ps=ps, cc=cc, kv=kv: e.matmul(ps[:, 0:255], W1[:, kv, cc, :], Kc2[:, kv, 2 * cc: 2 * cc + 16 * 254 + 1: 16],
                                                                start=(cc == 0), stop=(cc == 15)), r=[tW1, tKc2], w=[tps])
        s.add("act", lambda e, ps=ps, kv=kv: e.activation(out=HT[:, kv, 0:255], in_=ps[:, 0:255], func=AF.Gelu_apprx_tanh, bias=BP[:, kv:kv + 1]),
              r=[tps, tBP], w=[tHT])
    ps, tps = ps_score()
    s.add("pe", lambda e, ps=ps: e.matmul(ps[0:64, 0:256], W2[:, 0, :], HT[:, 0, :], start=True, stop=True), r=[tW2, tHT], w=[tps])
    evac(KCT[:, :], ps[0:64, 0:256], [tps], [tKCT], eng="dve")
    for cc in range(2):
        ps, tps = ps_score()
        s.add("pe", lambda e, ps=ps, cc=cc: e.matmul(ps[:, 0:64], HT[:, 1, cc * 128:(cc + 1) * 128], W2[:, 1, :], start=True, stop=True), r=[tW2, tHT], w=[tps])
        evac(RC[:, cc, 64:128], ps[:, 0:64], [tps], [tRC], eng="dve")

    NPT = 4
    LA = 2

    GP = []
    CAPT = 2

    def gp_flush_one():
        ent = GP.pop(0)
        ent["pv"]()
        if ent["after"] is not None:
            ent["after"]()

    def gp_push(n, pv, after=None):
        GP.append(dict(n=n, pv=pv, after=after))
        while sum(x["n"] for x in GP) > CAPT + n - 1 and len(GP) > 1:
            gp_flush_one()

    def gp_drain():
        while GP:
            gp_flush_one()

    def attn_unit(kbs, lhs_fn, ltoks, rhs, rtoks, bias_fn, vidx, o_acc, to_acc, after=None):
        attn_multi(kbs, [(lhs_fn, ltoks, rhs, rtoks, bias_fn, vidx, o_acc, to_acc)], after=after)

    def attn_multi(kbs, streams, after=None):
        ns = len(streams)
        first = [True] * ns
        last_kb = {}
        for (kb, mi, subs) in kbs:
            for sub in subs:
                last_kb[sub] = kb

        def make_pv(kb, subs, pks):
            def pv():
                for si, (lhs_fn, ltoks, rhs, rtoks, bias_fn, vidx, o_acc, to_acc) in enumerate(streams):
                    pk = pks[si]
                    for sub in subs:
                        s.add("pe", lambda e, pk=pk, sub=sub, kb=kb, st_=first[si], sp_=(last_kb[sub] == kb), o_acc=o_acc, vidx=vidx: e.matmul(
                            o_acc[:, sub * 128:sub * 128 + 65], PTb[pk][:, sub * 128:(sub + 1) * 128], VALL[:, kb, vidx, :], start=st_, stop=sp_,
                            skip_group_check=True),
                            r=[tPT[pk], tV], w=[to_acc])
                        first[si] = False
            return pv

        for idx_, (kb, mi, subs) in enumerate(kbs):
            c0, c1 = min(subs) * 128, (max(subs) + 1) * 128
            tiles = []
            for (lhs_fn, ltoks, rhs, rtoks, bias_fn, vidx, o_acc, to_acc) in streams:
                ps, tps = ps_score()
                s.add("pe", lambda e, ps=ps, kb=kb, mi=mi, c0=c0, c1=c1, lhs_fn=lhs_fn, rhs=rhs: e.matmul(ps[:, c0:c1], lhs_fn(kb), rhs[:, c0:c1], start=True, stop=(mi is None)),
                      r=ltoks + rtoks, w=[tps])
                tiles.append((ps, tps))
            if mi is not None:
                for (ps, tps) in tiles:
                    s.add("pe", lambda e, ps=ps, mi=mi, c0=c0, c1=c1: e.matmul(ps[:, c0:c1], IDNb[:], MK[:, mi, c0:c1], start=False, stop=True), r=[tIDNb, tMK], w=[tps])
            pks = []
            for si, (lhs_fn, ltoks, rhs, rtoks, bias_fn, vidx, o_acc, to_acc) in enumerate(streams):
                ps, tps = tiles[si]
                pk = ptrr[0] % NPT; ptrr[0] += 1
                s.add("act", lambda e, ps=ps, pk=pk, kb=kb, c0=c0, c1=c1, bias_fn=bias_fn: e.activation(out=PTb[pk][:, c0:c1], in_=ps[:, c0:c1], func=AF.Exp, bias=bias_fn(kb)),
                      r=[tps, tAB, tCB], w=[tPT[pk]])
                pks.append(pk)
            gp_push(ns, make_pv(kb, subs, pks), after if idx_ == len(kbs) - 1 else None)

    def kb_list(qt, kind):
        out = []
        lo = {"full": 0, "win": max(0, 4 * qt - 4), "swa": max(0, 4 * qt - 1)}[kind]
        for kb in range(lo, 4 * qt + 4):
            o = 4 * qt - kb
            if kind == "full":
                mi = (M_CAUS - o) if o <= 0 else None
            elif kind == "win":
                mi = (M_CAUS - o) if o <= 0 else (M_WM + o - 1)
            else:
                mi = M_SM + o + 3
            if mi is None:
                subs = [0, 1, 2, 3]
            else:
                subs = [sub for sub in range(4) if ALLOWED[mi][:, sub * 128:(sub + 1) * 128].any()]
                if ALLOWED[mi].all():
                    mi = None
            out.append((kb, mi, subs))
        return out

    def recip_den(o_acc, to_acc, width, col, rec_ap, extra=None):
        den = o_acc[:, 0:4 * width].rearrange("p (a b) -> p a b", b=width)[:, :, col]
        if extra is None:
            s.add("dve", lambda e: e.tensor_scalar(rec_ap, den, 1e-30, None, ALU.max), r=(to_acc if isinstance(to_acc, list) else [to_acc]), w=[tREC])
        else:
            s.add("dve", lambda e: e.tensor_tensor(out=rec_ap, in0=den, in1=extra, op=ALU.add), r=[to_acc, tSK], w=[tREC])
        s.add("dve", lambda e: e.reciprocal(out=rec_ap, in_=rec_ap), r=[tREC], w=[tREC])

    def oview(o_acc, width, c0, n=64):
        return o_acc[:, 0:4 * width].rearrange("p (a b) -> p a b", b=width)[:, :, c0:c0 + n]

    def bc(ap4):
        return ap4.unsqueeze(2).to_broadcast([128, 4, 64])

    outs = []
    toh = []
    def finish_tile(qt):
        for fc in range(4):
            ps, tps = ps_score()
            for sub in range(4):
                s.add("pe", lambda e, ps=ps, sub=sub, fc=fc: e.transpose(ps[:, sub * 128:(sub + 1) * 128], OCH[:, sub, fc * 128:(fc + 1) * 128], IDN[:]),
                      r=[tOCH, tIDN], w=[tps])
            evac(OTc[:, fc, :], ps, [tps], [tOTc], eng="dve")
        if "oTh" in io:
            tq = Tok()
            toh.append(tq)
            outs.append(s.dma("sp", io["oTh"][qt // 4].rearrange("(c p) t -> p c t", p=128)[:, :, (qt % 4) * 512:(qt % 4 + 1) * 512], OTc[:], r=[tOTc], w=[tq]))
            if qt % 4 == 3:
                j = qt // 4
                outs.append(s.add("pool", lambda e, j=j: e.collective_compute("AllGather", ALU.bypass, replica_groups=[[0, 1], [2, 3], [4, 5], [6, 7]],
                                                                              ins=[io["oTh"][j].opt()], outs=[io["oTg"][j].opt()]),
                                  r=toh[-4:], w=[Tok()], dma=True, inc=1))
        else:
            outs.append(s.dma("sp", io["oT"][:, :, qt * 512:(qt + 1) * 512].rearrange("c p t -> p c t"), OTc[:], r=[tOTc]))

    def q_proj(qt):
        k = qt % 2
        b = qt % 2
        load_xt(qt, k, eng="dve")
        for h in range(4):
            ps, tps = proj_fm(k, C_QA + 64 * h, 64)
            evac(QA[b][h][0:64, :], ps[0:64, :], [tps], [tQA[b][h]], scale=0.125, eng="dve")
        for h in range(2):
            ps, tps = proj_fm(k, C_QB + 128 * h, 128)
            evac(QB[b][h][:, :], ps, [tps], [tQB[b][h]], scale=32 ** -0.5, eng="dve")
        for h in range(4):
            ps, tps = proj_fm(k, C_QC + 64 * h, 64)
            evac(QC[b][h][0:64, :], ps[0:64, :], [tps], [tQC[b][h]], scale=0.125, eng="dve")

    q_proj(0)
    for qt in range(NQT):
        k = qt % 2
        b = qt % 2
        ps, tps = ps_score()
        for sub in range(4):
            for dc in range(8):
                s.add("pe", lambda e, ps=ps, dc=dc, sub=sub, k=k: e.matmul(ps[:, sub * 8:sub * 8 + 6], XTc[k][:, dc, sub * 128:(sub + 1) * 128], W[:, dc, C_G:C_G + 6],
                                                                      start=(dc == 0), stop=(dc == 7)), r=[tW, tXTc[k]], w=[tps])
        s.add("act", lambda e, ps=ps: e.activation(out=GT[:], in_=ps[:, 0:32].rearrange("p (a b) -> p a b", b=8)[:, :, 0:6], func=AF.Sigmoid), r=[tps], w=[tGT])

        chunks = [0] if qt < 4 else [0, 1]
        for h in range(4):
            oa, toa = ps_accfull()

            def post_cmp(h=h, oa=oa, toa=toa):
                recip_den(oa, toa, 256, 128, REC[:, 0:4])
                if h == 0:
                    s.add("dve", lambda e: e.tensor_tensor(out=IMP[:], in0=oview(oa, 256, 0), in1=bc(REC[:, 0:4]), op=ALU.mult),
                          r=[toa[0], toa[1], tREC], w=[tIMP])
                else:
                    tm = TMP[h % 2]; ttm = tTMP[h % 2]
                    s.add("dve", lambda e: e.tensor_tensor(out=tm[:], in0=oview(oa, 256, 0), in1=bc(REC[:, 0:4]), op=ALU.mult),
                          r=[toa[0], toa[1], tREC], w=[ttm])
                    s.add("dve", lambda e: e.tensor_tensor(out=IMP[:], in0=IMP[:], in1=tm[:], op=ALU.add), r=[ttm, tIMP], w=[tIMP])
                if h < 2:
                    s.add("dve", lambda e: e.tensor_tensor(out=OCc[:, h, :, :], in0=oview(oa, 256, 64), in1=bc(REC[:, 0:4]), op=ALU.mult),
                          r=[toa[0], toa[1], tREC], w=[tOCc])

            for cc in chunks:
                rel = qt - 4 * cc
                mi = (M_CM + rel) if rel < 5 else None
                ps, tps = ps_score()
                s.add("pe", lambda e, ps=ps, cc=cc, h=h, mi=mi, b=b: e.matmul(ps, KCT[:, cc * 128:(cc + 1) * 128], QA[b][h][0:64, :], start=True, stop=(mi is None)),
                      r=[tKCT, tQA[b][h]], w=[tps])
                if mi is not None:
                    s.add("pe", lambda e, ps=ps, mi=mi: e.matmul(ps, IDNb[:], MK[:, mi, :], start=False, stop=True), r=[tIDNb, tMK], w=[tps])
                pk = ptrr[0] % NPT; ptrr[0] += 1
                ci = h * 16 + cc * 8 + qt
                s.add("act", lambda e, ps=ps, pk=pk, ci=ci: e.activation(out=PTb[pk][:], in_=ps, func=AF.Exp, bias=CB[:, ci:ci + 1]),
                      r=[tps, tCB], w=[tPT[pk]])

                def pv_cmp(oa=oa, toa=toa, pk=pk, cc=cc, first=(cc == chunks[0]), last=(cc == chunks[-1])):
                    for sub in range(4):
                        s.add("pe", lambda e, sub=sub: e.matmul(
                            oa[:, sub * 256:sub * 256 + 129], PTb[pk][:, sub * 128:(sub + 1) * 128], RC[:, cc, :], start=(first and sub % 2 == 0), stop=last,
                            skip_group_check=True),
                            r=[tPT[pk], tRC], w=[toa[sub // 2]])

                gp_push(1, pv_cmp, post_cmp if cc == chunks[-1] else None)
        gp_drain()
        if qt >= 1:
            finish_tile(qt - 1)
        for sub in range(4):
            blk = qt * 4 + sub
            lo = 63 - 2 * blk
            s.add("dve", lambda e, sub=sub, lo=lo: e.tensor_tensor(out=IMP2[:, sub, :], in0=IMP[:, sub, :], in1=TA[:, lo:lo + 64], op=ALU.mult),
                  r=[tIMP, tTAB], w=[tIMP2])
            s.add("dve", lambda e, sub=sub, lo=lo: e.tensor_tensor(out=IMP2[:, sub, :], in0=IMP2[:, sub, :], in1=TBt[:, lo:lo + 64], op=ALU.add),
                  r=[tIMP2, tTAB], w=[tIMP2])
        s.add("dve", lambda e: e.memset(IMP2[:, :, 0:1], 1e9), w=[tIMP2])
        for sub in range(4):
            s.add("dve", lambda e, sub=sub: e.max(out=MX[:, sub, 0:8], in_=IMP2[:, sub, :]), r=[tIMP2], w=[tMX])
            s.add("dve", lambda e, sub=sub: e.match_replace(out=IMP[:, sub, :], in_to_replace=MX[:, sub, 0:8], in_values=IMP2[:, sub, :], imm_value=-1e30),
                  r=[tIMP2, tMX], w=[tIMP])
            s.add("dve", lambda e, sub=sub: e.max(out=MX[:, sub, 8:16], in_=IMP[:, sub, :]), r=[tIMP], w=[tMX])
            s.add("dve", lambda e, sub=sub: e.tensor_scalar(NM[:, sub, 64:128], IMP2[:, sub, :], MX[:, sub, 15:16], 1.0, ALU.is_ge, ALU.subtract),
                  r=[tIMP2, tMX], w=[tNM])
        for own in range(2):
            hs = 2 + own
            feat0 = 128 + 64 * own
            accs = []
            streams = []
            for mp in range(2):
                oa, toa = ps_acc()
                lhs_fn = lambda kb, own=own, mp=mp: KB[own][64 * mp:64 * mp + 64, kb * 128:(kb + 1) * 128]
                rhs = QB[b][own][64 * mp:64 * mp + 64, :]
                bias_fn = lambda kb, hs=hs, qt=qt: AB[:, hs, 4 * qt - kb + 3: 4 * qt - kb + 4]
                streams.append((lhs_fn, [tKB[own]], rhs, [tQB[b][own]], bias_fn, 2 + own, oa, toa))
                accs.append((oa, toa))

            def post_diff(accs=accs, feat0=feat0):
                (o1, to1), (o2, to2) = accs
                r1 = REC[:, 4:8]; r2 = REC[:, 8:12]
                recip_den(o1, to1, 128, 64, r1)
                recip_den(o2, to2, 128, 64, r2)
                s.add("dve", lambda e: e.tensor_scalar(r2, r2, NLAM, None, ALU.mult), r=[tREC, tSM], w=[tREC])
                t1 = TMP[0]; t2 = TMP[1]
                s.add("dve", lambda e: e.tensor_tensor(out=t1[:], in0=oview(o1, 128, 0), in1=bc(r1), op=ALU.mult), r=[to1, tREC], w=[tTMP[0]])
                s.add("dve", lambda e: e.tensor_tensor(out=t2[:], in0=oview(o2, 128, 0), in1=bc(r2), op=ALU.mult), r=[to2, tREC], w=[tTMP[1]])
                s.add("dve", lambda e: e.tensor_tensor(out=t1[:], in0=t1[:], in1=t2[:], op=ALU.add), r=[tTMP[0], tTMP[1]], w=[tTMP[0]])
                s.add("dve", lambda e: e.tensor_tensor(out=t2[:], in0=t1[:], in1=t1[:], op=ALU.mult), r=[tTMP[0]], w=[tTMP[1]])
                ss = REC[:, 12:16]
                s.add("dve", lambda e: e.reduce_sum(out=ss, in_=t2[:], axis=AX.X), r=[tTMP[1]], w=[tREC])
                s.add("dve", lambda e: e.tensor_scalar(ss, ss, 1.0 / 64.0, 1e-5, ALU.mult, ALU.add), r=[tREC], w=[tREC])
                s.add("act", lambda e: e.activation(out=ss, in_=ss, func=AF.Sqrt), r=[tREC], w=[tREC])
                s.add("dve", lambda e: e.reciprocal(out=ss, in_=ss), r=[tREC], w=[tREC])
                s.add("dve", lambda e: e.tensor_tensor(out=t1[:], in0=t1[:], in1=bc(ss), op=ALU.mult), r=[tTMP[0], tREC], w=[tTMP[0]])
                s.add("dve", lambda e: e.tensor_tensor(out=OCH[:, :, feat0:feat0 + 64], in0=t1[:], in1=SUBLN.unsqueeze(1).to_broadcast([128, 4, 64]), op=ALU.mult),
                      r=[tTMP[0], tSM], w=[tOCH])

            attn_multi(kb_list(qt, "full"), streams, after=post_diff)
        for r_ in range(4):
            hs = 4 + r_
            feat0 = 256 + 64 * r_
            oa, toa = ps_acc()
            lhs_fn = lambda kb: KC[0:65, kb * 128:(kb + 1) * 128]
            rhs = QC[b][r_][0:65, :]
            bias_fn = lambda kb, hs=hs, qt=qt: AB[:, hs, 4 * qt - kb + 3: 4 * qt - kb + 4]

            def post_swa(oa=oa, toa=toa, r_=r_, feat0=feat0):
                rec = REC[:, 4:8]
                recip_den(oa, toa, 128, 64, rec, extra=SK[:, :, r_])
                s.add("dve", lambda e: e.tensor_tensor(out=OCH[:, :, feat0:feat0 + 64], in0=oview(oa, 128, 0), in1=bc(rec), op=ALU.mult),
                      r=[toa, tREC], w=[tOCH])

            attn_unit(kb_list(qt, "swa"), lhs_fn, [tKC], rhs, [tQC[b][r_]], bias_fn, 4, oa, toa, after=post_swa)
        ps, tps = ps_score()
        for sub in range(4):
            s.add("pe", lambda e, ps=ps, sub=sub: e.matmul(ps[:, sub * 128:(sub + 1) * 128], NM[:, sub, :], I30K[:], start=True, stop=True),
                  r=[tNM, tI30K], w=[tps])
        for own in range(2):
            h = own
            evac(QA[b][h][64:128, :], ps[64:128, :], [tps], [tQA[b][h]], eng="dve")

        for own in range(2):
            h = own
            hs = own
            feat0 = 64 * own
            s.add("dve", lambda e, own=own, feat0=feat0: e.tensor_tensor(out=OCH[:, :, feat0:feat0 + 64], in0=OCc[:, own, :, :],
                                                                         in1=bc(GT[:, :, 3 * own + 0]), op=ALU.mult), r=[tOCc, tGT], w=[tOCH])
            for br, (kind, lhs_t, ltok, vidx) in enumerate([("full", KS, tKS, 0), ("win", KW, tKW, 1)]):
                oa, toa = ps_acc()
                if kind == "full":
                    lhs_fn = lambda kb: KS[:, kb * 128:(kb + 1) * 128]
                    rhs = QA[b][h][:, :]
                else:
                    lhs_fn = lambda kb: KW[0:64, kb * 128:(kb + 1) * 128]
                    rhs = QA[b][h][0:64, :]
                bias_fn = lambda kb, hs=hs, qt=qt: AB[:, hs, 4 * qt - kb + 3: 4 * qt - kb + 4]

                def post_nsa(oa=oa, toa=toa, own=own, br=br, feat0=feat0):
                    rec = REC[:, 4 + 4 * br: 8 + 4 * br]
                    recip_den(oa, toa, 128, 64, rec)
                    s.add("dve", lambda e: e.tensor_tensor(out=rec, in0=rec, in1=GT[:, :, 3 * own + 1 + br], op=ALU.mult),
                          r=[tREC, tGT], w=[tREC])
                    tm = TMP[br]; ttm = tTMP[br]
                    s.add("dve", lambda e: e.tensor_tensor(out=tm[:], in0=oview(oa, 128, 0), in1=bc(rec), op=ALU.mult),
                          r=[toa, tREC], w=[ttm])
                    s.add("dve", lambda e: e.tensor_tensor(out=OCH[:, :, feat0:feat0 + 64], in0=OCH[:, :, feat0:feat0 + 64], in1=tm[:], op=ALU.add),
                          r=[ttm, tOCH], w=[tOCH])

                attn_unit(kb_list(qt, kind), lhs_fn, [ltok], rhs, [tQA[b][h]], bias_fn, vidx, oa, toa, after=post_nsa)
        if qt + 1 < NQT:
            q_proj(qt + 1)
        gp_drain()
    finish_tile(NQT - 1)
    if io.get("debug"):
        dl_ = [("KC", KC, [65, T], BF16, tKC), ("KW", KW, [64, T], BF16, tKW), ("KS", KS, [128, T], BF16, tKS), ("KB0", KB[0], [128, T], BF16, tKB[0]),
               ("VALL", VALL, [128, 32, 5, 65], BF16, tV), ("KCT", KCT, [64, 256], BF16, tKCT), ("RC", RC, [128, 2, 129], BF16, tRC),
               ("HT", HT, [128, 2, 256], BF16, tHT), ("Kc2", Kc2, [128, 2, T], BF16, tKc2), ("QA0", QA[1][0], [128, 512], BF16, tQA[1][0]),
               ("QB0", QB[1][0], [128, 512], BF16, tQB[1][0]), ("QC0", QC[1][0], [65, 512], BF16, tQC[1][0]), ("GT", GT, [128, 4, 6], F32, tGT),
               ("IMP2", IMP2, [128, 4, 64], F32, tIMP2), ("NM", NM, [128, 4, 128], BF16, tNM), ("OCH", OCH, [128, 4, 512], F32, tOCH),
               ("SMALL", SM_, [128, 256], F32, tSM), ("SK", SK, [128, 4, 4], F32, tSK), ("OCc", OCc, [128, 2, 4, 64], F32, tOCc),
               ("XT", XTc[1], [128, 8, 512], BF16, tXTc[1]), ("W", W, [128, 8, NW], BF16, tW), ("MK", MK, [128, NMASK, 512], BF16, tMK)]
        for (nm, tl, shp, dt_, tk) in dl_:
            dtn = nc.dram_tensor("dbg_" + nm, shp, dt_, kind="ExternalOutput").ap()
            outs.append(s.dma("sp", dtn, tl[:], r=[tk]))
    return outs


from concourse.bass_utils import run_bass_kernel_spmd

B_DENSE_IN = dict(oT=([8, 128, NT], BF16), xres=([NT, D], F32), w_out=([D, D], F32), lnp=([6, D], F32), wg=([D, 2816], F32), wu=([D, 2816], F32),
                  wd=([2816, D], F32), ple_gate=([D, D], F32), ple_proj=([256, D], F32), p=([NT, 256], F32), idn=([128, 128], F32))
B_MOE_IN = dict(oT=([8, 128, NT], BF16), xres=([NT, D], F32), w_out=([D, D], F32), lnp=([6, D], F32), router=([D, 8], F32),
                mg=([8, D, 3584], F32), mu=([8, D, 3584], F32), md=([8, 3584, D], F32), ple_gate=([D, D], F32), ple_proj=([256, D], F32),
                p=([NT, 256], F32), idn=([128, 128], F32), ut=([128, 128], F32), iota=([128, CAP], F32), pcol=([128, 16, 2], F32))


def _prog_A(layer):
    nc = bass.Bass("TRN2", target_bir_lowering=False)
    io = {k: dram(nc, k, shp) for k, shp in A_IN.items()}
    io["oT"] = dram(nc, "oT", [4, 128, T], BF16, kind="ExternalOutput")
    with contextlib.ExitStack() as st:
        s = Sched(nc)
        c = Ctx()
        outs = build_A(nc, st, s, c, io, layer)
        s.emit(final_ops=outs)
    return nc


def _prog_B(moe):
    nc = bass.Bass("TRN2", target_bir_lowering=False)
    spec = B_MOE_IN if moe else B_DENSE_IN
    io = {k: dram(nc, k, shp, dt) for k, (shp, dt) in spec.items()}
    io["out"] = dram(nc, "out", [NT, D], kind="ExternalOutput")
    with contextlib.ExitStack() as st:
        s = Sched(nc)
        c = Ctx()
        setup_psum(nc, st, c)
        outs = (build_B_moe if moe else build_B_dense)(nc, st, s, c, io)
        s.emit(final_ops=outs)
    return nc


def _feat_perm(sidx):
    return np.concatenate([np.arange(128 * sidx, 128 * sidx + 128), 256 + np.arange(128 * sidx, 128 * sidx + 128),
                           512 + np.arange(256 * sidx, 256 * sidx + 256)])


def kernel_unfused(**inputs):
    inp = {k: np.asarray(v) for k, v in inputs.items()}
    x = np.ascontiguousarray(inp["x"], dtype=np.float32)
    nb = x.shape[0]
    cores = list(range(2 * nb))
    idn = np.eye(128, dtype=np.float32)
    ut = (np.arange(128)[:, None] < np.arange(128)[None, :]).astype(np.float32)
    iota = np.tile(np.arange(CAP, dtype=np.float32)[None, :], (128, 1))
    wperm = np.concatenate([_feat_perm(0), _feat_perm(1)])
    consts = [host_consts(0), host_consts(1)]
    for L in range(2):
        packs = [host_w(inp["w_in"][L], s_) for s_ in range(2)]
        smalls = [host_small(inp, L, s_) for s_ in range(2)]
        in_maps = []
        for cid in cores:
            b, s_ = cid // 2, cid % 2
            m = dict(x=np.ascontiguousarray(x[b]), w=packs[s_])
            m.update(consts[s_])
            m.update(smalls[s_])
            in_maps.append(m)
        res = run_bass_kernel_spmd(_prog_A(L), in_maps, core_ids=cores)
        oT = [np.asarray(r["oT"]) for r in res.results]
        moe = (L % 2 == 1)
        lnp = np.stack([inp["ln1_g"][L], inp["ln1_b"][L], inp["ln2_g"][L], inp["ln2_b"][L], inp["ln3_g"][L], inp["ln3_b"][L]]).astype(np.float32)
        w_out = np.ascontiguousarray(inp["w_out"][L][wperm, :])
        in_maps = []
        for cid in cores:
            b, h = cid // 2, cid % 2
            tk = slice(h * NT, (h + 1) * NT)
            oTB = np.ascontiguousarray(np.concatenate([oT[2 * b][:, :, tk], oT[2 * b + 1][:, :, tk]], axis=0))
            m = dict(oT=oTB, xres=np.ascontiguousarray(x[b, tk]), w_out=w_out, lnp=lnp, ple_gate=inp["ple_gate"][L], ple_proj=inp["ple_proj"][L],
                     p=np.ascontiguousarray(inp["p"][L, b, tk]), idn=idn)
            if moe:
                m.update(router=inp["moe_router"][L // 2], mg=inp["moe_w_gate"][L // 2], mu=inp["moe_w_up"][L // 2], md=inp["moe_w_down"][L // 2], ut=ut, iota=iota)
            else:
                m.update(wg=inp["ffn_w_gate"][L // 2], wu=inp["ffn_w_up"][L // 2], wd=inp["ffn_w_down"][L // 2])
            in_maps.append(m)
        res = run_bass_kernel_spmd(_prog_B(moe), in_maps, core_ids=cores)
        xn = np.empty_like(x)
        for cid in cores:
            b, h = cid // 2, cid % 2
            xn[b, h * NT:(h + 1) * NT] = np.asarray(res.results[cid]["out"], dtype=np.float32)
        x = xn
    return x


A_LAYER_KEYS = ("w", "w1", "w2", "pos", "dl", "subln", "sinks")
A_SHARED_KEYS = ("masks", "idn", "i30k", "ab", "cb", "ov", "emat", "ta", "tb", "ri", "gs")
B_COMMON = dict(w_out=([D, D], F32), lnp=([6, D], F32), ple_gate=([D, D], F32), ple_proj=([256, D], F32), p=([NT, 256], F32))


def _prog_fused(nlayers=2):
    PHASE[0] = 0
    Sched.NSCHED = 0
    nc = bass.Bass("TRN2", target_bir_lowering=False)
    ext = {}

    def inp(name, shp, dt=F32):
        ext[name] = dram(nc, name, shp, dt)
        return ext[name]

    x0 = inp("x0", [T, D])
    xres0 = inp("xres0", [NT, D])
    hsel = inp("hsel", [1, 2])
    shared = {k: inp(k, A_IN[k]) for k in A_SHARED_KEYS}
    perA = [{k: inp("%s_%d" % (k, L), A_IN[k]) for k in A_LAYER_KEYS} for L in range(2)]
    perB = [{k: inp("%s_%d" % (k, L), shp, dt) for k, (shp, dt) in B_COMMON.items()} for L in range(2)]
    dense = dict(wg=inp("wg", [D, 2816]), wu=inp("wu", [D, 2816]), wd=inp("wd", [2816, D]))
    moe = dict(router=inp("router", [D, 8]), mg=inp("mg", [8, D, 3584]), mu=inp("mu", [8, D, 3584]), md=inp("md", [8, 3584, D]),
               ut=inp("ut", [128, 128]), iota=inp("iota", [128, CAP]), pcol=inp("pcol", [128, 16, 2]))
    out = dram(nc, "out", [NT, D], kind="ExternalOutput")
    oTown = [dram(nc, "oTown%d" % L, [2, 512, NT], BF16, kind="Internal") for L in range(2)]
    oTg = [dram(nc, "oTg%d" % L, [2, 1024, NT], BF16, kind="Internal") for L in range(2)]
    xh = dram(nc, "xh", [NT, D], kind="Internal")
    xg = dram(nc, "xg", [4, 1024, D], kind="Internal")
    for L in range(nlayers):
        with nc.cleanup_on_exit():
            with contextlib.ExitStack() as st:
                PHASE[0] += 1
                s = Sched(nc)
                c = Ctx()
                io = dict(shared)
                io.update(perA[L])
                io["oTh"] = oTown[L]
                io["oTg"] = oTg[L]
                if L == 0:
                    io["x"] = x0
                else:
                    io["xchunk"] = lambda tc: xg[tc % 4][(tc // 4) * 512:(tc // 4 + 1) * 512, :]
                outs = build_A(nc, st, s, c, io, L)
                s.emit(final_ops=outs)
            nc.all_engine_barrier()
        with nc.cleanup_on_exit():
            with contextlib.ExitStack() as st:
                PHASE[0] += 1
                s = Sched(nc)
                c = Ctx()
                setup_psum(nc, st, c)
                io = dict(perB[L])
                io.update(idn=shared["idn"], hsel=hsel, oTown=oTown[L], oTg=oTg[L])
                io["xres"] = xres0 if L == 0 else xh
                io["out"] = xh if L < nlayers - 1 else out
                if L < nlayers - 1:
                    io["xg"] = xg
                if L % 2 == 0:
                    io.update(dense)
                    outs = build_B_dense(nc, st, s, c, io)
                else:
                    io.update(moe)
                    outs = build_B_moe(nc, st, s, c, io)
                s.emit(final_ops=outs)
            nc.all_engine_barrier()
    return nc


def kernel(**inputs):
    inp = {k: np.asarray(v) for k, v in inputs.items()}
    x = np.ascontiguousarray(inp["x"], dtype=np.float32)
    nb = x.shape[0]
    cores = list(range(2 * nb))
    idn = np.eye(128, dtype=np.float32)
    ut = (np.arange(128)[:, None] < np.arange(128)[None, :]).astype(np.float32)
    iota = np.tile(np.arange(CAP, dtype=np.float32)[None, :], (128, 1))
    pcol = np.stack([np.tile(np.arange(128, dtype=np.float32)[:, None], (1, 16)), np.tile(np.arange(16, dtype=np.float32)[None, :], (128, 1))], axis=-1)
    wperm = np.concatenate([_feat_perm(0), _feat_perm(1)])
    consts = [host_consts(0), host_consts(1)]
    packs = [[host_w(inp["w_in"][L], s_) for s_ in range(2)] for L in range(2)]
    smalls = [[host_small(inp, L, s_) for s_ in range(2)] for L in range(2)]
    lnps = [np.stack([inp["ln1_g"][L], inp["ln1_b"][L], inp["ln2_g"][L], inp["ln2_b"][L], inp["ln3_g"][L], inp["ln3_b"][L]]).astype(np.float32) for L in range(2)]
    w_outs = [np.ascontiguousarray(inp["w_out"][L][wperm, :]) for L in range(2)]
    in_maps = []
    for cid in cores:
        b, s_ = cid // 2, cid % 2
        tk = slice(s_ * NT, (s_ + 1) * NT)
        hs = np.zeros((1, 2), np.float32)
        hs[0, s_] = 1.0
        m = dict(x0=np.ascontiguousarray(x[b]), xres0=np.ascontiguousarray(x[b, tk]), hsel=hs)
        for k in A_SHARED_KEYS:
            m[k] = consts[s_][k]
        for L in range(2):
            m["w_%d" % L] = packs[L][s_]
            for k in A_LAYER_KEYS[1:]:
                m["%s_%d" % (k, L)] = smalls[L][s_][k]
            m["w_out_%d" % L] = w_outs[L]
            m["lnp_%d" % L] = lnps[L]
            m["ple_gate_%d" % L] = inp["ple_gate"][L]
            m["ple_proj_%d" % L] = inp["ple_proj"][L]
            m["p_%d" % L] = np.ascontiguousarray(inp["p"][L, b, tk])
        m.update(wg=inp["ffn_w_gate"][0], wu=inp["ffn_w_up"][0], wd=inp["ffn_w_down"][0], router=inp["moe_router"][0],
                 mg=inp["moe_w_gate"][0], mu=inp["moe_w_up"][0], md=inp["moe_w_down"][0], ut=ut, iota=iota, pcol=pcol)
        in_maps.append(m)
    res = run_bass_kernel_spmd(_prog_fused(), in_maps, core_ids=cores)
    out = np.empty_like(x)
    for cid in cores:
        b, s_ = cid // 2, cid % 2
        out[b, s_ * NT:(s_ + 1) * NT] = np.asarray(res.results[cid]["out"], dtype=np.float32)
    return out
```
